# Optimizing a Trainium2 kernel written in Bass

```python
import math
import jax, jax.numpy as jnp
from jax import lax
import numpy as np

D_MODEL = 1024
BATCH = 8
SEQ = 4096
DEPTH = 1

HEAD_DIM = 64
A_Q_HEADS = 8
A_KV_HEADS = 2
B_HEADS = 8
D_MIX = (A_Q_HEADS + B_HEADS) * HEAD_DIM
A_Q_W = A_Q_HEADS * HEAD_DIM
A_KV_W = A_KV_HEADS * HEAD_DIM
B_W = B_HEADS * HEAD_DIM
D_IN = A_Q_W + 2 * A_KV_W + 3 * B_W
WINDOW = 128
BLOCK = 128
ROT_DIMS = HEAD_DIM // 4
ROPE_THETA = 500000.0
GRID_W = 64
NA_ROWS_MAX = 8
NA_COLS = 16
N_EXPERTS = 32
TOP_K = 4
D_FF = 1024
SWIGLU_LIMIT = 7.0
SWIGLU_ALPHA = 1.702
MOE_BLOCK = 128
EPS = 1e-5
NEG = -1e30

kernel_name = "hybrid_swa_natten_moe_adaln"


def rms_norm(x):
    xf = x.astype(jnp.float32)
    return (xf * lax.rsqrt(jnp.mean(xf * xf, axis=-1, keepdims=True) + EPS)).astype(x.dtype)


def modulate(xn, shift, scale):
    return xn * (1.0 + scale[:, None, :]) + shift[:, None, :]


def apply_partial_rope(x, seq_len):
    half = ROT_DIMS // 2
    pos = jnp.arange(seq_len, dtype=jnp.float32)
    inv_freq = ROPE_THETA ** (-jnp.arange(0, ROT_DIMS, 2, dtype=jnp.float32) / ROT_DIMS)
    ang = pos[:, None] * inv_freq[None, :]
    cos = jnp.cos(ang)[None, :, None, :].astype(x.dtype)
    sin = jnp.sin(ang)[None, :, None, :].astype(x.dtype)
    x1 = x[..., :half]
    x2 = x[..., half:ROT_DIMS]
    return jnp.concatenate([x1 * cos - x2 * sin, x2 * cos + x1 * sin, x[..., ROT_DIMS:]], axis=-1)


def windowed_gqa_sink(q, k, v, sink):
    b, s, hq, d = q.shape
    hkv = k.shape[2]
    g = hq // hkv
    nb = s // BLOCK
    qb = q.reshape(b, nb, BLOCK, hkv, g, d)
    pad = ((0, 0), (BLOCK, BLOCK), (0, 0), (0, 0))
    kp = jnp.pad(k, pad).reshape(b, nb + 2, BLOCK, hkv, d)
    vp = jnp.pad(v, pad).reshape(b, nb + 2, BLOCK, hkv, d)
    kb = jnp.concatenate([kp[:, :-2], kp[:, 1:-1], kp[:, 2:]], axis=2)
    vb = jnp.concatenate([vp[:, :-2], vp[:, 1:-1], vp[:, 2:]], axis=2)
    qi = jnp.arange(BLOCK)
    kj = jnp.arange(3 * BLOCK)
    rel = kj[None, :] - BLOCK - qi[:, None]
    band = jnp.abs(rel) <= WINDOW
    kpos = jnp.arange(nb)[:, None] * BLOCK - BLOCK + kj[None, :]
    inb = (kpos >= 0) & (kpos < s)
    mask = band[None, :, :] & inb[:, None, :]
    scores = jnp.einsum('bnqhgd,bnkhd->bnhgqk', qb, kb).astype(jnp.float32) * (d ** -0.5)
    scores = jnp.where(mask[None, :, None, None], scores, NEG)
    sink_l = sink.astype(jnp.float32).reshape(hkv, g)[None, None, :, :, None, None]
    m = jnp.maximum(jnp.max(scores, axis=-1, keepdims=True), sink_l)
    p = jnp.exp(scores - m)
    denom = jnp.sum(p, axis=-1, keepdims=True) + jnp.exp(sink_l - m)
    out = jnp.einsum('bnhgqk,bnkhd->bnqhgd', (p / denom).astype(v.dtype), vb)
    return out.reshape(b, s, hq, d)


def neighbourhood_attn(q, k, v, rpb):
    b, s, h, d = q.shape
    rows = s // GRID_W
    kr = min(NA_ROWS_MAX, rows)
    kc = NA_COLS
    qg = q.reshape(b, rows, GRID_W, h, d)
    kg = k.reshape(b, rows, GRID_W, h, d)
    vg = v.reshape(b, rows, GRID_W, h, d)
    r = jnp.arange(rows)
    row_start = jnp.clip(r - kr // 2, 0, rows - kr)
    row_idx = row_start[:, None] + jnp.arange(kr)[None, :]
    kw = kg[:, row_idx].reshape(b, rows, kr * GRID_W, h, d)
    vw = vg[:, row_idx].reshape(b, rows, kr * GRID_W, h, d)
    col = jnp.arange(GRID_W)
    col_start = jnp.clip(col - kc // 2, 0, GRID_W - kc)
    col_mask = (col[None, :] >= col_start[:, None]) & (col[None, :] < col_start[:, None] + kc)
    mask = jnp.tile(col_mask, (1, kr))
    row_off = row_idx - r[:, None]
    col_off = jnp.clip(col[None, :] - col[:, None], -(kc - 1), kc - 1)
    bias = rpb[:, row_off[:, None, :, None] + (NA_ROWS_MAX - 1),
               col_off[None, :, None, :] + (NA_COLS - 1)]
    bias = bias.reshape(h, rows, GRID_W, kr * GRID_W).astype(jnp.float32)
    scores = jnp.einsum('brqhd,brkhd->bhrqk', qg, kw).astype(jnp.float32) * (d ** -0.5) + bias[None]
    scores = jnp.where(mask[None, None, None], scores, NEG)
    p = jax.nn.softmax(scores, axis=-1)
    out = jnp.einsum('bhrqk,brkhd->brqhd', p.astype(v.dtype), vw)
    return out.reshape(b, s, h, d)


def moe_ffn(h, w_router, b_router, w_gate_up, b_gate_up, w_down, b_down):
    b, s, d = h.shape
    t = b * s
    xt = h.reshape(t, d)
    logits = (xt @ w_router + b_router).astype(jnp.float32)
    top_val, top_idx = lax.top_k(logits, TOP_K)
    gates = jax.nn.softmax(top_val, axis=-1)
    a = t * TOP_K
    e_flat = top_idx.reshape(a)
    tok_flat = jnp.repeat(jnp.arange(t, dtype=jnp.int32), TOP_K)
    g_flat = gates.reshape(a)
    order = jnp.argsort(e_flat)
    e_sorted = e_flat[order]
    tok_sorted = tok_flat[order]
    g_sorted = g_flat[order]
    counts = jnp.bincount(e_flat, length=N_EXPERTS)
    starts = jnp.cumsum(counts) - counts
    padded = ((counts + MOE_BLOCK - 1) // MOE_BLOCK) * MOE_BLOCK
    pends = jnp.cumsum(padded)
    pstarts = pends - padded
    dest = pstarts[e_sorted] + (jnp.arange(a) - starts[e_sorted])
    n_slots = a + N_EXPERTS * MOE_BLOCK
    n_blk = n_slots // MOE_BLOCK
    slot_tok = jnp.full((n_slots,), t, dtype=jnp.int32).at[dest].set(tok_sorted)
    slot_gate = jnp.zeros((n_slots,), jnp.float32).at[dest].set(g_sorted)
    blk_expert = jnp.minimum(
        jnp.searchsorted(pends, jnp.arange(n_blk) * MOE_BLOCK, side='right'), N_EXPERTS - 1)
    x_pad = jnp.concatenate([xt, jnp.zeros((1, d), xt.dtype)], axis=0)
    xs = x_pad[slot_tok].reshape(n_blk, MOE_BLOCK, d)

    def expert_block(args):
        xb, e = args
        gu = xb @ w_gate_up[e] + b_gate_up[e]
        gate = jnp.minimum(gu[:, :D_FF], SWIGLU_LIMIT)
        up = jnp.clip(gu[:, D_FF:], -SWIGLU_LIMIT, SWIGLU_LIMIT)
        glu = gate * jax.nn.sigmoid(SWIGLU_ALPHA * gate)
        return ((up + 1.0) * glu) @ w_down[e] + b_down[e]

    ys = lax.map(expert_block, (xs, blk_expert)).reshape(n_slots, d)
    y = jax.ops.segment_sum(ys * slot_gate[:, None].astype(ys.dtype), slot_tok, num_segments=t + 1)[:t]
    return y.reshape(b, s, d)


def setup_inputs(seed: int = 0) -> dict:
    key = jax.random.key(seed)
    ks = jax.random.split(key, 20)
    f32 = jnp.float32
    nrm = lambda k, shape, sc: jax.random.normal(k, shape, f32) * sc
    return {
        "x": nrm(ks[0], (BATCH, SEQ, D_MODEL), 1.0),
        "c": nrm(ks[1], (BATCH, D_MODEL), 1.0),
        "w_ada": nrm(ks[2], (DEPTH, D_MODEL, 6 * D_MODEL), D_MODEL ** -0.5),
        "b_ada": nrm(ks[3], (DEPTH, 6 * D_MODEL), 0.02),
        "w_in": nrm(ks[4], (DEPTH, D_MODEL, D_IN), D_MODEL ** -0.5),
        "sink": nrm(ks[5], (DEPTH, A_Q_HEADS), 1.0),
        "rpb": nrm(ks[6], (DEPTH, B_HEADS, 2 * NA_ROWS_MAX - 1, 2 * NA_COLS - 1), 0.5),
        "g_out_a": 1.0 + nrm(ks[7], (DEPTH, A_Q_W), 0.02),
        "g_out_b": 1.0 + nrm(ks[8], (DEPTH, B_W), 0.02),
        "w_out": nrm(ks[9], (DEPTH, D_MIX, D_MODEL), D_MIX ** -0.5),
        "w_router": nrm(ks[10], (DEPTH, D_MODEL, N_EXPERTS), D_MODEL ** -0.5),
        "b_router": nrm(ks[11], (DEPTH, N_EXPERTS), 0.01),
        "w_gate_up": nrm(ks[12], (DEPTH, N_EXPERTS, D_MODEL, 2 * D_FF), D_MODEL ** -0.5),
        "b_gate_up": nrm(ks[13], (DEPTH, N_EXPERTS, 2 * D_FF), 0.01),
        "w_down": nrm(ks[14], (DEPTH, N_EXPERTS, D_FF, D_MODEL), D_FF ** -0.5),
        "b_down": nrm(ks[15], (DEPTH, N_EXPERTS, D_MODEL), 0.01),
        "g_final": 1.0 + nrm(ks[16], (D_MODEL,), 0.02),
    }


def reference(x, c, w_ada, b_ada, w_in, sink, rpb, g_out_a, g_out_b, w_out,
              w_router, b_router, w_gate_up, b_gate_up, w_down, b_down, g_final):
    b, s, _ = x.shape
    c_act = jax.nn.silu(c)
    for l in range(DEPTH):
        mod = c_act @ w_ada[l] + b_ada[l]
        shift_m, scale_m, gate_m, shift_f, scale_f, gate_f = jnp.split(mod, 6, axis=-1)

        h = modulate(rms_norm(x), shift_m, scale_m)
        proj = h @ w_in[l]
        qa, ka, va, qb, kb, vb = jnp.split(
            proj, np.cumsum([A_Q_W, A_KV_W, A_KV_W, B_W, B_W]).tolist(), axis=-1)
        qa = apply_partial_rope(qa.reshape(b, s, A_Q_HEADS, HEAD_DIM), s)
        ka = apply_partial_rope(ka.reshape(b, s, A_KV_HEADS, HEAD_DIM), s)
        va = va.reshape(b, s, A_KV_HEADS, HEAD_DIM)
        oa = windowed_gqa_sink(qa, ka, va, sink[l]).reshape(b, s, A_Q_W)
        ob = neighbourhood_attn(qb.reshape(b, s, B_HEADS, HEAD_DIM),
                                kb.reshape(b, s, B_HEADS, HEAD_DIM),
                                vb.reshape(b, s, B_HEADS, HEAD_DIM), rpb[l]).reshape(b, s, B_W)
        mixed = jnp.concatenate([rms_norm(oa) * g_out_a[l], rms_norm(ob) * g_out_b[l]], axis=-1)
        x = x + gate_m[:, None, :] * (mixed @ w_out[l])

        h = modulate(rms_norm(x), shift_f, scale_f)
        y = moe_ffn(h, w_router[l], b_router[l], w_gate_up[l], b_gate_up[l], w_down[l], b_down[l])
        x = x + gate_f[:, None, :] * y
    return rms_norm(x) * g_final
```

```python
import bisect
from contextlib import ExitStack

import numpy as np
import concourse.bass as bass
import concourse.mybir as mybir
from concourse.bass_utils import run_bass_kernel_spmd

F32 = mybir.dt.float32
BF16 = mybir.dt.bfloat16
I32 = mybir.dt.int32
AF = mybir.ActivationFunctionType
ALU = mybir.AluOpType
AX = mybir.AxisListType

S = 4096
D = 1024
NQB = 32
NE = 32
DFF = 1024
EPS = 1e-5
MASKV = -240000.0
MB = 512
NBLK = 64
NSLOT = NBLK * MB
THETA = 500000.0


class _Op:
    __slots__ = ("eng", "fn", "deps", "dma", "need_inc", "target")


class Sched:
    def __init__(self, nc, es, nchan=10):
        self.nc = nc
        self.engs = dict(pe=nc.tensor, act=nc.scalar, dve=nc.vector, pool=nc.gpsimd, sp=nc.sync)
        self.sem = {e: es.enter_context(nc.semaphore("sem_" + e)) for e in ("pe", "act", "dve", "pool")}
        nch = {"sp": 12, "pool": 28}
        self.chan = {q: [es.enter_context(nc.semaphore(f"ch_{q}{i}")) for i in range(nch[q])] for q in ("sp", "pool")}
        self.chan_cnt = {q: [0] * nch[q] for q in ("sp", "pool")}
        self.chan_next = {q: 0 for q in ("sp", "pool")}
        self.ops = []
        self.flushed = 0
        self.last_writer = {}
        self.readers = {}
        self.cnt = {e: 0 for e in self.sem}
        self.incs = {e: ([], []) for e in self.sem}
        self.waited = {}

    def add(self, eng, fn, reads=(), writes=(), dma=False):
        op = _Op()
        op.eng, op.fn, op.dma, op.need_inc, op.target = eng, fn, dma, False, None
        deps = set()
        for r in reads:
            w = self.last_writer.get(r)
            if w is not None:
                deps.add(w)
        for w_ in writes:
            w = self.last_writer.get(w_)
            if w is not None:
                deps.add(w)
            for r in self.readers.get(w_, ()):
                deps.add(r)
        idx = len(self.ops)
        deps.discard(idx)
        op.deps = deps
        for r in reads:
            self.readers.setdefault(r, []).append(idx)
        for w_ in writes:
            self.last_writer[w_] = idx
            self.readers[w_] = []
        self.ops.append(op)
        return idx

    def dma(self, q, fn, reads=(), writes=()):
        return self.add(q, fn, reads, writes, dma=True)

    def _wait(self, ceng, sem, val):
        key = (ceng, id(sem))
        if self.waited.get(key, 0) >= val:
            return
        self.waited[key] = val
        self.engs[ceng].wait_ge(sem, val)

    def flush(self, final=False):
        ops = self.ops
        lo, hi = self.flushed, len(ops)
        last_of = {}
        for i in range(lo, hi):
            op = ops[i]
            if not op.dma:
                last_of[op.eng] = i
            for d in op.deps:
                dop = ops[d]
                if d >= lo and not dop.dma:
                    if dop.eng == "pe" and op.eng == "pe" and not op.dma:
                        continue
                    dop.need_inc = True
        for e, i in last_of.items():
            ops[i].need_inc = True
        for i in range(lo, hi):
            op = ops[i]
            ceng = op.eng
            if op.dma:
                q = ceng
                c = self.chan_next[q]
                self.chan_next[q] = (c + 1) % len(self.chan[q])
                csem = self.chan[q][c]
                if self.chan_cnt[q][c] > 0:
                    self._wait(q, csem, 16 * self.chan_cnt[q][c])
            for d in sorted(op.deps):
                dop = ops[d]
                if dop.dma:
                    self._wait(ceng, dop.target[0], dop.target[1])
                else:
                    if dop.eng == "pe" and ceng == "pe" and not op.dma:
                        continue
                    il, cl = self.incs[dop.eng]
                    if dop.target is not None:
                        tv = dop.target[1]
                    else:
                        j = bisect.bisect_left(il, d)
                        if j < len(il):
                            tv = cl[j]
                        else:
                            raise RuntimeError("no covering inc")
                    self._wait(ceng, self.sem[dop.eng], tv)
            ins = op.fn()
            if op.dma:
                self.chan_cnt[q][c] += 1
                ins.then_inc(csem, 16)
                op.target = (csem, 16 * self.chan_cnt[q][c])
            elif op.need_inc:
                self.cnt[ceng] += 1
                ins.then_inc(self.sem[ceng], 1)
                op.target = (self.sem[ceng], self.cnt[ceng])
                self.incs[ceng][0].append(i)
                self.incs[ceng][1].append(self.cnt[ceng])
            op.fn = None
        self.flushed = hi

    def barrier(self):
        self.flush()
        for ceng in ("pe", "act", "dve", "pool", "sp"):
            for e, sem in self.sem.items():
                if self.cnt[e] > 0:
                    self._wait(ceng, sem, self.cnt[e])
            for q in ("sp", "pool"):
                for c, csem in enumerate(self.chan[q]):
                    if self.chan_cnt[q][c] > 0:
                        self._wait(ceng, csem, 16 * self.chan_cnt[q][c])

    def finish(self, out_keys):
        self.flush()
        for k in out_keys:
            w = self.last_writer.get(k)
            if w is not None:
                t = self.ops[w].target
                self._wait("sp", t[0], t[1])
        for q in ("sp", "pool"):
            for c, csem in enumerate(self.chan[q]):
                if self.chan_cnt[q][c] > 0:
                    self._wait("sp", csem, 16 * self.chan_cnt[q][c])


def _rope_tables():
    inv_freq = (np.float32(THETA) ** (-np.arange(0, 16, 2, dtype=np.float32) / np.float32(16))).astype(np.float32)
    pos = np.arange(S, dtype=np.float32)
    ang = (pos[:, None] * inv_freq[None, :]).astype(np.float32)
    cos = np.cos(ang).astype(np.float32)
    sin = np.sin(ang).astype(np.float32)
    cosT = np.ones((128, S), np.float32)
    sinT = np.zeros((128, S), np.float32)
    for hh in range(2):
        b = hh * 64
        for d in range(8):
            cosT[b + d] = cos[:, d]
            cosT[b + 8 + d] = cos[:, d]
            sinT[b + d] = -sin[:, d]
            sinT[b + 8 + d] = sin[:, d]
    return cosT, sinT


def _mask_a():
    k = np.arange(128)[:, None, None]
    m = np.arange(3)[None, :, None]
    q = np.arange(128)[None, None, :]
    rel = (m - 1) * 128 + k - q
    return np.where(np.abs(rel) <= 128, 0.0, MASKV).astype(np.float32)


def _b_geometry():
    rows = 64
    rs = np.clip(np.arange(rows) - 4, 0, rows - 8)
    cs = np.clip(np.arange(64) - 8, 0, 64 - 16)

    def valid_row(kr, r):
        return (0 <= kr < rows) and (rs[r] <= kr < rs[r] + 8)

    colmask = np.zeros((64, 64), bool)
    for qc in range(64):
        colmask[qc, cs[qc]:cs[qc] + 16] = True
    masks = {}
    kbs = {}
    for i in range(NQB):
        mk = np.full((128, 7, 128), MASKV, np.float32)
        used = []
        for m in range(7):
            kb = i + m - 3
            if kb < 0 or kb > 31:
                continue
            anyv = False
            for a in range(2):
                for b in range(2):
                    if valid_row(2 * kb + a, 2 * i + b):
                        anyv = True
                        blk = np.where(colmask.T, 0.0, MASKV)
                        mk[a * 64:(a + 1) * 64, m, b * 64:(b + 1) * 64] = blk
            if anyv:
                used.append(m)
        masks[i] = mk
        kbs[i] = used
    variants = []
    var_of = {}
    for i in range(NQB):
        for vi, v in enumerate(variants):
            if np.array_equal(v, masks[i]):
                var_of[i] = vi
                break
        else:
            var_of[i] = len(variants)
            variants.append(masks[i])
    return np.stack(variants), var_of, kbs


def _bias_index():
    a = np.arange(2)[:, None, None, None, None]
    kc = np.arange(64)[None, :, None, None, None]
    m = np.arange(7)[None, None, :, None, None]
    b = np.arange(2)[None, None, None, :, None]
    qc = np.arange(64)[None, None, None, None, :]
    dr = np.clip(2 * (m - 3) + a - b, -7, 7) + 7
    co = np.clip(kc - qc, -15, 15) + 15
    dr = np.broadcast_to(dr, (2, 64, 7, 2, 64)).reshape(128, 7, 128)
    co = np.broadcast_to(co, (2, 64, 7, 2, 64)).reshape(128, 7, 128)
    return dr, co


_MASKB, _VAR_OF, _KBS = _b_geometry()
_NVAR = _MASKB.shape[0]
_PERM = np.concatenate([np.arange(8, 16), np.arange(0, 8), np.arange(16, 64)])

NWA = 1536 + 128
NWB = 1536


def _layout_w_in(w):
    qa, ka, va = w[:, 0:512], w[:, 512:640], w[:, 640:768]
    qb, kb, vb = w[:, 768:1280], w[:, 1280:1792], w[:, 1792:2304]
    qap = qa.reshape(D, 8, 64)[:, :, _PERM].reshape(D, 512)
    k0, k1 = ka[:, 0:64], ka[:, 64:128]
    k0p, k1p = k0[:, _PERM], k1[:, _PERM]
    wa = np.concatenate([qa, qap, k0, k0, k1, k1, k0p, k0p, k1p, k1p, va], axis=1)
    wb = np.concatenate([qb, kb, vb], axis=1)
    return np.ascontiguousarray(wa), np.ascontiguousarray(wb)


def build_program(stop_after=None, dbg=False):
    nc = bass.Bass("TRN2", target_bir_lowering=False)
    es = ExitStack()

    def din(name, shape, dt=F32):
        return nc.dram_tensor(name, list(shape), dt, kind="ExternalInput").ap()

    x_d = din("x", [S, D])
    cT_d = din("cT", [128, 8])
    wada_d = din("w_ada", [D, 6 * D])
    bada_d = din("b_ada", [1, 6 * D])
    wa_d = din("w_a", [D, NWA])
    wb_d = din("w_b", [D, NWB])
    sink_d = din("sink", [1, 8])
    biasu_d = din("biasu", [8, 128, 7 * 128])
    goa_d = din("g_out_a", [1, 512])
    gob_d = din("g_out_b", [1, 512])
    wout_d = din("w_out", [D, D])
    wr_d = din("w_router", [D, NE])
    br_d = din("b_router", [1, NE])
    if stop_after is None:
        wgu_d = din("w_gate_up", [NE * D, 2 * DFF])
        bgu_d = din("b_gate_up", [NE * 128, 16])
        wd_d = din("w_down", [NE * DFF, D])
        bd_d = din("b_down", [NE, D])
    gfin_d = din("g_final", [1, D])
    cos_d = din("cosT", [128, S])
    sin_d = din("sinT", [128, S])
    maska_d = din("maska", [128, 3 * 128])
    maskb_d = din("maskb", [_NVAR, 128, 7 * 128])
    ident_d = din("ident", [128, 128])
    tri_d = din("tri", [128, 128])
    rowid_d = din("rowid", [128, 8])
    blkth_d = din("blkth", [128, NBLK * NE])
    out_d = nc.dram_tensor("out", [S, D], F32, kind="ExternalOutput").ap()

    def dscr(name, shape, dt):
        kind = "ExternalOutput" if (dbg and name not in ("xs_s", "ys_s")) else "Internal"
        return nc.dram_tensor(name, list(shape), dt, kind=kind).ap()

    mixa_d = dscr("mixa_s", [S, 512], BF16)
    mixb_d = dscr("mixb_s", [S, 512], BF16)
    x1_d = dscr("x1_s", [S, D], F32)
    h2_d = dscr("h2_s", [S, D], BF16)
    if stop_after not in ("A", "B"):
        xs_d = dscr("xs_s", [NSLOT, D], BF16)
        ys_d = dscr("ys_s", [NSLOT, D], F32)
    dbg_d = {}
    if dbg:
        dbg_d["qta"] = nc.dram_tensor("dbg_qta", [128, 6 * S], BF16, kind="ExternalOutput").ap()
        dbg_d["gw"] = nc.dram_tensor("dbg_gw", [128, NQB * NE], F32, kind="ExternalOutput").ap()
        dbg_d["dsel"] = nc.dram_tensor("dbg_dsel", [128, NQB * 4], I32, kind="ExternalOutput").ap()
        dbg_d["blke"] = nc.dram_tensor("dbg_blke", [128, NBLK], F32, kind="ExternalOutput").ap()
        dbg_d["mod"] = nc.dram_tensor("dbg_mod", [128, 6 * D], F32, kind="ExternalOutput").ap()

    sc = Sched(nc, es)
    dbg_keys = []

    DUMPS = dict(ptA=([128, 384], BF16), poA=([128, 1024], F32), vpa=([128, NQB * 2 * 65], BF16), esink=([128, 8], F32),
                 goa=([128, 512], F32), oaA=([128, 512], F32), denA=([128, 8], F32), ssqA=([128, 1], F32))
    dump_d = {k: nc.dram_tensor("dbg_" + k, v[0], v[1], kind="ExternalOutput").ap() for k, v in DUMPS.items()} if dbg else {}

    def dump(name, ap, shape, dt, reads):
        if not dbg:
            return
        d = dump_d[name]
        sc.dma("sp", lambda: nc.sync.dma_start(out=d, in_=ap), reads=reads, writes=["dbg_" + name])
        dbg_keys.append("dbg_" + name)
    A = sc.add
    T, V, G, ACT, SP = nc.tensor, nc.vector, nc.gpsimd, nc.scalar, nc.sync

    def sb(stack, name, shape, dt=F32):
        return stack.enter_context(nc.sbuf_tensor("s_" + name, list(shape), dt))

    PS = [es.enter_context(nc.psum_tensor(f"ps{i}", [128, 1024], F32)) for i in range(4)]

    def bank(i):
        return PS[i // 2][:, (i % 2) * 512:(i % 2 + 1) * 512], ("ps", i)

    ident_f = sb(es, "ident_f", [128, 128], F32)
    ident_b = sb(es, "ident_b", [128, 128], BF16)
    ones_f = sb(es, "ones_f", [128, 128], F32)
    mod = sb(es, "mod", [128, 6 * D], F32)
    epsb = sb(es, "epsb", [128, 1], F32)
    sc.dma("sp", lambda: SP.dma_start(out=ident_f[:], in_=ident_d[:, :]), writes=["ident_f"])
    sc.dma("pool", lambda: G.dma_start(out=ident_b[:], in_=ident_d[:, :]), writes=["ident_b"])
    A("dve", lambda: V.memset(ones_f[:], 1.0), writes=["ones_f"])
    A("dve", lambda: V.memset(epsb[:], EPS), writes=["epsb"])

    with ExitStack() as p0:
        cT = sb(p0, "cT", [128, 8], F32)
        cact = sb(p0, "cact", [128, 8], F32)
        csig = sb(p0, "csig", [128, 8], F32)
        crep = sb(p0, "crep", [128, 8 * 128], F32)
        bada = sb(p0, "bada", [1, 6 * D], F32)
        wsl = [sb(p0, f"wsl{i}", [128, 8 * 512], F32) for i in range(2)]
        sc.dma("sp", lambda: SP.dma_start(out=cT[:], in_=cT_d[:, :]), writes=["cT"])
        sc.dma("sp", lambda: SP.dma_start(out=bada[:], in_=bada_d[:, :]), writes=["bada"])
        A("act", lambda: ACT.activation(out=csig[:], in_=cT[:], func=AF.Sigmoid), reads=["cT"], writes=["csig"])
        A("dve", lambda: V.tensor_tensor(out=cact[:], in0=cT[:], in1=csig[:], op=ALU.mult), reads=["cT", "csig"], writes=["cact"])
        A("dve", lambda: V.tensor_copy(out=crep[:].rearrange("p (c m) -> p c m", m=128),
                                       in_=cact[:].unsqueeze(2).to_broadcast([128, 8, 128])),
          reads=["cact"], writes=["crep"])
        for n in range(12):
            slot = n % 2
            w_t = wsl[slot]
            sc.dma("sp", lambda w_t=w_t, n=n: SP.dma_start(
                out=w_t[:].rearrange("p (c n) -> p c n", n=512),
                in_=wada_d[:, n * 512:(n + 1) * 512].rearrange("(c p) n -> p c n", p=128)),
                writes=[("wsl", slot)])
            pb, pk = bank(n % 2)
            for c in range(8):
                A("pe", lambda pb=pb, w_t=w_t, c=c: T.matmul(pb, lhsT=crep[:, c * 128:(c + 1) * 128],
                                                             rhs=w_t[:, c * 512:(c + 1) * 512], start=(c == 0), stop=False),
                  reads=["crep", ("wsl", slot)], writes=[pk])
            A("pe", lambda pb=pb, n=n: T.matmul(pb, lhsT=ones_f[0:1, :], rhs=bada[0:1, n * 512:(n + 1) * 512],
                                                start=False, stop=True),
              reads=["ones_f", "bada"], writes=[pk])
            if (n // 2) % 3 == 1:
                A("dve", lambda pb=pb, n=n: V.tensor_scalar(out=mod[:, n * 512:(n + 1) * 512], in0=pb, scalar1=1.0,
                                                            scalar2=None, op0=ALU.add), reads=[pk], writes=[("mod", n)])
            else:
                A("act", lambda pb=pb, n=n: ACT.copy(out=mod[:, n * 512:(n + 1) * 512], in_=pb), reads=[pk], writes=[("mod", n)])
        sc.barrier()
    MODK = [("mod", n) for n in range(12)]
    shift_m, scale1_m, gate_m = mod[:, 0:D], mod[:, D:2 * D], mod[:, 2 * D:3 * D]
    shift_f, scale1_f, gate_f = mod[:, 3 * D:4 * D], mod[:, 4 * D:5 * D], mod[:, 5 * D:6 * D]
    if dbg:
        sc.dma("sp", lambda: SP.dma_start(out=dbg_d["mod"][:, :], in_=mod[:]), reads=MODK, writes=["dbg_mod"])

    def rmsnorm_mod(stack_tiles, src, src_keys, scale1, shift, out_bf=None, out_f32=None, out_keys=(), tag=""):
        junk, ssq, rstd, tmp = stack_tiles
        A("act", lambda: ACT.activation(out=junk[:], in_=src, func=AF.Square, accum_out=ssq[:]),
          reads=list(src_keys), writes=["junk" + tag, "ssq" + tag])
        A("dve", lambda: V.tensor_scalar(out=rstd[:], in0=ssq[:], scalar1=1.0 / D, scalar2=EPS, op0=ALU.mult, op1=ALU.add),
          reads=["ssq" + tag], writes=["rstd" + tag])
        A("act", lambda: ACT.activation(out=rstd[:], in_=rstd[:], func=AF.Ln), reads=["rstd" + tag], writes=["rstd" + tag])
        A("act", lambda: ACT.activation(out=rstd[:], in_=rstd[:], func=AF.Exp, scale=-0.5), reads=["rstd" + tag], writes=["rstd" + tag])
        A("dve", lambda: V.scalar_tensor_tensor(out=tmp[:], in0=src, scalar=rstd[:, 0:1], in1=scale1, op0=ALU.mult, op1=ALU.mult),
          reads=list(src_keys) + ["rstd" + tag] + MODK, writes=["tmp" + tag])
        if out_f32 is not None:
            A("pool", lambda: G.tensor_tensor(out=out_f32, in0=tmp[:], in1=shift, op=ALU.add),
              reads=["tmp" + tag] + MODK, writes=list(out_keys))
            if out_bf is not None:
                A("act", lambda: ACT.copy(out=out_bf, in_=out_f32), reads=list(out_keys), writes=[k + ("bf",) for k in out_keys])
        else:
            A("pool", lambda: G.tensor_tensor(out=out_bf, in0=tmp[:], in1=shift, op=ALU.add),
              reads=["tmp" + tag] + MODK, writes=list(out_keys))

    def projection_pass(ps_, wmat_d, ncols, ngroups, emit_group, emit_v, vcol0, nvcols, tag):
        wsb = sb(ps_, "wsb" + tag, [128, 8 * ncols], BF16)
        w3 = wsb[:].rearrange("p (c n) -> p c n", n=ncols)
        for c in range(8):
            sc.dma("pool", lambda c=c: G.dma_start(out=w3[:, c, :], in_=wmat_d[c * 128:(c + 1) * 128, :]),
                   writes=[("wsb" + tag, c)])
        WK = [("wsb" + tag, c) for c in range(8)]
        xbl = [sb(ps_, f"xbl{tag}{i}", [128, D], F32) for i in range(2)]
        hbf = [sb(ps_, f"hbf{tag}{i}", [128, D], BF16) for i in range(2)]
        hT = [sb(ps_, f"hT{tag}{i}", [128, 8 * 512], BF16) for i in range(2)]
        tiles = [(sb(ps_, f"junk{tag}{j}", [128, D], BF16), sb(ps_, f"ssq{tag}{j}", [128, 1], F32),
                  sb(ps_, f"rstd{tag}{j}", [128, 1], F32), sb(ps_, f"tmp{tag}{j}", [128, D], F32)) for j in range(2)]
        for tc in range(8):
            hslot = tc % 2
            hT3 = hT[hslot][:].rearrange("p (c t) -> p c t", t=512)
            for sub in range(4):
                i = tc * 4 + sub
                xs_ = i % 2
                sc.dma("sp", lambda i=i, xs_=xs_: SP.dma_start(out=xbl[xs_][:], in_=x_d[i * 128:(i + 1) * 128, :]),
                       writes=[("xbl" + tag, xs_)])
                rmsnorm_mod(tiles[xs_], xbl[xs_][:], [("xbl" + tag, xs_)], scale1_m, shift_m, out_bf=hbf[xs_][:],
                            out_keys=[("hbf" + tag, xs_)], tag=tag + str(xs_))
                pbT = PS[0][:, (i % 2) * 512:(i % 2 + 1) * 512].bitcast(BF16)
                pkT = ("ps", i % 2)
                for c in range(8):
                    A("pe", lambda c=c, pbT=pbT, xs_=xs_: T.transpose(out=pbT[:, c * 128:(c + 1) * 128],
                                                                      in_=hbf[xs_][:, c * 128:(c + 1) * 128], identity=ident_b[:]),
                      reads=[("hbf" + tag, xs_), "ident_b"], writes=[pkT])
                A("act", lambda pbT=pbT, hT3=hT3, sub=sub: ACT.copy(out=hT3[:, :, sub * 128:(sub + 1) * 128],
                                                                    in_=pbT.rearrange("p (c t) -> p c t", t=128)),
                  reads=[pkT], writes=[("hT" + tag, hslot, sub)])
                pv, pvk = bank(2 + (i % 2))
                for c in range(8):
                    A("pe", lambda c=c, pv=pv, hT3=hT3, sub=sub: T.matmul(
                        pv[:, 0:nvcols], lhsT=hT3[:, c, sub * 128:(sub + 1) * 128], rhs=w3[:, c, vcol0:vcol0 + nvcols],
                        start=(c == 0), stop=(c == 7)),
                      reads=[("hT" + tag, hslot, sub)] + WK, writes=[pvk])
                emit_v(i, pv, pvk)
            HK = [("hT" + tag, hslot, s_) for s_ in range(4)]
            emit_group(tc, hT3, HK, w3, WK)
        return

    with ExitStack() as pa:
        qTa = sb(pa, "qTa", [128, 6 * S], BF16)
        qTa3 = qTa[:].rearrange("p (g t) -> p g t", t=S)
        vpa = sb(pa, "vpa", [128, NQB * 2 * 65], BF16)
        vpa4 = vpa[:].rearrange("p (i g d) -> p i g d", g=2, d=65)
        A("pool", lambda: G.memset(vpa[:], 1.0), writes=["vpa_init"])
        with ExitStack() as pa1:
            cosT = sb(pa1, "cosT", [128, S], F32)
            sinT = sb(pa1, "sinT", [128, S], F32)
            sc.dma("sp", lambda: SP.dma_start(out=cosT[:], in_=cos_d[:, :]), writes=["cosT"])
            sc.dma("sp", lambda: SP.dma_start(out=sinT[:], in_=sin_d[:, :]), writes=["sinT"])
            rt = [sb(pa1, f"rt{i}", [128, 512], F32) for i in range(4)]

            def emit_v_a(i, pv, pvk):
                A("act", lambda: ACT.copy(out=vpa4[:, i, :, 0:64], in_=pv[:, 0:128].rearrange("p (g d) -> p g d", d=64)),
                  reads=[pvk, "vpa_init"], writes=[("vpa", i)])

            def emit_group_a(tc, hT3, HK, w3, WK):
                for g in range(6):
                    c0 = g * 128 if g < 4 else 1024 + (g - 4) * 256
                    c1 = 512 + g * 128 if g < 4 else 1024 + 512 + (g - 4) * 256
                    if g >= 4:
                        c0 = 1024 + (g - 4) * 128
                        c1 = 1024 + 256 + (g - 4) * 128
                    pq, pqk = bank(4 + (g % 2) * 2)
                    pp, ppk = bank(5 + (g % 2) * 2)
                    for (pb, pk, col) in ((pq, pqk, c0), (pp, ppk, c1)):
                        for c in range(8):
                            A("pe", lambda pb=pb, c=c, col=col: T.matmul(pb, lhsT=w3[:, c, col:col + 128], rhs=hT3[:, c, :],
                                                                         start=(c == 0), stop=(c == 7)),
                              reads=HK + WK, writes=[pk])
                    r0, r1 = rt[(g % 2) * 2], rt[(g % 2) * 2 + 1]
                    k0, k1 = ("rt", (g % 2) * 2), ("rt", (g % 2) * 2 + 1)
                    tsl = slice(tc * 512, (tc + 1) * 512)
                    A("dve", lambda pq=pq, r0=r0, tsl=tsl: V.tensor_tensor(out=r0[:], in0=pq, in1=cosT[:, tsl], op=ALU.mult),
                      reads=[pqk, "cosT"], writes=[k0])
                    A("dve", lambda pp=pp, r1=r1, tsl=tsl: V.tensor_tensor(out=r1[:], in0=pp, in1=sinT[:, tsl], op=ALU.mult),
                      reads=[ppk, "sinT"], writes=[k1])
                    A("pool", lambda r0=r0, r1=r1, g=g, tsl=tsl: G.tensor_tensor(out=qTa3[:, g, tsl], in0=r0[:], in1=r1[:], op=ALU.add),
                      reads=[k0, k1], writes=[("qTa", g, tc)])

            projection_pass(pa1, wa_d, NWA, 12, emit_group_a, emit_v_a, 1536, 128, "A")
            sc.barrier()
        if dbg:
            sc.dma("sp", lambda: SP.dma_start(out=dbg_d["qta"][:, :], in_=qTa[:]),
                   reads=[("qTa", g, tc) for g in range(6) for tc in range(8)], writes=["dbg_qta"])

        with ExitStack() as pa2:
            if stop_after not in ("A", "B"):
                zt = sb(pa2, "zt", [128, 4 * D], BF16)
                A("pool", lambda: G.memset(zt[:], 0.0), writes=["zt"])
                for b in range(NBLK):
                    sc.dma("sp", lambda b=b: SP.dma_start(out=xs_d[b * MB:(b + 1) * MB, :].rearrange("(s p) d -> p s d", p=128),
                                                        in_=zt[:].rearrange("p (s d) -> p s d", d=D)), reads=["zt"], writes=["xs_d"])
            maska = sb(pa2, "maska", [128, 384], BF16)
            sc.dma("pool", lambda: G.dma_start(out=maska[:], in_=maska_d[:, :]), writes=["maska"])
            esink = sb(pa2, "esink", [128, 8], F32)
            sc.dma("sp", lambda: SP.dma_start(out=esink[:], in_=sink_d[:, :].partition_broadcast(128)), writes=["esink0"])
            A("act", lambda: ACT.activation(out=esink[:], in_=esink[:], func=AF.Exp), reads=["esink0"], writes=["esink"])
            goa = sb(pa2, "goa", [128, 512], F32)
            sc.dma("sp", lambda: SP.dma_start(out=goa[:], in_=goa_d[:, :].partition_broadcast(128)), writes=["goa"])
            pt = [sb(pa2, f"pta{i}", [128, 384], BF16) for i in range(3)]
            den = sb(pa2, "dena", [128, 8], F32)
            oa = sb(pa2, "oa", [128, 512], F32)
            junk2 = sb(pa2, "junk2a", [128, 512], BF16)
            ssq2 = sb(pa2, "ssq2a", [128, 1], F32)
            mixa = [sb(pa2, f"mixa{i}", [128, 512], BF16) for i in range(2)]
            for i in range(NQB):
                tcq = i // 4
                ms = [m for m in range(3) if 0 <= i + m - 1 < NQB]
                po = PS[3 - (i % 2)]
                pok = ("ps", 6 - 2 * (i % 2))
                po4 = po[:].rearrange("p (b x) -> p b x", b=2)[:, :, 0:260].rearrange("p b (h d) -> p b h d", d=65)
                def qk_a(h):
                    g, off = h // 2, (h % 2) * 64
                    kg = 4 + h // 4
                    pst, pstk = bank(h % 3)
                    for m in ms:
                        kb = i + m - 1
                        A("pe", lambda pst=pst, m=m, kb=kb, g=g, off=off, kg=kg, i=i: T.matmul(
                            pst[:, m * 128:(m + 1) * 128], lhsT=qTa3[off:off + 64, kg, kb * 128:(kb + 1) * 128],
                            rhs=qTa3[off:off + 64, g, i * 128:(i + 1) * 128], start=True, stop=False),
                          reads=[("qTa", kg, kb // 4), ("qTa", g, tcq)], writes=[pstk])
                        A("pe", lambda pst=pst, m=m: T.matmul(pst[:, m * 128:(m + 1) * 128], lhsT=ident_b[:],
                                                              rhs=maska[:, m * 128:(m + 1) * 128], start=False, stop=True),
                          reads=["ident_b", "maska"], writes=[pstk])

                qk_a(0)
                for h in range(8):
                    if h + 1 < 8:
                        qk_a(h + 1)
                    pst, pstk = bank(h % 3)
                    ptt = pt[h % 3]
                    ptk = ("pta", h % 3)
                    lo, hi = ms[0] * 128, (ms[-1] + 1) * 128
                    A("act", lambda pst=pst, ptt=ptt, lo=lo, hi=hi: ACT.activation(out=ptt[:, lo:hi], in_=pst[:, lo:hi],
                                                                                   func=AF.Exp, scale=0.125),
                      reads=[pstk], writes=[ptk])
                    for m in ms:
                        kb = i + m - 1
                        A("pe", lambda ptt=ptt, m=m, kb=kb, h=h, ms=ms, po=po: T.matmul(
                            po[:, (h // 4) * 512 + (h % 4) * 65:(h // 4) * 512 + (h % 4) * 65 + 65],
                            lhsT=ptt[:, m * 128:(m + 1) * 128], rhs=vpa4[:, kb, h // 4, :],
                            start=(m == ms[0]), stop=(m == ms[-1])),
                          reads=[ptk, ("vpa", kb)], writes=[pok])
                if i == 4:
                    dump("ptA", pt[7 % 3][:], [128, 384], BF16, [("pta", 7 % 3)])
                    if dbg:
                        podbg = sb(pa2, "podbg", [128, 1024], F32)
                        A("act", lambda: ACT.copy(out=podbg[:], in_=po[:]), reads=[pok], writes=["podbg"])
                        dump("poA", podbg[:], [128, 1024], F32, ["podbg"])
                    dump("vpa", vpa[:], [128, NQB * 2 * 65], BF16, [("vpa", kk) for kk in range(NQB)])
                    dump("esink", esink[:], [128, 8], F32, ["esink"])
                    dump("goa", goa[:], [128, 512], F32, ["goa"])
                A("dve", lambda po4=po4: V.tensor_tensor(out=den[:].rearrange("p (b h) -> p b h", b=2), in0=po4[:, :, :, 64],
                                                         in1=esink[:].rearrange("p (b h) -> p b h", b=2), op=ALU.add),
                  reads=[pok, "esink"], writes=["dena"])
                A("dve", lambda: V.reciprocal(out=den[:], in_=den[:]), reads=["dena"], writes=["dena"])
                A("dve", lambda po4=po4: V.tensor_tensor(
                    out=oa[:].rearrange("p (b h d) -> p b h d", b=2, d=64), in0=po4[:, :, :, 0:64],
                    in1=den[:].rearrange("p (b h) -> p b h", b=2).unsqueeze(3).to_broadcast([128, 2, 4, 64]), op=ALU.mult),
                  reads=[pok, "dena"], writes=["oa"])
                A("act", lambda: ACT.activation(out=junk2[:], in_=oa[:], func=AF.Square, accum_out=ssq2[:]),
                  reads=["oa"], writes=["junk2a", "ssq2a"])
                A("dve", lambda: V.tensor_scalar(out=ssq2[:], in0=ssq2[:], scalar1=1.0 / 512, scalar2=EPS, op0=ALU.mult, op1=ALU.add),
                  reads=["ssq2a"], writes=["ssq2a"])
                A("act", lambda: ACT.activation(out=ssq2[:], in_=ssq2[:], func=AF.Ln), reads=["ssq2a"], writes=["ssq2a"])
                A("act", lambda: ACT.activation(out=ssq2[:], in_=ssq2[:], func=AF.Exp, scale=-0.5), reads=["ssq2a"], writes=["ssq2a"])
                if i == 4:
                    dump("oaA", oa[:], [128, 512], F32, ["oa"])
                    dump("denA", den[:], [128, 8], F32, ["dena"])
                    dump("ssqA", ssq2[:], [128, 1], F32, ["ssq2a"])
                mx = mixa[i % 2]
                A("dve", lambda mx=mx: V.scalar_tensor_tensor(out=mx[:], in0=oa[:], scalar=ssq2[:, 0:1], in1=goa[:],
                                                              op0=ALU.mult, op1=ALU.mult),
                  reads=["oa", "ssq2a", "goa"], writes=[("mixa", i % 2)])
                sc.dma("sp", lambda mx=mx, i=i: SP.dma_start(out=mixa_d[i * 128:(i + 1) * 128, :], in_=mx[:]),
                       reads=[("mixa", i % 2)], writes=[("mixa_d", i)])
            sc.barrier()
    if stop_after == "A":
        sc.finish([("mixa_d", i) for i in range(NQB)] + ["dbg_qta", "dbg_mod"] + dbg_keys)
        es.close()
        return nc

    with ExitStack() as pb_:
        qTb = sb(pb_, "qTb", [128, 8 * S], BF16)
        qTb3 = qTb[:].rearrange("p (g t) -> p g t", t=S)
        vpb = sb(pb_, "vpb", [128, NQB * 8 * 65], BF16)
        vpb4 = vpb[:].rearrange("p (i g d) -> p i g d", g=8, d=65)
        A("pool", lambda: G.memset(vpb[:], 1.0), writes=["vpb_init"])
        with ExitStack() as pb1:
            def emit_v_b(i, pv, pvk):
                A("act", lambda: ACT.copy(out=vpb4[:, i, :, 0:64], in_=pv[:, 0:512].rearrange("p (g d) -> p g d", d=64)),
                  reads=[pvk, "vpb_init"], writes=[("vpb", i)])

            def emit_group_b(tc, hT3, HK, w3, WK):
                for g in range(8):
                    pq, pqk = bank(4 + g % 4)
                    for c in range(8):
                        A("pe", lambda pq=pq, c=c, g=g: T.matmul(pq, lhsT=w3[:, c, g * 128:(g + 1) * 128], rhs=hT3[:, c, :],
                                                                 start=(c == 0), stop=(c == 7)),
                          reads=HK + WK, writes=[pqk])
                    tsl = slice(tc * 512, (tc + 1) * 512)
                    if g % 2 == 0:
                        A("dve", lambda pq=pq, g=g, tsl=tsl: V.tensor_copy(out=qTb3[:, g, tsl], in_=pq), reads=[pqk], writes=[("qTb", g, tc)])
                    else:
                        A("act", lambda pq=pq, g=g, tsl=tsl: ACT.copy(out=qTb3[:, g, tsl], in_=pq), reads=[pqk], writes=[("qTb", g, tc)])

            projection_pass(pb1, wb_d, NWB, 8, emit_group_b, emit_v_b, 1024, 512, "B")
            sc.barrier()

        with ExitStack() as pb2:
            maskb = sb(pb2, "maskb", [128, _NVAR * 896], BF16)
            for v in range(_NVAR):
                sc.dma("pool", lambda v=v: G.dma_start(out=maskb[:, v * 896:(v + 1) * 896], in_=maskb_d[v, :, :]), writes=[("maskb", v)])
            biasu = sb(pb2, "biasu", [128, 8 * 896], F32)
            for h in range(8):
                sc.dma("sp", lambda h=h: SP.dma_start(out=biasu[:, h * 896:(h + 1) * 896], in_=biasu_d[h, :, :]), writes=[("biasu", h)])
            gob = sb(pb2, "gob", [128, 512], F32)
            sc.dma("sp", lambda: SP.dma_start(out=gob[:], in_=gob_d[:, :].partition_broadcast(128)), writes=["gob"])
            tt = [sb(pb2, f"ttb{i}", [128, 896], F32) for i in range(2)]
            pt = [sb(pb2, f"ptb{i}", [128, 896], BF16) for i in range(2)]
            den = sb(pb2, "denb", [128, 8], F32)
            ob = sb(pb2, "ob", [128, 512], F32)
            junk2 = sb(pb2, "junk2b", [128, 512], BF16)
            ssq2 = sb(pb2, "ssq2b", [128, 1], F32)
            mixb = [sb(pb2, f"mixb{i}", [128, 512], BF16) for i in range(2)]
            for i in range(NQB):
                tcq = i // 4
                ms = _KBS[i]
                var = _VAR_OF[i]
                po = PS[3 - (i % 2)]
                pok = ("ps", 6 - 2 * (i % 2))
                po4 = po[:].rearrange("p (b x) -> p b x", b=2)[:, :, 0:260].rearrange("p b (h d) -> p b h d", d=65)
                lo, hi = ms[0] * 128, (ms[-1] + 1) * 128
                def qk_b(h):
                    g, off, kg = h // 2, (h % 2) * 64, 4 + h // 2
                    sl = h % 2
                    pst = PS[sl]
                    pstk = [("ps", 2 * sl), ("ps", 2 * sl + 1)]
                    for m in ms:
                        kb = i + m - 3
                        A("pe", lambda pst=pst, m=m, kb=kb, g=g, off=off, kg=kg, i=i: T.matmul(
                            pst[:, m * 128:(m + 1) * 128], lhsT=qTb3[off:off + 64, kg, kb * 128:(kb + 1) * 128],
                            rhs=qTb3[off:off + 64, g, i * 128:(i + 1) * 128], start=True, stop=False),
                          reads=[("qTb", kg, kb // 4), ("qTb", g, tcq)], writes=pstk)
                        A("pe", lambda pst=pst, m=m, var=var: T.matmul(pst[:, m * 128:(m + 1) * 128], lhsT=ident_b[:],
                                                                       rhs=maskb[:, var * 896 + m * 128:var * 896 + (m + 1) * 128],
                                                                       start=False, stop=True),
                          reads=["ident_b", ("maskb", var)], writes=pstk)

                qk_b(0)
                for h in range(8):
                    if h + 1 < 8:
                        qk_b(h + 1)
                    sl = h % 2
                    pst = PS[sl]
                    pstk = [("ps", 2 * sl), ("ps", 2 * sl + 1)]
                    ttt, ptt = tt[sl], pt[sl]
                    A("dve", lambda pst=pst, ttt=ttt, h=h, lo=lo, hi=hi: V.scalar_tensor_tensor(
                        out=ttt[:, lo:hi], in0=pst[:, lo:hi], scalar=0.125, in1=biasu[:, h * 896 + lo:h * 896 + hi],
                        op0=ALU.mult, op1=ALU.add), reads=pstk + [("biasu", h)], writes=[("ttb", sl)])
                    A("act", lambda ttt=ttt, ptt=ptt, lo=lo, hi=hi: ACT.activation(out=ptt[:, lo:hi], in_=ttt[:, lo:hi], func=AF.Exp),
                      reads=[("ttb", sl)], writes=[("ptb", sl)])
                    for m in ms:
                        kb = i + m - 3
                        A("pe", lambda ptt=ptt, m=m, kb=kb, h=h, ms=ms, po=po: T.matmul(
                            po[:, (h // 4) * 512 + (h % 4) * 65:(h // 4) * 512 + (h % 4) * 65 + 65],
                            lhsT=ptt[:, m * 128:(m + 1) * 128], rhs=vpb4[:, kb, h, :],
                            start=(m == ms[0]), stop=(m == ms[-1])),
                          reads=[("ptb", sl), ("vpb", kb)], writes=[pok])
                A("dve", lambda po4=po4: V.reciprocal(out=den[:].rearrange("p (b h) -> p b h", b=2), in_=po4[:, :, :, 64]),
                  reads=[pok], writes=["denb"])
                A("dve", lambda po4=po4: V.tensor_tensor(
                    out=ob[:].rearrange("p (b h d) -> p b h d", b=2, d=64), in0=po4[:, :, :, 0:64],
                    in1=den[:].rearrange("p (b h) -> p b h", b=2).unsqueeze(3).to_broadcast([128, 2, 4, 64]), op=ALU.mult),
                  reads=[pok, "denb"], writes=["ob"])
                A("act", lambda: ACT.activation(out=junk2[:], in_=ob[:], func=AF.Square, accum_out=ssq2[:]),
                  reads=["ob"], writes=["junk2b", "ssq2b"])
                A("dve", lambda: V.tensor_scalar(out=ssq2[:], in0=ssq2[:], scalar1=1.0 / 512, scalar2=EPS, op0=ALU.mult, op1=ALU.add),
                  reads=["ssq2b"], writes=["ssq2b"])
                A("act", lambda: ACT.activation(out=ssq2[:], in_=ssq2[:], func=AF.Ln), reads=["ssq2b"], writes=["ssq2b"])
                A("act", lambda: ACT.activation(out=ssq2[:], in_=ssq2[:], func=AF.Exp, scale=-0.5), reads=["ssq2b"], writes=["ssq2b"])
                mx = mixb[i % 2]
                A("dve", lambda mx=mx: V.scalar_tensor_tensor(out=mx[:], in0=ob[:], scalar=ssq2[:, 0:1], in1=gob[:],
                                                              op0=ALU.mult, op1=ALU.mult),
                  reads=["ob", "ssq2b", "gob"], writes=[("mixb", i % 2)])
                sc.dma("sp", lambda mx=mx, i=i: SP.dma_start(out=mixb_d[i * 128:(i + 1) * 128, :], in_=mx[:]),
                       reads=[("mixb", i % 2)], writes=[("mixb_d", i)])
            sc.barrier()
    if stop_after == "B":
        sc.finish([("mixa_d", i) for i in range(NQB)] + [("mixb_d", i) for i in range(NQB)] + ["dbg_qta", "dbg_mod"])
        es.close()
        return nc

    rt_ = ExitStack()
    lg_all = sb(rt_, "lg_all", [128, NQB * NE], F32)
    m8_all = sb(rt_, "m8_all", [128, NQB * 8], F32)
    lg3 = lg_all[:].rearrange("p (i e) -> p i e", e=NE)
    m83 = m8_all[:].rearrange("p (i k) -> p i k", k=8)
    with ExitStack() as pc:
        wout = sb(pc, "wout", [128, 8 * D], BF16)
        wout3 = wout[:].rearrange("p (c n) -> p c n", n=D)
        for c in range(8):
            sc.dma("pool", lambda c=c: G.dma_start(out=wout3[:, c, :], in_=wout_d[c * 128:(c + 1) * 128, :]), writes=[("wout", c)])
        WOK = [("wout", c) for c in range(8)]
        wr = sb(pc, "wr", [128, 8 * NE], F32)
        sc.dma("sp", lambda: SP.dma_start(out=wr[:].rearrange("p (c e) -> p c e", e=NE),
                                          in_=wr_d[:, :].rearrange("(c p) e -> p c e", p=128)), writes=["wr"])
        brt = sb(pc, "brt", [1, NE], F32)
        sc.dma("sp", lambda: SP.dma_start(out=brt[:], in_=br_d[:, :]), writes=["brt"])
        mixab = [sb(pc, f"mixab{i}", [128, D], BF16) for i in range(2)]
        xb_ = [sb(pc, f"xc{i}", [128, D], F32) for i in range(2)]
        mixT_ = [sb(pc, f"mixT{j}", [128, D], BF16) for j in range(2)]
        t1_ = [sb(pc, f"t1{j}", [128, D], F32) for j in range(2)]
        x1t = [sb(pc, f"x1t{i}", [128, D], F32) for i in range(2)]
        h2f_ = [sb(pc, f"h2f{j}", [128, D], F32) for j in range(2)]
        h2b = [sb(pc, f"h2b{i}", [128, D], BF16) for i in range(2)]
        h2T_ = [sb(pc, f"h2T{j}", [128, D], F32) for j in range(2)]
        tilesC_ = [(sb(pc, f"junkC{j}", [128, D], BF16), sb(pc, f"ssqC{j}", [128, 1], F32),
                    sb(pc, f"rstdC{j}", [128, 1], F32), sb(pc, f"tmpC{j}", [128, D], F32)) for j in range(2)]
        def c_loads(i):
            s2 = i % 2
            sc.dma("sp", lambda i=i, s2=s2: SP.dma_start(out=mixab[s2][:, 0:512], in_=mixa_d[i * 128:(i + 1) * 128, :]),
                   reads=[("mixa_d", i)], writes=[("mixab", s2, 0)])
            sc.dma("sp", lambda i=i, s2=s2: SP.dma_start(out=mixab[s2][:, 512:1024], in_=mixb_d[i * 128:(i + 1) * 128, :]),
                   reads=[("mixb_d", i)], writes=[("mixab", s2, 1)])
            sc.dma("sp", lambda i=i, s2=s2: SP.dma_start(out=xb_[s2][:], in_=x_d[i * 128:(i + 1) * 128, :]), writes=[("xc", s2)])

        def c_a(i):
            s2 = i % 2
            mixT, t1, h2f, h2T, tilesC = mixT_[s2], t1_[s2], h2f_[s2], h2T_[s2], tilesC_[s2]
            pbT = PS[0][:, s2 * 512:(s2 + 1) * 512].bitcast(BF16)
            for c in range(8):
                A("pe", lambda c=c, pbT=pbT, s2=s2: T.transpose(out=pbT[:, c * 128:(c + 1) * 128],
                                                                in_=mixab[s2][:, c * 128:(c + 1) * 128], identity=ident_b[:]),
                  reads=[("mixab", s2, 0), ("mixab", s2, 1), "ident_b"], writes=[("ps", s2)])
            A("act", lambda pbT=pbT, mixT=mixT: ACT.copy(out=mixT[:], in_=pbT), reads=[("ps", s2)], writes=[("mixT", s2)])
            for n in range(2):
                py, pyk = bank(2 + n)
                for c in range(8):
                    A("pe", lambda py=py, c=c, n=n, mixT=mixT: T.matmul(py, lhsT=mixT[:, c * 128:(c + 1) * 128],
                                                             rhs=wout3[:, c, n * 512:(n + 1) * 512], start=(c == 0), stop=(c == 7)),
                      reads=[("mixT", s2)] + WOK, writes=[pyk])
                A("dve", lambda py=py, n=n, t1=t1: V.tensor_tensor(out=t1[:, n * 512:(n + 1) * 512], in0=py,
                                                            in1=gate_m[:, n * 512:(n + 1) * 512], op=ALU.mult),
                  reads=[pyk] + MODK, writes=[("t1", s2, n)])
            xt = x1t[s2]
            A("pool", lambda xt=xt, s2=s2, t1=t1: G.tensor_tensor(out=xt[:], in0=t1[:], in1=xb_[s2][:], op=ALU.add),
              reads=[("t1", s2, 0), ("t1", s2, 1), ("xc", s2)], writes=[("x1t", s2)])
            sc.dma("sp", lambda xt=xt, i=i: SP.dma_start(out=x1_d[i * 128:(i + 1) * 128, :], in_=xt[:]),
                   reads=[("x1t", s2)], writes=[("x1_d", i)])
            rmsnorm_mod(tilesC, xt[:], [("x1t", s2)], scale1_f, shift_f, out_bf=h2b[s2][:], out_f32=h2f[:],
                        out_keys=[("h2f", s2)], tag="C" + str(s2))
            sc.dma("sp", lambda i=i, s2=s2: SP.dma_start(out=h2_d[i * 128:(i + 1) * 128, :], in_=h2b[s2][:]),
                   reads=[("h2f", s2, "bf")], writes=[("h2_d", i)])

        def c_b(i):
            s2 = i % 2
            mixT, t1, h2f, h2T, tilesC = mixT_[s2], t1_[s2], h2f_[s2], h2T_[s2], tilesC_[s2]
            for r_ in range(2):
                pt_, ptk_ = bank(4 + r_)
                for c4 in range(4):
                    c = r_ * 4 + c4
                    A("pe", lambda pt_=pt_, c=c, c4=c4, h2f=h2f: T.transpose(out=pt_[:, c4 * 128:(c4 + 1) * 128],
                                                                    in_=h2f[:, c * 128:(c + 1) * 128], identity=ident_f[:]),
                      reads=[("h2f", s2), "ident_f"], writes=[ptk_])
                if r_ == 0:
                    A("dve", lambda pt_=pt_, r_=r_, h2T=h2T: V.tensor_copy(out=h2T[:, r_ * 512:(r_ + 1) * 512], in_=pt_), reads=[ptk_], writes=[("h2T", s2, r_)])
                else:
                    A("act", lambda pt_=pt_, r_=r_, h2T=h2T: ACT.copy(out=h2T[:, r_ * 512:(r_ + 1) * 512], in_=pt_), reads=[ptk_], writes=[("h2T", s2, r_)])
            pl, plk = bank(6 + s2)
            for c in range(8):
                A("pe", lambda pl=pl, c=c, h2T=h2T: T.matmul(pl[:, 0:NE], lhsT=h2T[:, c * 128:(c + 1) * 128], rhs=wr[:, c * NE:(c + 1) * NE],
                                                    start=(c == 0), stop=False),
                  reads=[("h2T", s2, 0), ("h2T", s2, 1), "wr"], writes=[plk])
            A("pe", lambda pl=pl: T.matmul(pl[:, 0:NE], lhsT=ones_f[0:1, :], rhs=brt[0:1, :], start=False, stop=True),
              reads=["ones_f", "brt"], writes=[plk])
            A("dve", lambda pl=pl, i=i: V.tensor_copy(out=lg3[:, i, :], in_=pl[:, 0:NE]), reads=[plk], writes=[("lg", i)])
            A("dve", lambda i=i: V.max(out=m83[:, i, :], in_=lg3[:, i, :]), reads=[("lg", i)], writes=[("m8", i)])

        c_loads(0)
        for i in range(NQB):
            if i + 1 < NQB:
                c_loads(i + 1)
            c_a(i)
            if i >= 1:
                c_b(i - 1)
        c_b(NQB - 1)
        sc.barrier()
    LGK = [("lg", i) for i in range(NQB)] + [("m8", i) for i in range(NQB)]

    gw_all = sb(rt_, "gw_all", [128, NQB * NE], F32)
    gw3 = gw_all[:].rearrange("p (i e) -> p i e", e=NE)
    dsel_i = sb(rt_, "dsel_i", [128, 4 * NQB], I32)
    gk = sb(rt_, "gk", [128, 4 * NQB], F32)
    idxw_i = sb(rt_, "idxw_i", [128, NBLK * 8], I32)
    idxg_i = sb(rt_, "idxg_i", [128, NBLK * 8], I32)
    idxb_i = sb(rt_, "idxb_i", [128, NBLK], I32)
    with ExitStack() as pr:
        tri = sb(pr, "tri", [128, 128], F32)
        sc.dma("sp", lambda: SP.dma_start(out=tri[:], in_=tri_d[:, :]), writes=["tri"])
        rowid = sb(pr, "rowid", [128, 8], F32)
        sc.dma("sp", lambda: SP.dma_start(out=rowid[:], in_=rowid_d[:, :]), writes=["rowid"])
        blkth = sb(pr, "blkth", [128, NBLK * NE], F32)
        sc.dma("sp", lambda: SP.dma_start(out=blkth[:], in_=blkth_d[:, :]), writes=["blkth"])
        msk = sb(pr, "msk", [128, NQB * NE], F32)
        msk3 = msk[:].rearrange("p (i e) -> p i e", e=NE)
        ex = sb(pr, "ex", [128, NQB * NE], F32)
        ex3 = ex[:].rearrange("p (i e) -> p i e", e=NE)
        ssum = sb(pr, "ssum", [128, NQB], F32)
        pos = sb(pr, "pos", [128, NQB * NE], F32)
        pos3 = pos[:].rearrange("p (i e) -> p i e", e=NE)
        oh = sb(pr, "oh", [128, NQB * NE], F32)
        oh3 = oh[:].rearrange("p (i e) -> p i e", e=NE)
        prod = sb(pr, "prod", [128, NQB * NE], F32)
        prod3 = prod[:].rearrange("p (i e) -> p i e", e=NE)
        dself = sb(pr, "dself", [128, 4 * NQB], F32)
        cnt = sb(pr, "cnt", [128, NE], F32)
        cs = [sb(pr, f"cs{i}", [128, NE], F32) for i in range(2)]
        padded = sb(pr, "padded", [128, NE], F32)
        pstart = sb(pr, "pstart", [128, NE], F32)
        cmpb = sb(pr, "cmpb", [128, NBLK * NE], F32)
        blke = sb(pr, "blke", [128, NBLK], F32)
        idxwf = sb(pr, "idxwf", [128, NBLK * 8], F32)
        idxbf = sb(pr, "idxbf", [128, NBLK], F32)

        A("dve", lambda: V.tensor_tensor(out=msk3, in0=lg3, in1=m83[:, :, 3:4].to_broadcast([128, NQB, NE]), op=ALU.is_ge),
          reads=LGK, writes=["msk"])
        A("dve", lambda: V.tensor_tensor(out=ex3, in0=lg3, in1=m83[:, :, 0:1].to_broadcast([128, NQB, NE]), op=ALU.subtract),
          reads=LGK, writes=["ex"])
        A("act", lambda: ACT.activation(out=ex[:], in_=ex[:], func=AF.Exp), reads=["ex"], writes=["ex"])
        A("dve", lambda: V.tensor_tensor(out=ex[:], in0=ex[:], in1=msk[:], op=ALU.mult), reads=["ex", "msk"], writes=["ex"])
        A("dve", lambda: V.reduce_sum(out=ssum[:], in_=ex3, axis=AX.X), reads=["ex"], writes=["ssum"])
        A("dve", lambda: V.reciprocal(out=ssum[:], in_=ssum[:]), reads=["ssum"], writes=["ssum"])
        A("dve", lambda: V.tensor_tensor(out=gw3, in0=ex3, in1=ssum[:].unsqueeze(2).to_broadcast([128, NQB, NE]), op=ALU.mult),
          reads=["ex", "ssum"], writes=["gw"])
        for half in range(2):
            pp_, ppk_ = bank(half)
            for ii in range(16):
                i = half * 16 + ii
                for j in range(i):
                    A("pe", lambda pp_=pp_, ii=ii, j=j: T.matmul(pp_[:, ii * NE:(ii + 1) * NE], lhsT=ones_f[:], rhs=msk3[:, j, :],
                                                                 start=(j == 0), stop=False),
                      reads=["ones_f", "msk"], writes=[ppk_])
                A("pe", lambda pp_=pp_, ii=ii, i=i: T.matmul(pp_[:, ii * NE:(ii + 1) * NE], lhsT=tri[:], rhs=msk3[:, i, :],
                                                             start=(i == 0), stop=True),
                  reads=["tri", "msk"], writes=[ppk_])
            A("dve", lambda pp_=pp_, half=half: V.tensor_copy(out=pos[:, half * 512:(half + 1) * 512], in_=pp_),
              reads=[ppk_], writes=[("pos", half)])
        pc_, pck_ = bank(2)
        for j in range(NQB):
            A("pe", lambda j=j: T.matmul(pc_[:, 0:NE], lhsT=ones_f[:], rhs=msk3[:, j, :], start=(j == 0), stop=(j == NQB - 1)),
              reads=["ones_f", "msk"], writes=[pck_])
        A("dve", lambda: V.tensor_copy(out=cnt[:], in_=pc_[:, 0:NE]), reads=[pck_], writes=["cnt"])
        nbt = sb(pr, "nbt", [128, NE * 8], F32)
        A("dve", lambda: V.tensor_tensor(out=nbt[:].rearrange("p (e j) -> p e j", j=8),
                                         in0=cnt[:].unsqueeze(2).to_broadcast([128, NE, 8]),
                                         in1=blkth[:, 0:8 * NE].rearrange("p (b e) -> p e b", e=NE), op=ALU.is_gt),
          reads=["cnt", "blkth"], writes=["nbt"])
        A("dve", lambda: V.reduce_sum(out=padded[:], in_=nbt[:].rearrange("p (e j) -> p e j", j=8), axis=AX.X), reads=["nbt"], writes=["padded"])
        A("dve", lambda: V.tensor_scalar(out=padded[:], in0=padded[:], scalar1=float(MB), scalar2=None, op0=ALU.mult),
          reads=["padded"], writes=["padded"])
        A("dve", lambda: V.tensor_copy(out=cs[0][:], in_=padded[:]), reads=["padded"], writes=[("cs", 0)])
        cur = 0
        for sft in (1, 2, 4, 8, 16):
            nxt = 1 - cur
            A("dve", lambda cur=cur, nxt=nxt, sft=sft: V.tensor_copy(out=cs[nxt][:, 0:sft], in_=cs[cur][:, 0:sft]),
              reads=[("cs", cur)], writes=[("cs", nxt)])
            A("dve", lambda cur=cur, nxt=nxt, sft=sft: V.tensor_tensor(out=cs[nxt][:, sft:NE], in0=cs[cur][:, sft:NE],
                                                                       in1=cs[cur][:, 0:NE - sft], op=ALU.add),
              reads=[("cs", cur), ("cs", nxt)], writes=[("cs", nxt)])
            cur = nxt
        pend = cs[cur]
        pendk = ("cs", cur)
        A("dve", lambda: V.tensor_tensor(out=pstart[:], in0=pend[:], in1=padded[:], op=ALU.subtract), reads=[pendk, "padded"], writes=["pstart"])
        A("dve", lambda: V.tensor_tensor(out=pos3, in0=pos3, in1=pstart[:].unsqueeze(1).to_broadcast([128, NQB, NE]), op=ALU.add),
          reads=[("pos", 0), ("pos", 1), "pstart"], writes=["dest"])
        for k in range(4):
            A("dve", lambda k=k: V.tensor_tensor(out=oh3, in0=lg3, in1=m83[:, :, k:k + 1].to_broadcast([128, NQB, NE]), op=ALU.is_equal),
              reads=LGK, writes=["oh"])
            A("dve", lambda: V.tensor_tensor(out=prod[:], in0=oh[:], in1=pos[:], op=ALU.mult), reads=["oh", "dest"], writes=["prod"])
            A("dve", lambda k=k: V.reduce_sum(out=dself[:, k * NQB:(k + 1) * NQB], in_=prod3, axis=AX.X), reads=["prod"], writes=[("dself", k)])
            A("dve", lambda: V.tensor_tensor(out=prod[:], in0=oh[:], in1=gw_all[:], op=ALU.mult), reads=["oh", "gw"], writes=["prod"])
            A("dve", lambda k=k: V.reduce_sum(out=gk[:, k * NQB:(k + 1) * NQB], in_=prod3, axis=AX.X), reads=["prod"], writes=[("gk", k)])
        A("dve", lambda: V.tensor_copy(out=dsel_i[:], in_=dself[:]), reads=[("dself", k) for k in range(4)], writes=["dsel_i"])
        A("dve", lambda: V.tensor_tensor(out=cmpb[:].rearrange("p (b e) -> p b e", e=NE),
                                         in0=pend[:].unsqueeze(1).to_broadcast([128, NBLK, NE]),
                                         in1=blkth[:].rearrange("p (b e) -> p b e", e=NE), op=ALU.is_le),
          reads=[pendk, "blkth"], writes=["cmpb"])
        A("dve", lambda: V.reduce_sum(out=blke[:], in_=cmpb[:].rearrange("p (b e) -> p b e", e=NE), axis=AX.X), reads=["cmpb"], writes=["blke"])
        A("dve", lambda: V.tensor_scalar(out=blke[:], in0=blke[:], scalar1=float(NE - 1), scalar2=None, op0=ALU.min), reads=["blke"], writes=["blke"])
        A("dve", lambda: V.scalar_tensor_tensor(out=idxwf[:].rearrange("p (b c) -> p b c", c=8),
                                                in0=blke[:].unsqueeze(2).to_broadcast([128, NBLK, 8]), scalar=float(D),
                                                in1=rowid[:].unsqueeze(1).to_broadcast([128, NBLK, 8]), op0=ALU.mult, op1=ALU.add),
          reads=["blke", "rowid"], writes=["idxwf"])
        nused = sb(pr, "nused", [128, NBLK], F32)
        A("dve", lambda: V.tensor_scalar(out=nused[:], in0=blkth[:].rearrange("p (b e) -> p b e", e=NE)[:, :, 0],
                                         scalar1=pend[:, NE - 1:NE], scalar2=None, op0=ALU.is_ge),
          reads=[pendk, "blkth"], writes=["nused"])
        idxgf = sb(pr, "idxgf", [128, NBLK * 8], F32)
        A("dve", lambda: V.scalar_tensor_tensor(out=idxgf[:].rearrange("p (b c) -> p b c", c=8),
                                                in0=nused[:].unsqueeze(2).to_broadcast([128, NBLK, 8]), scalar=40000.0,
                                                in1=idxwf[:].rearrange("p (b c) -> p b c", c=8), op0=ALU.mult, op1=ALU.add),
          reads=["nused", "idxwf"], writes=["idxgf"])
        A("dve", lambda: V.tensor_copy(out=idxg_i[:], in_=idxgf[:]), reads=["idxgf"], writes=["idxg_i"])
        A("dve", lambda: V.tensor_copy(out=idxw_i[:], in_=idxwf[:]), reads=["idxwf"], writes=["idxw_i"])
        A("dve", lambda: V.scalar_tensor_tensor(out=idxbf[:], in0=blke[:], scalar=128.0, in1=rowid[:, 0:1].to_broadcast([128, NBLK]),
                                                op0=ALU.mult, op1=ALU.add), reads=["blke", "rowid"], writes=["idxbf"])
        A("dve", lambda: V.scalar_tensor_tensor(out=idxbf[:], in0=nused[:], scalar=40000.0, in1=idxbf[:], op0=ALU.mult, op1=ALU.add),
          reads=["nused", "idxbf"], writes=["idxbf"])
        A("dve", lambda: V.tensor_copy(out=idxb_i[:], in_=idxbf[:]), reads=["idxbf"], writes=["idxb_i"])
        if dbg:
            sc.dma("sp", lambda: SP.dma_start(out=dbg_d["gw"][:, :], in_=gw_all[:]), reads=["gw"], writes=["dbg_gw"])
            sc.dma("sp", lambda: SP.dma_start(out=dbg_d["dsel"][:, :], in_=dsel_i[:]), reads=["dsel_i"], writes=["dbg_dsel"])
            sc.dma("sp", lambda: SP.dma_start(out=dbg_d["blke"][:, :], in_=blke[:]), reads=["blke"], writes=["dbg_blke"])
        h2r = [sb(pr, f"h2r{i}", [128, D], BF16) for i in range(4)]
        for i in range(NQB):
            s4 = i % 4
            sc.dma("sp", lambda i=i, s4=s4: SP.dma_start(out=h2r[s4][:], in_=h2_d[i * 128:(i + 1) * 128, :]),
                   reads=[("h2_d", i)], writes=[("h2r", s4)])
            for k in range(4):
                sc.dma("pool", lambda i=i, k=k, s4=s4: G.indirect_dma_start(
                    out=xs_d[:, :], out_offset=bass.IndirectOffsetOnAxis(ap=dsel_i[:, k * NQB + i:k * NQB + i + 1], axis=0),
                    in_=h2r[s4][:], in_offset=None), reads=[("h2r", s4), "dsel_i"], writes=["xs_d"])
        sc.barrier()
    if stop_after == "R":
        sc.finish(["dbg_gw", "dbg_dsel", "dbg_blke", "xs_d"] + [("x1_d", i) for i in range(NQB)])
        rt_.close()
        es.close()
        return nc

    with ExitStack() as pm:
        wgu = [sb(pm, f"wgu{i}", [128, 8 * 2 * DFF], BF16) for i in range(2)]
        wdn = [sb(pm, f"wdn{i}", [128, 8 * D], BF16) for i in range(2)]
        bgu = [sb(pm, f"bgu{i}", [128, 16], F32) for i in range(2)]
        xst = [sb(pm, "xst0", [128, 4 * D], BF16)]
        xsT = sb(pm, "xsT", [128, 8 * MB], BF16)
        xsT3 = xsT[:].rearrange("p (c t) -> p c t", t=MB)
        actT_ = [sb(pm, f"actT{j}", [128, 8 * MB], BF16) for j in range(2)]
        actT3_ = [a_[:].rearrange("p (f t) -> p f t", t=MB) for a_ in actT_]
        gt = [sb(pm, f"gt{i}", [128, MB], F32) for i in range(2)]
        sg = [sb(pm, f"sg{i}", [128, MB], F32) for i in range(2)]
        ut = [sb(pm, f"ut{i}", [128, MB], F32) for i in range(2)]
        yst = [sb(pm, f"yst{i}", [128, D], F32) for i in range(2)]

        bc_reg = [G.to_reg(NE * D - 1), G.to_reg(NE * 128 - 1)]

        def load_weights(b, which):
            sl = b % 2
            w3g = wgu[sl][:].rearrange("p (c n) -> p c n", n=2 * DFF)
            w3d = wdn[sl][:].rearrange("p (c n) -> p c n", n=D)
            for c in range(8 if which == "gu" else 0):
                sc.dma("pool", lambda c=c, w3g=w3g, b=b: G.indirect_dma_start(
                    out=w3g[:, c, :], out_offset=None, in_=wgu_d[:, :],
                    in_offset=bass.IndirectOffsetOnAxis(ap=idxg_i[:, b * 8 + c:b * 8 + c + 1], axis=0),
                    bounds_check=bc_reg[0], oob_is_err=False),
                    reads=["idxg_i"], writes=[("wgu", sl, c)])
            for c in range(8 if which == "wd" else 0):
                sc.dma("pool", lambda c=c, w3d=w3d, b=b: G.indirect_dma_start(
                    out=w3d[:, c, :], out_offset=None, in_=wd_d[:, :],
                    in_offset=bass.IndirectOffsetOnAxis(ap=idxw_i[:, b * 8 + c:b * 8 + c + 1], axis=0)),
                    reads=["idxw_i"], writes=[("wdn", sl, c)])
            if which == "gu":
              sc.dma("pool", lambda b=b, sl=sl: G.indirect_dma_start(
                out=bgu[sl][:], out_offset=None, in_=bgu_d[:, :],
                in_offset=bass.IndirectOffsetOnAxis(ap=idxb_i[:, b:b + 1], axis=0),
                bounds_check=bc_reg[1], oob_is_err=False), reads=["idxb_i"], writes=[("bgu", sl)])

        def load_x(b):
            sl = 0
            sc.dma("sp", lambda b=b, sl=sl: SP.dma_start(out=xst[sl][:].rearrange("p (s d) -> p s d", d=D),
                                                        in_=xs_d[b * MB:(b + 1) * MB, :].rearrange("(s p) d -> p s d", p=128)),
                   reads=["xs_d"], writes=[("xst", sl)])

        for j in range(2):
            A("dve", lambda j=j: V.memset(wgu[j][:], 0.0), writes=[("wgu", j, c) for c in range(8)])
            A("dve", lambda j=j: V.memset(wdn[j][:], 0.0), writes=[("wdn", j, c) for c in range(8)])
            A("dve", lambda j=j: V.memset(bgu[j][:], 0.0), writes=[("bgu", j)])
        load_weights(0, "gu")
        load_x(0)

        def down_proj(b):
            sl = b % 2
            w3d = wdn[sl][:].rearrange("p (c n) -> p c n", n=D)
            WDK = [("wdn", sl, c) for c in range(8)]
            aT3 = actT3_[sl]
            AK = [("actT", sl, f) for f in range(8)]
            for s_ in range(4):
                ys_ = yst[s_ % 2]
                for n in range(2):
                    py, pyk = bank(6 + n)
                    for f in range(8):
                        A("pe", lambda py=py, f=f, s_=s_, n=n, w3d=w3d, aT3=aT3: T.matmul(
                            py, lhsT=aT3[:, f, s_ * 128:(s_ + 1) * 128], rhs=w3d[:, f, n * 512:(n + 1) * 512],
                            start=(f == 0), stop=(f == 7)), reads=AK + WDK, writes=[pyk])
                    A("act", lambda py=py, ys_=ys_, n=n: ACT.copy(out=ys_[:, n * 512:(n + 1) * 512], in_=py),
                      reads=[pyk], writes=[("yst", s_ % 2, n)])
                sc.dma("sp", lambda ys_=ys_, b=b, s_=s_: SP.dma_start(out=ys_d[b * MB + s_ * 128:b * MB + (s_ + 1) * 128, :], in_=ys_[:]),
                       reads=[("yst", s_ % 2, 0), ("yst", s_ % 2, 1)], writes=["ys_d"])

        for b in range(NBLK):
            sl = b % 2
            w3g = wgu[sl][:].rearrange("p (c n) -> p c n", n=2 * DFF)
            WGK = [("wgu", sl, c) for c in range(8)]
            aT3 = actT3_[sl]
            for s_ in range(4):
                pbT = PS[0][:, (s_ % 2) * 512:(s_ % 2 + 1) * 512].bitcast(BF16)
                pkT = ("ps", s_ % 2)
                for c in range(8):
                    A("pe", lambda c=c, pbT=pbT, s_=s_: T.transpose(
                        out=pbT[:, c * 128:(c + 1) * 128], in_=xst[0][:, s_ * D + c * 128:s_ * D + (c + 1) * 128], identity=ident_b[:]),
                      reads=[("xst", 0), "ident_b"], writes=[pkT])
                if s_ % 2 == 0:
                    A("dve", lambda pbT=pbT, s_=s_: V.tensor_copy(out=xsT3[:, :, s_ * 128:(s_ + 1) * 128],
                                                                  in_=pbT.rearrange("p (c t) -> p c t", t=128)),
                      reads=[pkT], writes=[("xsT", s_)])
                else:
                    A("act", lambda pbT=pbT, s_=s_: ACT.copy(out=xsT3[:, :, s_ * 128:(s_ + 1) * 128],
                                                             in_=pbT.rearrange("p (c t) -> p c t", t=128)),
                      reads=[pkT], writes=[("xsT", s_)])
            if b + 1 < NBLK:
                load_x(b + 1)
            if b >= 1:
                down_proj(b - 1)
            load_weights(b, "wd")
            if b + 1 < NBLK:
                load_weights(b + 1, "gu")
            XK = [("xsT", s_) for s_ in range(4)]
            for f in range(8):
                e2 = f % 2
                pg, pgk = bank(2 + e2 * 2)
                pu, puk = bank(3 + e2 * 2)
                for (pb, pk, col) in ((pg, pgk, f * 128), (pu, puk, DFF + f * 128)):
                    for c in range(8):
                        A("pe", lambda pb=pb, c=c, col=col, w3g=w3g: T.matmul(pb, lhsT=w3g[:, c, col:col + 128], rhs=xsT3[:, c, :],
                                                                              start=(c == 0), stop=(c == 7)),
                          reads=XK + WGK, writes=[pk])
                g_, s__, u_ = gt[e2], sg[e2], ut[e2]
                A("dve", lambda pg=pg, g_=g_, f=f, sl=sl: V.tensor_scalar(out=g_[:], in0=pg, scalar1=bgu[sl][:, f:f + 1], scalar2=7.0,
                                                                          op0=ALU.add, op1=ALU.min),
                  reads=[pgk, ("bgu", sl)], writes=[("gt", e2)])
                A("act", lambda g_=g_, s__=s__: ACT.activation(out=s__[:], in_=g_[:], func=AF.Sigmoid, scale=1.702),
                  reads=[("gt", e2)], writes=[("sg", e2)])
                A("dve", lambda pu=pu, u_=u_, f=f, sl=sl: V.tensor_scalar(out=u_[:], in0=pu, scalar1=bgu[sl][:, 8 + f:9 + f], scalar2=7.0,
                                                                          op0=ALU.add, op1=ALU.min),
                  reads=[puk, ("bgu", sl)], writes=[("ut", e2)])
                A("dve", lambda u_=u_: V.tensor_scalar(out=u_[:], in0=u_[:], scalar1=-7.0, scalar2=1.0, op0=ALU.max, op1=ALU.add),
                  reads=[("ut", e2)], writes=[("ut", e2)])
                A("dve", lambda g_=g_, s__=s__: V.tensor_tensor(out=g_[:], in0=g_[:], in1=s__[:], op=ALU.mult),
                  reads=[("gt", e2), ("sg", e2)], writes=[("gt", e2)])
                A("dve", lambda g_=g_, u_=u_, f=f, aT3=aT3: V.tensor_tensor(out=aT3[:, f, :], in0=g_[:], in1=u_[:], op=ALU.mult),
                  reads=[("gt", e2), ("ut", e2)], writes=[("actT", sl, f)])
            if b % 8 == 7:
                sc.flush()
        down_proj(NBLK - 1)
        sc.barrier()

    with ExitStack() as pf:
        bd = sb(pf, "bd", [NE, D], F32)
        sc.dma("sp", lambda: SP.dma_start(out=bd[:], in_=bd_d[:, :]), writes=["bd"])
        gfin = sb(pf, "gfin", [128, D], F32)
        sc.dma("sp", lambda: SP.dma_start(out=gfin[:], in_=gfin_d[:, :].partition_broadcast(128)), writes=["gfin"])
        x1f = [sb(pf, f"x1f{i}", [128, D], F32) for i in range(2)]
        yk_ = [[sb(pf, f"yk{j}_{i}", [128, D], F32) for i in range(4)] for j in range(2)]
        acc_ = [sb(pf, f"acc{j}", [128, D], F32) for j in range(2)]
        gwT_ = [sb(pf, f"gwT{j}", [NE, 128], F32) for j in range(2)]
        junkF = sb(pf, "junkF", [128, D], BF16)
        ssqF = sb(pf, "ssqF", [128, 1], F32)
        ot = [sb(pf, f"ot{i}", [128, D], F32) for i in range(2)]
        def f_loads(i):
            s2 = i % 2
            yk = yk_[s2]
            sc.dma("sp", lambda i=i, s2=s2: SP.dma_start(out=x1f[s2][:], in_=x1_d[i * 128:(i + 1) * 128, :]),
                   reads=[("x1_d", i)], writes=[("x1f", s2)])
            for k in range(4):
                sc.dma("pool", lambda i=i, k=k, yk=yk: G.indirect_dma_start(
                    out=yk[k][:], out_offset=None, in_=ys_d[:, :],
                    in_offset=bass.IndirectOffsetOnAxis(ap=dsel_i[:, k * NQB + i:k * NQB + i + 1], axis=0)),
                    reads=["ys_d", "dsel_i"], writes=[("yk", s2, k)])

        for i in range(NQB):
            s2 = i % 2
            yk, acc, gwT = yk_[s2], acc_[s2], gwT_[s2]
            ACCK, GWTK = ("acc", s2), ("gwT", s2)
            if i == 0:
                f_loads(0)
            if i + 1 < NQB:
                f_loads(i + 1)
            pt_, ptk_ = bank(s2)
            A("pe", lambda pt_=pt_, i=i: T.transpose(out=pt_[0:NE, 0:128], in_=gw3[:, i, :], identity=ident_f[:]),
              reads=["gw", "ident_f"], writes=[ptk_])
            A("act", lambda pt_=pt_, gwT=gwT: ACT.copy(out=gwT[:], in_=pt_[0:NE, 0:128]), reads=[ptk_], writes=[GWTK])
            pbs = []
            for n in range(2):
                pb, pbk = bank(2 + 2 * s2 + n)
                A("pe", lambda pb=pb, n=n, gwT=gwT: T.matmul(pb, lhsT=gwT[:], rhs=bd[:, n * 512:(n + 1) * 512], start=True, stop=True),
                  reads=[GWTK, "bd"], writes=[pbk])
                pbs.append((pb, pbk))
            A("dve", lambda i=i, acc=acc, yk=yk: V.tensor_scalar(out=acc[:], in0=yk[0][:], scalar1=gk[:, i:i + 1], scalar2=None, op0=ALU.mult),
              reads=[("yk", s2, 0)] + [("gk", k) for k in range(4)], writes=[ACCK])
            for k in range(1, 4):
                A("dve", lambda i=i, k=k, acc=acc, yk=yk: V.scalar_tensor_tensor(out=acc[:], in0=yk[k][:], scalar=gk[:, k * NQB + i:k * NQB + i + 1],
                                                                 in1=acc[:], op0=ALU.mult, op1=ALU.add),
                  reads=[("yk", s2, k), ACCK] + [("gk", kk) for kk in range(4)], writes=[ACCK])
            for n in range(2):
                pb, pbk = pbs[n]
                A("dve", lambda pb=pb, n=n, acc=acc: V.tensor_tensor(out=acc[:, n * 512:(n + 1) * 512], in0=pb, in1=acc[:, n * 512:(n + 1) * 512], op=ALU.add),
                  reads=[pbk, ACCK], writes=[ACCK])
            A("pool", lambda acc=acc: G.tensor_tensor(out=acc[:], in0=acc[:], in1=gate_f, op=ALU.mult), reads=[ACCK] + MODK, writes=[ACCK])
            A("pool", lambda s2=s2, acc=acc: G.tensor_tensor(out=acc[:], in0=acc[:], in1=x1f[s2][:], op=ALU.add), reads=[ACCK, ("x1f", s2)], writes=[ACCK])
            A("act", lambda acc=acc: ACT.activation(out=junkF[:], in_=acc[:], func=AF.Square, accum_out=ssqF[:]), reads=[ACCK], writes=["junkF", "ssqF"])
            A("dve", lambda: V.tensor_scalar(out=ssqF[:], in0=ssqF[:], scalar1=1.0 / D, scalar2=EPS, op0=ALU.mult, op1=ALU.add),
              reads=["ssqF"], writes=["ssqF"])
            A("act", lambda: ACT.activation(out=ssqF[:], in_=ssqF[:], func=AF.Ln), reads=["ssqF"], writes=["ssqF"])
            A("act", lambda: ACT.activation(out=ssqF[:], in_=ssqF[:], func=AF.Exp, scale=-0.5), reads=["ssqF"], writes=["ssqF"])
            o_ = ot[s2]
            A("dve", lambda o_=o_, acc=acc: V.scalar_tensor_tensor(out=o_[:], in0=acc[:], scalar=ssqF[:, 0:1], in1=gfin[:], op0=ALU.mult, op1=ALU.mult),
              reads=[ACCK, "ssqF", "gfin"], writes=[("ot", s2)])
            sc.dma("sp", lambda o_=o_, i=i: SP.dma_start(out=out_d[i * 128:(i + 1) * 128, :], in_=o_[:]),
                   reads=[("ot", s2)], writes=[("out_d", i)])
        sc.barrier()
    sc.finish([("out_d", i) for i in range(NQB)])
    rt_.close()
    es.close()
    return nc


def _prep_inputs(inputs):
    f = lambda a: np.ascontiguousarray(np.asarray(a, dtype=np.float32))
    x = f(inputs["x"])
    c = f(inputs["c"])
    w_in = f(inputs["w_in"])[0]
    wa, wb = _layout_w_in(w_in)
    cosT, sinT = _rope_tables()
    dr, co = _bias_index()
    rpb = f(inputs["rpb"])[0]
    biasu = np.ascontiguousarray(rpb[:, dr, co].reshape(8, 128, 7 * 128))
    bgu = f(inputs["b_gate_up"])[0]
    bgu_l = np.ascontiguousarray(bgu.reshape(NE, 16, 128).transpose(0, 2, 1).reshape(NE * 128, 16))
    rowid = (np.arange(8)[None, :] * 128 + np.arange(128)[:, None]).astype(np.float32)
    blkth = np.broadcast_to((np.arange(NBLK, dtype=np.float32) * MB)[None, :, None], (128, NBLK, NE)).reshape(128, NBLK * NE)
    shared = {
        "w_ada": f(inputs["w_ada"])[0], "b_ada": f(inputs["b_ada"]).reshape(1, 6 * D),
        "w_a": wa, "w_b": wb, "sink": f(inputs["sink"]).reshape(1, 8), "biasu": biasu,
        "g_out_a": f(inputs["g_out_a"]).reshape(1, 512), "g_out_b": f(inputs["g_out_b"]).reshape(1, 512),
        "w_out": f(inputs["w_out"])[0], "w_router": f(inputs["w_router"])[0], "b_router": f(inputs["b_router"]).reshape(1, NE),
        "w_gate_up": f(inputs["w_gate_up"])[0].reshape(NE * D, 2 * DFF), "b_gate_up": bgu_l,
        "w_down": f(inputs["w_down"])[0].reshape(NE * DFF, D), "b_down": f(inputs["b_down"])[0],
        "g_final": f(inputs["g_final"]).reshape(1, D), "cosT": cosT, "sinT": sinT,
        "maska": np.ascontiguousarray(_mask_a().reshape(128, 384)),
        "maskb": np.ascontiguousarray(_MASKB.reshape(_NVAR, 128, 7 * 128)),
        "ident": np.eye(128, dtype=np.float32), "tri": np.triu(np.ones((128, 128), np.float32), 1),
        "rowid": rowid, "blkth": np.ascontiguousarray(blkth),
    }
    in_maps = []
    for b in range(8):
        m = dict(shared)
        m["x"] = x[b]
        m["cT"] = np.ascontiguousarray(c[b].reshape(8, 128).T)
        in_maps.append(m)
    return in_maps


def kernel(**inputs):
    in_maps = _prep_inputs(inputs)
    nc = build_program()
    res = run_bass_kernel_spmd(nc, in_maps, core_ids=list(range(8)))
    return np.stack([np.asarray(r["out"], dtype=np.float32) for r in res.results], axis=0)
```

```python
import bisect
from contextlib import ExitStack

import numpy as np
import concourse.bass as bass
import concourse.mybir as mybir
from concourse.bass_utils import run_bass_kernel_spmd

F32 = mybir.dt.float32
BF16 = mybir.dt.bfloat16
I32 = mybir.dt.int32
AF = mybir.ActivationFunctionType
ALU = mybir.AluOpType
AX = mybir.AxisListType

S = 4096
D = 1024
NQB = 32
NE = 32
DFF = 1024
EPS = 1e-5
MASKV = -240000.0
MB = 512
NBLK = 64
NSLOT = NBLK * MB
THETA = 500000.0


class _Op:
    __slots__ = ("eng", "fn", "deps", "dma", "need_inc", "target")


class Sched:
    def __init__(self, nc, es, nchan=10):
        self.nc = nc
        self.engs = dict(pe=nc.tensor, act=nc.scalar, dve=nc.vector, pool=nc.gpsimd, sp=nc.sync)
        self.sem = {e: es.enter_context(nc.semaphore("sem_" + e)) for e in ("pe", "act", "dve", "pool")}
        nch = {"sp": 12, "pool": 28}
        self.chan = {q: [es.enter_context(nc.semaphore(f"ch_{q}{i}")) for i in range(nch[q])] for q in ("sp", "pool")}
        self.chan_cnt = {q: [0] * nch[q] for q in ("sp", "pool")}
        self.chan_next = {q: 0 for q in ("sp", "pool")}
        self.ops = []
        self.flushed = 0
        self.last_writer = {}
        self.readers = {}
        self.cnt = {e: 0 for e in self.sem}
        self.incs = {e: ([], []) for e in self.sem}
        self.waited = {}

    def add(self, eng, fn, reads=(), writes=(), dma=False):
        op = _Op()
        op.eng, op.fn, op.dma, op.need_inc, op.target = eng, fn, dma, False, None
        deps = set()
        for r in reads:
            w = self.last_writer.get(r)
            if w is not None:
                deps.add(w)
        for w_ in writes:
            w = self.last_writer.get(w_)
            if w is not None:
                deps.add(w)
            for r in self.readers.get(w_, ()):
                deps.add(r)
        idx = len(self.ops)
        deps.discard(idx)
        op.deps = deps
        for r in reads:
            self.readers.setdefault(r, []).append(idx)
        for w_ in writes:
            self.last_writer[w_] = idx
            self.readers[w_] = []
        self.ops.append(op)
        return idx

    def dma(self, q, fn, reads=(), writes=()):
        return self.add(q, fn, reads, writes, dma=True)

    def _wait(self, ceng, sem, val):
        key = (ceng, id(sem))
        if self.waited.get(key, 0) >= val:
            return
        self.waited[key] = val
        self.engs[ceng].wait_ge(sem, val)

    def flush(self, final=False):
        ops = self.ops
        lo, hi = self.flushed, len(ops)
        last_of = {}
        for i in range(lo, hi):
            op = ops[i]
            if not op.dma:
                last_of[op.eng] = i
            for d in op.deps:
                dop = ops[d]
                if d >= lo and not dop.dma:
                    if dop.eng == "pe" and op.eng == "pe" and not op.dma:
                        continue
                    dop.need_inc = True
        for e, i in last_of.items():
            ops[i].need_inc = True
        for i in range(lo, hi):
            op = ops[i]
            ceng = op.eng
            if op.dma:
                q = ceng
                c = self.chan_next[q]
                self.chan_next[q] = (c + 1) % len(self.chan[q])
                csem = self.chan[q][c]
                if self.chan_cnt[q][c] > 0:
                    self._wait(q, csem, 16 * self.chan_cnt[q][c])
            for d in sorted(op.deps):
                dop = ops[d]
                if dop.dma:
                    self._wait(ceng, dop.target[0], dop.target[1])
                else:
                    if dop.eng == "pe" and ceng == "pe" and not op.dma:
                        continue
                    il, cl = self.incs[dop.eng]
                    if dop.target is not None:
                        tv = dop.target[1]
                    else:
                        j = bisect.bisect_left(il, d)
                        if j < len(il):
                            tv = cl[j]
                        else:
                            raise RuntimeError("no covering inc")
                    self._wait(ceng, self.sem[dop.eng], tv)
            ins = op.fn()
            if op.dma:
                self.chan_cnt[q][c] += 1
                ins.then_inc(csem, 16)
                op.target = (csem, 16 * self.chan_cnt[q][c])
            elif op.need_inc:
                self.cnt[ceng] += 1
                ins.then_inc(self.sem[ceng], 1)
                op.target = (self.sem[ceng], self.cnt[ceng])
                self.incs[ceng][0].append(i)
                self.incs[ceng][1].append(self.cnt[ceng])
            op.fn = None
        self.flushed = hi

    def barrier(self):
        self.flush()
        for ceng in ("pe", "act", "dve", "pool", "sp"):
            for e, sem in self.sem.items():
                if self.cnt[e] > 0:
                    self._wait(ceng, sem, self.cnt[e])
            for q in ("sp", "pool"):
                for c, csem in enumerate(self.chan[q]):
                    if self.chan_cnt[q][c] > 0:
                        self._wait(ceng, csem, 16 * self.chan_cnt[q][c])

    def finish(self, out_keys):
        self.flush()
        for k in out_keys:
            w = self.last_writer.get(k)
            if w is not None:
                t = self.ops[w].target
                self._wait("sp", t[0], t[1])
        for q in ("sp", "pool"):
            for c, csem in enumerate(self.chan[q]):
                if self.chan_cnt[q][c] > 0:
                    self._wait("sp", csem, 16 * self.chan_cnt[q][c])


def _rope_tables():
    inv_freq = (np.float32(THETA) ** (-np.arange(0, 16, 2, dtype=np.float32) / np.float32(16))).astype(np.float32)
    pos = np.arange(S, dtype=np.float32)
    ang = (pos[:, None] * inv_freq[None, :]).astype(np.float32)
    cos = np.cos(ang).astype(np.float32)
    sin = np.sin(ang).astype(np.float32)
    cosT = np.ones((128, S), np.float32)
    sinT = np.zeros((128, S), np.float32)
    for hh in range(2):
        b = hh * 64
        for d in range(8):
            cosT[b + d] = cos[:, d]
            cosT[b + 8 + d] = cos[:, d]
            sinT[b + d] = -sin[:, d]
            sinT[b + 8 + d] = sin[:, d]
    return cosT, sinT


def _mask_a():
    k = np.arange(128)[:, None, None]
    m = np.arange(3)[None, :, None]
    q = np.arange(128)[None, None, :]
    rel = (m - 1) * 128 + k - q
    return np.where(np.abs(rel) <= 128, 0.0, MASKV).astype(np.float32)


def _b_geometry():
    rows = 64
    rs = np.clip(np.arange(rows) - 4, 0, rows - 8)
    cs = np.clip(np.arange(64) - 8, 0, 64 - 16)

    def valid_row(kr, r):
        return (0 <= kr < rows) and (rs[r] <= kr < rs[r] + 8)

    colmask = np.zeros((64, 64), bool)
    for qc in range(64):
        colmask[qc, cs[qc]:cs[qc] + 16] = True
    masks = {}
    kbs = {}
    for i in range(NQB):
        mk = np.full((128, 7, 128), MASKV, np.float32)
        used = []
        for m in range(7):
            kb = i + m - 3
            if kb < 0 or kb > 31:
                continue
            anyv = False
            for a in range(2):
                for b in range(2):
                    if valid_row(2 * kb + a, 2 * i + b):
                        anyv = True
                        blk = np.where(colmask.T, 0.0, MASKV)
                        mk[a * 64:(a + 1) * 64, m, b * 64:(b + 1) * 64] = blk
            if anyv:
                used.append(m)
        masks[i] = mk
        kbs[i] = used
    variants = []
    var_of = {}
    for i in range(NQB):
        for vi, v in enumerate(variants):
            if np.array_equal(v, masks[i]):
                var_of[i] = vi
                break
        else:
            var_of[i] = len(variants)
            variants.append(masks[i])
    return np.stack(variants), var_of, kbs


def _bias_index():
    a = np.arange(2)[:, None, None, None, None]
    kc = np.arange(64)[None, :, None, None, None]
    m = np.arange(7)[None, None, :, None, None]
    b = np.arange(2)[None, None, None, :, None]
    qc = np.arange(64)[None, None, None, None, :]
    dr = np.clip(2 * (m - 3) + a - b, -7, 7) + 7
    co = np.clip(kc - qc, -15, 15) + 15
    dr = np.broadcast_to(dr, (2, 64, 7, 2, 64)).reshape(128, 7, 128)
    co = np.broadcast_to(co, (2, 64, 7, 2, 64)).reshape(128, 7, 128)
    return dr, co


_MASKB, _VAR_OF, _KBS = _b_geometry()
_NVAR = _MASKB.shape[0]
_PERM = np.concatenate([np.arange(8, 16), np.arange(0, 8), np.arange(16, 64)])

NWA = 1536 + 128
NWB = 1536


def _layout_w_in(w):
    qa, ka, va = w[:, 0:512], w[:, 512:640], w[:, 640:768]
    qb, kb, vb = w[:, 768:1280], w[:, 1280:1792], w[:, 1792:2304]
    qap = qa.reshape(D, 8, 64)[:, :, _PERM].reshape(D, 512)
    k0, k1 = ka[:, 0:64], ka[:, 64:128]
    k0p, k1p = k0[:, _PERM], k1[:, _PERM]
    wa = np.concatenate([qa, qap, k0, k0, k1, k1, k0p, k0p, k1p, k1p, va], axis=1)
    wb = np.concatenate([qb, kb, vb], axis=1)
    return np.ascontiguousarray(wa), np.ascontiguousarray(wb)


def build_program(stop_after=None, dbg=False):
    nc = bass.Bass("TRN2", target_bir_lowering=False)
    es = ExitStack()

    def din(name, shape, dt=F32):
        return nc.dram_tensor(name, list(shape), dt, kind="ExternalInput").ap()

    x_d = din("x", [S, D])
    cT_d = din("cT", [128, 8])
    wada_d = din("w_ada", [D, 6 * D])
    bada_d = din("b_ada", [1, 6 * D])
    wa_d = din("w_a", [D, NWA])
    wb_d = din("w_b", [D, NWB])
    sink_d = din("sink", [1, 8])
    biasu_d = din("biasu", [8, 128, 7 * 128])
    goa_d = din("g_out_a", [1, 512])
    gob_d = din("g_out_b", [1, 512])
    wout_d = din("w_out", [D, D])
    wr_d = din("w_router", [D, NE])
    br_d = din("b_router", [1, NE])
    if stop_after is None:
        wgu_d = din("w_gate_up", [NE * D, 2 * DFF])
        bgu_d = din("b_gate_up", [NE * 128, 16])
        wd_d = din("w_down", [NE * DFF, D])
        bd_d = din("b_down", [NE, D])
    gfin_d = din("g_final", [1, D])
    cos_d = din("cosT", [128, S])
    sin_d = din("sinT", [128, S])
    maska_d = din("maska", [128, 3 * 128])
    maskb_d = din("maskb", [_NVAR, 128, 7 * 128])
    ident_d = din("ident", [128, 128])
    tri_d = din("tri", [128, 128])
    rowid_d = din("rowid", [128, 8])
    blkth_d = din("blkth", [128, NBLK * NE])
    out_d = nc.dram_tensor("out", [S, D], F32, kind="ExternalOutput").ap()

    def dscr(name, shape, dt):
        kind = "ExternalOutput" if (dbg and name not in ("xs_s", "ys_s")) else "Internal"
        return nc.dram_tensor(name, list(shape), dt, kind=kind).ap()

    mixa_d = dscr("mixa_s", [S, 512], BF16)
    mixb_d = dscr("mixb_s", [S, 512], BF16)
    x1_d = dscr("x1_s", [S, D], F32)
    h2_d = dscr("h2_s", [S, D], BF16)
    if stop_after not in ("A", "B"):
        xs_d = dscr("xs_s", [NSLOT, D], BF16)
        ys_d = dscr("ys_s", [NSLOT, D], F32)
    dbg_d = {}
    if dbg:
        dbg_d["qta"] = nc.dram_tensor("dbg_qta", [128, 6 * S], BF16, kind="ExternalOutput").ap()
        dbg_d["gw"] = nc.dram_tensor("dbg_gw", [128, NQB * NE], F32, kind="ExternalOutput").ap()
        dbg_d["dsel"] = nc.dram_tensor("dbg_dsel", [128, NQB * 4], I32, kind="ExternalOutput").ap()
        dbg_d["blke"] = nc.dram_tensor("dbg_blke", [128, NBLK], F32, kind="ExternalOutput").ap()
        dbg_d["mod"] = nc.dram_tensor("dbg_mod", [128, 6 * D], F32, kind="ExternalOutput").ap()

    sc = Sched(nc, es)
    dbg_keys = []

    DUMPS = dict(ptA=([128, 384], BF16), poA=([128, 1024], F32), vpa=([128, NQB * 2 * 65], BF16), esink=([128, 8], F32),
                 goa=([128, 512], F32), oaA=([128, 512], F32), denA=([128, 8], F32), ssqA=([128, 1], F32))
    dump_d = {k: nc.dram_tensor("dbg_" + k, v[0], v[1], kind="ExternalOutput").ap() for k, v in DUMPS.items()} if dbg else {}

    def dump(name, ap, shape, dt, reads):
        if not dbg:
            return
        d = dump_d[name]
        sc.dma("sp", lambda: nc.sync.dma_start(out=d, in_=ap), reads=reads, writes=["dbg_" + name])
        dbg_keys.append("dbg_" + name)
    A = sc.add
    T, V, G, ACT, SP = nc.tensor, nc.vector, nc.gpsimd, nc.scalar, nc.sync

    def sb(stack, name, shape, dt=F32):
        return stack.enter_context(nc.sbuf_tensor("s_" + name, list(shape), dt))

    PS = [es.enter_context(nc.psum_tensor(f"ps{i}", [128, 1024], F32)) for i in range(4)]

    def bank(i):
        return PS[i // 2][:, (i % 2) * 512:(i % 2 + 1) * 512], ("ps", i)

    ident_f = sb(es, "ident_f", [128, 128], F32)
    ident_b = sb(es, "ident_b", [128, 128], BF16)
    ones_f = sb(es, "ones_f", [128, 128], F32)
    mod = sb(es, "mod", [128, 6 * D], F32)
    epsb = sb(es, "epsb", [128, 1], F32)
    sc.dma("sp", lambda: SP.dma_start(out=ident_f[:], in_=ident_d[:, :]), writes=["ident_f"])
    sc.dma("pool", lambda: G.dma_start(out=ident_b[:], in_=ident_d[:, :]), writes=["ident_b"])
    A("dve", lambda: V.memset(ones_f[:], 1.0), writes=["ones_f"])
    A("dve", lambda: V.memset(epsb[:], EPS), writes=["epsb"])

    with ExitStack() as p0:
        cT = sb(p0, "cT", [128, 8], F32)
        cact = sb(p0, "cact", [128, 8], F32)
        csig = sb(p0, "csig", [128, 8], F32)
        crep = sb(p0, "crep", [128, 8 * 128], F32)
        bada = sb(p0, "bada", [1, 6 * D], F32)
        wsl = [sb(p0, f"wsl{i}", [128, 8 * 512], F32) for i in range(2)]
        sc.dma("sp", lambda: SP.dma_start(out=cT[:], in_=cT_d[:, :]), writes=["cT"])
        sc.dma("sp", lambda: SP.dma_start(out=bada[:], in_=bada_d[:, :]), writes=["bada"])
        A("act", lambda: ACT.activation(out=csig[:], in_=cT[:], func=AF.Sigmoid), reads=["cT"], writes=["csig"])
        A("dve", lambda: V.tensor_tensor(out=cact[:], in0=cT[:], in1=csig[:], op=ALU.mult), reads=["cT", "csig"], writes=["cact"])
        A("dve", lambda: V.tensor_copy(out=crep[:].rearrange("p (c m) -> p c m", m=128),
                                       in_=cact[:].unsqueeze(2).to_broadcast([128, 8, 128])),
          reads=["cact"], writes=["crep"])
        for n in range(12):
            slot = n % 2
            w_t = wsl[slot]
            sc.dma("sp", lambda w_t=w_t, n=n: SP.dma_start(
                out=w_t[:].rearrange("p (c n) -> p c n", n=512),
                in_=wada_d[:, n * 512:(n + 1) * 512].rearrange("(c p) n -> p c n", p=128)),
                writes=[("wsl", slot)])
            pb, pk = bank(n % 2)
            for c in range(8):
                A("pe", lambda pb=pb, w_t=w_t, c=c: T.matmul(pb, lhsT=crep[:, c * 128:(c + 1) * 128],
                                                             rhs=w_t[:, c * 512:(c + 1) * 512], start=(c == 0), stop=False),
                  reads=["crep", ("wsl", slot)], writes=[pk])
            A("pe", lambda pb=pb, n=n: T.matmul(pb, lhsT=ones_f[0:1, :], rhs=bada[0:1, n * 512:(n + 1) * 512],
                                                start=False, stop=True),
              reads=["ones_f", "bada"], writes=[pk])
            if (n // 2) % 3 == 1:
                A("dve", lambda pb=pb, n=n: V.tensor_scalar(out=mod[:, n * 512:(n + 1) * 512], in0=pb, scalar1=1.0,
                                                            scalar2=None, op0=ALU.add), reads=[pk], writes=[("mod", n)])
            else:
                A("act", lambda pb=pb, n=n: ACT.copy(out=mod[:, n * 512:(n + 1) * 512], in_=pb), reads=[pk], writes=[("mod", n)])
        sc.barrier()
    MODK = [("mod", n) for n in range(12)]
    shift_m, scale1_m, gate_m = mod[:, 0:D], mod[:, D:2 * D], mod[:, 2 * D:3 * D]
    shift_f, scale1_f, gate_f = mod[:, 3 * D:4 * D], mod[:, 4 * D:5 * D], mod[:, 5 * D:6 * D]
    if dbg:
        sc.dma("sp", lambda: SP.dma_start(out=dbg_d["mod"][:, :], in_=mod[:]), reads=MODK, writes=["dbg_mod"])

    def rmsnorm_mod(stack_tiles, src, src_keys, scale1, shift, out_bf=None, out_f32=None, out_keys=(), tag=""):
        junk, ssq, rstd, tmp = stack_tiles
        A("act", lambda: ACT.activation(out=junk[:], in_=src, func=AF.Square, accum_out=ssq[:]),
          reads=list(src_keys), writes=["junk" + tag, "ssq" + tag])
        A("dve", lambda: V.tensor_scalar(out=rstd[:], in0=ssq[:], scalar1=1.0 / D, scalar2=EPS, op0=ALU.mult, op1=ALU.add),
          reads=["ssq" + tag], writes=["rstd" + tag])
        A("act", lambda: ACT.activation(out=rstd[:], in_=rstd[:], func=AF.Ln), reads=["rstd" + tag], writes=["rstd" + tag])
        A("act", lambda: ACT.activation(out=rstd[:], in_=rstd[:], func=AF.Exp, scale=-0.5), reads=["rstd" + tag], writes=["rstd" + tag])
        A("dve", lambda: V.scalar_tensor_tensor(out=tmp[:], in0=src, scalar=rstd[:, 0:1], in1=scale1, op0=ALU.mult, op1=ALU.mult),
          reads=list(src_keys) + ["rstd" + tag] + MODK, writes=["tmp" + tag])
        if out_f32 is not None:
            A("pool", lambda: G.tensor_tensor(out=out_f32, in0=tmp[:], in1=shift, op=ALU.add),
              reads=["tmp" + tag] + MODK, writes=list(out_keys))
            if out_bf is not None:
                A("act", lambda: ACT.copy(out=out_bf, in_=out_f32), reads=list(out_keys), writes=[k + ("bf",) for k in out_keys])
        else:
            A("pool", lambda: G.tensor_tensor(out=out_bf, in0=tmp[:], in1=shift, op=ALU.add),
              reads=["tmp" + tag] + MODK, writes=list(out_keys))

    def projection_pass(ps_, wmat_d, ncols, ngroups, emit_group, emit_v, vcol0, nvcols, tag):
        wsb = sb(ps_, "wsb" + tag, [128, 8 * ncols], BF16)
        w3 = wsb[:].rearrange("p (c n) -> p c n", n=ncols)
        for c in range(8):
            sc.dma("pool", lambda c=c: G.dma_start(out=w3[:, c, :], in_=wmat_d[c * 128:(c + 1) * 128, :]),
                   writes=[("wsb" + tag, c)])
        WK = [("wsb" + tag, c) for c in range(8)]
        xbl = [sb(ps_, f"xbl{tag}{i}", [128, D], F32) for i in range(2)]
        hbf = [sb(ps_, f"hbf{tag}{i}", [128, D], BF16) for i in range(2)]
        hT = [sb(ps_, f"hT{tag}{i}", [128, 8 * 512], BF16) for i in range(2)]
        tiles = [(sb(ps_, f"junk{tag}{j}", [128, D], BF16), sb(ps_, f"ssq{tag}{j}", [128, 1], F32),
                  sb(ps_, f"rstd{tag}{j}", [128, 1], F32), sb(ps_, f"tmp{tag}{j}", [128, D], F32)) for j in range(2)]
        for tc in range(8):
            hslot = tc % 2
            hT3 = hT[hslot][:].rearrange("p (c t) -> p c t", t=512)
            for sub in range(4):
                i = tc * 4 + sub
                xs_ = i % 2
                sc.dma("sp", lambda i=i, xs_=xs_: SP.dma_start(out=xbl[xs_][:], in_=x_d[i * 128:(i + 1) * 128, :]),
                       writes=[("xbl" + tag, xs_)])
                rmsnorm_mod(tiles[xs_], xbl[xs_][:], [("xbl" + tag, xs_)], scale1_m, shift_m, out_bf=hbf[xs_][:],
                            out_keys=[("hbf" + tag, xs_)], tag=tag + str(xs_))
                pbT = PS[0][:, (i % 2) * 512:(i % 2 + 1) * 512].bitcast(BF16)
                pkT = ("ps", i % 2)
                for c in range(8):
                    A("pe", lambda c=c, pbT=pbT, xs_=xs_: T.transpose(out=pbT[:, c * 128:(c + 1) * 128],
                                                                      in_=hbf[xs_][:, c * 128:(c + 1) * 128], identity=ident_b[:]),
                      reads=[("hbf" + tag, xs_), "ident_b"], writes=[pkT])
                A("act", lambda pbT=pbT, hT3=hT3, sub=sub: ACT.copy(out=hT3[:, :, sub * 128:(sub + 1) * 128],
                                                                    in_=pbT.rearrange("p (c t) -> p c t", t=128)),
                  reads=[pkT], writes=[("hT" + tag, hslot, sub)])
                pv, pvk = bank(2 + (i % 2))
                for c in range(8):
                    A("pe", lambda c=c, pv=pv, hT3=hT3, sub=sub: T.matmul(
                        pv[:, 0:nvcols], lhsT=hT3[:, c, sub * 128:(sub + 1) * 128], rhs=w3[:, c, vcol0:vcol0 + nvcols],
                        start=(c == 0), stop=(c == 7)),
                      reads=[("hT" + tag, hslot, sub)] + WK, writes=[pvk])
                emit_v(i, pv, pvk)
            HK = [("hT" + tag, hslot, s_) for s_ in range(4)]
            emit_group(tc, hT3, HK, w3, WK)
        return

    with ExitStack() as pa:
        qTa = sb(pa, "qTa", [128, 6 * S], BF16)
        qTa3 = qTa[:].rearrange("p (g t) -> p g t", t=S)
        vpa = sb(pa, "vpa", [128, NQB * 2 * 65], BF16)
        vpa4 = vpa[:].rearrange("p (i g d) -> p i g d", g=2, d=65)
        A("pool", lambda: G.memset(vpa[:], 1.0), writes=["vpa_init"])
        with ExitStack() as pa1:
            cosT = sb(pa1, "cosT", [128, S], F32)
            sinT = sb(pa1, "sinT", [128, S], F32)
            sc.dma("sp", lambda: SP.dma_start(out=cosT[:], in_=cos_d[:, :]), writes=["cosT"])
            sc.dma("sp", lambda: SP.dma_start(out=sinT[:], in_=sin_d[:, :]), writes=["sinT"])
            rt = [sb(pa1, f"rt{i}", [128, 512], F32) for i in range(4)]

            def emit_v_a(i, pv, pvk):
                A("act", lambda: ACT.copy(out=vpa4[:, i, :, 0:64], in_=pv[:, 0:128].rearrange("p (g d) -> p g d", d=64)),
                  reads=[pvk, "vpa_init"], writes=[("vpa", i)])

            def emit_group_a(tc, hT3, HK, w3, WK):
                for g in range(6):
                    c0 = g * 128 if g < 4 else 1024 + (g - 4) * 256
                    c1 = 512 + g * 128 if g < 4 else 1024 + 512 + (g - 4) * 256
                    if g >= 4:
                        c0 = 1024 + (g - 4) * 128
                        c1 = 1024 + 256 + (g - 4) * 128
                    pq, pqk = bank(4 + (g % 2) * 2)
                    pp, ppk = bank(5 + (g % 2) * 2)
                    for (pb, pk, col) in ((pq, pqk, c0), (pp, ppk, c1)):
                        for c in range(8):
                            A("pe", lambda pb=pb, c=c, col=col: T.matmul(pb, lhsT=w3[:, c, col:col + 128], rhs=hT3[:, c, :],
                                                                         start=(c == 0), stop=(c == 7)),
                              reads=HK + WK, writes=[pk])
                    r0, r1 = rt[(g % 2) * 2], rt[(g % 2) * 2 + 1]
                    k0, k1 = ("rt", (g % 2) * 2), ("rt", (g % 2) * 2 + 1)
                    tsl = slice(tc * 512, (tc + 1) * 512)
                    A("dve", lambda pq=pq, r0=r0, tsl=tsl: V.tensor_tensor(out=r0[:], in0=pq, in1=cosT[:, tsl], op=ALU.mult),
                      reads=[pqk, "cosT"], writes=[k0])
                    A("dve", lambda pp=pp, r1=r1, tsl=tsl: V.tensor_tensor(out=r1[:], in0=pp, in1=sinT[:, tsl], op=ALU.mult),
                      reads=[ppk, "sinT"], writes=[k1])
                    A("pool", lambda r0=r0, r1=r1, g=g, tsl=tsl: G.tensor_tensor(out=qTa3[:, g, tsl], in0=r0[:], in1=r1[:], op=ALU.add),
                      reads=[k0, k1], writes=[("qTa", g, tc)])

            projection_pass(pa1, wa_d, NWA, 12, emit_group_a, emit_v_a, 1536, 128, "A")
            sc.barrier()
        if dbg:
            sc.dma("sp", lambda: SP.dma_start(out=dbg_d["qta"][:, :], in_=qTa[:]),
                   reads=[("qTa", g, tc) for g in range(6) for tc in range(8)], writes=["dbg_qta"])

        with ExitStack() as pa2:
            if stop_after not in ("A", "B"):
                zt = sb(pa2, "zt", [128, 4 * D], BF16)
                A("pool", lambda: G.memset(zt[:], 0.0), writes=["zt"])
                for b in range(NBLK):
                    sc.dma("sp", lambda b=b: SP.dma_start(out=xs_d[b * MB:(b + 1) * MB, :].rearrange("(s p) d -> p s d", p=128),
                                                        in_=zt[:].rearrange("p (s d) -> p s d", d=D)), reads=["zt"], writes=["xs_d"])
            maska = sb(pa2, "maska", [128, 384], BF16)
            sc.dma("pool", lambda: G.dma_start(out=maska[:], in_=maska_d[:, :]), writes=["maska"])
            esink = sb(pa2, "esink", [128, 8], F32)
            sc.dma("sp", lambda: SP.dma_start(out=esink[:], in_=sink_d[:, :].partition_broadcast(128)), writes=["esink0"])
            A("act", lambda: ACT.activation(out=esink[:], in_=esink[:], func=AF.Exp), reads=["esink0"], writes=["esink"])
            goa = sb(pa2, "goa", [128, 512], F32)
            sc.dma("sp", lambda: SP.dma_start(out=goa[:], in_=goa_d[:, :].partition_broadcast(128)), writes=["goa"])
            pt = [sb(pa2, f"pta{i}", [128, 384], BF16) for i in range(3)]
            den = sb(pa2, "dena", [128, 8], F32)
            oa = sb(pa2, "oa", [128, 512], F32)
            junk2 = sb(pa2, "junk2a", [128, 512], BF16)
            ssq2 = sb(pa2, "ssq2a", [128, 1], F32)
            mixa = [sb(pa2, f"mixa{i}", [128, 512], BF16) for i in range(2)]
            for i in range(NQB):
                tcq = i // 4
                ms = [m for m in range(3) if 0 <= i + m - 1 < NQB]
                po = PS[3 - (i % 2)]
                pok = ("ps", 6 - 2 * (i % 2))
                po4 = po[:].rearrange("p (b x) -> p b x", b=2)[:, :, 0:260].rearrange("p b (h d) -> p b h d", d=65)
                def qk_a(h):
                    g, off = h // 2, (h % 2) * 64
                    kg = 4 + h // 4
                    pst, pstk = bank(h % 3)
                    for m in ms:
                        kb = i + m - 1
                        A("pe", lambda pst=pst, m=m, kb=kb, g=g, off=off, kg=kg, i=i: T.matmul(
                            pst[:, m * 128:(m + 1) * 128], lhsT=qTa3[off:off + 64, kg, kb * 128:(kb + 1) * 128],
                            rhs=qTa3[off:off + 64, g, i * 128:(i + 1) * 128], start=True, stop=False),
                          reads=[("qTa", kg, kb // 4), ("qTa", g, tcq)], writes=[pstk])
                        A("pe", lambda pst=pst, m=m: T.matmul(pst[:, m * 128:(m + 1) * 128], lhsT=ident_b[:],
                                                              rhs=maska[:, m * 128:(m + 1) * 128], start=False, stop=True),
                          reads=["ident_b", "maska"], writes=[pstk])

                qk_a(0)
                for h in range(8):
                    if h + 1 < 8:
                        qk_a(h + 1)
                    pst, pstk = bank(h % 3)
                    ptt = pt[h % 3]
                    ptk = ("pta", h % 3)
                    lo, hi = ms[0] * 128, (ms[-1] + 1) * 128
                    A("act", lambda pst=pst, ptt=ptt, lo=lo, hi=hi: ACT.activation(out=ptt[:, lo:hi], in_=pst[:, lo:hi],
                                                                                   func=AF.Exp, scale=0.125),
                      reads=[pstk], writes=[ptk])
                    for m in ms:
                        kb = i + m - 1
                        A("pe", lambda ptt=ptt, m=m, kb=kb, h=h, ms=ms, po=po: T.matmul(
                            po[:, (h // 4) * 512 + (h % 4) * 65:(h // 4) * 512 + (h % 4) * 65 + 65],
                            lhsT=ptt[:, m * 128:(m + 1) * 128], rhs=vpa4[:, kb, h // 4, :],
                            start=(m == ms[0]), stop=(m == ms[-1])),
                          reads=[ptk, ("vpa", kb)], writes=[pok])
                if i == 4:
                    dump("ptA", pt[7 % 3][:], [128, 384], BF16, [("pta", 7 % 3)])
                    if dbg:
                        podbg = sb(pa2, "podbg", [128, 1024], F32)
                        A("act", lambda: ACT.copy(out=podbg[:], in_=po[:]), reads=[pok], writes=["podbg"])
                        dump("poA", podbg[:], [128, 1024], F32, ["podbg"])
                    dump("vpa", vpa[:], [128, NQB * 2 * 65], BF16, [("vpa", kk) for kk in range(NQB)])
                    dump("esink", esink[:], [128, 8], F32, ["esink"])
                    dump("goa", goa[:], [128, 512], F32, ["goa"])
                A("dve", lambda po4=po4: V.tensor_tensor(out=den[:].rearrange("p (b h) -> p b h", b=2), in0=po4[:, :, :, 64],
                                                         in1=esink[:].rearrange("p (b h) -> p b h", b=2), op=ALU.add),
                  reads=[pok, "esink"], writes=["dena"])
                A("dve", lambda: V.reciprocal(out=den[:], in_=den[:]), reads=["dena"], writes=["dena"])
                A("dve", lambda po4=po4: V.tensor_tensor(
                    out=oa[:].rearrange("p (b h d) -> p b h d", b=2, d=64), in0=po4[:, :, :, 0:64],
                    in1=den[:].rearrange("p (b h) -> p b h", b=2).unsqueeze(3).to_broadcast([128, 2, 4, 64]), op=ALU.mult),
                  reads=[pok, "dena"], writes=["oa"])
                A("act", lambda: ACT.activation(out=junk2[:], in_=oa[:], func=AF.Square, accum_out=ssq2[:]),
                  reads=["oa"], writes=["junk2a", "ssq2a"])
                A("dve", lambda: V.tensor_scalar(out=ssq2[:], in0=ssq2[:], scalar1=1.0 / 512, scalar2=EPS, op0=ALU.mult, op1=ALU.add),
                  reads=["ssq2a"], writes=["ssq2a"])
                A("act", lambda: ACT.activation(out=ssq2[:], in_=ssq2[:], func=AF.Ln), reads=["ssq2a"], writes=["ssq2a"])
                A("act", lambda: ACT.activation(out=ssq2[:], in_=ssq2[:], func=AF.Exp, scale=-0.5), reads=["ssq2a"], writes=["ssq2a"])
                if i == 4:
                    dump("oaA", oa[:], [128, 512], F32, ["oa"])
                    dump("denA", den[:], [128, 8], F32, ["dena"])
                    dump("ssqA", ssq2[:], [128, 1], F32, ["ssq2a"])
                mx = mixa[i % 2]
                A("dve", lambda mx=mx: V.scalar_tensor_tensor(out=mx[:], in0=oa[:], scalar=ssq2[:, 0:1], in1=goa[:],
                                                              op0=ALU.mult, op1=ALU.mult),
                  reads=["oa", "ssq2a", "goa"], writes=[("mixa", i % 2)])
                sc.dma("sp", lambda mx=mx, i=i: SP.dma_start(out=mixa_d[i * 128:(i + 1) * 128, :], in_=mx[:]),
                       reads=[("mixa", i % 2)], writes=[("mixa_d", i)])
            sc.barrier()
    if stop_after == "A":
        sc.finish([("mixa_d", i) for i in range(NQB)] + ["dbg_qta", "dbg_mod"] + dbg_keys)
        es.close()
        return nc

    with ExitStack() as pb_:
        qTb = sb(pb_, "qTb", [128, 8 * S], BF16)
        qTb3 = qTb[:].rearrange("p (g t) -> p g t", t=S)
        vpb = sb(pb_, "vpb", [128, NQB * 8 * 65], BF16)
        vpb4 = vpb[:].rearrange("p (i g d) -> p i g d", g=8, d=65)
        A("pool", lambda: G.memset(vpb[:], 1.0), writes=["vpb_init"])
        with ExitStack() as pb1:
            def emit_v_b(i, pv, pvk):
                A("act", lambda: ACT.copy(out=vpb4[:, i, :, 0:64], in_=pv[:, 0:512].rearrange("p (g d) -> p g d", d=64)),
                  reads=[pvk, "vpb_init"], writes=[("vpb", i)])

            def emit_group_b(tc, hT3, HK, w3, WK):
                for g in range(8):
                    pq, pqk = bank(4 + g % 4)
                    for c in range(8):
                        A("pe", lambda pq=pq, c=c, g=g: T.matmul(pq, lhsT=w3[:, c, g * 128:(g + 1) * 128], rhs=hT3[:, c, :],
                                                                 start=(c == 0), stop=(c == 7)),
                          reads=HK + WK, writes=[pqk])
                    tsl = slice(tc * 512, (tc + 1) * 512)
                    if g % 2 == 0:
                        A("dve", lambda pq=pq, g=g, tsl=tsl: V.tensor_copy(out=qTb3[:, g, tsl], in_=pq), reads=[pqk], writes=[("qTb", g, tc)])
                    else:
                        A("act", lambda pq=pq, g=g, tsl=tsl: ACT.copy(out=qTb3[:, g, tsl], in_=pq), reads=[pqk], writes=[("qTb", g, tc)])

            projection_pass(pb1, wb_d, NWB, 8, emit_group_b, emit_v_b, 1024, 512, "B")
            sc.barrier()

        with ExitStack() as pb2:
            maskb = sb(pb2, "maskb", [128, _NVAR * 896], BF16)
            for v in range(_NVAR):
                sc.dma("pool", lambda v=v: G.dma_start(out=maskb[:, v * 896:(v + 1) * 896], in_=maskb_d[v, :, :]), writes=[("maskb", v)])
            biasu = sb(pb2, "biasu", [128, 8 * 896], F32)
            for h in range(8):
                sc.dma("sp", lambda h=h: SP.dma_start(out=biasu[:, h * 896:(h + 1) * 896], in_=biasu_d[h, :, :]), writes=[("biasu", h)])
            gob = sb(pb2, "gob", [128, 512], F32)
            sc.dma("sp", lambda: SP.dma_start(out=gob[:], in_=gob_d[:, :].partition_broadcast(128)), writes=["gob"])
            tt = [sb(pb2, f"ttb{i}", [128, 896], F32) for i in range(2)]
            pt = [sb(pb2, f"ptb{i}", [128, 896], BF16) for i in range(2)]
            den = sb(pb2, "denb", [128, 8], F32)
            ob = sb(pb2, "ob", [128, 512], F32)
            junk2 = sb(pb2, "junk2b", [128, 512], BF16)
            ssq2 = sb(pb2, "ssq2b", [128, 1], F32)
            mixb = [sb(pb2, f"mixb{i}", [128, 512], BF16) for i in range(2)]
            for i in range(NQB):
                tcq = i // 4
                ms = _KBS[i]
                var = _VAR_OF[i]
                po = PS[3 - (i % 2)]
                pok = ("ps", 6 - 2 * (i % 2))
                po4 = po[:].rearrange("p (b x) -> p b x", b=2)[:, :, 0:260].rearrange("p b (h d) -> p b h d", d=65)
                lo, hi = ms[0] * 128, (ms[-1] + 1) * 128
                def qk_b(h):
                    g, off, kg = h // 2, (h % 2) * 64, 4 + h // 2
                    sl = h % 2
                    pst = PS[sl]
                    pstk = [("ps", 2 * sl), ("ps", 2 * sl + 1)]
                    for m in ms:
                        kb = i + m - 3
                        A("pe", lambda pst=pst, m=m, kb=kb, g=g, off=off, kg=kg, i=i: T.matmul(
                            pst[:, m * 128:(m + 1) * 128], lhsT=qTb3[off:off + 64, kg, kb * 128:(kb + 1) * 128],
                            rhs=qTb3[off:off + 64, g, i * 128:(i + 1) * 128], start=True, stop=False),
                          reads=[("qTb", kg, kb // 4), ("qTb", g, tcq)], writes=pstk)
                        A("pe", lambda pst=pst, m=m, var=var: T.matmul(pst[:, m * 128:(m + 1) * 128], lhsT=ident_b[:],
                                                                       rhs=maskb[:, var * 896 + m * 128:var * 896 + (m + 1) * 128],
                                                                       start=False, stop=True),
                          reads=["ident_b", ("maskb", var)], writes=pstk)

                qk_b(0)
                for h in range(8):
                    if h + 1 < 8:
                        qk_b(h + 1)
                    sl = h % 2
                    pst = PS[sl]
                    pstk = [("ps", 2 * sl), ("ps", 2 * sl + 1)]
                    ttt, ptt = tt[sl], pt[sl]
                    A("dve", lambda pst=pst, ttt=ttt, h=h, lo=lo, hi=hi: V.scalar_tensor_tensor(
                        out=ttt[:, lo:hi], in0=pst[:, lo:hi], scalar=0.125, in1=biasu[:, h * 896 + lo:h * 896 + hi],
                        op0=ALU.mult, op1=ALU.add), reads=pstk + [("biasu", h)], writes=[("ttb", sl)])
                    A("act", lambda ttt=ttt, ptt=ptt, lo=lo, hi=hi: ACT.activation(out=ptt[:, lo:hi], in_=ttt[:, lo:hi], func=AF.Exp),
                      reads=[("ttb", sl)], writes=[("ptb", sl)])
                    for m in ms:
                        kb = i + m - 3
                        A("pe", lambda ptt=ptt, m=m, kb=kb, h=h, ms=ms, po=po: T.matmul(
                            po[:, (h // 4) * 512 + (h % 4) * 65:(h // 4) * 512 + (h % 4) * 65 + 65],
                            lhsT=ptt[:, m * 128:(m + 1) * 128], rhs=vpb4[:, kb, h, :],
                            start=(m == ms[0]), stop=(m == ms[-1])),
                          reads=[("ptb", sl), ("vpb", kb)], writes=[pok])
                A("dve", lambda po4=po4: V.reciprocal(out=den[:].rearrange("p (b h) -> p b h", b=2), in_=po4[:, :, :, 64]),
                  reads=[pok], writes=["denb"])
                A("dve", lambda po4=po4: V.tensor_tensor(
                    out=ob[:].rearrange("p (b h d) -> p b h d", b=2, d=64), in0=po4[:, :, :, 0:64],
                    in1=den[:].rearrange("p (b h) -> p b h", b=2).unsqueeze(3).to_broadcast([128, 2, 4, 64]), op=ALU.mult),
                  reads=[pok, "denb"], writes=["ob"])
                A("act", lambda: ACT.activation(out=junk2[:], in_=ob[:], func=AF.Square, accum_out=ssq2[:]),
                  reads=["ob"], writes=["junk2b", "ssq2b"])
                A("dve", lambda: V.tensor_scalar(out=ssq2[:], in0=ssq2[:], scalar1=1.0 / 512, scalar2=EPS, op0=ALU.mult, op1=ALU.add),
                  reads=["ssq2b"], writes=["ssq2b"])
                A("act", lambda: ACT.activation(out=ssq2[:], in_=ssq2[:], func=AF.Ln), reads=["ssq2b"], writes=["ssq2b"])
                A("act", lambda: ACT.activation(out=ssq2[:], in_=ssq2[:], func=AF.Exp, scale=-0.5), reads=["ssq2b"], writes=["ssq2b"])
                mx = mixb[i % 2]
                A("dve", lambda mx=mx: V.scalar_tensor_tensor(out=mx[:], in0=ob[:], scalar=ssq2[:, 0:1], in1=gob[:],
                                                              op0=ALU.mult, op1=ALU.mult),
                  reads=["ob", "ssq2b", "gob"], writes=[("mixb", i % 2)])
                sc.dma("sp", lambda mx=mx, i=i: SP.dma_start(out=mixb_d[i * 128:(i + 1) * 128, :], in_=mx[:]),
                       reads=[("mixb", i % 2)], writes=[("mixb_d", i)])
            sc.barrier()
    if stop_after == "B":
        sc.finish([("mixa_d", i) for i in range(NQB)] + [("mixb_d", i) for i in range(NQB)] + ["dbg_qta", "dbg_mod"])
        es.close()
        return nc

    rt_ = ExitStack()
    lg_all = sb(rt_, "lg_all", [128, NQB * NE], F32)
    m8_all = sb(rt_, "m8_all", [128, NQB * 8], F32)
    lg3 = lg_all[:].rearrange("p (i e) -> p i e", e=NE)
    m83 = m8_all[:].rearrange("p (i k) -> p i k", k=8)
    with ExitStack() as pc:
        wout = sb(pc, "wout", [128, 8 * D], BF16)
        wout3 = wout[:].rearrange("p (c n) -> p c n", n=D)
        for c in range(8):
            sc.dma("pool", lambda c=c: G.dma_start(out=wout3[:, c, :], in_=wout_d[c * 128:(c + 1) * 128, :]), writes=[("wout", c)])
        WOK = [("wout", c) for c in range(8)]
        wr = sb(pc, "wr", [128, 8 * NE], F32)
        sc.dma("sp", lambda: SP.dma_start(out=wr[:].rearrange("p (c e) -> p c e", e=NE),
                                          in_=wr_d[:, :].rearrange("(c p) e -> p c e", p=128)), writes=["wr"])
        brt = sb(pc, "brt", [1, NE], F32)
        sc.dma("sp", lambda: SP.dma_start(out=brt[:], in_=br_d[:, :]), writes=["brt"])
        mixab = [sb(pc, f"mixab{i}", [128, D], BF16) for i in range(2)]
        xb_ = [sb(pc, f"xc{i}", [128, D], F32) for i in range(2)]
        mixT_ = [sb(pc, f"mixT{j}", [128, D], BF16) for j in range(2)]
        t1_ = [sb(pc, f"t1{j}", [128, D], F32) for j in range(2)]
        x1t = [sb(pc, f"x1t{i}", [128, D], F32) for i in range(2)]
        h2f_ = [sb(pc, f"h2f{j}", [128, D], F32) for j in range(2)]
        h2b = [sb(pc, f"h2b{i}", [128, D], BF16) for i in range(2)]
        h2T_ = [sb(pc, f"h2T{j}", [128, D], F32) for j in range(2)]
        tilesC_ = [(sb(pc, f"junkC{j}", [128, D], BF16), sb(pc, f"ssqC{j}", [128, 1], F32),
                    sb(pc, f"rstdC{j}", [128, 1], F32), sb(pc, f"tmpC{j}", [128, D], F32)) for j in range(2)]
        def c_loads(i):
            s2 = i % 2
            sc.dma("sp", lambda i=i, s2=s2: SP.dma_start(out=mixab[s2][:, 0:512], in_=mixa_d[i * 128:(i + 1) * 128, :]),
                   reads=[("mixa_d", i)], writes=[("mixab", s2, 0)])
            sc.dma("sp", lambda i=i, s2=s2: SP.dma_start(out=mixab[s2][:, 512:1024], in_=mixb_d[i * 128:(i + 1) * 128, :]),
                   reads=[("mixb_d", i)], writes=[("mixab", s2, 1)])
            sc.dma("sp", lambda i=i, s2=s2: SP.dma_start(out=xb_[s2][:], in_=x_d[i * 128:(i + 1) * 128, :]), writes=[("xc", s2)])

        def c_a(i):
            s2 = i % 2
            mixT, t1, h2f, h2T, tilesC = mixT_[s2], t1_[s2], h2f_[s2], h2T_[s2], tilesC_[s2]
            pbT = PS[0][:, s2 * 512:(s2 + 1) * 512].bitcast(BF16)
            for c in range(8):
                A("pe", lambda c=c, pbT=pbT, s2=s2: T.transpose(out=pbT[:, c * 128:(c + 1) * 128],
                                                                in_=mixab[s2][:, c * 128:(c + 1) * 128], identity=ident_b[:]),
                  reads=[("mixab", s2, 0), ("mixab", s2, 1), "ident_b"], writes=[("ps", s2)])
            A("act", lambda pbT=pbT, mixT=mixT: ACT.copy(out=mixT[:], in_=pbT), reads=[("ps", s2)], writes=[("mixT", s2)])
            for n in range(2):
                py, pyk = bank(2 + n)
                for c in range(8):
                    A("pe", lambda py=py, c=c, n=n, mixT=mixT: T.matmul(py, lhsT=mixT[:, c * 128:(c + 1) * 128],
                                                             rhs=wout3[:, c, n * 512:(n + 1) * 512], start=(c == 0), stop=(c == 7)),
                      reads=[("mixT", s2)] + WOK, writes=[pyk])
                A("dve", lambda py=py, n=n, t1=t1: V.tensor_tensor(out=t1[:, n * 512:(n + 1) * 512], in0=py,
                                                            in1=gate_m[:, n * 512:(n + 1) * 512], op=ALU.mult),
                  reads=[pyk] + MODK, writes=[("t1", s2, n)])
            xt = x1t[s2]
            A("pool", lambda xt=xt, s2=s2, t1=t1: G.tensor_tensor(out=xt[:], in0=t1[:], in1=xb_[s2][:], op=ALU.add),
              reads=[("t1", s2, 0), ("t1", s2, 1), ("xc", s2)], writes=[("x1t", s2)])
            sc.dma("sp", lambda xt=xt, i=i: SP.dma_start(out=x1_d[i * 128:(i + 1) * 128, :], in_=xt[:]),
                   reads=[("x1t", s2)], writes=[("x1_d", i)])
            rmsnorm_mod(tilesC, xt[:], [("x1t", s2)], scale1_f, shift_f, out_bf=h2b[s2][:], out_f32=h2f[:],
                        out_keys=[("h2f", s2)], tag="C" + str(s2))
            sc.dma("sp", lambda i=i, s2=s2: SP.dma_start(out=h2_d[i * 128:(i + 1) * 128, :], in_=h2b[s2][:]),
                   reads=[("h2f", s2, "bf")], writes=[("h2_d", i)])

        def c_b(i):
            s2 = i % 2
            mixT, t1, h2f, h2T, tilesC = mixT_[s2], t1_[s2], h2f_[s2], h2T_[s2], tilesC_[s2]
            for r_ in range(2):
                pt_, ptk_ = bank(4 + r_)
                for c4 in range(4):
                    c = r_ * 4 + c4
                    A("pe", lambda pt_=pt_, c=c, c4=c4, h2f=h2f: T.transpose(out=pt_[:, c4 * 128:(c4 + 1) * 128],
                                                                    in_=h2f[:, c * 128:(c + 1) * 128], identity=ident_f[:]),
                      reads=[("h2f", s2), "ident_f"], writes=[ptk_])
                if r_ == 0:
                    A("dve", lambda pt_=pt_, r_=r_, h2T=h2T: V.tensor_copy(out=h2T[:, r_ * 512:(r_ + 1) * 512], in_=pt_), reads=[ptk_], writes=[("h2T", s2, r_)])
                else:
                    A("act", lambda pt_=pt_, r_=r_, h2T=h2T: ACT.copy(out=h2T[:, r_ * 512:(r_ + 1) * 512], in_=pt_), reads=[ptk_], writes=[("h2T", s2, r_)])
            pl, plk = bank(6 + s2)
            for c in range(8):
                A("pe", lambda pl=pl, c=c, h2T=h2T: T.matmul(pl[:, 0:NE], lhsT=h2T[:, c * 128:(c + 1) * 128], rhs=wr[:, c * NE:(c + 1) * NE],
                                                    start=(c == 0), stop=False),
                  reads=[("h2T", s2, 0), ("h2T", s2, 1), "wr"], writes=[plk])
            A("pe", lambda pl=pl: T.matmul(pl[:, 0:NE], lhsT=ones_f[0:1, :], rhs=brt[0:1, :], start=False, stop=True),
              reads=["ones_f", "brt"], writes=[plk])
            A("dve", lambda pl=pl, i=i: V.tensor_copy(out=lg3[:, i, :], in_=pl[:, 0:NE]), reads=[plk], writes=[("lg", i)])
            A("dve", lambda i=i: V.max(out=m83[:, i, :], in_=lg3[:, i, :]), reads=[("lg", i)], writes=[("m8", i)])

        c_loads(0)
        for i in range(NQB):
            if i + 1 < NQB:
                c_loads(i + 1)
            c_a(i)
            if i >= 1:
                c_b(i - 1)
        c_b(NQB - 1)
        sc.barrier()
    LGK = [("lg", i) for i in range(NQB)] + [("m8", i) for i in range(NQB)]

    gw_all = sb(rt_, "gw_all", [128, NQB * NE], F32)
    gw3 = gw_all[:].rearrange("p (i e) -> p i e", e=NE)
    dsel_i = sb(rt_, "dsel_i", [128, 4 * NQB], I32)
    gk = sb(rt_, "gk", [128, 4 * NQB], F32)
    idxw_i = sb(rt_, "idxw_i", [128, NBLK * 8], I32)
    idxg_i = sb(rt_, "idxg_i", [128, NBLK * 8], I32)
    idxb_i = sb(rt_, "idxb_i", [128, NBLK], I32)
    with ExitStack() as pr:
        tri = sb(pr, "tri", [128, 128], F32)
        sc.dma("sp", lambda: SP.dma_start(out=tri[:], in_=tri_d[:, :]), writes=["tri"])
        rowid = sb(pr, "rowid", [128, 8], F32)
        sc.dma("sp", lambda: SP.dma_start(out=rowid[:], in_=rowid_d[:, :]), writes=["rowid"])
        blkth = sb(pr, "blkth", [128, NBLK * NE], F32)
        sc.dma("sp", lambda: SP.dma_start(out=blkth[:], in_=blkth_d[:, :]), writes=["blkth"])
        msk = sb(pr, "msk", [128, NQB * NE], F32)
        msk3 = msk[:].rearrange("p (i e) -> p i e", e=NE)
        ex = sb(pr, "ex", [128, NQB * NE], F32)
        ex3 = ex[:].rearrange("p (i e) -> p i e", e=NE)
        ssum = sb(pr, "ssum", [128, NQB], F32)
        pos = sb(pr, "pos", [128, NQB * NE], F32)
        pos3 = pos[:].rearrange("p (i e) -> p i e", e=NE)
        oh = sb(pr, "oh", [128, NQB * NE], F32)
        oh3 = oh[:].rearrange("p (i e) -> p i e", e=NE)
        prod = sb(pr, "prod", [128, NQB * NE], F32)
        prod3 = prod[:].rearrange("p (i e) -> p i e", e=NE)
        dself = sb(pr, "dself", [128, 4 * NQB], F32)
        cnt = sb(pr, "cnt", [128, NE], F32)
        cs = [sb(pr, f"cs{i}", [128, NE], F32) for i in range(2)]
        padded = sb(pr, "padded", [128, NE], F32)
        pstart = sb(pr, "pstart", [128, NE], F32)
        cmpb = sb(pr, "cmpb", [128, NBLK * NE], F32)
        blke = sb(pr, "blke", [128, NBLK], F32)
        idxwf = sb(pr, "idxwf", [128, NBLK * 8], F32)
        idxbf = sb(pr, "idxbf", [128, NBLK], F32)

        A("dve", lambda: V.tensor_tensor(out=msk3, in0=lg3, in1=m83[:, :, 3:4].to_broadcast([128, NQB, NE]), op=ALU.is_ge),
          reads=LGK, writes=["msk"])
        A("dve", lambda: V.tensor_tensor(out=ex3, in0=lg3, in1=m83[:, :, 0:1].to_broadcast([128, NQB, NE]), op=ALU.subtract),
          reads=LGK, writes=["ex"])
        A("act", lambda: ACT.activation(out=ex[:], in_=ex[:], func=AF.Exp), reads=["ex"], writes=["ex"])
        A("dve", lambda: V.tensor_tensor(out=ex[:], in0=ex[:], in1=msk[:], op=ALU.mult), reads=["ex", "msk"], writes=["ex"])
        A("dve", lambda: V.reduce_sum(out=ssum[:], in_=ex3, axis=AX.X), reads=["ex"], writes=["ssum"])
        A("dve", lambda: V.reciprocal(out=ssum[:], in_=ssum[:]), reads=["ssum"], writes=["ssum"])
        A("dve", lambda: V.tensor_tensor(out=gw3, in0=ex3, in1=ssum[:].unsqueeze(2).to_broadcast([128, NQB, NE]), op=ALU.mult),
          reads=["ex", "ssum"], writes=["gw"])
        for half in range(2):
            pp_, ppk_ = bank(half)
            for ii in range(16):
                i = half * 16 + ii
                for j in range(i):
                    A("pe", lambda pp_=pp_, ii=ii, j=j: T.matmul(pp_[:, ii * NE:(ii + 1) * NE], lhsT=ones_f[:], rhs=msk3[:, j, :],
                                                                 start=(j == 0), stop=False),
                      reads=["ones_f", "msk"], writes=[ppk_])
                A("pe", lambda pp_=pp_, ii=ii, i=i: T.matmul(pp_[:, ii * NE:(ii + 1) * NE], lhsT=tri[:], rhs=msk3[:, i, :],
                                                             start=(i == 0), stop=True),
                  reads=["tri", "msk"], writes=[ppk_])
            A("dve", lambda pp_=pp_, half=half: V.tensor_copy(out=pos[:, half * 512:(half + 1) * 512], in_=pp_),
              reads=[ppk_], writes=[("pos", half)])
        pc_, pck_ = bank(2)
        for j in range(NQB):
            A("pe", lambda j=j: T.matmul(pc_[:, 0:NE], lhsT=ones_f[:], rhs=msk3[:, j, :], start=(j == 0), stop=(j == NQB - 1)),
              reads=["ones_f", "msk"], writes=[pck_])
        A("dve", lambda: V.tensor_copy(out=cnt[:], in_=pc_[:, 0:NE]), reads=[pck_], writes=["cnt"])
        nbt = sb(pr, "nbt", [128, NE * 8], F32)
        A("dve", lambda: V.tensor_tensor(out=nbt[:].rearrange("p (e j) -> p e j", j=8),
                                         in0=cnt[:].unsqueeze(2).to_broadcast([128, NE, 8]),
                                         in1=blkth[:, 0:8 * NE].rearrange("p (b e) -> p e b", e=NE), op=ALU.is_gt),
          reads=["cnt", "blkth"], writes=["nbt"])
        A("dve", lambda: V.reduce_sum(out=padded[:], in_=nbt[:].rearrange("p (e j) -> p e j", j=8), axis=AX.X), reads=["nbt"], writes=["padded"])
        A("dve", lambda: V.tensor_scalar(out=padded[:], in0=padded[:], scalar1=float(MB), scalar2=None, op0=ALU.mult),
          reads=["padded"], writes=["padded"])
        A("dve", lambda: V.tensor_copy(out=cs[0][:], in_=padded[:]), reads=["padded"], writes=[("cs", 0)])
        cur = 0
        for sft in (1, 2, 4, 8, 16):
            nxt = 1 - cur
            A("dve", lambda cur=cur, nxt=nxt, sft=sft: V.tensor_copy(out=cs[nxt][:, 0:sft], in_=cs[cur][:, 0:sft]),
              reads=[("cs", cur)], writes=[("cs", nxt)])
            A("dve", lambda cur=cur, nxt=nxt, sft=sft: V.tensor_tensor(out=cs[nxt][:, sft:NE], in0=cs[cur][:, sft:NE],
                                                                       in1=cs[cur][:, 0:NE - sft], op=ALU.add),
              reads=[("cs", cur), ("cs", nxt)], writes=[("cs", nxt)])
            cur = nxt
        pend = cs[cur]
        pendk = ("cs", cur)
        A("dve", lambda: V.tensor_tensor(out=pstart[:], in0=pend[:], in1=padded[:], op=ALU.subtract), reads=[pendk, "padded"], writes=["pstart"])
        A("dve", lambda: V.tensor_tensor(out=pos3, in0=pos3, in1=pstart[:].unsqueeze(1).to_broadcast([128, NQB, NE]), op=ALU.add),
          reads=[("pos", 0), ("pos", 1), "pstart"], writes=["dest"])
        for k in range(4):
            A("dve", lambda k=k: V.tensor_tensor(out=oh3, in0=lg3, in1=m83[:, :, k:k + 1].to_broadcast([128, NQB, NE]), op=ALU.is_equal),
              reads=LGK, writes=["oh"])
            A("dve", lambda: V.tensor_tensor(out=prod[:], in0=oh[:], in1=pos[:], op=ALU.mult), reads=["oh", "dest"], writes=["prod"])
            A("dve", lambda k=k: V.reduce_sum(out=dself[:, k * NQB:(k + 1) * NQB], in_=prod3, axis=AX.X), reads=["prod"], writes=[("dself", k)])
            A("dve", lambda: V.tensor_tensor(out=prod[:], in0=oh[:], in1=gw_all[:], op=ALU.mult), reads=["oh", "gw"], writes=["prod"])
            A("dve", lambda k=k: V.reduce_sum(out=gk[:, k * NQB:(k + 1) * NQB], in_=prod3, axis=AX.X), reads=["prod"], writes=[("gk", k)])
        A("dve", lambda: V.tensor_copy(out=dsel_i[:], in_=dself[:]), reads=[("dself", k) for k in range(4)], writes=["dsel_i"])
        A("dve", lambda: V.tensor_tensor(out=cmpb[:].rearrange("p (b e) -> p b e", e=NE),
                                         in0=pend[:].unsqueeze(1).to_broadcast([128, NBLK, NE]),
                                         in1=blkth[:].rearrange("p (b e) -> p b e", e=NE), op=ALU.is_le),
          reads=[pendk, "blkth"], writes=["cmpb"])
        A("dve", lambda: V.reduce_sum(out=blke[:], in_=cmpb[:].rearrange("p (b e) -> p b e", e=NE), axis=AX.X), reads=["cmpb"], writes=["blke"])
        A("dve", lambda: V.tensor_scalar(out=blke[:], in0=blke[:], scalar1=float(NE - 1), scalar2=None, op0=ALU.min), reads=["blke"], writes=["blke"])
        A("dve", lambda: V.scalar_tensor_tensor(out=idxwf[:].rearrange("p (b c) -> p b c", c=8),
                                                in0=blke[:].unsqueeze(2).to_broadcast([128, NBLK, 8]), scalar=float(D),
                                                in1=rowid[:].unsqueeze(1).to_broadcast([128, NBLK, 8]), op0=ALU.mult, op1=ALU.add),
          reads=["blke", "rowid"], writes=["idxwf"])
        nused = sb(pr, "nused", [128, NBLK], F32)
        A("dve", lambda: V.tensor_scalar(out=nused[:], in0=blkth[:].rearrange("p (b e) -> p b e", e=NE)[:, :, 0],
                                         scalar1=pend[:, NE - 1:NE], scalar2=None, op0=ALU.is_ge),
          reads=[pendk, "blkth"], writes=["nused"])
        sameb = sb(pr, "sameb", [128, NBLK], F32)
        A("dve", lambda: V.memset(sameb[:], 0.0), writes=["sameb"])
        A("dve", lambda: V.tensor_tensor(out=sameb[:, 2:NBLK], in0=blke[:, 2:NBLK], in1=blke[:, 0:NBLK - 2], op=ALU.is_equal),
          reads=["blke", "sameb"], writes=["sameb"])
        A("dve", lambda: V.tensor_tensor(out=nused[:], in0=nused[:], in1=sameb[:], op=ALU.max), reads=["nused", "sameb"], writes=["nused"])
        idxgf = sb(pr, "idxgf", [128, NBLK * 8], F32)
        A("dve", lambda: V.scalar_tensor_tensor(out=idxgf[:].rearrange("p (b c) -> p b c", c=8),
                                                in0=nused[:].unsqueeze(2).to_broadcast([128, NBLK, 8]), scalar=40000.0,
                                                in1=idxwf[:].rearrange("p (b c) -> p b c", c=8), op0=ALU.mult, op1=ALU.add),
          reads=["nused", "idxwf"], writes=["idxgf"])
        A("dve", lambda: V.tensor_copy(out=idxg_i[:], in_=idxgf[:]), reads=["idxgf"], writes=["idxg_i"])
        A("dve", lambda: V.tensor_copy(out=idxw_i[:], in_=idxwf[:]), reads=["idxwf"], writes=["idxw_i"])
        A("dve", lambda: V.scalar_tensor_tensor(out=idxbf[:], in0=blke[:], scalar=128.0, in1=rowid[:, 0:1].to_broadcast([128, NBLK]),
                                                op0=ALU.mult, op1=ALU.add), reads=["blke", "rowid"], writes=["idxbf"])
        A("dve", lambda: V.scalar_tensor_tensor(out=idxbf[:], in0=nused[:], scalar=40000.0, in1=idxbf[:], op0=ALU.mult, op1=ALU.add),
          reads=["nused", "idxbf"], writes=["idxbf"])
        A("dve", lambda: V.tensor_copy(out=idxb_i[:], in_=idxbf[:]), reads=["idxbf"], writes=["idxb_i"])
        if dbg:
            sc.dma("sp", lambda: SP.dma_start(out=dbg_d["gw"][:, :], in_=gw_all[:]), reads=["gw"], writes=["dbg_gw"])
            sc.dma("sp", lambda: SP.dma_start(out=dbg_d["dsel"][:, :], in_=dsel_i[:]), reads=["dsel_i"], writes=["dbg_dsel"])
            sc.dma("sp", lambda: SP.dma_start(out=dbg_d["blke"][:, :], in_=blke[:]), reads=["blke"], writes=["dbg_blke"])
        h2r = [sb(pr, f"h2r{i}", [128, D], BF16) for i in range(4)]
        for i in range(NQB):
            s4 = i % 4
            sc.dma("sp", lambda i=i, s4=s4: SP.dma_start(out=h2r[s4][:], in_=h2_d[i * 128:(i + 1) * 128, :]),
                   reads=[("h2_d", i)], writes=[("h2r", s4)])
            for k in range(4):
                sc.dma("pool", lambda i=i, k=k, s4=s4: G.indirect_dma_start(
                    out=xs_d[:, :], out_offset=bass.IndirectOffsetOnAxis(ap=dsel_i[:, k * NQB + i:k * NQB + i + 1], axis=0),
                    in_=h2r[s4][:], in_offset=None), reads=[("h2r", s4), "dsel_i"], writes=["xs_d"])
        sc.barrier()
    if stop_after == "R":
        sc.finish(["dbg_gw", "dbg_dsel", "dbg_blke", "xs_d"] + [("x1_d", i) for i in range(NQB)])
        rt_.close()
        es.close()
        return nc

    with ExitStack() as pm:
        wgu = [sb(pm, f"wgu{i}", [128, 8 * 2 * DFF], BF16) for i in range(2)]
        wdn = [sb(pm, f"wdn{i}", [128, 8 * D], BF16) for i in range(2)]
        bgu = [sb(pm, f"bgu{i}", [128, 16], F32) for i in range(2)]
        xst = [sb(pm, "xst0", [128, 4 * D], BF16)]
        xsT = sb(pm, "xsT", [128, 8 * MB], BF16)
        xsT3 = xsT[:].rearrange("p (c t) -> p c t", t=MB)
        actT_ = [sb(pm, f"actT{j}", [128, 8 * MB], BF16) for j in range(2)]
        actT3_ = [a_[:].rearrange("p (f t) -> p f t", t=MB) for a_ in actT_]
        gt = [sb(pm, f"gt{i}", [128, MB], F32) for i in range(2)]
        sg = [sb(pm, f"sg{i}", [128, MB], F32) for i in range(2)]
        ut = [sb(pm, f"ut{i}", [128, MB], F32) for i in range(2)]
        yst = [sb(pm, f"yst{i}", [128, D], F32) for i in range(2)]

        bc_reg = [G.to_reg(NE * D - 1), G.to_reg(NE * 128 - 1)]

        def load_weights(b, which):
            sl = b % 2
            w3g = wgu[sl][:].rearrange("p (c n) -> p c n", n=2 * DFF)
            w3d = wdn[sl][:].rearrange("p (c n) -> p c n", n=D)
            for c in range(8 if which == "gu" else 0):
                sc.dma("pool", lambda c=c, w3g=w3g, b=b: G.indirect_dma_start(
                    out=w3g[:, c, :], out_offset=None, in_=wgu_d[:, :],
                    in_offset=bass.IndirectOffsetOnAxis(ap=idxg_i[:, b * 8 + c:b * 8 + c + 1], axis=0),
                    bounds_check=bc_reg[0], oob_is_err=False),
                    reads=["idxg_i"], writes=[("wgu", sl, c)])
            for c in range(8 if which == "wd" else 0):
                sc.dma("pool", lambda c=c, w3d=w3d, b=b: G.indirect_dma_start(
                    out=w3d[:, c, :], out_offset=None, in_=wd_d[:, :],
                    in_offset=bass.IndirectOffsetOnAxis(ap=idxg_i[:, b * 8 + c:b * 8 + c + 1], axis=0),
                    bounds_check=bc_reg[0], oob_is_err=False),
                    reads=["idxg_i"], writes=[("wdn", sl, c)])
            if which == "gu":
              sc.dma("pool", lambda b=b, sl=sl: G.indirect_dma_start(
                out=bgu[sl][:], out_offset=None, in_=bgu_d[:, :],
                in_offset=bass.IndirectOffsetOnAxis(ap=idxb_i[:, b:b + 1], axis=0),
                bounds_check=bc_reg[1], oob_is_err=False), reads=["idxb_i"], writes=[("bgu", sl)])

        def load_x(b):
            sl = 0
            sc.dma("sp", lambda b=b, sl=sl: SP.dma_start(out=xst[sl][:].rearrange("p (s d) -> p s d", d=D),
                                                        in_=xs_d[b * MB:(b + 1) * MB, :].rearrange("(s p) d -> p s d", p=128)),
                   reads=["xs_d"], writes=[("xst", sl)])

        for j in range(2):
            A("dve", lambda j=j: V.memset(wgu[j][:], 0.0), writes=[("wgu", j, c) for c in range(8)])
            A("dve", lambda j=j: V.memset(wdn[j][:], 0.0), writes=[("wdn", j, c) for c in range(8)])
            A("dve", lambda j=j: V.memset(bgu[j][:], 0.0), writes=[("bgu", j)])
        load_weights(0, "gu")
        load_x(0)

        def down_proj(b):
            sl = b % 2
            w3d = wdn[sl][:].rearrange("p (c n) -> p c n", n=D)
            WDK = [("wdn", sl, c) for c in range(8)]
            aT3 = actT3_[sl]
            AK = [("actT", sl, f) for f in range(8)]
            for s_ in range(4):
                ys_ = yst[s_ % 2]
                for n in range(2):
                    py, pyk = bank(6 + n)
                    for f in range(8):
                        A("pe", lambda py=py, f=f, s_=s_, n=n, w3d=w3d, aT3=aT3: T.matmul(
                            py, lhsT=aT3[:, f, s_ * 128:(s_ + 1) * 128], rhs=w3d[:, f, n * 512:(n + 1) * 512],
                            start=(f == 0), stop=(f == 7)), reads=AK + WDK, writes=[pyk])
                    A("act", lambda py=py, ys_=ys_, n=n: ACT.copy(out=ys_[:, n * 512:(n + 1) * 512], in_=py),
                      reads=[pyk], writes=[("yst", s_ % 2, n)])
                sc.dma("sp", lambda ys_=ys_, b=b, s_=s_: SP.dma_start(out=ys_d[b * MB + s_ * 128:b * MB + (s_ + 1) * 128, :], in_=ys_[:]),
                       reads=[("yst", s_ % 2, 0), ("yst", s_ % 2, 1)], writes=["ys_d"])

        for b in range(NBLK):
            sl = b % 2
            w3g = wgu[sl][:].rearrange("p (c n) -> p c n", n=2 * DFF)
            WGK = [("wgu", sl, c) for c in range(8)]
            aT3 = actT3_[sl]
            for s_ in range(4):
                pbT = PS[0][:, (s_ % 2) * 512:(s_ % 2 + 1) * 512].bitcast(BF16)
                pkT = ("ps", s_ % 2)
                for c in range(8):
                    A("pe", lambda c=c, pbT=pbT, s_=s_: T.transpose(
                        out=pbT[:, c * 128:(c + 1) * 128], in_=xst[0][:, s_ * D + c * 128:s_ * D + (c + 1) * 128], identity=ident_b[:]),
                      reads=[("xst", 0), "ident_b"], writes=[pkT])
                if s_ % 2 == 0:
                    A("dve", lambda pbT=pbT, s_=s_: V.tensor_copy(out=xsT3[:, :, s_ * 128:(s_ + 1) * 128],
                                                                  in_=pbT.rearrange("p (c t) -> p c t", t=128)),
                      reads=[pkT], writes=[("xsT", s_)])
                else:
                    A("act", lambda pbT=pbT, s_=s_: ACT.copy(out=xsT3[:, :, s_ * 128:(s_ + 1) * 128],
                                                             in_=pbT.rearrange("p (c t) -> p c t", t=128)),
                      reads=[pkT], writes=[("xsT", s_)])
            if b + 1 < NBLK:
                load_x(b + 1)
            if b >= 1:
                down_proj(b - 1)
            load_weights(b, "wd")
            if b + 1 < NBLK:
                load_weights(b + 1, "gu")
            XK = [("xsT", s_) for s_ in range(4)]
            for f in range(8):
                e2 = f % 2
                pg, pgk = bank(2 + e2 * 2)
                pu, puk = bank(3 + e2 * 2)
                for (pb, pk, col) in ((pg, pgk, f * 128), (pu, puk, DFF + f * 128)):
                    for c in range(8):
                        A("pe", lambda pb=pb, c=c, col=col, w3g=w3g: T.matmul(pb, lhsT=w3g[:, c, col:col + 128], rhs=xsT3[:, c, :],
                                                                              start=(c == 0), stop=(c == 7)),
                          reads=XK + WGK, writes=[pk])
                g_, s__, u_ = gt[e2], sg[e2], ut[e2]
                A("dve", lambda pg=pg, g_=g_, f=f, sl=sl: V.tensor_scalar(out=g_[:], in0=pg, scalar1=bgu[sl][:, f:f + 1], scalar2=7.0,
                                                                          op0=ALU.add, op1=ALU.min),
                  reads=[pgk, ("bgu", sl)], writes=[("gt", e2)])
                A("act", lambda g_=g_, s__=s__: ACT.activation(out=s__[:], in_=g_[:], func=AF.Sigmoid, scale=1.702),
                  reads=[("gt", e2)], writes=[("sg", e2)])
                A("dve", lambda pu=pu, u_=u_, f=f, sl=sl: V.tensor_scalar(out=u_[:], in0=pu, scalar1=bgu[sl][:, 8 + f:9 + f], scalar2=7.0,
                                                                          op0=ALU.add, op1=ALU.min),
                  reads=[puk, ("bgu", sl)], writes=[("ut", e2)])
                A("dve", lambda u_=u_: V.tensor_scalar(out=u_[:], in0=u_[:], scalar1=-7.0, scalar2=1.0, op0=ALU.max, op1=ALU.add),
                  reads=[("ut", e2)], writes=[("ut", e2)])
                A("dve", lambda g_=g_, s__=s__: V.tensor_tensor(out=g_[:], in0=g_[:], in1=s__[:], op=ALU.mult),
                  reads=[("gt", e2), ("sg", e2)], writes=[("gt", e2)])
                A("dve", lambda g_=g_, u_=u_, f=f, aT3=aT3: V.tensor_tensor(out=aT3[:, f, :], in0=g_[:], in1=u_[:], op=ALU.mult),
                  reads=[("gt", e2), ("ut", e2)], writes=[("actT", sl, f)])
            if b % 8 == 7:
                sc.flush()
        down_proj(NBLK - 1)
        sc.barrier()

    with ExitStack() as pf:
        bd = sb(pf, "bd", [NE, D], F32)
        sc.dma("sp", lambda: SP.dma_start(out=bd[:], in_=bd_d[:, :]), writes=["bd"])
        gfin = sb(pf, "gfin", [128, D], F32)
        sc.dma("sp", lambda: SP.dma_start(out=gfin[:], in_=gfin_d[:, :].partition_broadcast(128)), writes=["gfin"])
        x1f = [sb(pf, f"x1f{i}", [128, D], F32) for i in range(2)]
        yk_ = [[sb(pf, f"yk{j}_{i}", [128, D], F32) for i in range(4)] for j in range(2)]
        acc_ = [sb(pf, f"acc{j}", [128, D], F32) for j in range(2)]
        gwT_ = [sb(pf, f"gwT{j}", [NE, 128], F32) for j in range(2)]
        junkF = sb(pf, "junkF", [128, D], BF16)
        ssqF = sb(pf, "ssqF", [128, 1], F32)
        ot = [sb(pf, f"ot{i}", [128, D], F32) for i in range(2)]
        def f_loads(i):
            s2 = i % 2
            yk = yk_[s2]
            sc.dma("sp", lambda i=i, s2=s2: SP.dma_start(out=x1f[s2][:], in_=x1_d[i * 128:(i + 1) * 128, :]),
                   reads=[("x1_d", i)], writes=[("x1f", s2)])
            for k in range(4):
                sc.dma("pool", lambda i=i, k=k, yk=yk: G.indirect_dma_start(
                    out=yk[k][:], out_offset=None, in_=ys_d[:, :],
                    in_offset=bass.IndirectOffsetOnAxis(ap=dsel_i[:, k * NQB + i:k * NQB + i + 1], axis=0)),
                    reads=["ys_d", "dsel_i"], writes=[("yk", s2, k)])

        for i in range(NQB):
            s2 = i % 2
            yk, acc, gwT = yk_[s2], acc_[s2], gwT_[s2]
            ACCK, GWTK = ("acc", s2), ("gwT", s2)
            if i == 0:
                f_loads(0)
            if i + 1 < NQB:
                f_loads(i + 1)
            pt_, ptk_ = bank(s2)
            A("pe", lambda pt_=pt_, i=i: T.transpose(out=pt_[0:NE, 0:128], in_=gw3[:, i, :], identity=ident_f[:]),
              reads=["gw", "ident_f"], writes=[ptk_])
            A("act", lambda pt_=pt_, gwT=gwT: ACT.copy(out=gwT[:], in_=pt_[0:NE, 0:128]), reads=[ptk_], writes=[GWTK])
            pbs = []
            for n in range(2):
                pb, pbk = bank(2 + 2 * s2 + n)
                A("pe", lambda pb=pb, n=n, gwT=gwT: T.matmul(pb, lhsT=gwT[:], rhs=bd[:, n * 512:(n + 1) * 512], start=True, stop=True),
                  reads=[GWTK, "bd"], writes=[pbk])
                pbs.append((pb, pbk))
            A("dve", lambda i=i, acc=acc, yk=yk: V.tensor_scalar(out=acc[:], in0=yk[0][:], scalar1=gk[:, i:i + 1], scalar2=None, op0=ALU.mult),
              reads=[("yk", s2, 0)] + [("gk", k) for k in range(4)], writes=[ACCK])
            for k in range(1, 4):
                A("dve", lambda i=i, k=k, acc=acc, yk=yk: V.scalar_tensor_tensor(out=acc[:], in0=yk[k][:], scalar=gk[:, k * NQB + i:k * NQB + i + 1],
                                                                 in1=acc[:], op0=ALU.mult, op1=ALU.add),
                  reads=[("yk", s2, k), ACCK] + [("gk", kk) for kk in range(4)], writes=[ACCK])
            for n in range(2):
                pb, pbk = pbs[n]
                A("dve", lambda pb=pb, n=n, acc=acc: V.tensor_tensor(out=acc[:, n * 512:(n + 1) * 512], in0=pb, in1=acc[:, n * 512:(n + 1) * 512], op=ALU.add),
                  reads=[pbk, ACCK], writes=[ACCK])
            A("pool", lambda acc=acc: G.tensor_tensor(out=acc[:], in0=acc[:], in1=gate_f, op=ALU.mult), reads=[ACCK] + MODK, writes=[ACCK])
            A("pool", lambda s2=s2, acc=acc: G.tensor_tensor(out=acc[:], in0=acc[:], in1=x1f[s2][:], op=ALU.add), reads=[ACCK, ("x1f", s2)], writes=[ACCK])
            A("act", lambda acc=acc: ACT.activation(out=junkF[:], in_=acc[:], func=AF.Square, accum_out=ssqF[:]), reads=[ACCK], writes=["junkF", "ssqF"])
            A("dve", lambda: V.tensor_scalar(out=ssqF[:], in0=ssqF[:], scalar1=1.0 / D, scalar2=EPS, op0=ALU.mult, op1=ALU.add),
              reads=["ssqF"], writes=["ssqF"])
            A("act", lambda: ACT.activation(out=ssqF[:], in_=ssqF[:], func=AF.Ln), reads=["ssqF"], writes=["ssqF"])
            A("act", lambda: ACT.activation(out=ssqF[:], in_=ssqF[:], func=AF.Exp, scale=-0.5), reads=["ssqF"], writes=["ssqF"])
            o_ = ot[s2]
            A("dve", lambda o_=o_, acc=acc: V.scalar_tensor_tensor(out=o_[:], in0=acc[:], scalar=ssqF[:, 0:1], in1=gfin[:], op0=ALU.mult, op1=ALU.mult),
              reads=[ACCK, "ssqF", "gfin"], writes=[("ot", s2)])
            sc.dma("sp", lambda o_=o_, i=i: SP.dma_start(out=out_d[i * 128:(i + 1) * 128, :], in_=o_[:]),
                   reads=[("ot", s2)], writes=[("out_d", i)])
        sc.barrier()
    sc.finish([("out_d", i) for i in range(NQB)])
    rt_.close()
    es.close()
    return nc


def _prep_inputs(inputs):
    f = lambda a: np.ascontiguousarray(np.asarray(a, dtype=np.float32))
    x = f(inputs["x"])
    c = f(inputs["c"])
    w_in = f(inputs["w_in"])[0]
    wa, wb = _layout_w_in(w_in)
    cosT, sinT = _rope_tables()
    dr, co = _bias_index()
    rpb = f(inputs["rpb"])[0]
    biasu = np.ascontiguousarray(rpb[:, dr, co].reshape(8, 128, 7 * 128))
    bgu = f(inputs["b_gate_up"])[0]
    bgu_l = np.ascontiguousarray(bgu.reshape(NE, 16, 128).transpose(0, 2, 1).reshape(NE * 128, 16))
    rowid = (np.arange(8)[None, :] * 128 + np.arange(128)[:, None]).astype(np.float32)
    blkth = np.broadcast_to((np.arange(NBLK, dtype=np.float32) * MB)[None, :, None], (128, NBLK, NE)).reshape(128, NBLK * NE)
    shared = {
        "w_ada": f(inputs["w_ada"])[0], "b_ada": f(inputs["b_ada"]).reshape(1, 6 * D),
        "w_a": wa, "w_b": wb, "sink": f(inputs["sink"]).reshape(1, 8), "biasu": biasu,
        "g_out_a": f(inputs["g_out_a"]).reshape(1, 512), "g_out_b": f(inputs["g_out_b"]).reshape(1, 512),
        "w_out": f(inputs["w_out"])[0], "w_router": f(inputs["w_router"])[0], "b_router": f(inputs["b_router"]).reshape(1, NE),
        "w_gate_up": f(inputs["w_gate_up"])[0].reshape(NE * D, 2 * DFF), "b_gate_up": bgu_l,
        "w_down": f(inputs["w_down"])[0].reshape(NE * DFF, D), "b_down": f(inputs["b_down"])[0],
        "g_final": f(inputs["g_final"]).reshape(1, D), "cosT": cosT, "sinT": sinT,
        "maska": np.ascontiguousarray(_mask_a().reshape(128, 384)),
        "maskb": np.ascontiguousarray(_MASKB.reshape(_NVAR, 128, 7 * 128)),
        "ident": np.eye(128, dtype=np.float32), "tri": np.triu(np.ones((128, 128), np.float32), 1),
        "rowid": rowid, "blkth": np.ascontiguousarray(blkth),
    }
    in_maps = []
    for b in range(8):
        m = dict(shared)
        m["x"] = x[b]
        m["cT"] = np.ascontiguousarray(c[b].reshape(8, 128).T)
        in_maps.append(m)
    return in_maps


def kernel(**inputs):
    in_maps = _prep_inputs(inputs)
    nc = build_program()
    res = run_bass_kernel_spmd(nc, in_maps, core_ids=list(range(8)))
    return np.stack([np.asarray(r["out"], dtype=np.float32) for r in res.results], axis=0)
```

```python
import bisect
from contextlib import ExitStack

import numpy as np
import concourse.bass as bass
import concourse.mybir as mybir
from concourse.bass_utils import run_bass_kernel_spmd

F32 = mybir.dt.float32
BF16 = mybir.dt.bfloat16
I32 = mybir.dt.int32
AF = mybir.ActivationFunctionType
ALU = mybir.AluOpType
AX = mybir.AxisListType

S = 4096
D = 1024
NQB = 32
NE = 32
DFF = 1024
EPS = 1e-5
MASKV = -240000.0
MB = 512
NBLK = 64
NSLOT = NBLK * MB
THETA = 500000.0


class _Op:
    __slots__ = ("eng", "fn", "deps", "dma", "need_inc", "target")


class Sched:
    def __init__(self, nc, es, nchan=10):
        self.nc = nc
        self.engs = dict(pe=nc.tensor, act=nc.scalar, dve=nc.vector, pool=nc.gpsimd, sp=nc.sync)
        self.sem = {e: es.enter_context(nc.semaphore("sem_" + e)) for e in ("pe", "act", "dve", "pool")}
        nch = {"sp": 12, "pool": 28}
        self.chan = {q: [es.enter_context(nc.semaphore(f"ch_{q}{i}")) for i in range(nch[q])] for q in ("sp", "pool")}
        self.chan_cnt = {q: [0] * nch[q] for q in ("sp", "pool")}
        self.chan_next = {q: 0 for q in ("sp", "pool")}
        self.ops = []
        self.flushed = 0
        self.last_writer = {}
        self.readers = {}
        self.cnt = {e: 0 for e in self.sem}
        self.incs = {e: ([], []) for e in self.sem}
        self.waited = {}

    def add(self, eng, fn, reads=(), writes=(), dma=False):
        op = _Op()
        op.eng, op.fn, op.dma, op.need_inc, op.target = eng, fn, dma, False, None
        deps = set()
        for r in reads:
            w = self.last_writer.get(r)
            if w is not None:
                deps.add(w)
        for w_ in writes:
            w = self.last_writer.get(w_)
            if w is not None:
                deps.add(w)
            for r in self.readers.get(w_, ()):
                deps.add(r)
        idx = len(self.ops)
        deps.discard(idx)
        op.deps = deps
        for r in reads:
            self.readers.setdefault(r, []).append(idx)
        for w_ in writes:
            self.last_writer[w_] = idx
            self.readers[w_] = []
        self.ops.append(op)
        return idx

    def dma(self, q, fn, reads=(), writes=()):
        return self.add(q, fn, reads, writes, dma=True)

    def _wait(self, ceng, sem, val):
        key = (ceng, id(sem))
        if self.waited.get(key, 0) >= val:
            return
        self.waited[key] = val
        self.engs[ceng].wait_ge(sem, val)

    def flush(self, final=False):
        ops = self.ops
        lo, hi = self.flushed, len(ops)
        last_of = {}
        for i in range(lo, hi):
            op = ops[i]
            if not op.dma:
                last_of[op.eng] = i
            for d in op.deps:
                dop = ops[d]
                if d >= lo and not dop.dma:
                    if dop.eng == "pe" and op.eng == "pe" and not op.dma:
                        continue
                    dop.need_inc = True
        for e, i in last_of.items():
            ops[i].need_inc = True
        for i in range(lo, hi):
            op = ops[i]
            ceng = op.eng
            if op.dma:
                q = ceng
                c = self.chan_next[q]
                self.chan_next[q] = (c + 1) % len(self.chan[q])
                csem = self.chan[q][c]
                if self.chan_cnt[q][c] > 0:
                    self._wait(q, csem, 16 * self.chan_cnt[q][c])
            for d in sorted(op.deps):
                dop = ops[d]
                if dop.dma:
                    self._wait(ceng, dop.target[0], dop.target[1])
                else:
                    if dop.eng == "pe" and ceng == "pe" and not op.dma:
                        continue
                    il, cl = self.incs[dop.eng]
                    if dop.target is not None:
                        tv = dop.target[1]
                    else:
                        j = bisect.bisect_left(il, d)
                        if j < len(il):
                            tv = cl[j]
                        else:
                            raise RuntimeError("no covering inc")
                    self._wait(ceng, self.sem[dop.eng], tv)
            ins = op.fn()
            if op.dma:
                self.chan_cnt[q][c] += 1
                ins.then_inc(csem, 16)
                op.target = (csem, 16 * self.chan_cnt[q][c])
            elif op.need_inc:
                self.cnt[ceng] += 1
                ins.then_inc(self.sem[ceng], 1)
                op.target = (self.sem[ceng], self.cnt[ceng])
                self.incs[ceng][0].append(i)
                self.incs[ceng][1].append(self.cnt[ceng])
            op.fn = None
        self.flushed = hi

    def barrier(self):
        self.flush()
        for ceng in ("pe", "act", "dve", "pool", "sp"):
            for e, sem in self.sem.items():
                if self.cnt[e] > 0:
                    self._wait(ceng, sem, self.cnt[e])
            for q in ("sp", "pool"):
                for c, csem in enumerate(self.chan[q]):
                    if self.chan_cnt[q][c] > 0:
                        self._wait(ceng, csem, 16 * self.chan_cnt[q][c])

    def finish(self, out_keys):
        self.flush()
        for k in out_keys:
            w = self.last_writer.get(k)
            if w is not None:
                t = self.ops[w].target
                self._wait("sp", t[0], t[1])
        for q in ("sp", "pool"):
            for c, csem in enumerate(self.chan[q]):
                if self.chan_cnt[q][c] > 0:
                    self._wait("sp", csem, 16 * self.chan_cnt[q][c])


def _rope_tables():
    inv_freq = (np.float32(THETA) ** (-np.arange(0, 16, 2, dtype=np.float32) / np.float32(16))).astype(np.float32)
    pos = np.arange(S, dtype=np.float32)
    ang = (pos[:, None] * inv_freq[None, :]).astype(np.float32)
    cos = np.cos(ang).astype(np.float32)
    sin = np.sin(ang).astype(np.float32)
    cosT = np.ones((128, S), np.float32)
    sinT = np.zeros((128, S), np.float32)
    for hh in range(2):
        b = hh * 64
        for d in range(8):
            cosT[b + d] = cos[:, d]
            cosT[b + 8 + d] = cos[:, d]
            sinT[b + d] = -sin[:, d]
            sinT[b + 8 + d] = sin[:, d]
    return cosT, sinT


def _mask_a():
    k = np.arange(128)[:, None, None]
    m = np.arange(3)[None, :, None]
    q = np.arange(128)[None, None, :]
    rel = (m - 1) * 128 + k - q
    return np.where(np.abs(rel) <= 128, 0.0, MASKV).astype(np.float32)


def _b_geometry():
    rows = 64
    rs = np.clip(np.arange(rows) - 4, 0, rows - 8)
    cs = np.clip(np.arange(64) - 8, 0, 64 - 16)

    def valid_row(kr, r):
        return (0 <= kr < rows) and (rs[r] <= kr < rs[r] + 8)

    colmask = np.zeros((64, 64), bool)
    for qc in range(64):
        colmask[qc, cs[qc]:cs[qc] + 16] = True
    masks = {}
    kbs = {}
    for i in range(NQB):
        mk = np.full((128, 7, 128), MASKV, np.float32)
        used = []
        for m in range(7):
            kb = i + m - 3
            if kb < 0 or kb > 31:
                continue
            anyv = False
            for a in range(2):
                for b in range(2):
                    if valid_row(2 * kb + a, 2 * i + b):
                        anyv = True
                        blk = np.where(colmask.T, 0.0, MASKV)
                        mk[a * 64:(a + 1) * 64, m, b * 64:(b + 1) * 64] = blk
            if anyv:
                used.append(m)
        masks[i] = mk
        kbs[i] = used
    variants = []
    var_of = {}
    for i in range(NQB):
        for vi, v in enumerate(variants):
            if np.array_equal(v, masks[i]):
                var_of[i] = vi
                break
        else:
            var_of[i] = len(variants)
            variants.append(masks[i])
    return np.stack(variants), var_of, kbs


def _bias_index():
    a = np.arange(2)[:, None, None, None, None]
    kc = np.arange(64)[None, :, None, None, None]
    m = np.arange(7)[None, None, :, None, None]
    b = np.arange(2)[None, None, None, :, None]
    qc = np.arange(64)[None, None, None, None, :]
    dr = np.clip(2 * (m - 3) + a - b, -7, 7) + 7
    co = np.clip(kc - qc, -15, 15) + 15
    dr = np.broadcast_to(dr, (2, 64, 7, 2, 64)).reshape(128, 7, 128)
    co = np.broadcast_to(co, (2, 64, 7, 2, 64)).reshape(128, 7, 128)
    return dr, co


_MASKB, _VAR_OF, _KBS = _b_geometry()
_NVAR = _MASKB.shape[0]
_PERM = np.concatenate([np.arange(8, 16), np.arange(0, 8), np.arange(16, 64)])

NWA = 1536 + 128
NWB = 1536


def _layout_w_in(w):
    qa, ka, va = w[:, 0:512], w[:, 512:640], w[:, 640:768]
    qb, kb, vb = w[:, 768:1280], w[:, 1280:1792], w[:, 1792:2304]
    qap = qa.reshape(D, 8, 64)[:, :, _PERM].reshape(D, 512)
    k0, k1 = ka[:, 0:64], ka[:, 64:128]
    k0p, k1p = k0[:, _PERM], k1[:, _PERM]
    wa = np.concatenate([qa, qap, k0, k0, k1, k1, k0p, k0p, k1p, k1p, va], axis=1)
    wb = np.concatenate([qb, kb, vb], axis=1)
    return np.ascontiguousarray(wa), np.ascontiguousarray(wb)


def build_program(stop_after=None, dbg=False):
    nc = bass.Bass("TRN2", target_bir_lowering=False)
    es = ExitStack()

    def din(name, shape, dt=F32):
        return nc.dram_tensor(name, list(shape), dt, kind="ExternalInput").ap()

    x_d = din("x", [S, D])
    cT_d = din("cT", [128, 8])
    wada_d = din("w_ada", [D, 6 * D])
    bada_d = din("b_ada", [1, 6 * D])
    wa_d = din("w_a", [D, NWA])
    wb_d = din("w_b", [D, NWB])
    sink_d = din("sink", [1, 8])
    biasu_d = din("biasu", [8, 128, 7 * 128])
    goa_d = din("g_out_a", [1, 512])
    gob_d = din("g_out_b", [1, 512])
    wout_d = din("w_out", [D, D])
    wr_d = din("w_router", [D, NE])
    br_d = din("b_router", [1, NE])
    if stop_after is None:
        wgu_d = din("w_gate_up", [NE * D, 2 * DFF])
        bgu_d = din("b_gate_up", [NE * 128, 16])
        wd_d = din("w_down", [NE * DFF, D])
        bd_d = din("b_down", [NE, D])
    gfin_d = din("g_final", [1, D])
    cos_d = din("cosT", [128, S])
    sin_d = din("sinT", [128, S])
    maska_d = din("maska", [128, 3 * 128])
    maskb_d = din("maskb", [_NVAR, 128, 7 * 128])
    ident_d = din("ident", [128, 128])
    tri_d = din("tri", [128, 128])
    rowid_d = din("rowid", [128, 8])
    blkth_d = din("blkth", [128, NBLK * NE])
    out_d = nc.dram_tensor("out", [S, D], F32, kind="ExternalOutput").ap()

    def dscr(name, shape, dt):
        kind = "ExternalOutput" if (dbg and name not in ("xs_s", "ys_s")) else "Internal"
        return nc.dram_tensor(name, list(shape), dt, kind=kind).ap()

    mixa_d = dscr("mixa_s", [S, 512], BF16)
    mixb_d = dscr("mixb_s", [S, 512], BF16)
    x1_d = dscr("x1_s", [S, D], F32)
    h2_d = dscr("h2_s", [S, D], BF16)
    if stop_after not in ("A", "B"):
        xs_d = dscr("xs_s", [NSLOT, D], BF16)
        ys_d = dscr("ys_s", [NSLOT, D], F32)
    dbg_d = {}
    if dbg:
        dbg_d["qta"] = nc.dram_tensor("dbg_qta", [128, 6 * S], BF16, kind="ExternalOutput").ap()
        dbg_d["gw"] = nc.dram_tensor("dbg_gw", [128, NQB * NE], F32, kind="ExternalOutput").ap()
        dbg_d["dsel"] = nc.dram_tensor("dbg_dsel", [128, NQB * 4], I32, kind="ExternalOutput").ap()
        dbg_d["blke"] = nc.dram_tensor("dbg_blke", [128, NBLK], F32, kind="ExternalOutput").ap()
        dbg_d["mod"] = nc.dram_tensor("dbg_mod", [128, 6 * D], F32, kind="ExternalOutput").ap()

    sc = Sched(nc, es)
    dbg_keys = []

    DUMPS = dict(ptA=([128, 384], BF16), poA=([128, 1024], F32), vpa=([128, NQB * 2 * 65], BF16), esink=([128, 8], F32),
                 goa=([128, 512], F32), oaA=([128, 512], F32), denA=([128, 8], F32), ssqA=([128, 1], F32))
    dump_d = {k: nc.dram_tensor("dbg_" + k, v[0], v[1], kind="ExternalOutput").ap() for k, v in DUMPS.items()} if dbg else {}

    def dump(name, ap, shape, dt, reads):
        if not dbg:
            return
        d = dump_d[name]
        sc.dma("sp", lambda: nc.sync.dma_start(out=d, in_=ap), reads=reads, writes=["dbg_" + name])
        dbg_keys.append("dbg_" + name)
    A = sc.add
    T, V, G, ACT, SP = nc.tensor, nc.vector, nc.gpsimd, nc.scalar, nc.sync

    def sb(stack, name, shape, dt=F32):
        return stack.enter_context(nc.sbuf_tensor("s_" + name, list(shape), dt))

    PS = [es.enter_context(nc.psum_tensor(f"ps{i}", [128, 1024], F32)) for i in range(4)]

    def bank(i):
        return PS[i // 2][:, (i % 2) * 512:(i % 2 + 1) * 512], ("ps", i)

    ident_f = sb(es, "ident_f", [128, 128], F32)
    ident_b = sb(es, "ident_b", [128, 128], BF16)
    ones_f = sb(es, "ones_f", [128, 128], F32)
    mod = sb(es, "mod", [128, 6 * D], F32)
    epsb = sb(es, "epsb", [128, 1], F32)
    sc.dma("sp", lambda: SP.dma_start(out=ident_f[:], in_=ident_d[:, :]), writes=["ident_f"])
    sc.dma("pool", lambda: G.dma_start(out=ident_b[:], in_=ident_d[:, :]), writes=["ident_b"])
    A("dve", lambda: V.memset(ones_f[:], 1.0), writes=["ones_f"])
    A("dve", lambda: V.memset(epsb[:], EPS), writes=["epsb"])

    with ExitStack() as p0:
        cT = sb(p0, "cT", [128, 8], F32)
        cact = sb(p0, "cact", [128, 8], F32)
        csig = sb(p0, "csig", [128, 8], F32)
        crep = sb(p0, "crep", [128, 8 * 128], F32)
        bada = sb(p0, "bada", [1, 6 * D], F32)
        wsl = [sb(p0, f"wsl{i}", [128, 8 * 512], F32) for i in range(2)]
        sc.dma("sp", lambda: SP.dma_start(out=cT[:], in_=cT_d[:, :]), writes=["cT"])
        sc.dma("sp", lambda: SP.dma_start(out=bada[:], in_=bada_d[:, :]), writes=["bada"])
        A("act", lambda: ACT.activation(out=csig[:], in_=cT[:], func=AF.Sigmoid), reads=["cT"], writes=["csig"])
        A("dve", lambda: V.tensor_tensor(out=cact[:], in0=cT[:], in1=csig[:], op=ALU.mult), reads=["cT", "csig"], writes=["cact"])
        A("dve", lambda: V.tensor_copy(out=crep[:].rearrange("p (c m) -> p c m", m=128),
                                       in_=cact[:].unsqueeze(2).to_broadcast([128, 8, 128])),
          reads=["cact"], writes=["crep"])
        for n in range(12):
            slot = n % 2
            w_t = wsl[slot]
            sc.dma("sp", lambda w_t=w_t, n=n: SP.dma_start(
                out=w_t[:].rearrange("p (c n) -> p c n", n=512),
                in_=wada_d[:, n * 512:(n + 1) * 512].rearrange("(c p) n -> p c n", p=128)),
                writes=[("wsl", slot)])
            pb, pk = bank(n % 2)
            for c in range(8):
                A("pe", lambda pb=pb, w_t=w_t, c=c: T.matmul(pb, lhsT=crep[:, c * 128:(c + 1) * 128],
                                                             rhs=w_t[:, c * 512:(c + 1) * 512], start=(c == 0), stop=False),
                  reads=["crep", ("wsl", slot)], writes=[pk])
            A("pe", lambda pb=pb, n=n: T.matmul(pb, lhsT=ones_f[0:1, :], rhs=bada[0:1, n * 512:(n + 1) * 512],
                                                start=False, stop=True),
              reads=["ones_f", "bada"], writes=[pk])
            if (n // 2) % 3 == 1:
                A("dve", lambda pb=pb, n=n: V.tensor_scalar(out=mod[:, n * 512:(n + 1) * 512], in0=pb, scalar1=1.0,
                                                            scalar2=None, op0=ALU.add), reads=[pk], writes=[("mod", n)])
            else:
                A("act", lambda pb=pb, n=n: ACT.copy(out=mod[:, n * 512:(n + 1) * 512], in_=pb), reads=[pk], writes=[("mod", n)])
        sc.barrier()
    MODK = [("mod", n) for n in range(12)]
    shift_m, scale1_m, gate_m = mod[:, 0:D], mod[:, D:2 * D], mod[:, 2 * D:3 * D]
    shift_f, scale1_f, gate_f = mod[:, 3 * D:4 * D], mod[:, 4 * D:5 * D], mod[:, 5 * D:6 * D]
    if dbg:
        sc.dma("sp", lambda: SP.dma_start(out=dbg_d["mod"][:, :], in_=mod[:]), reads=MODK, writes=["dbg_mod"])

    def rmsnorm_mod(stack_tiles, src, src_keys, scale1, shift, out_bf=None, out_f32=None, out_keys=(), tag=""):
        junk, ssq, rstd, tmp = stack_tiles
        A("act", lambda: ACT.activation(out=junk[:], in_=src, func=AF.Square, accum_out=ssq[:]),
          reads=list(src_keys), writes=["junk" + tag, "ssq" + tag])
        A("dve", lambda: V.tensor_scalar(out=rstd[:], in0=ssq[:], scalar1=1.0 / D, scalar2=EPS, op0=ALU.mult, op1=ALU.add),
          reads=["ssq" + tag], writes=["rstd" + tag])
        A("act", lambda: ACT.activation(out=rstd[:], in_=rstd[:], func=AF.Ln), reads=["rstd" + tag], writes=["rstd" + tag])
        A("act", lambda: ACT.activation(out=rstd[:], in_=rstd[:], func=AF.Exp, scale=-0.5), reads=["rstd" + tag], writes=["rstd" + tag])
        A("dve", lambda: V.scalar_tensor_tensor(out=tmp[:], in0=src, scalar=rstd[:, 0:1], in1=scale1, op0=ALU.mult, op1=ALU.mult),
          reads=list(src_keys) + ["rstd" + tag] + MODK, writes=["tmp" + tag])
        if out_f32 is not None:
            A("pool", lambda: G.tensor_tensor(out=out_f32, in0=tmp[:], in1=shift, op=ALU.add),
              reads=["tmp" + tag] + MODK, writes=list(out_keys))
            if out_bf is not None:
                A("act", lambda: ACT.copy(out=out_bf, in_=out_f32), reads=list(out_keys), writes=[k + ("bf",) for k in out_keys])
        else:
            A("pool", lambda: G.tensor_tensor(out=out_bf, in0=tmp[:], in1=shift, op=ALU.add),
              reads=["tmp" + tag] + MODK, writes=list(out_keys))

    def projection_pass(ps_, wmat_d, ncols, ngroups, emit_group, emit_v, vcol0, nvcols, tag):
        wsb = sb(ps_, "wsb" + tag, [128, 8 * ncols], BF16)
        w3 = wsb[:].rearrange("p (c n) -> p c n", n=ncols)
        for c in range(8):
            sc.dma("pool", lambda c=c: G.dma_start(out=w3[:, c, :], in_=wmat_d[c * 128:(c + 1) * 128, :]),
                   writes=[("wsb" + tag, c)])
        WK = [("wsb" + tag, c) for c in range(8)]
        xbl = [sb(ps_, f"xbl{tag}{i}", [128, D], F32) for i in range(2)]
        hbf = [sb(ps_, f"hbf{tag}{i}", [128, D], BF16) for i in range(2)]
        hT = [sb(ps_, f"hT{tag}{i}", [128, 8 * 512], BF16) for i in range(2)]
        tiles = [(sb(ps_, f"junk{tag}{j}", [128, D], BF16), sb(ps_, f"ssq{tag}{j}", [128, 1], F32),
                  sb(ps_, f"rstd{tag}{j}", [128, 1], F32), sb(ps_, f"tmp{tag}{j}", [128, D], F32)) for j in range(2)]
        for tc in range(8):
            hslot = tc % 2
            hT3 = hT[hslot][:].rearrange("p (c t) -> p c t", t=512)
            for sub in range(4):
                i = tc * 4 + sub
                xs_ = i % 2
                sc.dma("sp", lambda i=i, xs_=xs_: SP.dma_start(out=xbl[xs_][:], in_=x_d[i * 128:(i + 1) * 128, :]),
                       writes=[("xbl" + tag, xs_)])
                rmsnorm_mod(tiles[xs_], xbl[xs_][:], [("xbl" + tag, xs_)], scale1_m, shift_m, out_bf=hbf[xs_][:],
                            out_keys=[("hbf" + tag, xs_)], tag=tag + str(xs_))
                pbT = PS[0][:, (i % 2) * 512:(i % 2 + 1) * 512].bitcast(BF16)
                pkT = ("ps", i % 2)
                for c in range(8):
                    A("pe", lambda c=c, pbT=pbT, xs_=xs_: T.transpose(out=pbT[:, c * 128:(c + 1) * 128],
                                                                      in_=hbf[xs_][:, c * 128:(c + 1) * 128], identity=ident_b[:]),
                      reads=[("hbf" + tag, xs_), "ident_b"], writes=[pkT])
                A("act", lambda pbT=pbT, hT3=hT3, sub=sub: ACT.copy(out=hT3[:, :, sub * 128:(sub + 1) * 128],
                                                                    in_=pbT.rearrange("p (c t) -> p c t", t=128)),
                  reads=[pkT], writes=[("hT" + tag, hslot, sub)])
                pv, pvk = bank(2 + (i % 2))
                for c in range(8):
                    A("pe", lambda c=c, pv=pv, hT3=hT3, sub=sub: T.matmul(
                        pv[:, 0:nvcols], lhsT=hT3[:, c, sub * 128:(sub + 1) * 128], rhs=w3[:, c, vcol0:vcol0 + nvcols],
                        start=(c == 0), stop=(c == 7)),
                      reads=[("hT" + tag, hslot, sub)] + WK, writes=[pvk])
                emit_v(i, pv, pvk)
            HK = [("hT" + tag, hslot, s_) for s_ in range(4)]
            emit_group(tc, hT3, HK, w3, WK)
        return

    with ExitStack() as pa:
        qTa = sb(pa, "qTa", [128, 6 * S], BF16)
        qTa3 = qTa[:].rearrange("p (g t) -> p g t", t=S)
        vpa = sb(pa, "vpa", [128, NQB * 2 * 65], BF16)
        vpa4 = vpa[:].rearrange("p (i g d) -> p i g d", g=2, d=65)
        A("pool", lambda: G.memset(vpa[:], 1.0), writes=["vpa_init"])
        with ExitStack() as pa1:
            cosT = sb(pa1, "cosT", [128, S], F32)
            sinT = sb(pa1, "sinT", [128, S], F32)
            sc.dma("sp", lambda: SP.dma_start(out=cosT[:], in_=cos_d[:, :]), writes=["cosT"])
            sc.dma("sp", lambda: SP.dma_start(out=sinT[:], in_=sin_d[:, :]), writes=["sinT"])
            rt = [sb(pa1, f"rt{i}", [128, 512], F32) for i in range(4)]

            def emit_v_a(i, pv, pvk):
                A("act", lambda: ACT.copy(out=vpa4[:, i, :, 0:64], in_=pv[:, 0:128].rearrange("p (g d) -> p g d", d=64)),
                  reads=[pvk, "vpa_init"], writes=[("vpa", i)])

            def emit_group_a(tc, hT3, HK, w3, WK):
                for g in range(6):
                    c0 = g * 128 if g < 4 else 1024 + (g - 4) * 256
                    c1 = 512 + g * 128 if g < 4 else 1024 + 512 + (g - 4) * 256
                    if g >= 4:
                        c0 = 1024 + (g - 4) * 128
                        c1 = 1024 + 256 + (g - 4) * 128
                    pq, pqk = bank(4 + (g % 2) * 2)
                    pp, ppk = bank(5 + (g % 2) * 2)
                    for (pb, pk, col) in ((pq, pqk, c0), (pp, ppk, c1)):
                        for c in range(8):
                            A("pe", lambda pb=pb, c=c, col=col: T.matmul(pb, lhsT=w3[:, c, col:col + 128], rhs=hT3[:, c, :],
                                                                         start=(c == 0), stop=(c == 7)),
                              reads=HK + WK, writes=[pk])
                    r0, r1 = rt[(g % 2) * 2], rt[(g % 2) * 2 + 1]
                    k0, k1 = ("rt", (g % 2) * 2), ("rt", (g % 2) * 2 + 1)
                    tsl = slice(tc * 512, (tc + 1) * 512)
                    A("dve", lambda pq=pq, r0=r0, tsl=tsl: V.tensor_tensor(out=r0[:], in0=pq, in1=cosT[:, tsl], op=ALU.mult),
                      reads=[pqk, "cosT"], writes=[k0])
                    A("dve", lambda pp=pp, r1=r1, tsl=tsl: V.tensor_tensor(out=r1[:], in0=pp, in1=sinT[:, tsl], op=ALU.mult),
                      reads=[ppk, "sinT"], writes=[k1])
                    A("pool", lambda r0=r0, r1=r1, g=g, tsl=tsl: G.tensor_tensor(out=qTa3[:, g, tsl], in0=r0[:], in1=r1[:], op=ALU.add),
                      reads=[k0, k1], writes=[("qTa", g, tc)])

            projection_pass(pa1, wa_d, NWA, 12, emit_group_a, emit_v_a, 1536, 128, "A")
            sc.barrier()
        if dbg:
            sc.dma("sp", lambda: SP.dma_start(out=dbg_d["qta"][:, :], in_=qTa[:]),
                   reads=[("qTa", g, tc) for g in range(6) for tc in range(8)], writes=["dbg_qta"])

        with ExitStack() as pa2:
            if stop_after not in ("A", "B"):
                zt = sb(pa2, "zt", [128, 4 * D], BF16)
                A("pool", lambda: G.memset(zt[:], 0.0), writes=["zt"])
                for b in range(NBLK):
                    sc.dma("sp", lambda b=b: SP.dma_start(out=xs_d[b * MB:(b + 1) * MB, :].rearrange("(s p) d -> p s d", p=128),
                                                        in_=zt[:].rearrange("p (s d) -> p s d", d=D)), reads=["zt"], writes=["xs_d"])
            maska = sb(pa2, "maska", [128, 384], BF16)
            sc.dma("pool", lambda: G.dma_start(out=maska[:], in_=maska_d[:, :]), writes=["maska"])
            esink = sb(pa2, "esink", [128, 8], F32)
            sc.dma("sp", lambda: SP.dma_start(out=esink[:], in_=sink_d[:, :].partition_broadcast(128)), writes=["esink0"])
            A("act", lambda: ACT.activation(out=esink[:], in_=esink[:], func=AF.Exp), reads=["esink0"], writes=["esink"])
            goa = sb(pa2, "goa", [128, 512], F32)
            sc.dma("sp", lambda: SP.dma_start(out=goa[:], in_=goa_d[:, :].partition_broadcast(128)), writes=["goa"])
            pt = [sb(pa2, f"pta{i}", [128, 384], BF16) for i in range(3)]
            den = sb(pa2, "dena", [128, 8], F32)
            oa = sb(pa2, "oa", [128, 512], F32)
            junk2 = sb(pa2, "junk2a", [128, 512], BF16)
            ssq2 = sb(pa2, "ssq2a", [128, 1], F32)
            mixa = [sb(pa2, f"mixa{i}", [128, 512], BF16) for i in range(2)]
            for i in range(NQB):
                tcq = i // 4
                ms = [m for m in range(3) if 0 <= i + m - 1 < NQB]
                po = PS[3 - (i % 2)]
                pok = ("ps", 6 - 2 * (i % 2))
                po4 = po[:].rearrange("p (b x) -> p b x", b=2)[:, :, 0:260].rearrange("p b (h d) -> p b h d", d=65)
                def qk_a(h):
                    g, off = h // 2, (h % 2) * 64
                    kg = 4 + h // 4
                    pst, pstk = bank(h % 3)
                    for m in ms:
                        kb = i + m - 1
                        A("pe", lambda pst=pst, m=m, kb=kb, g=g, off=off, kg=kg, i=i: T.matmul(
                            pst[:, m * 128:(m + 1) * 128], lhsT=qTa3[off:off + 64, kg, kb * 128:(kb + 1) * 128],
                            rhs=qTa3[off:off + 64, g, i * 128:(i + 1) * 128], start=True, stop=False),
                          reads=[("qTa", kg, kb // 4), ("qTa", g, tcq)], writes=[pstk])
                        A("pe", lambda pst=pst, m=m: T.matmul(pst[:, m * 128:(m + 1) * 128], lhsT=ident_b[:],
                                                              rhs=maska[:, m * 128:(m + 1) * 128], start=False, stop=True),
                          reads=["ident_b", "maska"], writes=[pstk])

                qk_a(0)
                for h in range(8):
                    if h + 1 < 8:
                        qk_a(h + 1)
                    pst, pstk = bank(h % 3)
                    ptt = pt[h % 3]
                    ptk = ("pta", h % 3)
                    lo, hi = ms[0] * 128, (ms[-1] + 1) * 128
                    A("act", lambda pst=pst, ptt=ptt, lo=lo, hi=hi: ACT.activation(out=ptt[:, lo:hi], in_=pst[:, lo:hi],
                                                                                   func=AF.Exp, scale=0.125),
                      reads=[pstk], writes=[ptk])
                    for m in ms:
                        kb = i + m - 1
                        A("pe", lambda ptt=ptt, m=m, kb=kb, h=h, ms=ms, po=po: T.matmul(
                            po[:, (h // 4) * 512 + (h % 4) * 65:(h // 4) * 512 + (h % 4) * 65 + 65],
                            lhsT=ptt[:, m * 128:(m + 1) * 128], rhs=vpa4[:, kb, h // 4, :],
                            start=(m == ms[0]), stop=(m == ms[-1])),
                          reads=[ptk, ("vpa", kb)], writes=[pok])
                if i == 4:
                    dump("ptA", pt[7 % 3][:], [128, 384], BF16, [("pta", 7 % 3)])
                    if dbg:
                        podbg = sb(pa2, "podbg", [128, 1024], F32)
                        A("act", lambda: ACT.copy(out=podbg[:], in_=po[:]), reads=[pok], writes=["podbg"])
                        dump("poA", podbg[:], [128, 1024], F32, ["podbg"])
                    dump("vpa", vpa[:], [128, NQB * 2 * 65], BF16, [("vpa", kk) for kk in range(NQB)])
                    dump("esink", esink[:], [128, 8], F32, ["esink"])
                    dump("goa", goa[:], [128, 512], F32, ["goa"])
                A("dve", lambda po4=po4: V.tensor_tensor(out=den[:].rearrange("p (b h) -> p b h", b=2), in0=po4[:, :, :, 64],
                                                         in1=esink[:].rearrange("p (b h) -> p b h", b=2), op=ALU.add),
                  reads=[pok, "esink"], writes=["dena"])
                A("dve", lambda: V.reciprocal(out=den[:], in_=den[:]), reads=["dena"], writes=["dena"])
                A("dve", lambda po4=po4: V.tensor_tensor(
                    out=oa[:].rearrange("p (b h d) -> p b h d", b=2, d=64), in0=po4[:, :, :, 0:64],
                    in1=den[:].rearrange("p (b h) -> p b h", b=2).unsqueeze(3).to_broadcast([128, 2, 4, 64]), op=ALU.mult),
                  reads=[pok, "dena"], writes=["oa"])
                A("act", lambda: ACT.activation(out=junk2[:], in_=oa[:], func=AF.Square, accum_out=ssq2[:]),
                  reads=["oa"], writes=["junk2a", "ssq2a"])
                A("dve", lambda: V.tensor_scalar(out=ssq2[:], in0=ssq2[:], scalar1=1.0 / 512, scalar2=EPS, op0=ALU.mult, op1=ALU.add),
                  reads=["ssq2a"], writes=["ssq2a"])
                A("act", lambda: ACT.activation(out=ssq2[:], in_=ssq2[:], func=AF.Ln), reads=["ssq2a"], writes=["ssq2a"])
                A("act", lambda: ACT.activation(out=ssq2[:], in_=ssq2[:], func=AF.Exp, scale=-0.5), reads=["ssq2a"], writes=["ssq2a"])
                if i == 4:
                    dump("oaA", oa[:], [128, 512], F32, ["oa"])
                    dump("denA", den[:], [128, 8], F32, ["dena"])
                    dump("ssqA", ssq2[:], [128, 1], F32, ["ssq2a"])
                mx = mixa[i % 2]
                A("dve", lambda mx=mx: V.scalar_tensor_tensor(out=mx[:], in0=oa[:], scalar=ssq2[:, 0:1], in1=goa[:],
                                                              op0=ALU.mult, op1=ALU.mult),
                  reads=["oa", "ssq2a", "goa"], writes=[("mixa", i % 2)])
                sc.dma("sp", lambda mx=mx, i=i: SP.dma_start(out=mixa_d[i * 128:(i + 1) * 128, :], in_=mx[:]),
                       reads=[("mixa", i % 2)], writes=[("mixa_d", i)])
            sc.barrier()
    if stop_after == "A":
        sc.finish([("mixa_d", i) for i in range(NQB)] + ["dbg_qta", "dbg_mod"] + dbg_keys)
        es.close()
        return nc

    with ExitStack() as pb_:
        qTb = sb(pb_, "qTb", [128, 8 * S], BF16)
        qTb3 = qTb[:].rearrange("p (g t) -> p g t", t=S)
        vpb = sb(pb_, "vpb", [128, NQB * 8 * 65], BF16)
        vpb4 = vpb[:].rearrange("p (i g d) -> p i g d", g=8, d=65)
        A("pool", lambda: G.memset(vpb[:], 1.0), writes=["vpb_init"])
        with ExitStack() as pb1:
            def emit_v_b(i, pv, pvk):
                A("act", lambda: ACT.copy(out=vpb4[:, i, :, 0:64], in_=pv[:, 0:512].rearrange("p (g d) -> p g d", d=64)),
                  reads=[pvk, "vpb_init"], writes=[("vpb", i)])

            def emit_group_b(tc, hT3, HK, w3, WK):
                for g in range(8):
                    pq, pqk = bank(4 + g % 4)
                    for c in range(8):
                        A("pe", lambda pq=pq, c=c, g=g: T.matmul(pq, lhsT=w3[:, c, g * 128:(g + 1) * 128], rhs=hT3[:, c, :],
                                                                 start=(c == 0), stop=(c == 7)),
                          reads=HK + WK, writes=[pqk])
                    tsl = slice(tc * 512, (tc + 1) * 512)
                    if g % 2 == 0:
                        A("dve", lambda pq=pq, g=g, tsl=tsl: V.tensor_copy(out=qTb3[:, g, tsl], in_=pq), reads=[pqk], writes=[("qTb", g, tc)])
                    else:
                        A("act", lambda pq=pq, g=g, tsl=tsl: ACT.copy(out=qTb3[:, g, tsl], in_=pq), reads=[pqk], writes=[("qTb", g, tc)])

            projection_pass(pb1, wb_d, NWB, 8, emit_group_b, emit_v_b, 1024, 512, "B")
            sc.barrier()

        with ExitStack() as pb2:
            maskb = sb(pb2, "maskb", [128, _NVAR * 896], BF16)
            for v in range(_NVAR):
                sc.dma("pool", lambda v=v: G.dma_start(out=maskb[:, v * 896:(v + 1) * 896], in_=maskb_d[v, :, :]), writes=[("maskb", v)])
            biasu = sb(pb2, "biasu", [128, 8 * 896], F32)
            for h in range(8):
                sc.dma("sp", lambda h=h: SP.dma_start(out=biasu[:, h * 896:(h + 1) * 896], in_=biasu_d[h, :, :]), writes=[("biasu", h)])
            gob = sb(pb2, "gob", [128, 512], F32)
            sc.dma("sp", lambda: SP.dma_start(out=gob[:], in_=gob_d[:, :].partition_broadcast(128)), writes=["gob"])
            tt = [sb(pb2, f"ttb{i}", [128, 896], F32) for i in range(2)]
            pt = [sb(pb2, f"ptb{i}", [128, 896], BF16) for i in range(2)]
            den = sb(pb2, "denb", [128, 8], F32)
            ob = sb(pb2, "ob", [128, 512], F32)
            junk2 = sb(pb2, "junk2b", [128, 512], BF16)
            ssq2 = sb(pb2, "ssq2b", [128, 1], F32)
            mixb = [sb(pb2, f"mixb{i}", [128, 512], BF16) for i in range(2)]
            for i in range(NQB):
                tcq = i // 4
                ms = _KBS[i]
                var = _VAR_OF[i]
                po = PS[3 - (i % 2)]
                pok = ("ps", 6 - 2 * (i % 2))
                po4 = po[:].rearrange("p (b x) -> p b x", b=2)[:, :, 0:260].rearrange("p b (h d) -> p b h d", d=65)
                lo, hi = ms[0] * 128, (ms[-1] + 1) * 128
                def qk_b(h):
                    g, off, kg = h // 2, (h % 2) * 64, 4 + h // 2
                    sl = h % 2
                    pst = PS[sl]
                    pstk = [("ps", 2 * sl), ("ps", 2 * sl + 1)]
                    for m in ms:
                        kb = i + m - 3
                        A("pe", lambda pst=pst, m=m, kb=kb, g=g, off=off, kg=kg, i=i: T.matmul(
                            pst[:, m * 128:(m + 1) * 128], lhsT=qTb3[off:off + 64, kg, kb * 128:(kb + 1) * 128],
                            rhs=qTb3[off:off + 64, g, i * 128:(i + 1) * 128], start=True, stop=False),
                          reads=[("qTb", kg, kb // 4), ("qTb", g, tcq)], writes=pstk)
                        A("pe", lambda pst=pst, m=m, var=var: T.matmul(pst[:, m * 128:(m + 1) * 128], lhsT=ident_b[:],
                                                                       rhs=maskb[:, var * 896 + m * 128:var * 896 + (m + 1) * 128],
                                                                       start=False, stop=True),
                          reads=["ident_b", ("maskb", var)], writes=pstk)

                qk_b(0)
                for h in range(8):
                    if h + 1 < 8:
                        qk_b(h + 1)
                    sl = h % 2
                    pst = PS[sl]
                    pstk = [("ps", 2 * sl), ("ps", 2 * sl + 1)]
                    ttt, ptt = tt[sl], pt[sl]
                    A("dve", lambda pst=pst, ttt=ttt, h=h, lo=lo, hi=hi: V.scalar_tensor_tensor(
                        out=ttt[:, lo:hi], in0=pst[:, lo:hi], scalar=0.125, in1=biasu[:, h * 896 + lo:h * 896 + hi],
                        op0=ALU.mult, op1=ALU.add), reads=pstk + [("biasu", h)], writes=[("ttb", sl)])
                    A("act", lambda ttt=ttt, ptt=ptt, lo=lo, hi=hi: ACT.activation(out=ptt[:, lo:hi], in_=ttt[:, lo:hi], func=AF.Exp),
                      reads=[("ttb", sl)], writes=[("ptb", sl)])
                    for m in ms:
                        kb = i + m - 3
                        A("pe", lambda ptt=ptt, m=m, kb=kb, h=h, ms=ms, po=po: T.matmul(
                            po[:, (h // 4) * 512 + (h % 4) * 65:(h // 4) * 512 + (h % 4) * 65 + 65],
                            lhsT=ptt[:, m * 128:(m + 1) * 128], rhs=vpb4[:, kb, h, :],
                            start=(m == ms[0]), stop=(m == ms[-1])),
                          reads=[("ptb", sl), ("vpb", kb)], writes=[pok])
                A("dve", lambda po4=po4: V.reciprocal(out=den[:].rearrange("p (b h) -> p b h", b=2), in_=po4[:, :, :, 64]),
                  reads=[pok], writes=["denb"])
                A("dve", lambda po4=po4: V.tensor_tensor(
                    out=ob[:].rearrange("p (b h d) -> p b h d", b=2, d=64), in0=po4[:, :, :, 0:64],
                    in1=den[:].rearrange("p (b h) -> p b h", b=2).unsqueeze(3).to_broadcast([128, 2, 4, 64]), op=ALU.mult),
                  reads=[pok, "denb"], writes=["ob"])
                A("act", lambda: ACT.activation(out=junk2[:], in_=ob[:], func=AF.Square, accum_out=ssq2[:]),
                  reads=["ob"], writes=["junk2b", "ssq2b"])
                A("dve", lambda: V.tensor_scalar(out=ssq2[:], in0=ssq2[:], scalar1=1.0 / 512, scalar2=EPS, op0=ALU.mult, op1=ALU.add),
                  reads=["ssq2b"], writes=["ssq2b"])
                A("act", lambda: ACT.activation(out=ssq2[:], in_=ssq2[:], func=AF.Ln), reads=["ssq2b"], writes=["ssq2b"])
                A("act", lambda: ACT.activation(out=ssq2[:], in_=ssq2[:], func=AF.Exp, scale=-0.5), reads=["ssq2b"], writes=["ssq2b"])
                mx = mixb[i % 2]
                A("dve", lambda mx=mx: V.scalar_tensor_tensor(out=mx[:], in0=ob[:], scalar=ssq2[:, 0:1], in1=gob[:],
                                                              op0=ALU.mult, op1=ALU.mult),
                  reads=["ob", "ssq2b", "gob"], writes=[("mixb", i % 2)])
                sc.dma("sp", lambda mx=mx, i=i: SP.dma_start(out=mixb_d[i * 128:(i + 1) * 128, :], in_=mx[:]),
                       reads=[("mixb", i % 2)], writes=[("mixb_d", i)])
            sc.barrier()
    if stop_after == "B":
        sc.finish([("mixa_d", i) for i in range(NQB)] + [("mixb_d", i) for i in range(NQB)] + ["dbg_qta", "dbg_mod"])
        es.close()
        return nc

    rt_ = ExitStack()
    lg_all = sb(rt_, "lg_all", [128, NQB * NE], F32)
    m8_all = sb(rt_, "m8_all", [128, NQB * 8], F32)
    lg3 = lg_all[:].rearrange("p (i e) -> p i e", e=NE)
    m83 = m8_all[:].rearrange("p (i k) -> p i k", k=8)
    with ExitStack() as pc:
        wout = sb(pc, "wout", [128, 8 * D], BF16)
        wout3 = wout[:].rearrange("p (c n) -> p c n", n=D)
        for c in range(8):
            sc.dma("pool", lambda c=c: G.dma_start(out=wout3[:, c, :], in_=wout_d[c * 128:(c + 1) * 128, :]), writes=[("wout", c)])
        WOK = [("wout", c) for c in range(8)]
        wr = sb(pc, "wr", [128, 8 * NE], F32)
        sc.dma("sp", lambda: SP.dma_start(out=wr[:].rearrange("p (c e) -> p c e", e=NE),
                                          in_=wr_d[:, :].rearrange("(c p) e -> p c e", p=128)), writes=["wr"])
        brt = sb(pc, "brt", [1, NE], F32)
        sc.dma("sp", lambda: SP.dma_start(out=brt[:], in_=br_d[:, :]), writes=["brt"])
        mixab = [sb(pc, f"mixab{i}", [128, D], BF16) for i in range(2)]
        xb_ = [sb(pc, f"xc{i}", [128, D], F32) for i in range(2)]
        mixT_ = [sb(pc, f"mixT{j}", [128, D], BF16) for j in range(2)]
        t1_ = [sb(pc, f"t1{j}", [128, D], F32) for j in range(2)]
        x1t = [sb(pc, f"x1t{i}", [128, D], F32) for i in range(2)]
        h2f_ = [sb(pc, f"h2f{j}", [128, D], F32) for j in range(2)]
        h2b = [sb(pc, f"h2b{i}", [128, D], BF16) for i in range(2)]
        h2T_ = [sb(pc, f"h2T{j}", [128, D], F32) for j in range(2)]
        tilesC_ = [(sb(pc, f"junkC{j}", [128, D], BF16), sb(pc, f"ssqC{j}", [128, 1], F32),
                    sb(pc, f"rstdC{j}", [128, 1], F32), sb(pc, f"tmpC{j}", [128, D], F32)) for j in range(2)]
        def c_loads(i):
            s2 = i % 2
            sc.dma("sp", lambda i=i, s2=s2: SP.dma_start(out=mixab[s2][:, 0:512], in_=mixa_d[i * 128:(i + 1) * 128, :]),
                   reads=[("mixa_d", i)], writes=[("mixab", s2, 0)])
            sc.dma("sp", lambda i=i, s2=s2: SP.dma_start(out=mixab[s2][:, 512:1024], in_=mixb_d[i * 128:(i + 1) * 128, :]),
                   reads=[("mixb_d", i)], writes=[("mixab", s2, 1)])
            sc.dma("sp", lambda i=i, s2=s2: SP.dma_start(out=xb_[s2][:], in_=x_d[i * 128:(i + 1) * 128, :]), writes=[("xc", s2)])

        def c_a(i):
            s2 = i % 2
            mixT, t1, h2f, h2T, tilesC = mixT_[s2], t1_[s2], h2f_[s2], h2T_[s2], tilesC_[s2]
            pbT = PS[0][:, s2 * 512:(s2 + 1) * 512].bitcast(BF16)
            for c in range(8):
                A("pe", lambda c=c, pbT=pbT, s2=s2: T.transpose(out=pbT[:, c * 128:(c + 1) * 128],
                                                                in_=mixab[s2][:, c * 128:(c + 1) * 128], identity=ident_b[:]),
                  reads=[("mixab", s2, 0), ("mixab", s2, 1), "ident_b"], writes=[("ps", s2)])
            A("act", lambda pbT=pbT, mixT=mixT: ACT.copy(out=mixT[:], in_=pbT), reads=[("ps", s2)], writes=[("mixT", s2)])
            for n in range(2):
                py, pyk = bank(2 + n)
                for c in range(8):
                    A("pe", lambda py=py, c=c, n=n, mixT=mixT: T.matmul(py, lhsT=mixT[:, c * 128:(c + 1) * 128],
                                                             rhs=wout3[:, c, n * 512:(n + 1) * 512], start=(c == 0), stop=(c == 7)),
                      reads=[("mixT", s2)] + WOK, writes=[pyk])
                A("dve", lambda py=py, n=n, t1=t1: V.tensor_tensor(out=t1[:, n * 512:(n + 1) * 512], in0=py,
                                                            in1=gate_m[:, n * 512:(n + 1) * 512], op=ALU.mult),
                  reads=[pyk] + MODK, writes=[("t1", s2, n)])
            xt = x1t[s2]
            A("pool", lambda xt=xt, s2=s2, t1=t1: G.tensor_tensor(out=xt[:], in0=t1[:], in1=xb_[s2][:], op=ALU.add),
              reads=[("t1", s2, 0), ("t1", s2, 1), ("xc", s2)], writes=[("x1t", s2)])
            sc.dma("sp", lambda xt=xt, i=i: SP.dma_start(out=x1_d[i * 128:(i + 1) * 128, :], in_=xt[:]),
                   reads=[("x1t", s2)], writes=[("x1_d", i)])
            rmsnorm_mod(tilesC, xt[:], [("x1t", s2)], scale1_f, shift_f, out_bf=h2b[s2][:], out_f32=h2f[:],
                        out_keys=[("h2f", s2)], tag="C" + str(s2))
            sc.dma("sp", lambda i=i, s2=s2: SP.dma_start(out=h2_d[i * 128:(i + 1) * 128, :], in_=h2b[s2][:]),
                   reads=[("h2f", s2, "bf")], writes=[("h2_d", i)])

        def c_b(i):
            s2 = i % 2
            mixT, t1, h2f, h2T, tilesC = mixT_[s2], t1_[s2], h2f_[s2], h2T_[s2], tilesC_[s2]
            for r_ in range(2):
                pt_, ptk_ = bank(4 + r_)
                for c4 in range(4):
                    c = r_ * 4 + c4
                    A("pe", lambda pt_=pt_, c=c, c4=c4, h2f=h2f: T.transpose(out=pt_[:, c4 * 128:(c4 + 1) * 128],
                                                                    in_=h2f[:, c * 128:(c + 1) * 128], identity=ident_f[:]),
                      reads=[("h2f", s2), "ident_f"], writes=[ptk_])
                if r_ == 0:
                    A("dve", lambda pt_=pt_, r_=r_, h2T=h2T: V.tensor_copy(out=h2T[:, r_ * 512:(r_ + 1) * 512], in_=pt_), reads=[ptk_], writes=[("h2T", s2, r_)])
                else:
                    A("act", lambda pt_=pt_, r_=r_, h2T=h2T: ACT.copy(out=h2T[:, r_ * 512:(r_ + 1) * 512], in_=pt_), reads=[ptk_], writes=[("h2T", s2, r_)])
            pl, plk = bank(6 + s2)
            for c in range(8):
                A("pe", lambda pl=pl, c=c, h2T=h2T: T.matmul(pl[:, 0:NE], lhsT=h2T[:, c * 128:(c + 1) * 128], rhs=wr[:, c * NE:(c + 1) * NE],
                                                    start=(c == 0), stop=False),
                  reads=[("h2T", s2, 0), ("h2T", s2, 1), "wr"], writes=[plk])
            A("pe", lambda pl=pl: T.matmul(pl[:, 0:NE], lhsT=ones_f[0:1, :], rhs=brt[0:1, :], start=False, stop=True),
              reads=["ones_f", "brt"], writes=[plk])
            A("dve", lambda pl=pl, i=i: V.tensor_copy(out=lg3[:, i, :], in_=pl[:, 0:NE]), reads=[plk], writes=[("lg", i)])
            A("dve", lambda i=i: V.max(out=m83[:, i, :], in_=lg3[:, i, :]), reads=[("lg", i)], writes=[("m8", i)])

        c_loads(0)
        for i in range(NQB):
            if i + 1 < NQB:
                c_loads(i + 1)
            c_a(i)
            if i >= 1:
                c_b(i - 1)
        c_b(NQB - 1)
        sc.barrier()
    LGK = [("lg", i) for i in range(NQB)] + [("m8", i) for i in range(NQB)]

    gw_all = sb(rt_, "gw_all", [128, NQB * NE], F32)
    gw3 = gw_all[:].rearrange("p (i e) -> p i e", e=NE)
    dsel_i = sb(rt_, "dsel_i", [128, 4 * NQB], I32)
    gk = sb(rt_, "gk", [128, 4 * NQB], F32)
    idxw_i = sb(rt_, "idxw_i", [128, NBLK * 8], I32)
    idxg_i = sb(rt_, "idxg_i", [128, NBLK * 8], I32)
    idxb_i = sb(rt_, "idxb_i", [128, NBLK], I32)
    with ExitStack() as pr:
        tri = sb(pr, "tri", [128, 128], F32)
        sc.dma("sp", lambda: SP.dma_start(out=tri[:], in_=tri_d[:, :]), writes=["tri"])
        rowid = sb(pr, "rowid", [128, 8], F32)
        sc.dma("sp", lambda: SP.dma_start(out=rowid[:], in_=rowid_d[:, :]), writes=["rowid"])
        blkth = sb(pr, "blkth", [128, NBLK * NE], F32)
        sc.dma("sp", lambda: SP.dma_start(out=blkth[:], in_=blkth_d[:, :]), writes=["blkth"])
        msk = sb(pr, "msk", [128, NQB * NE], F32)
        msk3 = msk[:].rearrange("p (i e) -> p i e", e=NE)
        ex = sb(pr, "ex", [128, NQB * NE], F32)
        ex3 = ex[:].rearrange("p (i e) -> p i e", e=NE)
        ssum = sb(pr, "ssum", [128, NQB], F32)
        pos = sb(pr, "pos", [128, NQB * NE], F32)
        pos3 = pos[:].rearrange("p (i e) -> p i e", e=NE)
        oh = sb(pr, "oh", [128, NQB * NE], F32)
        oh3 = oh[:].rearrange("p (i e) -> p i e", e=NE)
        prod = sb(pr, "prod", [128, NQB * NE], F32)
        prod3 = prod[:].rearrange("p (i e) -> p i e", e=NE)
        dself = sb(pr, "dself", [128, 4 * NQB], F32)
        cnt = sb(pr, "cnt", [128, NE], F32)
        cs = [sb(pr, f"cs{i}", [128, NE], F32) for i in range(2)]
        padded = sb(pr, "padded", [128, NE], F32)
        pstart = sb(pr, "pstart", [128, NE], F32)
        cmpb = sb(pr, "cmpb", [128, NBLK * NE], F32)
        blke = sb(pr, "blke", [128, NBLK], F32)
        idxwf = sb(pr, "idxwf", [128, NBLK * 8], F32)
        idxbf = sb(pr, "idxbf", [128, NBLK], F32)

        A("dve", lambda: V.tensor_tensor(out=msk3, in0=lg3, in1=m83[:, :, 3:4].to_broadcast([128, NQB, NE]), op=ALU.is_ge),
          reads=LGK, writes=["msk"])
        A("dve", lambda: V.tensor_tensor(out=ex3, in0=lg3, in1=m83[:, :, 0:1].to_broadcast([128, NQB, NE]), op=ALU.subtract),
          reads=LGK, writes=["ex"])
        A("act", lambda: ACT.activation(out=ex[:], in_=ex[:], func=AF.Exp), reads=["ex"], writes=["ex"])
        A("dve", lambda: V.tensor_tensor(out=ex[:], in0=ex[:], in1=msk[:], op=ALU.mult), reads=["ex", "msk"], writes=["ex"])
        A("dve", lambda: V.reduce_sum(out=ssum[:], in_=ex3, axis=AX.X), reads=["ex"], writes=["ssum"])
        A("dve", lambda: V.reciprocal(out=ssum[:], in_=ssum[:]), reads=["ssum"], writes=["ssum"])
        A("dve", lambda: V.tensor_tensor(out=gw3, in0=ex3, in1=ssum[:].unsqueeze(2).to_broadcast([128, NQB, NE]), op=ALU.mult),
          reads=["ex", "ssum"], writes=["gw"])
        for half in range(2):
            pp_, ppk_ = bank(half)
            for ii in range(16):
                i = half * 16 + ii
                for j in range(i):
                    A("pe", lambda pp_=pp_, ii=ii, j=j: T.matmul(pp_[:, ii * NE:(ii + 1) * NE], lhsT=ones_f[:], rhs=msk3[:, j, :],
                                                                 start=(j == 0), stop=False),
                      reads=["ones_f", "msk"], writes=[ppk_])
                A("pe", lambda pp_=pp_, ii=ii, i=i: T.matmul(pp_[:, ii * NE:(ii + 1) * NE], lhsT=tri[:], rhs=msk3[:, i, :],
                                                             start=(i == 0), stop=True),
                  reads=["tri", "msk"], writes=[ppk_])
            A("dve", lambda pp_=pp_, half=half: V.tensor_copy(out=pos[:, half * 512:(half + 1) * 512], in_=pp_),
              reads=[ppk_], writes=[("pos", half)])
        pc_, pck_ = bank(2)
        for j in range(NQB):
            A("pe", lambda j=j: T.matmul(pc_[:, 0:NE], lhsT=ones_f[:], rhs=msk3[:, j, :], start=(j == 0), stop=(j == NQB - 1)),
              reads=["ones_f", "msk"], writes=[pck_])
        A("dve", lambda: V.tensor_copy(out=cnt[:], in_=pc_[:, 0:NE]), reads=[pck_], writes=["cnt"])
        nbt = sb(pr, "nbt", [128, NE * 8], F32)
        A("dve", lambda: V.tensor_tensor(out=nbt[:].rearrange("p (e j) -> p e j", j=8),
                                         in0=cnt[:].unsqueeze(2).to_broadcast([128, NE, 8]),
                                         in1=blkth[:, 0:8 * NE].rearrange("p (b e) -> p e b", e=NE), op=ALU.is_gt),
          reads=["cnt", "blkth"], writes=["nbt"])
        A("dve", lambda: V.reduce_sum(out=padded[:], in_=nbt[:].rearrange("p (e j) -> p e j", j=8), axis=AX.X), reads=["nbt"], writes=["padded"])
        A("dve", lambda: V.tensor_scalar(out=padded[:], in0=padded[:], scalar1=float(MB), scalar2=None, op0=ALU.mult),
          reads=["padded"], writes=["padded"])
        A("dve", lambda: V.tensor_copy(out=cs[0][:], in_=padded[:]), reads=["padded"], writes=[("cs", 0)])
        cur = 0
        for sft in (1, 2, 4, 8, 16):
            nxt = 1 - cur
            A("dve", lambda cur=cur, nxt=nxt, sft=sft: V.tensor_copy(out=cs[nxt][:, 0:sft], in_=cs[cur][:, 0:sft]),
              reads=[("cs", cur)], writes=[("cs", nxt)])
            A("dve", lambda cur=cur, nxt=nxt, sft=sft: V.tensor_tensor(out=cs[nxt][:, sft:NE], in0=cs[cur][:, sft:NE],
                                                                       in1=cs[cur][:, 0:NE - sft], op=ALU.add),
              reads=[("cs", cur), ("cs", nxt)], writes=[("cs", nxt)])
            cur = nxt
        pend = cs[cur]
        pendk = ("cs", cur)
        A("dve", lambda: V.tensor_tensor(out=pstart[:], in0=pend[:], in1=padded[:], op=ALU.subtract), reads=[pendk, "padded"], writes=["pstart"])
        A("dve", lambda: V.tensor_tensor(out=pos3, in0=pos3, in1=pstart[:].unsqueeze(1).to_broadcast([128, NQB, NE]), op=ALU.add),
          reads=[("pos", 0), ("pos", 1), "pstart"], writes=["dest"])
        for k in range(4):
            A("dve", lambda k=k: V.tensor_tensor(out=oh3, in0=lg3, in1=m83[:, :, k:k + 1].to_broadcast([128, NQB, NE]), op=ALU.is_equal),
              reads=LGK, writes=["oh"])
            A("dve", lambda: V.tensor_tensor(out=prod[:], in0=oh[:], in1=pos[:], op=ALU.mult), reads=["oh", "dest"], writes=["prod"])
            A("dve", lambda k=k: V.reduce_sum(out=dself[:, k * NQB:(k + 1) * NQB], in_=prod3, axis=AX.X), reads=["prod"], writes=[("dself", k)])
            A("dve", lambda: V.tensor_tensor(out=prod[:], in0=oh[:], in1=gw_all[:], op=ALU.mult), reads=["oh", "gw"], writes=["prod"])
            A("dve", lambda k=k: V.reduce_sum(out=gk[:, k * NQB:(k + 1) * NQB], in_=prod3, axis=AX.X), reads=["prod"], writes=[("gk", k)])
        A("dve", lambda: V.tensor_copy(out=dsel_i[:], in_=dself[:]), reads=[("dself", k) for k in range(4)], writes=["dsel_i"])
        A("dve", lambda: V.tensor_tensor(out=cmpb[:].rearrange("p (b e) -> p b e", e=NE),
                                         in0=pend[:].unsqueeze(1).to_broadcast([128, NBLK, NE]),
                                         in1=blkth[:].rearrange("p (b e) -> p b e", e=NE), op=ALU.is_le),
          reads=[pendk, "blkth"], writes=["cmpb"])
        A("dve", lambda: V.reduce_sum(out=blke[:], in_=cmpb[:].rearrange("p (b e) -> p b e", e=NE), axis=AX.X), reads=["cmpb"], writes=["blke"])
        A("dve", lambda: V.tensor_scalar(out=blke[:], in0=blke[:], scalar1=float(NE - 1), scalar2=None, op0=ALU.min), reads=["blke"], writes=["blke"])
        A("dve", lambda: V.scalar_tensor_tensor(out=idxwf[:].rearrange("p (b c) -> p b c", c=8),
                                                in0=blke[:].unsqueeze(2).to_broadcast([128, NBLK, 8]), scalar=float(D),
                                                in1=rowid[:].unsqueeze(1).to_broadcast([128, NBLK, 8]), op0=ALU.mult, op1=ALU.add),
          reads=["blke", "rowid"], writes=["idxwf"])
        nused = sb(pr, "nused", [128, NBLK], F32)
        A("dve", lambda: V.tensor_scalar(out=nused[:], in0=blkth[:].rearrange("p (b e) -> p b e", e=NE)[:, :, 0],
                                         scalar1=pend[:, NE - 1:NE], scalar2=None, op0=ALU.is_ge),
          reads=[pendk, "blkth"], writes=["nused"])
        sameb = sb(pr, "sameb", [128, NBLK], F32)
        A("dve", lambda: V.memset(sameb[:], 0.0), writes=["sameb"])
        A("dve", lambda: V.tensor_tensor(out=sameb[:, 2:NBLK], in0=blke[:, 2:NBLK], in1=blke[:, 0:NBLK - 2], op=ALU.is_equal),
          reads=["blke", "sameb"], writes=["sameb"])
        A("dve", lambda: V.tensor_tensor(out=nused[:], in0=nused[:], in1=sameb[:], op=ALU.max), reads=["nused", "sameb"], writes=["nused"])
        idxgf = sb(pr, "idxgf", [128, NBLK * 8], F32)
        A("dve", lambda: V.scalar_tensor_tensor(out=idxgf[:].rearrange("p (b c) -> p b c", c=8),
                                                in0=nused[:].unsqueeze(2).to_broadcast([128, NBLK, 8]), scalar=40000.0,
                                                in1=idxwf[:].rearrange("p (b c) -> p b c", c=8), op0=ALU.mult, op1=ALU.add),
          reads=["nused", "idxwf"], writes=["idxgf"])
        A("dve", lambda: V.tensor_copy(out=idxg_i[:], in_=idxgf[:]), reads=["idxgf"], writes=["idxg_i"])
        A("dve", lambda: V.tensor_copy(out=idxw_i[:], in_=idxwf[:]), reads=["idxwf"], writes=["idxw_i"])
        A("dve", lambda: V.scalar_tensor_tensor(out=idxbf[:], in0=blke[:], scalar=128.0, in1=rowid[:, 0:1].to_broadcast([128, NBLK]),
                                                op0=ALU.mult, op1=ALU.add), reads=["blke", "rowid"], writes=["idxbf"])
        A("dve", lambda: V.scalar_tensor_tensor(out=idxbf[:], in0=nused[:], scalar=40000.0, in1=idxbf[:], op0=ALU.mult, op1=ALU.add),
          reads=["nused", "idxbf"], writes=["idxbf"])
        A("dve", lambda: V.tensor_copy(out=idxb_i[:], in_=idxbf[:]), reads=["idxbf"], writes=["idxb_i"])
        if dbg:
            sc.dma("sp", lambda: SP.dma_start(out=dbg_d["gw"][:, :], in_=gw_all[:]), reads=["gw"], writes=["dbg_gw"])
            sc.dma("sp", lambda: SP.dma_start(out=dbg_d["dsel"][:, :], in_=dsel_i[:]), reads=["dsel_i"], writes=["dbg_dsel"])
            sc.dma("sp", lambda: SP.dma_start(out=dbg_d["blke"][:, :], in_=blke[:]), reads=["blke"], writes=["dbg_blke"])
        h2r = [sb(pr, f"h2r{i}", [128, D], BF16) for i in range(4)]
        for i in range(NQB):
            s4 = i % 4
            sc.dma("sp", lambda i=i, s4=s4: SP.dma_start(out=h2r[s4][:], in_=h2_d[i * 128:(i + 1) * 128, :]),
                   reads=[("h2_d", i)], writes=[("h2r", s4)])
            for k in range(4):
                sc.dma("pool", lambda i=i, k=k, s4=s4: G.indirect_dma_start(
                    out=xs_d[:, :], out_offset=bass.IndirectOffsetOnAxis(ap=dsel_i[:, k * NQB + i:k * NQB + i + 1], axis=0),
                    in_=h2r[s4][:], in_offset=None), reads=[("h2r", s4), "dsel_i"], writes=["xs_d"])
        sc.barrier()
    if stop_after == "R":
        sc.finish(["dbg_gw", "dbg_dsel", "dbg_blke", "xs_d"] + [("x1_d", i) for i in range(NQB)])
        rt_.close()
        es.close()
        return nc

    with ExitStack() as pm:
        wgu = [sb(pm, f"wgu{i}", [128, 8 * 2 * DFF], BF16) for i in range(2)]
        wdn = [sb(pm, f"wdn{i}", [128, 8 * D], BF16) for i in range(2)]
        bgu = [sb(pm, f"bgu{i}", [128, 16], F32) for i in range(2)]
        xst = [sb(pm, "xst0", [128, 4 * D], BF16)]
        xsT = sb(pm, "xsT", [128, 8 * MB], BF16)
        xsT3 = xsT[:].rearrange("p (c t) -> p c t", t=MB)
        actT_ = [sb(pm, f"actT{j}", [128, 8 * MB], BF16) for j in range(2)]
        actT3_ = [a_[:].rearrange("p (f t) -> p f t", t=MB) for a_ in actT_]
        gt = [sb(pm, f"gt{i}", [128, MB], F32) for i in range(2)]
        sg = [sb(pm, f"sg{i}", [128, MB], F32) for i in range(2)]
        ut = [sb(pm, f"ut{i}", [128, MB], F32) for i in range(2)]
        yst = [sb(pm, f"yst{i}", [128, D], F32) for i in range(4)]

        bc_reg = [G.to_reg(NE * D - 1), G.to_reg(NE * 128 - 1)]

        def load_weights(b, which):
            sl = b % 2
            w3g = wgu[sl][:].rearrange("p (c n) -> p c n", n=2 * DFF)
            w3d = wdn[sl][:].rearrange("p (c n) -> p c n", n=D)
            for c in range(8 if which == "gu" else 0):
                sc.dma("pool", lambda c=c, w3g=w3g, b=b: G.indirect_dma_start(
                    out=w3g[:, c, :], out_offset=None, in_=wgu_d[:, :],
                    in_offset=bass.IndirectOffsetOnAxis(ap=idxg_i[:, b * 8 + c:b * 8 + c + 1], axis=0),
                    bounds_check=bc_reg[0], oob_is_err=False),
                    reads=["idxg_i"], writes=[("wgu", sl, c)])
            for c in range(8 if which == "wd" else 0):
                sc.dma("pool", lambda c=c, w3d=w3d, b=b: G.indirect_dma_start(
                    out=w3d[:, c, :], out_offset=None, in_=wd_d[:, :],
                    in_offset=bass.IndirectOffsetOnAxis(ap=idxg_i[:, b * 8 + c:b * 8 + c + 1], axis=0),
                    bounds_check=bc_reg[0], oob_is_err=False),
                    reads=["idxg_i"], writes=[("wdn", sl, c)])
            if which == "gu":
              sc.dma("pool", lambda b=b, sl=sl: G.indirect_dma_start(
                out=bgu[sl][:], out_offset=None, in_=bgu_d[:, :],
                in_offset=bass.IndirectOffsetOnAxis(ap=idxb_i[:, b:b + 1], axis=0),
                bounds_check=bc_reg[1], oob_is_err=False), reads=["idxb_i"], writes=[("bgu", sl)])

        def load_x(b):
            sl = 0
            sc.dma("sp", lambda b=b, sl=sl: SP.dma_start(out=xst[sl][:].rearrange("p (s d) -> p s d", d=D),
                                                        in_=xs_d[b * MB:(b + 1) * MB, :].rearrange("(s p) d -> p s d", p=128)),
                   reads=["xs_d"], writes=[("xst", sl)])

        for j in range(2):
            A("dve", lambda j=j: V.memset(wgu[j][:], 0.0), writes=[("wgu", j, c) for c in range(8)])
            A("dve", lambda j=j: V.memset(wdn[j][:], 0.0), writes=[("wdn", j, c) for c in range(8)])
            A("dve", lambda j=j: V.memset(bgu[j][:], 0.0), writes=[("bgu", j)])
        load_weights(0, "gu")
        load_x(0)

        def down_proj(b):
            sl = b % 2
            w3d = wdn[sl][:].rearrange("p (c n) -> p c n", n=D)
            WDK = [("wdn", sl, c) for c in range(8)]
            aT3 = actT3_[sl]
            AK = [("actT", sl, f) for f in range(8)]
            for s_ in range(4):
                ys_ = yst[s_]
                for n in range(2):
                    py, pyk = bank(6 + n)
                    for f in range(8):
                        A("pe", lambda py=py, f=f, s_=s_, n=n, w3d=w3d, aT3=aT3: T.matmul(
                            py, lhsT=aT3[:, f, s_ * 128:(s_ + 1) * 128], rhs=w3d[:, f, n * 512:(n + 1) * 512],
                            start=(f == 0), stop=(f == 7)), reads=AK + WDK, writes=[pyk])
                    A("act", lambda py=py, ys_=ys_, n=n: ACT.copy(out=ys_[:, n * 512:(n + 1) * 512], in_=py),
                      reads=[pyk], writes=[("yst", s_, n)])
                sc.dma("sp", lambda ys_=ys_, b=b, s_=s_: SP.dma_start(out=ys_d[b * MB + s_ * 128:b * MB + (s_ + 1) * 128, :], in_=ys_[:]),
                       reads=[("yst", s_, 0), ("yst", s_, 1)], writes=["ys_d"])

        for b in range(NBLK):
            sl = b % 2
            w3g = wgu[sl][:].rearrange("p (c n) -> p c n", n=2 * DFF)
            WGK = [("wgu", sl, c) for c in range(8)]
            aT3 = actT3_[sl]
            for s_ in range(4):
                pbT = PS[0][:, (s_ % 2) * 512:(s_ % 2 + 1) * 512].bitcast(BF16)
                pkT = ("ps", s_ % 2)
                for c in range(8):
                    A("pe", lambda c=c, pbT=pbT, s_=s_: T.transpose(
                        out=pbT[:, c * 128:(c + 1) * 128], in_=xst[0][:, s_ * D + c * 128:s_ * D + (c + 1) * 128], identity=ident_b[:]),
                      reads=[("xst", 0), "ident_b"], writes=[pkT])
                if s_ % 2 == 0:
                    A("dve", lambda pbT=pbT, s_=s_: V.tensor_copy(out=xsT3[:, :, s_ * 128:(s_ + 1) * 128],
                                                                  in_=pbT.rearrange("p (c t) -> p c t", t=128)),
                      reads=[pkT], writes=[("xsT", s_)])
                else:
                    A("act", lambda pbT=pbT, s_=s_: ACT.copy(out=xsT3[:, :, s_ * 128:(s_ + 1) * 128],
                                                             in_=pbT.rearrange("p (c t) -> p c t", t=128)),
                      reads=[pkT], writes=[("xsT", s_)])
            if b + 1 < NBLK:
                load_x(b + 1)
            if b >= 1:
                down_proj(b - 1)
            load_weights(b, "wd")
            if b + 1 < NBLK:
                load_weights(b + 1, "gu")
            XK = [("xsT", s_) for s_ in range(4)]
            for f in range(8):
                e2 = f % 2
                pg, pgk = bank(2 + e2 * 2)
                pu, puk = bank(3 + e2 * 2)
                for (pb, pk, col) in ((pg, pgk, f * 128), (pu, puk, DFF + f * 128)):
                    for c in range(8):
                        A("pe", lambda pb=pb, c=c, col=col, w3g=w3g: T.matmul(pb, lhsT=w3g[:, c, col:col + 128], rhs=xsT3[:, c, :],
                                                                              start=(c == 0), stop=(c == 7)),
                          reads=XK + WGK, writes=[pk])
                g_, s__, u_ = gt[e2], sg[e2], ut[e2]
                A("dve", lambda pg=pg, g_=g_, f=f, sl=sl: V.tensor_scalar(out=g_[:], in0=pg, scalar1=bgu[sl][:, f:f + 1], scalar2=7.0,
                                                                          op0=ALU.add, op1=ALU.min),
                  reads=[pgk, ("bgu", sl)], writes=[("gt", e2)])
                A("act", lambda g_=g_, s__=s__: ACT.activation(out=s__[:], in_=g_[:], func=AF.Sigmoid, scale=1.702),
                  reads=[("gt", e2)], writes=[("sg", e2)])
                A("dve", lambda pu=pu, u_=u_, f=f, sl=sl: V.tensor_scalar(out=u_[:], in0=pu, scalar1=bgu[sl][:, 8 + f:9 + f], scalar2=7.0,
                                                                          op0=ALU.add, op1=ALU.min),
                  reads=[puk, ("bgu", sl)], writes=[("ut", e2)])
                A("dve", lambda u_=u_: V.tensor_scalar(out=u_[:], in0=u_[:], scalar1=-7.0, scalar2=1.0, op0=ALU.max, op1=ALU.add),
                  reads=[("ut", e2)], writes=[("ut", e2)])
                A("dve", lambda g_=g_, s__=s__: V.tensor_tensor(out=g_[:], in0=g_[:], in1=s__[:], op=ALU.mult),
                  reads=[("gt", e2), ("sg", e2)], writes=[("gt", e2)])
                A("dve", lambda g_=g_, u_=u_, f=f, aT3=aT3: V.tensor_tensor(out=aT3[:, f, :], in0=g_[:], in1=u_[:], op=ALU.mult),
                  reads=[("gt", e2), ("ut", e2)], writes=[("actT", sl, f)])
            if b % 8 == 7:
                sc.flush()
        down_proj(NBLK - 1)
        sc.barrier()

    with ExitStack() as pf:
        bd = sb(pf, "bd", [NE, D], F32)
        sc.dma("sp", lambda: SP.dma_start(out=bd[:], in_=bd_d[:, :]), writes=["bd"])
        gfin = sb(pf, "gfin", [128, D], F32)
        sc.dma("sp", lambda: SP.dma_start(out=gfin[:], in_=gfin_d[:, :].partition_broadcast(128)), writes=["gfin"])
        x1f = [sb(pf, f"x1f{i}", [128, D], F32) for i in range(2)]
        yk_ = [[sb(pf, f"yk{j}_{i}", [128, D], F32) for i in range(4)] for j in range(2)]
        acc_ = [sb(pf, f"acc{j}", [128, D], F32) for j in range(2)]
        gwT_ = [sb(pf, f"gwT{j}", [NE, 128], F32) for j in range(2)]
        junkF = sb(pf, "junkF", [128, D], BF16)
        ssqF = sb(pf, "ssqF", [128, 1], F32)
        ot = [sb(pf, f"ot{i}", [128, D], F32) for i in range(2)]
        def f_loads(i):
            s2 = i % 2
            yk = yk_[s2]
            sc.dma("sp", lambda i=i, s2=s2: SP.dma_start(out=x1f[s2][:], in_=x1_d[i * 128:(i + 1) * 128, :]),
                   reads=[("x1_d", i)], writes=[("x1f", s2)])
            for k in range(4):
                sc.dma("pool", lambda i=i, k=k, yk=yk: G.indirect_dma_start(
                    out=yk[k][:], out_offset=None, in_=ys_d[:, :],
                    in_offset=bass.IndirectOffsetOnAxis(ap=dsel_i[:, k * NQB + i:k * NQB + i + 1], axis=0)),
                    reads=["ys_d", "dsel_i"], writes=[("yk", s2, k)])

        for i in range(NQB):
            s2 = i % 2
            yk, acc, gwT = yk_[s2], acc_[s2], gwT_[s2]
            ACCK, GWTK = ("acc", s2), ("gwT", s2)
            if i == 0:
                f_loads(0)
            if i + 1 < NQB:
                f_loads(i + 1)
            pt_, ptk_ = bank(s2)
            A("pe", lambda pt_=pt_, i=i: T.transpose(out=pt_[0:NE, 0:128], in_=gw3[:, i, :], identity=ident_f[:]),
              reads=["gw", "ident_f"], writes=[ptk_])
            A("act", lambda pt_=pt_, gwT=gwT: ACT.copy(out=gwT[:], in_=pt_[0:NE, 0:128]), reads=[ptk_], writes=[GWTK])
            pbs = []
            for n in range(2):
                pb, pbk = bank(2 + 2 * s2 + n)
                A("pe", lambda pb=pb, n=n, gwT=gwT: T.matmul(pb, lhsT=gwT[:], rhs=bd[:, n * 512:(n + 1) * 512], start=True, stop=True),
                  reads=[GWTK, "bd"], writes=[pbk])
                pbs.append((pb, pbk))
            A("dve", lambda i=i, acc=acc, yk=yk: V.tensor_scalar(out=acc[:], in0=yk[0][:], scalar1=gk[:, i:i + 1], scalar2=None, op0=ALU.mult),
              reads=[("yk", s2, 0)] + [("gk", k) for k in range(4)], writes=[ACCK])
            for k in range(1, 4):
                A("dve", lambda i=i, k=k, acc=acc, yk=yk: V.scalar_tensor_tensor(out=acc[:], in0=yk[k][:], scalar=gk[:, k * NQB + i:k * NQB + i + 1],
                                                                 in1=acc[:], op0=ALU.mult, op1=ALU.add),
                  reads=[("yk", s2, k), ACCK] + [("gk", kk) for kk in range(4)], writes=[ACCK])
            for n in range(2):
                pb, pbk = pbs[n]
                A("dve", lambda pb=pb, n=n, acc=acc: V.tensor_tensor(out=acc[:, n * 512:(n + 1) * 512], in0=pb, in1=acc[:, n * 512:(n + 1) * 512], op=ALU.add),
                  reads=[pbk, ACCK], writes=[ACCK])
            A("pool", lambda acc=acc: G.tensor_tensor(out=acc[:], in0=acc[:], in1=gate_f, op=ALU.mult), reads=[ACCK] + MODK, writes=[ACCK])
            A("pool", lambda s2=s2, acc=acc: G.tensor_tensor(out=acc[:], in0=acc[:], in1=x1f[s2][:], op=ALU.add), reads=[ACCK, ("x1f", s2)], writes=[ACCK])
            A("act", lambda acc=acc: ACT.activation(out=junkF[:], in_=acc[:], func=AF.Square, accum_out=ssqF[:]), reads=[ACCK], writes=["junkF", "ssqF"])
            A("dve", lambda: V.tensor_scalar(out=ssqF[:], in0=ssqF[:], scalar1=1.0 / D, scalar2=EPS, op0=ALU.mult, op1=ALU.add),
              reads=["ssqF"], writes=["ssqF"])
            A("act", lambda: ACT.activation(out=ssqF[:], in_=ssqF[:], func=AF.Ln), reads=["ssqF"], writes=["ssqF"])
            A("act", lambda: ACT.activation(out=ssqF[:], in_=ssqF[:], func=AF.Exp, scale=-0.5), reads=["ssqF"], writes=["ssqF"])
            o_ = ot[s2]
            A("dve", lambda o_=o_, acc=acc: V.scalar_tensor_tensor(out=o_[:], in0=acc[:], scalar=ssqF[:, 0:1], in1=gfin[:], op0=ALU.mult, op1=ALU.mult),
              reads=[ACCK, "ssqF", "gfin"], writes=[("ot", s2)])
            sc.dma("sp", lambda o_=o_, i=i: SP.dma_start(out=out_d[i * 128:(i + 1) * 128, :], in_=o_[:]),
                   reads=[("ot", s2)], writes=[("out_d", i)])
        sc.barrier()
    sc.finish([("out_d", i) for i in range(NQB)])
    rt_.close()
    es.close()
    return nc


def _prep_inputs(inputs):
    f = lambda a: np.ascontiguousarray(np.asarray(a, dtype=np.float32))
    x = f(inputs["x"])
    c = f(inputs["c"])
    w_in = f(inputs["w_in"])[0]
    wa, wb = _layout_w_in(w_in)
    cosT, sinT = _rope_tables()
    dr, co = _bias_index()
    rpb = f(inputs["rpb"])[0]
    biasu = np.ascontiguousarray(rpb[:, dr, co].reshape(8, 128, 7 * 128))
    bgu = f(inputs["b_gate_up"])[0]
    bgu_l = np.ascontiguousarray(bgu.reshape(NE, 16, 128).transpose(0, 2, 1).reshape(NE * 128, 16))
    rowid = (np.arange(8)[None, :] * 128 + np.arange(128)[:, None]).astype(np.float32)
    blkth = np.broadcast_to((np.arange(NBLK, dtype=np.float32) * MB)[None, :, None], (128, NBLK, NE)).reshape(128, NBLK * NE)
    shared = {
        "w_ada": f(inputs["w_ada"])[0], "b_ada": f(inputs["b_ada"]).reshape(1, 6 * D),
        "w_a": wa, "w_b": wb, "sink": f(inputs["sink"]).reshape(1, 8), "biasu": biasu,
        "g_out_a": f(inputs["g_out_a"]).reshape(1, 512), "g_out_b": f(inputs["g_out_b"]).reshape(1, 512),
        "w_out": f(inputs["w_out"])[0], "w_router": f(inputs["w_router"])[0], "b_router": f(inputs["b_router"]).reshape(1, NE),
        "w_gate_up": f(inputs["w_gate_up"])[0].reshape(NE * D, 2 * DFF), "b_gate_up": bgu_l,
        "w_down": f(inputs["w_down"])[0].reshape(NE * DFF, D), "b_down": f(inputs["b_down"])[0],
        "g_final": f(inputs["g_final"]).reshape(1, D), "cosT": cosT, "sinT": sinT,
        "maska": np.ascontiguousarray(_mask_a().reshape(128, 384)),
        "maskb": np.ascontiguousarray(_MASKB.reshape(_NVAR, 128, 7 * 128)),
        "ident": np.eye(128, dtype=np.float32), "tri": np.triu(np.ones((128, 128), np.float32), 1),
        "rowid": rowid, "blkth": np.ascontiguousarray(blkth),
    }
    in_maps = []
    for b in range(8):
        m = dict(shared)
        m["x"] = x[b]
        m["cT"] = np.ascontiguousarray(c[b].reshape(8, 128).T)
        in_maps.append(m)
    return in_maps


def kernel(**inputs):
    in_maps = _prep_inputs(inputs)
    nc = build_program()
    res = run_bass_kernel_spmd(nc, in_maps, core_ids=list(range(8)))
    return np.stack([np.asarray(r["out"], dtype=np.float32) for r in res.results], axis=0)
```

```python
import bisect
from contextlib import ExitStack

import numpy as np
import concourse.bass as bass
import concourse.mybir as mybir
from concourse.bass_utils import run_bass_kernel_spmd

F32 = mybir.dt.float32
BF16 = mybir.dt.bfloat16
I32 = mybir.dt.int32
AF = mybir.ActivationFunctionType
ALU = mybir.AluOpType
AX = mybir.AxisListType

S = 4096
D = 1024
NQB = 32
NE = 32
DFF = 1024
EPS = 1e-5
MASKV = -240000.0
MB = 512
NBLK = 64
NSLOT = NBLK * MB
THETA = 500000.0


class _Op:
    __slots__ = ("eng", "fn", "deps", "dma", "need_inc", "target")


class Sched:
    def __init__(self, nc, es, nchan=10):
        self.nc = nc
        self.engs = dict(pe=nc.tensor, act=nc.scalar, dve=nc.vector, pool=nc.gpsimd, sp=nc.sync)
        self.sem = {e: es.enter_context(nc.semaphore("sem_" + e)) for e in ("pe", "act", "dve", "pool")}
        nch = {"sp": 12, "pool": 28}
        self.chan = {q: [es.enter_context(nc.semaphore(f"ch_{q}{i}")) for i in range(nch[q])] for q in ("sp", "pool")}
        self.chan_cnt = {q: [0] * nch[q] for q in ("sp", "pool")}
        self.chan_next = {q: 0 for q in ("sp", "pool")}
        self.ops = []
        self.flushed = 0
        self.last_writer = {}
        self.readers = {}
        self.cnt = {e: 0 for e in self.sem}
        self.incs = {e: ([], []) for e in self.sem}
        self.waited = {}

    def add(self, eng, fn, reads=(), writes=(), dma=False):
        op = _Op()
        op.eng, op.fn, op.dma, op.need_inc, op.target = eng, fn, dma, False, None
        deps = set()
        for r in reads:
            w = self.last_writer.get(r)
            if w is not None:
                deps.add(w)
        for w_ in writes:
            w = self.last_writer.get(w_)
            if w is not None:
                deps.add(w)
            for r in self.readers.get(w_, ()):
                deps.add(r)
        idx = len(self.ops)
        deps.discard(idx)
        op.deps = deps
        for r in reads:
            self.readers.setdefault(r, []).append(idx)
        for w_ in writes:
            self.last_writer[w_] = idx
            self.readers[w_] = []
        self.ops.append(op)
        return idx

    def dma(self, q, fn, reads=(), writes=()):
        return self.add(q, fn, reads, writes, dma=True)

    def _wait(self, ceng, sem, val):
        key = (ceng, id(sem))
        if self.waited.get(key, 0) >= val:
            return
        self.waited[key] = val
        self.engs[ceng].wait_ge(sem, val)

    def flush(self, final=False):
        ops = self.ops
        lo, hi = self.flushed, len(ops)
        last_of = {}
        for i in range(lo, hi):
            op = ops[i]
            if not op.dma:
                last_of[op.eng] = i
            for d in op.deps:
                dop = ops[d]
                if d >= lo and not dop.dma:
                    if dop.eng == "pe" and op.eng == "pe" and not op.dma:
                        continue
                    dop.need_inc = True
        for e, i in last_of.items():
            ops[i].need_inc = True
        for i in range(lo, hi):
            op = ops[i]
            ceng = op.eng
            if op.dma:
                q = ceng
                c = self.chan_next[q]
                self.chan_next[q] = (c + 1) % len(self.chan[q])
                csem = self.chan[q][c]
                if self.chan_cnt[q][c] > 0:
                    self._wait(q, csem, 16 * self.chan_cnt[q][c])
            for d in sorted(op.deps):
                dop = ops[d]
                if dop.dma:
                    self._wait(ceng, dop.target[0], dop.target[1])
                else:
                    if dop.eng == "pe" and ceng == "pe" and not op.dma:
                        continue
                    il, cl = self.incs[dop.eng]
                    if dop.target is not None:
                        tv = dop.target[1]
                    else:
                        j = bisect.bisect_left(il, d)
                        if j < len(il):
                            tv = cl[j]
                        else:
                            raise RuntimeError("no covering inc")
                    self._wait(ceng, self.sem[dop.eng], tv)
            ins = op.fn()
            if op.dma:
                self.chan_cnt[q][c] += 1
                ins.then_inc(csem, 16)
                op.target = (csem, 16 * self.chan_cnt[q][c])
            elif op.need_inc:
                self.cnt[ceng] += 1
                ins.then_inc(self.sem[ceng], 1)
                op.target = (self.sem[ceng], self.cnt[ceng])
                self.incs[ceng][0].append(i)
                self.incs[ceng][1].append(self.cnt[ceng])
            op.fn = None
        self.flushed = hi

    def barrier(self):
        self.flush()
        for ceng in ("pe", "act", "dve", "pool", "sp"):
            for e, sem in self.sem.items():
                if self.cnt[e] > 0:
                    self._wait(ceng, sem, self.cnt[e])
            for q in ("sp", "pool"):
                for c, csem in enumerate(self.chan[q]):
                    if self.chan_cnt[q][c] > 0:
                        self._wait(ceng, csem, 16 * self.chan_cnt[q][c])

    def finish(self, out_keys):
        self.flush()
        for k in out_keys:
            w = self.last_writer.get(k)
            if w is not None:
                t = self.ops[w].target
                self._wait("sp", t[0], t[1])
        for q in ("sp", "pool"):
            for c, csem in enumerate(self.chan[q]):
                if self.chan_cnt[q][c] > 0:
                    self._wait("sp", csem, 16 * self.chan_cnt[q][c])


def _rope_tables():
    inv_freq = (np.float32(THETA) ** (-np.arange(0, 16, 2, dtype=np.float32) / np.float32(16))).astype(np.float32)
    pos = np.arange(S, dtype=np.float32)
    ang = (pos[:, None] * inv_freq[None, :]).astype(np.float32)
    cos = np.cos(ang).astype(np.float32)
    sin = np.sin(ang).astype(np.float32)
    cosT = np.ones((128, S), np.float32)
    sinT = np.zeros((128, S), np.float32)
    for hh in range(2):
        b = hh * 64
        for d in range(8):
            cosT[b + d] = cos[:, d]
            cosT[b + 8 + d] = cos[:, d]
            sinT[b + d] = -sin[:, d]
            sinT[b + 8 + d] = sin[:, d]
    return cosT, sinT


def _mask_a():
    k = np.arange(128)[:, None, None]
    m = np.arange(3)[None, :, None]
    q = np.arange(128)[None, None, :]
    rel = (m - 1) * 128 + k - q
    return np.where(np.abs(rel) <= 128, 0.0, MASKV).astype(np.float32)


def _b_geometry():
    rows = 64
    rs = np.clip(np.arange(rows) - 4, 0, rows - 8)
    cs = np.clip(np.arange(64) - 8, 0, 64 - 16)

    def valid_row(kr, r):
        return (0 <= kr < rows) and (rs[r] <= kr < rs[r] + 8)

    colmask = np.zeros((64, 64), bool)
    for qc in range(64):
        colmask[qc, cs[qc]:cs[qc] + 16] = True
    masks = {}
    kbs = {}
    for i in range(NQB):
        mk = np.full((128, 7, 128), MASKV, np.float32)
        used = []
        for m in range(7):
            kb = i + m - 3
            if kb < 0 or kb > 31:
                continue
            anyv = False
            for a in range(2):
                for b in range(2):
                    if valid_row(2 * kb + a, 2 * i + b):
                        anyv = True
                        blk = np.where(colmask.T, 0.0, MASKV)
                        mk[a * 64:(a + 1) * 64, m, b * 64:(b + 1) * 64] = blk
            if anyv:
                used.append(m)
        masks[i] = mk
        kbs[i] = used
    variants = []
    var_of = {}
    for i in range(NQB):
        for vi, v in enumerate(variants):
            if np.array_equal(v, masks[i]):
                var_of[i] = vi
                break
        else:
            var_of[i] = len(variants)
            variants.append(masks[i])
    return np.stack(variants), var_of, kbs


def _bias_index():
    a = np.arange(2)[:, None, None, None, None]
    kc = np.arange(64)[None, :, None, None, None]
    m = np.arange(7)[None, None, :, None, None]
    b = np.arange(2)[None, None, None, :, None]
    qc = np.arange(64)[None, None, None, None, :]
    dr = np.clip(2 * (m - 3) + a - b, -7, 7) + 7
    co = np.clip(kc - qc, -15, 15) + 15
    dr = np.broadcast_to(dr, (2, 64, 7, 2, 64)).reshape(128, 7, 128)
    co = np.broadcast_to(co, (2, 64, 7, 2, 64)).reshape(128, 7, 128)
    return dr, co


_MASKB, _VAR_OF, _KBS = _b_geometry()
_NVAR = _MASKB.shape[0]
_PERM = np.concatenate([np.arange(8, 16), np.arange(0, 8), np.arange(16, 64)])

NWA = 1536 + 128
NWB = 1536


def _layout_w_in(w):
    qa, ka, va = w[:, 0:512], w[:, 512:640], w[:, 640:768]
    qb, kb, vb = w[:, 768:1280], w[:, 1280:1792], w[:, 1792:2304]
    qap = qa.reshape(D, 8, 64)[:, :, _PERM].reshape(D, 512)
    k0, k1 = ka[:, 0:64], ka[:, 64:128]
    k0p, k1p = k0[:, _PERM], k1[:, _PERM]
    wa = np.concatenate([qa, qap, k0, k0, k1, k1, k0p, k0p, k1p, k1p, va], axis=1)
    wb = np.concatenate([qb, kb, vb], axis=1)
    return np.ascontiguousarray(wa), np.ascontiguousarray(wb)


def build_program(stop_after=None, dbg=False):
    nc = bass.Bass("TRN2", target_bir_lowering=False)
    es = ExitStack()

    def din(name, shape, dt=F32):
        return nc.dram_tensor(name, list(shape), dt, kind="ExternalInput").ap()

    x_d = din("x", [S, D])
    cT_d = din("cT", [128, 8])
    wada_d = din("w_ada", [D, 6 * D])
    bada_d = din("b_ada", [1, 6 * D])
    wa_d = din("w_a", [D, NWA])
    wb_d = din("w_b", [D, NWB])
    sink_d = din("sink", [1, 8])
    biasu_d = din("biasu", [8, 128, 7 * 128])
    goa_d = din("g_out_a", [1, 512])
    gob_d = din("g_out_b", [1, 512])
    wout_d = din("w_out", [D, D])
    wr_d = din("w_router", [D, NE])
    br_d = din("b_router", [1, NE])
    if stop_after is None:
        wgu_d = din("w_gate_up", [NE * D, 2 * DFF])
        bgu_d = din("b_gate_up", [NE * 128, 16])
        wd_d = din("w_down", [NE * DFF, D])
        bd_d = din("b_down", [NE, D])
    gfin_d = din("g_final", [1, D])
    cos_d = din("cosT", [128, S])
    sin_d = din("sinT", [128, S])
    maska_d = din("maska", [128, 3 * 128])
    maskb_d = din("maskb", [_NVAR, 128, 7 * 128])
    ident_d = din("ident", [128, 128])
    tri_d = din("tri", [128, 128])
    rowid_d = din("rowid", [128, 8])
    blkth_d = din("blkth", [128, NBLK * NE])
    out_d = nc.dram_tensor("out", [S, D], F32, kind="ExternalOutput").ap()

    def dscr(name, shape, dt):
        kind = "ExternalOutput" if (dbg and name not in ("xs_s", "ys_s")) else "Internal"
        return nc.dram_tensor(name, list(shape), dt, kind=kind).ap()

    mixa_d = dscr("mixa_s", [S, 512], BF16)
    mixb_d = dscr("mixb_s", [S, 512], BF16)
    x1_d = dscr("x1_s", [S, D], F32)
    h2_d = dscr("h2_s", [S, D], BF16)
    if stop_after not in ("A", "B"):
        xs_d = dscr("xs_s", [NSLOT, D], BF16)
        ys_d = dscr("ys_s", [NSLOT, D], F32)
    dbg_d = {}
    if dbg:
        dbg_d["qta"] = nc.dram_tensor("dbg_qta", [128, 6 * S], BF16, kind="ExternalOutput").ap()
        dbg_d["gw"] = nc.dram_tensor("dbg_gw", [128, NQB * NE], F32, kind="ExternalOutput").ap()
        dbg_d["dsel"] = nc.dram_tensor("dbg_dsel", [128, NQB * 4], I32, kind="ExternalOutput").ap()
        dbg_d["blke"] = nc.dram_tensor("dbg_blke", [128, NBLK], F32, kind="ExternalOutput").ap()
        dbg_d["mod"] = nc.dram_tensor("dbg_mod", [128, 6 * D], F32, kind="ExternalOutput").ap()

    sc = Sched(nc, es)
    dbg_keys = []

    DUMPS = dict(ptA=([128, 384], BF16), poA=([128, 1024], F32), vpa=([128, NQB * 2 * 65], BF16), esink=([128, 8], F32),
                 goa=([128, 512], F32), oaA=([128, 512], F32), denA=([128, 8], F32), ssqA=([128, 1], F32))
    dump_d = {k: nc.dram_tensor("dbg_" + k, v[0], v[1], kind="ExternalOutput").ap() for k, v in DUMPS.items()} if dbg else {}

    def dump(name, ap, shape, dt, reads):
        if not dbg:
            return
        d = dump_d[name]
        sc.dma("sp", lambda: nc.sync.dma_start(out=d, in_=ap), reads=reads, writes=["dbg_" + name])
        dbg_keys.append("dbg_" + name)
    A = sc.add
    T, V, G, ACT, SP = nc.tensor, nc.vector, nc.gpsimd, nc.scalar, nc.sync

    def sb(stack, name, shape, dt=F32):
        return stack.enter_context(nc.sbuf_tensor("s_" + name, list(shape), dt))

    PS = [es.enter_context(nc.psum_tensor(f"ps{i}", [128, 1024], F32)) for i in range(4)]

    def bank(i):
        return PS[i // 2][:, (i % 2) * 512:(i % 2 + 1) * 512], ("ps", i)

    ident_f = sb(es, "ident_f", [128, 128], F32)
    ident_b = sb(es, "ident_b", [128, 128], BF16)
    ones_f = sb(es, "ones_f", [128, 128], F32)
    mod = sb(es, "mod", [128, 6 * D], F32)
    epsb = sb(es, "epsb", [128, 1], F32)
    sc.dma("sp", lambda: SP.dma_start(out=ident_f[:], in_=ident_d[:, :]), writes=["ident_f"])
    sc.dma("pool", lambda: G.dma_start(out=ident_b[:], in_=ident_d[:, :]), writes=["ident_b"])
    A("dve", lambda: V.memset(ones_f[:], 1.0), writes=["ones_f"])
    A("dve", lambda: V.memset(epsb[:], EPS), writes=["epsb"])

    with ExitStack() as p0:
        cT = sb(p0, "cT", [128, 8], F32)
        cact = sb(p0, "cact", [128, 8], F32)
        csig = sb(p0, "csig", [128, 8], F32)
        crep = sb(p0, "crep", [128, 8 * 128], F32)
        bada = sb(p0, "bada", [1, 6 * D], F32)
        wsl = [sb(p0, f"wsl{i}", [128, 8 * 512], F32) for i in range(2)]
        sc.dma("sp", lambda: SP.dma_start(out=cT[:], in_=cT_d[:, :]), writes=["cT"])
        sc.dma("sp", lambda: SP.dma_start(out=bada[:], in_=bada_d[:, :]), writes=["bada"])
        A("act", lambda: ACT.activation(out=csig[:], in_=cT[:], func=AF.Sigmoid), reads=["cT"], writes=["csig"])
        A("dve", lambda: V.tensor_tensor(out=cact[:], in0=cT[:], in1=csig[:], op=ALU.mult), reads=["cT", "csig"], writes=["cact"])
        A("dve", lambda: V.tensor_copy(out=crep[:].rearrange("p (c m) -> p c m", m=128),
                                       in_=cact[:].unsqueeze(2).to_broadcast([128, 8, 128])),
          reads=["cact"], writes=["crep"])
        for n in range(12):
            slot = n % 2
            w_t = wsl[slot]
            sc.dma("sp", lambda w_t=w_t, n=n: SP.dma_start(
                out=w_t[:].rearrange("p (c n) -> p c n", n=512),
                in_=wada_d[:, n * 512:(n + 1) * 512].rearrange("(c p) n -> p c n", p=128)),
                writes=[("wsl", slot)])
            pb, pk = bank(n % 2)
            for c in range(8):
                A("pe", lambda pb=pb, w_t=w_t, c=c: T.matmul(pb, lhsT=crep[:, c * 128:(c + 1) * 128],
                                                             rhs=w_t[:, c * 512:(c + 1) * 512], start=(c == 0), stop=False),
                  reads=["crep", ("wsl", slot)], writes=[pk])
            A("pe", lambda pb=pb, n=n: T.matmul(pb, lhsT=ones_f[0:1, :], rhs=bada[0:1, n * 512:(n + 1) * 512],
                                                start=False, stop=True),
              reads=["ones_f", "bada"], writes=[pk])
            if (n // 2) % 3 == 1:
                A("dve", lambda pb=pb, n=n: V.tensor_scalar(out=mod[:, n * 512:(n + 1) * 512], in0=pb, scalar1=1.0,
                                                            scalar2=None, op0=ALU.add), reads=[pk], writes=[("mod", n)])
            else:
                A("act", lambda pb=pb, n=n: ACT.copy(out=mod[:, n * 512:(n + 1) * 512], in_=pb), reads=[pk], writes=[("mod", n)])
        sc.barrier()
    MODK = [("mod", n) for n in range(12)]
    shift_m, scale1_m, gate_m = mod[:, 0:D], mod[:, D:2 * D], mod[:, 2 * D:3 * D]
    shift_f, scale1_f, gate_f = mod[:, 3 * D:4 * D], mod[:, 4 * D:5 * D], mod[:, 5 * D:6 * D]
    if dbg:
        sc.dma("sp", lambda: SP.dma_start(out=dbg_d["mod"][:, :], in_=mod[:]), reads=MODK, writes=["dbg_mod"])

    def rmsnorm_mod(stack_tiles, src, src_keys, scale1, shift, out_bf=None, out_f32=None, out_keys=(), tag=""):
        junk, ssq, rstd, tmp = stack_tiles
        A("act", lambda: ACT.activation(out=junk[:], in_=src, func=AF.Square, accum_out=ssq[:]),
          reads=list(src_keys), writes=["junk" + tag, "ssq" + tag])
        A("dve", lambda: V.tensor_scalar(out=rstd[:], in0=ssq[:], scalar1=1.0 / D, scalar2=EPS, op0=ALU.mult, op1=ALU.add),
          reads=["ssq" + tag], writes=["rstd" + tag])
        A("act", lambda: ACT.activation(out=rstd[:], in_=rstd[:], func=AF.Ln), reads=["rstd" + tag], writes=["rstd" + tag])
        A("act", lambda: ACT.activation(out=rstd[:], in_=rstd[:], func=AF.Exp, scale=-0.5), reads=["rstd" + tag], writes=["rstd" + tag])
        A("dve", lambda: V.scalar_tensor_tensor(out=tmp[:], in0=src, scalar=rstd[:, 0:1], in1=scale1, op0=ALU.mult, op1=ALU.mult),
          reads=list(src_keys) + ["rstd" + tag] + MODK, writes=["tmp" + tag])
        if out_f32 is not None:
            A("dve", lambda: V.tensor_tensor(out=out_f32, in0=tmp[:], in1=shift, op=ALU.add),
              reads=["tmp" + tag] + MODK, writes=list(out_keys))
            if out_bf is not None:
                A("act", lambda: ACT.copy(out=out_bf, in_=out_f32), reads=list(out_keys), writes=[k + ("bf",) for k in out_keys])
        else:
            A("dve", lambda: V.tensor_tensor(out=out_bf, in0=tmp[:], in1=shift, op=ALU.add),
              reads=["tmp" + tag] + MODK, writes=list(out_keys))

    def projection_pass(ps_, wmat_d, ncols, ngroups, emit_group, emit_v, vcol0, nvcols, tag):
        wsb = sb(ps_, "wsb" + tag, [128, 8 * ncols], BF16)
        w3 = wsb[:].rearrange("p (c n) -> p c n", n=ncols)
        for c in range(8):
            sc.dma("pool", lambda c=c: G.dma_start(out=w3[:, c, :], in_=wmat_d[c * 128:(c + 1) * 128, :]),
                   writes=[("wsb" + tag, c)])
        WK = [("wsb" + tag, c) for c in range(8)]
        xbl = [sb(ps_, f"xbl{tag}{i}", [128, D], F32) for i in range(2)]
        hbf = [sb(ps_, f"hbf{tag}{i}", [128, D], BF16) for i in range(2)]
        hT = [sb(ps_, f"hT{tag}{i}", [128, 8 * 512], BF16) for i in range(2)]
        tiles = [(sb(ps_, f"junk{tag}{j}", [128, D], BF16), sb(ps_, f"ssq{tag}{j}", [128, 1], F32),
                  sb(ps_, f"rstd{tag}{j}", [128, 1], F32), sb(ps_, f"tmp{tag}{j}", [128, D], F32)) for j in range(2)]
        for tc in range(8):
            hslot = tc % 2
            hT3 = hT[hslot][:].rearrange("p (c t) -> p c t", t=512)
            for sub in range(4):
                i = tc * 4 + sub
                xs_ = i % 2
                sc.dma("sp", lambda i=i, xs_=xs_: SP.dma_start(out=xbl[xs_][:], in_=x_d[i * 128:(i + 1) * 128, :]),
                       writes=[("xbl" + tag, xs_)])
                rmsnorm_mod(tiles[xs_], xbl[xs_][:], [("xbl" + tag, xs_)], scale1_m, shift_m, out_bf=hbf[xs_][:],
                            out_keys=[("hbf" + tag, xs_)], tag=tag + str(xs_))
                pbT = PS[0][:, (i % 2) * 512:(i % 2 + 1) * 512].bitcast(BF16)
                pkT = ("ps", i % 2)
                for c in range(8):
                    A("pe", lambda c=c, pbT=pbT, xs_=xs_: T.transpose(out=pbT[:, c * 128:(c + 1) * 128],
                                                                      in_=hbf[xs_][:, c * 128:(c + 1) * 128], identity=ident_b[:]),
                      reads=[("hbf" + tag, xs_), "ident_b"], writes=[pkT])
                A("act", lambda pbT=pbT, hT3=hT3, sub=sub: ACT.copy(out=hT3[:, :, sub * 128:(sub + 1) * 128],
                                                                    in_=pbT.rearrange("p (c t) -> p c t", t=128)),
                  reads=[pkT], writes=[("hT" + tag, hslot, sub)])
                pv, pvk = bank(2 + (i % 2))
                for c in range(8):
                    A("pe", lambda c=c, pv=pv, hT3=hT3, sub=sub: T.matmul(
                        pv[:, 0:nvcols], lhsT=hT3[:, c, sub * 128:(sub + 1) * 128], rhs=w3[:, c, vcol0:vcol0 + nvcols],
                        start=(c == 0), stop=(c == 7)),
                      reads=[("hT" + tag, hslot, sub)] + WK, writes=[pvk])
                emit_v(i, pv, pvk)
            HK = [("hT" + tag, hslot, s_) for s_ in range(4)]
            emit_group(tc, hT3, HK, w3, WK)
        return

    with ExitStack() as pa:
        qTa = sb(pa, "qTa", [128, 6 * S], BF16)
        qTa3 = qTa[:].rearrange("p (g t) -> p g t", t=S)
        vpa = sb(pa, "vpa", [128, NQB * 2 * 65], BF16)
        vpa4 = vpa[:].rearrange("p (i g d) -> p i g d", g=2, d=65)
        A("pool", lambda: G.memset(vpa[:], 1.0), writes=["vpa_init"])
        with ExitStack() as pa1:
            cosT = sb(pa1, "cosT", [128, S], F32)
            sinT = sb(pa1, "sinT", [128, S], F32)
            sc.dma("sp", lambda: SP.dma_start(out=cosT[:], in_=cos_d[:, :]), writes=["cosT"])
            sc.dma("sp", lambda: SP.dma_start(out=sinT[:], in_=sin_d[:, :]), writes=["sinT"])
            rt = [sb(pa1, f"rt{i}", [128, 512], F32) for i in range(4)]

            def emit_v_a(i, pv, pvk):
                A("act", lambda: ACT.copy(out=vpa4[:, i, :, 0:64], in_=pv[:, 0:128].rearrange("p (g d) -> p g d", d=64)),
                  reads=[pvk, "vpa_init"], writes=[("vpa", i)])

            def emit_group_a(tc, hT3, HK, w3, WK):
                for g in range(6):
                    c0 = g * 128 if g < 4 else 1024 + (g - 4) * 256
                    c1 = 512 + g * 128 if g < 4 else 1024 + 512 + (g - 4) * 256
                    if g >= 4:
                        c0 = 1024 + (g - 4) * 128
                        c1 = 1024 + 256 + (g - 4) * 128
                    pq, pqk = bank(4 + (g % 2) * 2)
                    pp, ppk = bank(5 + (g % 2) * 2)
                    for (pb, pk, col) in ((pq, pqk, c0), (pp, ppk, c1)):
                        for c in range(8):
                            A("pe", lambda pb=pb, c=c, col=col: T.matmul(pb, lhsT=w3[:, c, col:col + 128], rhs=hT3[:, c, :],
                                                                         start=(c == 0), stop=(c == 7)),
                              reads=HK + WK, writes=[pk])
                    r0, r1 = rt[(g % 2) * 2], rt[(g % 2) * 2 + 1]
                    k0, k1 = ("rt", (g % 2) * 2), ("rt", (g % 2) * 2 + 1)
                    tsl = slice(tc * 512, (tc + 1) * 512)
                    A("dve", lambda pq=pq, r0=r0, tsl=tsl: V.tensor_tensor(out=r0[:], in0=pq, in1=cosT[:, tsl], op=ALU.mult),
                      reads=[pqk, "cosT"], writes=[k0])
                    A("dve", lambda pp=pp, r1=r1, tsl=tsl: V.tensor_tensor(out=r1[:], in0=pp, in1=sinT[:, tsl], op=ALU.mult),
                      reads=[ppk, "sinT"], writes=[k1])
                    A("pool", lambda r0=r0, r1=r1, g=g, tsl=tsl: G.tensor_tensor(out=qTa3[:, g, tsl], in0=r0[:], in1=r1[:], op=ALU.add),
                      reads=[k0, k1], writes=[("qTa", g, tc)])

            projection_pass(pa1, wa_d, NWA, 12, emit_group_a, emit_v_a, 1536, 128, "A")
            sc.barrier()
        if dbg:
            sc.dma("sp", lambda: SP.dma_start(out=dbg_d["qta"][:, :], in_=qTa[:]),
                   reads=[("qTa", g, tc) for g in range(6) for tc in range(8)], writes=["dbg_qta"])

        with ExitStack() as pa2:
            if stop_after not in ("A", "B"):
                zt = sb(pa2, "zt", [128, 4 * D], BF16)
                A("pool", lambda: G.memset(zt[:], 0.0), writes=["zt"])
                for b in range(NBLK):
                    sc.dma("sp", lambda b=b: SP.dma_start(out=xs_d[b * MB:(b + 1) * MB, :].rearrange("(s p) d -> p s d", p=128),
                                                        in_=zt[:].rearrange("p (s d) -> p s d", d=D)), reads=["zt"], writes=["xs_d"])
            maska = sb(pa2, "maska", [128, 384], BF16)
            sc.dma("pool", lambda: G.dma_start(out=maska[:], in_=maska_d[:, :]), writes=["maska"])
            esink = sb(pa2, "esink", [128, 8], F32)
            sc.dma("sp", lambda: SP.dma_start(out=esink[:], in_=sink_d[:, :].partition_broadcast(128)), writes=["esink0"])
            A("act", lambda: ACT.activation(out=esink[:], in_=esink[:], func=AF.Exp), reads=["esink0"], writes=["esink"])
            goa = sb(pa2, "goa", [128, 512], F32)
            sc.dma("sp", lambda: SP.dma_start(out=goa[:], in_=goa_d[:, :].partition_broadcast(128)), writes=["goa"])
            pt = [sb(pa2, f"pta{i}", [128, 384], BF16) for i in range(3)]
            den = sb(pa2, "dena", [128, 8], F32)
            oa = sb(pa2, "oa", [128, 512], F32)
            junk2 = sb(pa2, "junk2a", [128, 512], BF16)
            ssq2 = sb(pa2, "ssq2a", [128, 1], F32)
            mixa = [sb(pa2, f"mixa{i}", [128, 512], BF16) for i in range(2)]
            for i in range(NQB):
                tcq = i // 4
                ms = [m for m in range(3) if 0 <= i + m - 1 < NQB]
                po = PS[3 - (i % 2)]
                pok = ("ps", 6 - 2 * (i % 2))
                po4 = po[:].rearrange("p (b x) -> p b x", b=2)[:, :, 0:260].rearrange("p b (h d) -> p b h d", d=65)
                def qk_a(h):
                    g, off = h // 2, (h % 2) * 64
                    kg = 4 + h // 4
                    pst, pstk = bank(h % 3)
                    for m in ms:
                        kb = i + m - 1
                        A("pe", lambda pst=pst, m=m, kb=kb, g=g, off=off, kg=kg, i=i: T.matmul(
                            pst[:, m * 128:(m + 1) * 128], lhsT=qTa3[off:off + 64, kg, kb * 128:(kb + 1) * 128],
                            rhs=qTa3[off:off + 64, g, i * 128:(i + 1) * 128], start=True, stop=False),
                          reads=[("qTa", kg, kb // 4), ("qTa", g, tcq)], writes=[pstk])
                        A("pe", lambda pst=pst, m=m: T.matmul(pst[:, m * 128:(m + 1) * 128], lhsT=ident_b[:],
                                                              rhs=maska[:, m * 128:(m + 1) * 128], start=False, stop=True),
                          reads=["ident_b", "maska"], writes=[pstk])

                qk_a(0)
                for h in range(8):
                    if h + 1 < 8:
                        qk_a(h + 1)
                    pst, pstk = bank(h % 3)
                    ptt = pt[h % 3]
                    ptk = ("pta", h % 3)
                    lo, hi = ms[0] * 128, (ms[-1] + 1) * 128
                    A("act", lambda pst=pst, ptt=ptt, lo=lo, hi=hi: ACT.activation(out=ptt[:, lo:hi], in_=pst[:, lo:hi],
                                                                                   func=AF.Exp, scale=0.125),
                      reads=[pstk], writes=[ptk])
                    for m in ms:
                        kb = i + m - 1
                        A("pe", lambda ptt=ptt, m=m, kb=kb, h=h, ms=ms, po=po: T.matmul(
                            po[:, (h // 4) * 512 + (h % 4) * 65:(h // 4) * 512 + (h % 4) * 65 + 65],
                            lhsT=ptt[:, m * 128:(m + 1) * 128], rhs=vpa4[:, kb, h // 4, :],
                            start=(m == ms[0]), stop=(m == ms[-1])),
                          reads=[ptk, ("vpa", kb)], writes=[pok])
                if i == 4:
                    dump("ptA", pt[7 % 3][:], [128, 384], BF16, [("pta", 7 % 3)])
                    if dbg:
                        podbg = sb(pa2, "podbg", [128, 1024], F32)
                        A("act", lambda: ACT.copy(out=podbg[:], in_=po[:]), reads=[pok], writes=["podbg"])
                        dump("poA", podbg[:], [128, 1024], F32, ["podbg"])
                    dump("vpa", vpa[:], [128, NQB * 2 * 65], BF16, [("vpa", kk) for kk in range(NQB)])
                    dump("esink", esink[:], [128, 8], F32, ["esink"])
                    dump("goa", goa[:], [128, 512], F32, ["goa"])
                A("dve", lambda po4=po4: V.tensor_tensor(out=den[:].rearrange("p (b h) -> p b h", b=2), in0=po4[:, :, :, 64],
                                                         in1=esink[:].rearrange("p (b h) -> p b h", b=2), op=ALU.add),
                  reads=[pok, "esink"], writes=["dena"])
                A("dve", lambda: V.reciprocal(out=den[:], in_=den[:]), reads=["dena"], writes=["dena"])
                A("dve", lambda po4=po4: V.tensor_tensor(
                    out=oa[:].rearrange("p (b h d) -> p b h d", b=2, d=64), in0=po4[:, :, :, 0:64],
                    in1=den[:].rearrange("p (b h) -> p b h", b=2).unsqueeze(3).to_broadcast([128, 2, 4, 64]), op=ALU.mult),
                  reads=[pok, "dena"], writes=["oa"])
                A("act", lambda: ACT.activation(out=junk2[:], in_=oa[:], func=AF.Square, accum_out=ssq2[:]),
                  reads=["oa"], writes=["junk2a", "ssq2a"])
                A("dve", lambda: V.tensor_scalar(out=ssq2[:], in0=ssq2[:], scalar1=1.0 / 512, scalar2=EPS, op0=ALU.mult, op1=ALU.add),
                  reads=["ssq2a"], writes=["ssq2a"])
                A("act", lambda: ACT.activation(out=ssq2[:], in_=ssq2[:], func=AF.Ln), reads=["ssq2a"], writes=["ssq2a"])
                A("act", lambda: ACT.activation(out=ssq2[:], in_=ssq2[:], func=AF.Exp, scale=-0.5), reads=["ssq2a"], writes=["ssq2a"])
                if i == 4:
                    dump("oaA", oa[:], [128, 512], F32, ["oa"])
                    dump("denA", den[:], [128, 8], F32, ["dena"])
                    dump("ssqA", ssq2[:], [128, 1], F32, ["ssq2a"])
                mx = mixa[i % 2]
                A("dve", lambda mx=mx: V.scalar_tensor_tensor(out=mx[:], in0=oa[:], scalar=ssq2[:, 0:1], in1=goa[:],
                                                              op0=ALU.mult, op1=ALU.mult),
                  reads=["oa", "ssq2a", "goa"], writes=[("mixa", i % 2)])
                sc.dma("sp", lambda mx=mx, i=i: SP.dma_start(out=mixa_d[i * 128:(i + 1) * 128, :], in_=mx[:]),
                       reads=[("mixa", i % 2)], writes=[("mixa_d", i)])
            sc.barrier()
    if stop_after == "A":
        sc.finish([("mixa_d", i) for i in range(NQB)] + ["dbg_qta", "dbg_mod"] + dbg_keys)
        es.close()
        return nc

    with ExitStack() as pb_:
        qTb = sb(pb_, "qTb", [128, 8 * S], BF16)
        qTb3 = qTb[:].rearrange("p (g t) -> p g t", t=S)
        vpb = sb(pb_, "vpb", [128, NQB * 8 * 65], BF16)
        vpb4 = vpb[:].rearrange("p (i g d) -> p i g d", g=8, d=65)
        A("pool", lambda: G.memset(vpb[:], 1.0), writes=["vpb_init"])
        with ExitStack() as pb1:
            def emit_v_b(i, pv, pvk):
                A("act", lambda: ACT.copy(out=vpb4[:, i, :, 0:64], in_=pv[:, 0:512].rearrange("p (g d) -> p g d", d=64)),
                  reads=[pvk, "vpb_init"], writes=[("vpb", i)])

            def emit_group_b(tc, hT3, HK, w3, WK):
                for g in range(8):
                    pq, pqk = bank(4 + g % 4)
                    for c in range(8):
                        A("pe", lambda pq=pq, c=c, g=g: T.matmul(pq, lhsT=w3[:, c, g * 128:(g + 1) * 128], rhs=hT3[:, c, :],
                                                                 start=(c == 0), stop=(c == 7)),
                          reads=HK + WK, writes=[pqk])
                    tsl = slice(tc * 512, (tc + 1) * 512)
                    if g % 2 == 0:
                        A("dve", lambda pq=pq, g=g, tsl=tsl: V.tensor_copy(out=qTb3[:, g, tsl], in_=pq), reads=[pqk], writes=[("qTb", g, tc)])
                    else:
                        A("act", lambda pq=pq, g=g, tsl=tsl: ACT.copy(out=qTb3[:, g, tsl], in_=pq), reads=[pqk], writes=[("qTb", g, tc)])

            projection_pass(pb1, wb_d, NWB, 8, emit_group_b, emit_v_b, 1024, 512, "B")
            sc.barrier()

        with ExitStack() as pb2:
            maskb = sb(pb2, "maskb", [128, _NVAR * 896], BF16)
            for v in range(_NVAR):
                sc.dma("pool", lambda v=v: G.dma_start(out=maskb[:, v * 896:(v + 1) * 896], in_=maskb_d[v, :, :]), writes=[("maskb", v)])
            biasu = sb(pb2, "biasu", [128, 8 * 896], F32)
            for h in range(8):
                sc.dma("sp", lambda h=h: SP.dma_start(out=biasu[:, h * 896:(h + 1) * 896], in_=biasu_d[h, :, :]), writes=[("biasu", h)])
            gob = sb(pb2, "gob", [128, 512], F32)
            sc.dma("sp", lambda: SP.dma_start(out=gob[:], in_=gob_d[:, :].partition_broadcast(128)), writes=["gob"])
            tt = [sb(pb2, f"ttb{i}", [128, 896], F32) for i in range(2)]
            pt = [sb(pb2, f"ptb{i}", [128, 896], BF16) for i in range(2)]
            den = sb(pb2, "denb", [128, 8], F32)
            ob = sb(pb2, "ob", [128, 512], F32)
            junk2 = sb(pb2, "junk2b", [128, 512], BF16)
            ssq2 = sb(pb2, "ssq2b", [128, 1], F32)
            mixb = [sb(pb2, f"mixb{i}", [128, 512], BF16) for i in range(2)]
            for i in range(NQB):
                tcq = i // 4
                ms = _KBS[i]
                var = _VAR_OF[i]
                po = PS[3 - (i % 2)]
                pok = ("ps", 6 - 2 * (i % 2))
                po4 = po[:].rearrange("p (b x) -> p b x", b=2)[:, :, 0:260].rearrange("p b (h d) -> p b h d", d=65)
                lo, hi = ms[0] * 128, (ms[-1] + 1) * 128
                def qk_b(h):
                    g, off, kg = h // 2, (h % 2) * 64, 4 + h // 2
                    sl = h % 2
                    pst = PS[sl]
                    pstk = [("ps", 2 * sl), ("ps", 2 * sl + 1)]
                    for m in ms:
                        kb = i + m - 3
                        A("pe", lambda pst=pst, m=m, kb=kb, g=g, off=off, kg=kg, i=i: T.matmul(
                            pst[:, m * 128:(m + 1) * 128], lhsT=qTb3[off:off + 64, kg, kb * 128:(kb + 1) * 128],
                            rhs=qTb3[off:off + 64, g, i * 128:(i + 1) * 128], start=True, stop=False),
                          reads=[("qTb", kg, kb // 4), ("qTb", g, tcq)], writes=pstk)
                        A("pe", lambda pst=pst, m=m, var=var: T.matmul(pst[:, m * 128:(m + 1) * 128], lhsT=ident_b[:],
                                                                       rhs=maskb[:, var * 896 + m * 128:var * 896 + (m + 1) * 128],
                                                                       start=False, stop=True),
                          reads=["ident_b", ("maskb", var)], writes=pstk)

                qk_b(0)
                for h in range(8):
                    if h + 1 < 8:
                        qk_b(h + 1)
                    sl = h % 2
                    pst = PS[sl]
                    pstk = [("ps", 2 * sl), ("ps", 2 * sl + 1)]
                    ttt, ptt = tt[sl], pt[sl]
                    A("dve", lambda pst=pst, ttt=ttt, h=h, lo=lo, hi=hi: V.scalar_tensor_tensor(
                        out=ttt[:, lo:hi], in0=pst[:, lo:hi], scalar=0.125, in1=biasu[:, h * 896 + lo:h * 896 + hi],
                        op0=ALU.mult, op1=ALU.add), reads=pstk + [("biasu", h)], writes=[("ttb", sl)])
                    A("act", lambda ttt=ttt, ptt=ptt, lo=lo, hi=hi: ACT.activation(out=ptt[:, lo:hi], in_=ttt[:, lo:hi], func=AF.Exp),
                      reads=[("ttb", sl)], writes=[("ptb", sl)])
                    for m in ms:
                        kb = i + m - 3
                        A("pe", lambda ptt=ptt, m=m, kb=kb, h=h, ms=ms, po=po: T.matmul(
                            po[:, (h // 4) * 512 + (h % 4) * 65:(h // 4) * 512 + (h % 4) * 65 + 65],
                            lhsT=ptt[:, m * 128:(m + 1) * 128], rhs=vpb4[:, kb, h, :],
                            start=(m == ms[0]), stop=(m == ms[-1])),
                          reads=[("ptb", sl), ("vpb", kb)], writes=[pok])
                A("dve", lambda po4=po4: V.reciprocal(out=den[:].rearrange("p (b h) -> p b h", b=2), in_=po4[:, :, :, 64]),
                  reads=[pok], writes=["denb"])
                A("dve", lambda po4=po4: V.tensor_tensor(
                    out=ob[:].rearrange("p (b h d) -> p b h d", b=2, d=64), in0=po4[:, :, :, 0:64],
                    in1=den[:].rearrange("p (b h) -> p b h", b=2).unsqueeze(3).to_broadcast([128, 2, 4, 64]), op=ALU.mult),
                  reads=[pok, "denb"], writes=["ob"])
                A("act", lambda: ACT.activation(out=junk2[:], in_=ob[:], func=AF.Square, accum_out=ssq2[:]),
                  reads=["ob"], writes=["junk2b", "ssq2b"])
                A("dve", lambda: V.tensor_scalar(out=ssq2[:], in0=ssq2[:], scalar1=1.0 / 512, scalar2=EPS, op0=ALU.mult, op1=ALU.add),
                  reads=["ssq2b"], writes=["ssq2b"])
                A("act", lambda: ACT.activation(out=ssq2[:], in_=ssq2[:], func=AF.Ln), reads=["ssq2b"], writes=["ssq2b"])
                A("act", lambda: ACT.activation(out=ssq2[:], in_=ssq2[:], func=AF.Exp, scale=-0.5), reads=["ssq2b"], writes=["ssq2b"])
                mx = mixb[i % 2]
                A("dve", lambda mx=mx: V.scalar_tensor_tensor(out=mx[:], in0=ob[:], scalar=ssq2[:, 0:1], in1=gob[:],
                                                              op0=ALU.mult, op1=ALU.mult),
                  reads=["ob", "ssq2b", "gob"], writes=[("mixb", i % 2)])
                sc.dma("sp", lambda mx=mx, i=i: SP.dma_start(out=mixb_d[i * 128:(i + 1) * 128, :], in_=mx[:]),
                       reads=[("mixb", i % 2)], writes=[("mixb_d", i)])
            sc.barrier()
    if stop_after == "B":
        sc.finish([("mixa_d", i) for i in range(NQB)] + [("mixb_d", i) for i in range(NQB)] + ["dbg_qta", "dbg_mod"])
        es.close()
        return nc

    rt_ = ExitStack()
    lg_all = sb(rt_, "lg_all", [128, NQB * NE], F32)
    m8_all = sb(rt_, "m8_all", [128, NQB * 8], F32)
    lg3 = lg_all[:].rearrange("p (i e) -> p i e", e=NE)
    m83 = m8_all[:].rearrange("p (i k) -> p i k", k=8)
    with ExitStack() as pc:
        wout = sb(pc, "wout", [128, 8 * D], BF16)
        wout3 = wout[:].rearrange("p (c n) -> p c n", n=D)
        for c in range(8):
            sc.dma("pool", lambda c=c: G.dma_start(out=wout3[:, c, :], in_=wout_d[c * 128:(c + 1) * 128, :]), writes=[("wout", c)])
        WOK = [("wout", c) for c in range(8)]
        wr = sb(pc, "wr", [128, 8 * NE], F32)
        sc.dma("sp", lambda: SP.dma_start(out=wr[:].rearrange("p (c e) -> p c e", e=NE),
                                          in_=wr_d[:, :].rearrange("(c p) e -> p c e", p=128)), writes=["wr"])
        brt = sb(pc, "brt", [1, NE], F32)
        sc.dma("sp", lambda: SP.dma_start(out=brt[:], in_=br_d[:, :]), writes=["brt"])
        mixab = [sb(pc, f"mixab{i}", [128, D], BF16) for i in range(2)]
        xb_ = [sb(pc, f"xc{i}", [128, D], F32) for i in range(2)]
        mixT_ = [sb(pc, f"mixT{j}", [128, D], BF16) for j in range(2)]
        t1_ = [sb(pc, f"t1{j}", [128, D], F32) for j in range(2)]
        x1t = [sb(pc, f"x1t{i}", [128, D], F32) for i in range(2)]
        h2f_ = [sb(pc, f"h2f{j}", [128, D], F32) for j in range(2)]
        h2b = [sb(pc, f"h2b{i}", [128, D], BF16) for i in range(2)]
        h2T_ = [sb(pc, f"h2T{j}", [128, D], F32) for j in range(2)]
        tilesC_ = [(sb(pc, f"junkC{j}", [128, D], BF16), sb(pc, f"ssqC{j}", [128, 1], F32),
                    sb(pc, f"rstdC{j}", [128, 1], F32), sb(pc, f"tmpC{j}", [128, D], F32)) for j in range(2)]
        def c_loads(i):
            s2 = i % 2
            sc.dma("sp", lambda i=i, s2=s2: SP.dma_start(out=mixab[s2][:, 0:512], in_=mixa_d[i * 128:(i + 1) * 128, :]),
                   reads=[("mixa_d", i)], writes=[("mixab", s2, 0)])
            sc.dma("sp", lambda i=i, s2=s2: SP.dma_start(out=mixab[s2][:, 512:1024], in_=mixb_d[i * 128:(i + 1) * 128, :]),
                   reads=[("mixb_d", i)], writes=[("mixab", s2, 1)])
            sc.dma("sp", lambda i=i, s2=s2: SP.dma_start(out=xb_[s2][:], in_=x_d[i * 128:(i + 1) * 128, :]), writes=[("xc", s2)])

        def c_a(i):
            s2 = i % 2
            mixT, t1, h2f, h2T, tilesC = mixT_[s2], t1_[s2], h2f_[s2], h2T_[s2], tilesC_[s2]
            pbT = PS[0][:, s2 * 512:(s2 + 1) * 512].bitcast(BF16)
            for c in range(8):
                A("pe", lambda c=c, pbT=pbT, s2=s2: T.transpose(out=pbT[:, c * 128:(c + 1) * 128],
                                                                in_=mixab[s2][:, c * 128:(c + 1) * 128], identity=ident_b[:]),
                  reads=[("mixab", s2, 0), ("mixab", s2, 1), "ident_b"], writes=[("ps", s2)])
            A("act", lambda pbT=pbT, mixT=mixT: ACT.copy(out=mixT[:], in_=pbT), reads=[("ps", s2)], writes=[("mixT", s2)])
            for n in range(2):
                py, pyk = bank(2 + n)
                for c in range(8):
                    A("pe", lambda py=py, c=c, n=n, mixT=mixT: T.matmul(py, lhsT=mixT[:, c * 128:(c + 1) * 128],
                                                             rhs=wout3[:, c, n * 512:(n + 1) * 512], start=(c == 0), stop=(c == 7)),
                      reads=[("mixT", s2)] + WOK, writes=[pyk])
                A("dve", lambda py=py, n=n, t1=t1: V.tensor_tensor(out=t1[:, n * 512:(n + 1) * 512], in0=py,
                                                            in1=gate_m[:, n * 512:(n + 1) * 512], op=ALU.mult),
                  reads=[pyk] + MODK, writes=[("t1", s2, n)])
            xt = x1t[s2]
            A("dve", lambda xt=xt, s2=s2, t1=t1: V.tensor_tensor(out=xt[:], in0=t1[:], in1=xb_[s2][:], op=ALU.add),
              reads=[("t1", s2, 0), ("t1", s2, 1), ("xc", s2)], writes=[("x1t", s2)])
            sc.dma("sp", lambda xt=xt, i=i: SP.dma_start(out=x1_d[i * 128:(i + 1) * 128, :], in_=xt[:]),
                   reads=[("x1t", s2)], writes=[("x1_d", i)])
            rmsnorm_mod(tilesC, xt[:], [("x1t", s2)], scale1_f, shift_f, out_bf=h2b[s2][:], out_f32=h2f[:],
                        out_keys=[("h2f", s2)], tag="C" + str(s2))
            sc.dma("sp", lambda i=i, s2=s2: SP.dma_start(out=h2_d[i * 128:(i + 1) * 128, :], in_=h2b[s2][:]),
                   reads=[("h2f", s2, "bf")], writes=[("h2_d", i)])

        def c_b(i):
            s2 = i % 2
            mixT, t1, h2f, h2T, tilesC = mixT_[s2], t1_[s2], h2f_[s2], h2T_[s2], tilesC_[s2]
            for r_ in range(2):
                pt_, ptk_ = bank(4 + r_)
                for c4 in range(4):
                    c = r_ * 4 + c4
                    A("pe", lambda pt_=pt_, c=c, c4=c4, h2f=h2f: T.transpose(out=pt_[:, c4 * 128:(c4 + 1) * 128],
                                                                    in_=h2f[:, c * 128:(c + 1) * 128], identity=ident_f[:]),
                      reads=[("h2f", s2), "ident_f"], writes=[ptk_])
                if r_ == 0:
                    A("dve", lambda pt_=pt_, r_=r_, h2T=h2T: V.tensor_copy(out=h2T[:, r_ * 512:(r_ + 1) * 512], in_=pt_), reads=[ptk_], writes=[("h2T", s2, r_)])
                else:
                    A("act", lambda pt_=pt_, r_=r_, h2T=h2T: ACT.copy(out=h2T[:, r_ * 512:(r_ + 1) * 512], in_=pt_), reads=[ptk_], writes=[("h2T", s2, r_)])
            pl, plk = bank(6 + s2)
            for c in range(8):
                A("pe", lambda pl=pl, c=c, h2T=h2T: T.matmul(pl[:, 0:NE], lhsT=h2T[:, c * 128:(c + 1) * 128], rhs=wr[:, c * NE:(c + 1) * NE],
                                                    start=(c == 0), stop=False),
                  reads=[("h2T", s2, 0), ("h2T", s2, 1), "wr"], writes=[plk])
            A("pe", lambda pl=pl: T.matmul(pl[:, 0:NE], lhsT=ones_f[0:1, :], rhs=brt[0:1, :], start=False, stop=True),
              reads=["ones_f", "brt"], writes=[plk])
            A("dve", lambda pl=pl, i=i: V.tensor_copy(out=lg3[:, i, :], in_=pl[:, 0:NE]), reads=[plk], writes=[("lg", i)])
            A("dve", lambda i=i: V.max(out=m83[:, i, :], in_=lg3[:, i, :]), reads=[("lg", i)], writes=[("m8", i)])

        c_loads(0)
        for i in range(NQB):
            if i + 1 < NQB:
                c_loads(i + 1)
            c_a(i)
            if i >= 1:
                c_b(i - 1)
        c_b(NQB - 1)
        sc.barrier()
    LGK = [("lg", i) for i in range(NQB)] + [("m8", i) for i in range(NQB)]

    gw_all = sb(rt_, "gw_all", [128, NQB * NE], F32)
    gw3 = gw_all[:].rearrange("p (i e) -> p i e", e=NE)
    dsel_i = sb(rt_, "dsel_i", [128, 4 * NQB], I32)
    gk = sb(rt_, "gk", [128, 4 * NQB], F32)
    idxw_i = sb(rt_, "idxw_i", [128, NBLK * 8], I32)
    idxg_i = sb(rt_, "idxg_i", [128, NBLK * 8], I32)
    idxb_i = sb(rt_, "idxb_i", [128, NBLK], I32)
    with ExitStack() as pr:
        tri = sb(pr, "tri", [128, 128], F32)
        sc.dma("sp", lambda: SP.dma_start(out=tri[:], in_=tri_d[:, :]), writes=["tri"])
        rowid = sb(pr, "rowid", [128, 8], F32)
        sc.dma("sp", lambda: SP.dma_start(out=rowid[:], in_=rowid_d[:, :]), writes=["rowid"])
        blkth = sb(pr, "blkth", [128, NBLK * NE], F32)
        sc.dma("sp", lambda: SP.dma_start(out=blkth[:], in_=blkth_d[:, :]), writes=["blkth"])
        msk = sb(pr, "msk", [128, NQB * NE], F32)
        msk3 = msk[:].rearrange("p (i e) -> p i e", e=NE)
        ex = sb(pr, "ex", [128, NQB * NE], F32)
        ex3 = ex[:].rearrange("p (i e) -> p i e", e=NE)
        ssum = sb(pr, "ssum", [128, NQB], F32)
        pos = sb(pr, "pos", [128, NQB * NE], F32)
        pos3 = pos[:].rearrange("p (i e) -> p i e", e=NE)
        oh = sb(pr, "oh", [128, NQB * NE], F32)
        oh3 = oh[:].rearrange("p (i e) -> p i e", e=NE)
        prod = sb(pr, "prod", [128, NQB * NE], F32)
        prod3 = prod[:].rearrange("p (i e) -> p i e", e=NE)
        dself = sb(pr, "dself", [128, 4 * NQB], F32)
        cnt = sb(pr, "cnt", [128, NE], F32)
        cs = [sb(pr, f"cs{i}", [128, NE], F32) for i in range(2)]
        padded = sb(pr, "padded", [128, NE], F32)
        pstart = sb(pr, "pstart", [128, NE], F32)
        cmpb = sb(pr, "cmpb", [128, NBLK * NE], F32)
        blke = sb(pr, "blke", [128, NBLK], F32)
        idxwf = sb(pr, "idxwf", [128, NBLK * 8], F32)
        idxbf = sb(pr, "idxbf", [128, NBLK], F32)

        A("dve", lambda: V.tensor_tensor(out=msk3, in0=lg3, in1=m83[:, :, 3:4].to_broadcast([128, NQB, NE]), op=ALU.is_ge),
          reads=LGK, writes=["msk"])
        mskb = sb(pr, "mskb", [128, NQB * NE], BF16)
        mskb3 = mskb[:].rearrange("p (i e) -> p i e", e=NE)
        ones_b = sb(pr, "ones_b", [128, 128], BF16)
        tri_b = sb(pr, "tri_b", [128, 128], BF16)
        A("act", lambda: ACT.copy(out=mskb[:], in_=msk[:]), reads=["msk"], writes=["mskb"])
        A("pool", lambda: G.memset(ones_b[:], 1.0), writes=["ones_b"])
        sc.dma("pool", lambda: G.dma_start(out=tri_b[:], in_=tri_d[:, :]), writes=["tri_b"])
        A("dve", lambda: V.tensor_tensor(out=ex3, in0=lg3, in1=m83[:, :, 0:1].to_broadcast([128, NQB, NE]), op=ALU.subtract),
          reads=LGK, writes=["ex"])
        A("act", lambda: ACT.activation(out=ex[:], in_=ex[:], func=AF.Exp), reads=["ex"], writes=["ex"])
        A("dve", lambda: V.tensor_tensor(out=ex[:], in0=ex[:], in1=msk[:], op=ALU.mult), reads=["ex", "msk"], writes=["ex"])
        A("dve", lambda: V.reduce_sum(out=ssum[:], in_=ex3, axis=AX.X), reads=["ex"], writes=["ssum"])
        A("dve", lambda: V.reciprocal(out=ssum[:], in_=ssum[:]), reads=["ssum"], writes=["ssum"])
        A("dve", lambda: V.tensor_tensor(out=gw3, in0=ex3, in1=ssum[:].unsqueeze(2).to_broadcast([128, NQB, NE]), op=ALU.mult),
          reads=["ex", "ssum"], writes=["gw"])
        for half in range(2):
            pp_, ppk_ = bank(half)
            for ii in range(16):
                i = half * 16 + ii
                for j in range(i):
                    A("pe", lambda pp_=pp_, ii=ii, j=j: T.matmul(pp_[:, ii * NE:(ii + 1) * NE], lhsT=ones_b[:], rhs=mskb3[:, j, :],
                                                                 start=(j == 0), stop=False),
                      reads=["ones_b", "mskb"], writes=[ppk_])
                A("pe", lambda pp_=pp_, ii=ii, i=i: T.matmul(pp_[:, ii * NE:(ii + 1) * NE], lhsT=tri_b[:], rhs=mskb3[:, i, :],
                                                             start=(i == 0), stop=True),
                  reads=["tri_b", "mskb"], writes=[ppk_])
            A("dve", lambda pp_=pp_, half=half: V.tensor_copy(out=pos[:, half * 512:(half + 1) * 512], in_=pp_),
              reads=[ppk_], writes=[("pos", half)])
        pc_, pck_ = bank(2)
        for j in range(NQB):
            A("pe", lambda j=j: T.matmul(pc_[:, 0:NE], lhsT=ones_b[:], rhs=mskb3[:, j, :], start=(j == 0), stop=(j == NQB - 1)),
              reads=["ones_b", "mskb"], writes=[pck_])
        A("dve", lambda: V.tensor_copy(out=cnt[:], in_=pc_[:, 0:NE]), reads=[pck_], writes=["cnt"])
        nbt = sb(pr, "nbt", [128, NE * 8], F32)
        A("dve", lambda: V.tensor_tensor(out=nbt[:].rearrange("p (e j) -> p e j", j=8),
                                         in0=cnt[:].unsqueeze(2).to_broadcast([128, NE, 8]),
                                         in1=blkth[:, 0:8 * NE].rearrange("p (b e) -> p e b", e=NE), op=ALU.is_gt),
          reads=["cnt", "blkth"], writes=["nbt"])
        A("dve", lambda: V.reduce_sum(out=padded[:], in_=nbt[:].rearrange("p (e j) -> p e j", j=8), axis=AX.X), reads=["nbt"], writes=["padded"])
        A("dve", lambda: V.tensor_scalar(out=padded[:], in0=padded[:], scalar1=float(MB), scalar2=None, op0=ALU.mult),
          reads=["padded"], writes=["padded"])
        A("dve", lambda: V.tensor_copy(out=cs[0][:], in_=padded[:]), reads=["padded"], writes=[("cs", 0)])
        cur = 0
        for sft in (1, 2, 4, 8, 16):
            nxt = 1 - cur
            A("dve", lambda cur=cur, nxt=nxt, sft=sft: V.tensor_copy(out=cs[nxt][:, 0:sft], in_=cs[cur][:, 0:sft]),
              reads=[("cs", cur)], writes=[("cs", nxt)])
            A("dve", lambda cur=cur, nxt=nxt, sft=sft: V.tensor_tensor(out=cs[nxt][:, sft:NE], in0=cs[cur][:, sft:NE],
                                                                       in1=cs[cur][:, 0:NE - sft], op=ALU.add),
              reads=[("cs", cur), ("cs", nxt)], writes=[("cs", nxt)])
            cur = nxt
        pend = cs[cur]
        pendk = ("cs", cur)
        A("dve", lambda: V.tensor_tensor(out=pstart[:], in0=pend[:], in1=padded[:], op=ALU.subtract), reads=[pendk, "padded"], writes=["pstart"])
        A("dve", lambda: V.tensor_tensor(out=pos3, in0=pos3, in1=pstart[:].unsqueeze(1).to_broadcast([128, NQB, NE]), op=ALU.add),
          reads=[("pos", 0), ("pos", 1), "pstart"], writes=["dest"])
        for k in range(4):
            A("dve", lambda k=k: V.tensor_tensor(out=oh3, in0=lg3, in1=m83[:, :, k:k + 1].to_broadcast([128, NQB, NE]), op=ALU.is_equal),
              reads=LGK, writes=["oh"])
            A("dve", lambda: V.tensor_tensor(out=prod[:], in0=oh[:], in1=pos[:], op=ALU.mult), reads=["oh", "dest"], writes=["prod"])
            A("dve", lambda k=k: V.reduce_sum(out=dself[:, k * NQB:(k + 1) * NQB], in_=prod3, axis=AX.X), reads=["prod"], writes=[("dself", k)])
            A("dve", lambda: V.tensor_tensor(out=prod[:], in0=oh[:], in1=gw_all[:], op=ALU.mult), reads=["oh", "gw"], writes=["prod"])
            A("dve", lambda k=k: V.reduce_sum(out=gk[:, k * NQB:(k + 1) * NQB], in_=prod3, axis=AX.X), reads=["prod"], writes=[("gk", k)])
        A("dve", lambda: V.tensor_copy(out=dsel_i[:], in_=dself[:]), reads=[("dself", k) for k in range(4)], writes=["dsel_i"])
        A("dve", lambda: V.tensor_tensor(out=cmpb[:].rearrange("p (b e) -> p b e", e=NE),
                                         in0=pend[:].unsqueeze(1).to_broadcast([128, NBLK, NE]),
                                         in1=blkth[:].rearrange("p (b e) -> p b e", e=NE), op=ALU.is_le),
          reads=[pendk, "blkth"], writes=["cmpb"])
        A("dve", lambda: V.reduce_sum(out=blke[:], in_=cmpb[:].rearrange("p (b e) -> p b e", e=NE), axis=AX.X), reads=["cmpb"], writes=["blke"])
        A("dve", lambda: V.tensor_scalar(out=blke[:], in0=blke[:], scalar1=float(NE - 1), scalar2=None, op0=ALU.min), reads=["blke"], writes=["blke"])
        A("dve", lambda: V.scalar_tensor_tensor(out=idxwf[:].rearrange("p (b c) -> p b c", c=8),
                                                in0=blke[:].unsqueeze(2).to_broadcast([128, NBLK, 8]), scalar=float(D),
                                                in1=rowid[:].unsqueeze(1).to_broadcast([128, NBLK, 8]), op0=ALU.mult, op1=ALU.add),
          reads=["blke", "rowid"], writes=["idxwf"])
        nused = sb(pr, "nused", [128, NBLK], F32)
        A("dve", lambda: V.tensor_scalar(out=nused[:], in0=blkth[:].rearrange("p (b e) -> p b e", e=NE)[:, :, 0],
                                         scalar1=pend[:, NE - 1:NE], scalar2=None, op0=ALU.is_ge),
          reads=[pendk, "blkth"], writes=["nused"])
        sameb = sb(pr, "sameb", [128, NBLK], F32)
        A("dve", lambda: V.memset(sameb[:], 0.0), writes=["sameb"])
        A("dve", lambda: V.tensor_tensor(out=sameb[:, 2:NBLK], in0=blke[:, 2:NBLK], in1=blke[:, 0:NBLK - 2], op=ALU.is_equal),
          reads=["blke", "sameb"], writes=["sameb"])
        A("dve", lambda: V.tensor_tensor(out=nused[:], in0=nused[:], in1=sameb[:], op=ALU.max), reads=["nused", "sameb"], writes=["nused"])
        idxgf = sb(pr, "idxgf", [128, NBLK * 8], F32)
        A("dve", lambda: V.scalar_tensor_tensor(out=idxgf[:].rearrange("p (b c) -> p b c", c=8),
                                                in0=nused[:].unsqueeze(2).to_broadcast([128, NBLK, 8]), scalar=40000.0,
                                                in1=idxwf[:].rearrange("p (b c) -> p b c", c=8), op0=ALU.mult, op1=ALU.add),
          reads=["nused", "idxwf"], writes=["idxgf"])
        A("dve", lambda: V.tensor_copy(out=idxg_i[:], in_=idxgf[:]), reads=["idxgf"], writes=["idxg_i"])
        A("dve", lambda: V.tensor_copy(out=idxw_i[:], in_=idxwf[:]), reads=["idxwf"], writes=["idxw_i"])
        A("dve", lambda: V.scalar_tensor_tensor(out=idxbf[:], in0=blke[:], scalar=128.0, in1=rowid[:, 0:1].to_broadcast([128, NBLK]),
                                                op0=ALU.mult, op1=ALU.add), reads=["blke", "rowid"], writes=["idxbf"])
        A("dve", lambda: V.scalar_tensor_tensor(out=idxbf[:], in0=nused[:], scalar=40000.0, in1=idxbf[:], op0=ALU.mult, op1=ALU.add),
          reads=["nused", "idxbf"], writes=["idxbf"])
        A("dve", lambda: V.tensor_copy(out=idxb_i[:], in_=idxbf[:]), reads=["idxbf"], writes=["idxb_i"])
        if dbg:
            sc.dma("sp", lambda: SP.dma_start(out=dbg_d["gw"][:, :], in_=gw_all[:]), reads=["gw"], writes=["dbg_gw"])
            sc.dma("sp", lambda: SP.dma_start(out=dbg_d["dsel"][:, :], in_=dsel_i[:]), reads=["dsel_i"], writes=["dbg_dsel"])
            sc.dma("sp", lambda: SP.dma_start(out=dbg_d["blke"][:, :], in_=blke[:]), reads=["blke"], writes=["dbg_blke"])
        h2r = [sb(pr, f"h2r{i}", [128, D], BF16) for i in range(4)]
        for i in range(NQB):
            s4 = i % 4
            sc.dma("sp", lambda i=i, s4=s4: SP.dma_start(out=h2r[s4][:], in_=h2_d[i * 128:(i + 1) * 128, :]),
                   reads=[("h2_d", i)], writes=[("h2r", s4)])
            for k in range(4):
                sc.dma("pool", lambda i=i, k=k, s4=s4: G.indirect_dma_start(
                    out=xs_d[:, :], out_offset=bass.IndirectOffsetOnAxis(ap=dsel_i[:, k * NQB + i:k * NQB + i + 1], axis=0),
                    in_=h2r[s4][:], in_offset=None), reads=[("h2r", s4), "dsel_i"], writes=["xs_d"])
        sc.barrier()
    if stop_after == "R":
        sc.finish(["dbg_gw", "dbg_dsel", "dbg_blke", "xs_d"] + [("x1_d", i) for i in range(NQB)])
        rt_.close()
        es.close()
        return nc

    with ExitStack() as pm:
        wgu = [sb(pm, f"wgu{i}", [128, 8 * 2 * DFF], BF16) for i in range(2)]
        wdn = [sb(pm, f"wdn{i}", [128, 8 * D], BF16) for i in range(2)]
        bgu = [sb(pm, f"bgu{i}", [128, 16], F32) for i in range(2)]
        xst = [sb(pm, "xst0", [128, 4 * D], BF16)]
        xsT = sb(pm, "xsT", [128, 8 * MB], BF16)
        xsT3 = xsT[:].rearrange("p (c t) -> p c t", t=MB)
        actT_ = [sb(pm, f"actT{j}", [128, 8 * MB], BF16) for j in range(2)]
        actT3_ = [a_[:].rearrange("p (f t) -> p f t", t=MB) for a_ in actT_]
        gt = [sb(pm, f"gt{i}", [128, MB], F32) for i in range(2)]
        sg = [sb(pm, f"sg{i}", [128, MB], F32) for i in range(2)]
        ut = [sb(pm, f"ut{i}", [128, MB], F32) for i in range(2)]
        yst = [sb(pm, f"yst{i}", [128, D], F32) for i in range(4)]

        bc_reg = [G.to_reg(NE * D - 1), G.to_reg(NE * 128 - 1)]

        def load_weights(b, which):
            sl = b % 2
            w3g = wgu[sl][:].rearrange("p (c n) -> p c n", n=2 * DFF)
            w3d = wdn[sl][:].rearrange("p (c n) -> p c n", n=D)
            for c in range(8 if which == "gu" else 0):
                sc.dma("pool", lambda c=c, w3g=w3g, b=b: G.indirect_dma_start(
                    out=w3g[:, c, :], out_offset=None, in_=wgu_d[:, :],
                    in_offset=bass.IndirectOffsetOnAxis(ap=idxg_i[:, b * 8 + c:b * 8 + c + 1], axis=0),
                    bounds_check=bc_reg[0], oob_is_err=False),
                    reads=["idxg_i"], writes=[("wgu", sl, c)])
            for c in range(8 if which == "wd" else 0):
                sc.dma("pool", lambda c=c, w3d=w3d, b=b: G.indirect_dma_start(
                    out=w3d[:, c, :], out_offset=None, in_=wd_d[:, :],
                    in_offset=bass.IndirectOffsetOnAxis(ap=idxg_i[:, b * 8 + c:b * 8 + c + 1], axis=0),
                    bounds_check=bc_reg[0], oob_is_err=False),
                    reads=["idxg_i"], writes=[("wdn", sl, c)])
            if which == "gu":
              sc.dma("pool", lambda b=b, sl=sl: G.indirect_dma_start(
                out=bgu[sl][:], out_offset=None, in_=bgu_d[:, :],
                in_offset=bass.IndirectOffsetOnAxis(ap=idxb_i[:, b:b + 1], axis=0),
                bounds_check=bc_reg[1], oob_is_err=False), reads=["idxb_i"], writes=[("bgu", sl)])

        def load_x(b):
            sl = 0
            sc.dma("sp", lambda b=b, sl=sl: SP.dma_start(out=xst[sl][:].rearrange("p (s d) -> p s d", d=D),
                                                        in_=xs_d[b * MB:(b + 1) * MB, :].rearrange("(s p) d -> p s d", p=128)),
                   reads=["xs_d"], writes=[("xst", sl)])

        for j in range(2):
            A("dve", lambda j=j: V.memset(wgu[j][:], 0.0), writes=[("wgu", j, c) for c in range(8)])
            A("dve", lambda j=j: V.memset(wdn[j][:], 0.0), writes=[("wdn", j, c) for c in range(8)])
            A("dve", lambda j=j: V.memset(bgu[j][:], 0.0), writes=[("bgu", j)])
        load_weights(0, "gu")
        load_x(0)

        def down_proj(b):
            sl = b % 2
            w3d = wdn[sl][:].rearrange("p (c n) -> p c n", n=D)
            WDK = [("wdn", sl, c) for c in range(8)]
            aT3 = actT3_[sl]
            AK = [("actT", sl, f) for f in range(8)]
            for s_ in range(4):
                ys_ = yst[s_]
                for n in range(2):
                    py, pyk = bank(6 + n)
                    for f in range(8):
                        A("pe", lambda py=py, f=f, s_=s_, n=n, w3d=w3d, aT3=aT3: T.matmul(
                            py, lhsT=aT3[:, f, s_ * 128:(s_ + 1) * 128], rhs=w3d[:, f, n * 512:(n + 1) * 512],
                            start=(f == 0), stop=(f == 7)), reads=AK + WDK, writes=[pyk])
                    A("act", lambda py=py, ys_=ys_, n=n: ACT.copy(out=ys_[:, n * 512:(n + 1) * 512], in_=py),
                      reads=[pyk], writes=[("yst", s_, n)])
                sc.dma("sp", lambda ys_=ys_, b=b, s_=s_: SP.dma_start(out=ys_d[b * MB + s_ * 128:b * MB + (s_ + 1) * 128, :], in_=ys_[:]),
                       reads=[("yst", s_, 0), ("yst", s_, 1)], writes=["ys_d"])

        for b in range(NBLK):
            sl = b % 2
            w3g = wgu[sl][:].rearrange("p (c n) -> p c n", n=2 * DFF)
            WGK = [("wgu", sl, c) for c in range(8)]
            aT3 = actT3_[sl]
            for s_ in range(4):
                pbT = PS[0][:, (s_ % 2) * 512:(s_ % 2 + 1) * 512].bitcast(BF16)
                pkT = ("ps", s_ % 2)
                for c in range(8):
                    A("pe", lambda c=c, pbT=pbT, s_=s_: T.transpose(
                        out=pbT[:, c * 128:(c + 1) * 128], in_=xst[0][:, s_ * D + c * 128:s_ * D + (c + 1) * 128], identity=ident_b[:]),
                      reads=[("xst", 0), "ident_b"], writes=[pkT])
                if s_ % 2 == 0:
                    A("dve", lambda pbT=pbT, s_=s_: V.tensor_copy(out=xsT3[:, :, s_ * 128:(s_ + 1) * 128],
                                                                  in_=pbT.rearrange("p (c t) -> p c t", t=128)),
                      reads=[pkT], writes=[("xsT", s_)])
                else:
                    A("act", lambda pbT=pbT, s_=s_: ACT.copy(out=xsT3[:, :, s_ * 128:(s_ + 1) * 128],
                                                             in_=pbT.rearrange("p (c t) -> p c t", t=128)),
                      reads=[pkT], writes=[("xsT", s_)])
            if b + 1 < NBLK:
                load_x(b + 1)
            if b >= 1:
                down_proj(b - 1)
            load_weights(b, "wd")
            if b + 1 < NBLK:
                load_weights(b + 1, "gu")
            XK = [("xsT", s_) for s_ in range(4)]
            for f in range(8):
                e2 = f % 2
                pg, pgk = bank(2 + e2 * 2)
                pu, puk = bank(3 + e2 * 2)
                for (pb, pk, col) in ((pg, pgk, f * 128), (pu, puk, DFF + f * 128)):
                    for c in range(8):
                        A("pe", lambda pb=pb, c=c, col=col, w3g=w3g: T.matmul(pb, lhsT=w3g[:, c, col:col + 128], rhs=xsT3[:, c, :],
                                                                              start=(c == 0), stop=(c == 7)),
                          reads=XK + WGK, writes=[pk])
                g_, s__, u_ = gt[e2], sg[e2], ut[e2]
                A("dve", lambda pg=pg, g_=g_, f=f, sl=sl: V.tensor_scalar(out=g_[:], in0=pg, scalar1=bgu[sl][:, f:f + 1], scalar2=7.0,
                                                                          op0=ALU.add, op1=ALU.min),
                  reads=[pgk, ("bgu", sl)], writes=[("gt", e2)])
                A("act", lambda g_=g_, s__=s__: ACT.activation(out=s__[:], in_=g_[:], func=AF.Sigmoid, scale=1.702),
                  reads=[("gt", e2)], writes=[("sg", e2)])
                A("dve", lambda pu=pu, u_=u_, f=f, sl=sl: V.tensor_scalar(out=u_[:], in0=pu, scalar1=bgu[sl][:, 8 + f:9 + f], scalar2=7.0,
                                                                          op0=ALU.add, op1=ALU.min),
                  reads=[puk, ("bgu", sl)], writes=[("ut", e2)])
                A("dve", lambda u_=u_: V.tensor_scalar(out=u_[:], in0=u_[:], scalar1=-7.0, scalar2=1.0, op0=ALU.max, op1=ALU.add),
                  reads=[("ut", e2)], writes=[("ut", e2)])
                A("dve", lambda g_=g_, s__=s__: V.tensor_tensor(out=g_[:], in0=g_[:], in1=s__[:], op=ALU.mult),
                  reads=[("gt", e2), ("sg", e2)], writes=[("gt", e2)])
                A("dve", lambda g_=g_, u_=u_, f=f, aT3=aT3: V.tensor_tensor(out=aT3[:, f, :], in0=g_[:], in1=u_[:], op=ALU.mult),
                  reads=[("gt", e2), ("ut", e2)], writes=[("actT", sl, f)])
            if b % 8 == 7:
                sc.flush()
        down_proj(NBLK - 1)
        sc.barrier()

    with ExitStack() as pf:
        bd = sb(pf, "bd", [NE, D], F32)
        sc.dma("sp", lambda: SP.dma_start(out=bd[:], in_=bd_d[:, :]), writes=["bd"])
        gfin = sb(pf, "gfin", [128, D], F32)
        sc.dma("sp", lambda: SP.dma_start(out=gfin[:], in_=gfin_d[:, :].partition_broadcast(128)), writes=["gfin"])
        x1f = [sb(pf, f"x1f{i}", [128, D], F32) for i in range(2)]
        yk_ = [[sb(pf, f"yk{j}_{i}", [128, D], F32) for i in range(4)] for j in range(2)]
        acc_ = [sb(pf, f"acc{j}", [128, D], F32) for j in range(2)]
        gwT_ = [sb(pf, f"gwT{j}", [NE, 128], F32) for j in range(2)]
        junkF = sb(pf, "junkF", [128, D], BF16)
        ssqF = sb(pf, "ssqF", [128, 1], F32)
        ot = [sb(pf, f"ot{i}", [128, D], F32) for i in range(2)]
        def f_loads(i):
            s2 = i % 2
            yk = yk_[s2]
            sc.dma("sp", lambda i=i, s2=s2: SP.dma_start(out=x1f[s2][:], in_=x1_d[i * 128:(i + 1) * 128, :]),
                   reads=[("x1_d", i)], writes=[("x1f", s2)])
            for k in range(4):
                sc.dma("pool", lambda i=i, k=k, yk=yk: G.indirect_dma_start(
                    out=yk[k][:], out_offset=None, in_=ys_d[:, :],
                    in_offset=bass.IndirectOffsetOnAxis(ap=dsel_i[:, k * NQB + i:k * NQB + i + 1], axis=0)),
                    reads=["ys_d", "dsel_i"], writes=[("yk", s2, k)])

        for i in range(NQB):
            s2 = i % 2
            yk, acc, gwT = yk_[s2], acc_[s2], gwT_[s2]
            ACCK, GWTK = ("acc", s2), ("gwT", s2)
            if i == 0:
                f_loads(0)
            if i + 1 < NQB:
                f_loads(i + 1)
            pt_, ptk_ = bank(s2)
            A("pe", lambda pt_=pt_, i=i: T.transpose(out=pt_[0:NE, 0:128], in_=gw3[:, i, :], identity=ident_f[:]),
              reads=["gw", "ident_f"], writes=[ptk_])
            A("act", lambda pt_=pt_, gwT=gwT: ACT.copy(out=gwT[:], in_=pt_[0:NE, 0:128]), reads=[ptk_], writes=[GWTK])
            pbs = []
            for n in range(2):
                pb, pbk = bank(2 + 2 * s2 + n)
                A("pe", lambda pb=pb, n=n, gwT=gwT: T.matmul(pb, lhsT=gwT[:], rhs=bd[:, n * 512:(n + 1) * 512], start=True, stop=True),
                  reads=[GWTK, "bd"], writes=[pbk])
                pbs.append((pb, pbk))
            A("dve", lambda i=i, acc=acc, yk=yk: V.tensor_scalar(out=acc[:], in0=yk[0][:], scalar1=gk[:, i:i + 1], scalar2=None, op0=ALU.mult),
              reads=[("yk", s2, 0)] + [("gk", k) for k in range(4)], writes=[ACCK])
            for k in range(1, 4):
                A("dve", lambda i=i, k=k, acc=acc, yk=yk: V.scalar_tensor_tensor(out=acc[:], in0=yk[k][:], scalar=gk[:, k * NQB + i:k * NQB + i + 1],
                                                                 in1=acc[:], op0=ALU.mult, op1=ALU.add),
                  reads=[("yk", s2, k), ACCK] + [("gk", kk) for kk in range(4)], writes=[ACCK])
            for n in range(2):
                pb, pbk = pbs[n]
                A("dve", lambda pb=pb, n=n, acc=acc: V.tensor_tensor(out=acc[:, n * 512:(n + 1) * 512], in0=pb, in1=acc[:, n * 512:(n + 1) * 512], op=ALU.add),
                  reads=[pbk, ACCK], writes=[ACCK])
            A("pool", lambda acc=acc: G.tensor_tensor(out=acc[:], in0=acc[:], in1=gate_f, op=ALU.mult), reads=[ACCK] + MODK, writes=[ACCK])
            A("pool", lambda s2=s2, acc=acc: G.tensor_tensor(out=acc[:], in0=acc[:], in1=x1f[s2][:], op=ALU.add), reads=[ACCK, ("x1f", s2)], writes=[ACCK])
            A("act", lambda acc=acc: ACT.activation(out=junkF[:], in_=acc[:], func=AF.Square, accum_out=ssqF[:]), reads=[ACCK], writes=["junkF", "ssqF"])
            A("dve", lambda: V.tensor_scalar(out=ssqF[:], in0=ssqF[:], scalar1=1.0 / D, scalar2=EPS, op0=ALU.mult, op1=ALU.add),
              reads=["ssqF"], writes=["ssqF"])
            A("act", lambda: ACT.activation(out=ssqF[:], in_=ssqF[:], func=AF.Ln), reads=["ssqF"], writes=["ssqF"])
            A("act", lambda: ACT.activation(out=ssqF[:], in_=ssqF[:], func=AF.Exp, scale=-0.5), reads=["ssqF"], writes=["ssqF"])
            o_ = ot[s2]
            A("dve", lambda o_=o_, acc=acc: V.scalar_tensor_tensor(out=o_[:], in0=acc[:], scalar=ssqF[:, 0:1], in1=gfin[:], op0=ALU.mult, op1=ALU.mult),
              reads=[ACCK, "ssqF", "gfin"], writes=[("ot", s2)])
            sc.dma("sp", lambda o_=o_, i=i: SP.dma_start(out=out_d[i * 128:(i + 1) * 128, :], in_=o_[:]),
                   reads=[("ot", s2)], writes=[("out_d", i)])
        sc.barrier()
    sc.finish([("out_d", i) for i in range(NQB)])
    rt_.close()
    es.close()
    return nc


def _prep_inputs(inputs):
    f = lambda a: np.ascontiguousarray(np.asarray(a, dtype=np.float32))
    x = f(inputs["x"])
    c = f(inputs["c"])
    w_in = f(inputs["w_in"])[0]
    wa, wb = _layout_w_in(w_in)
    cosT, sinT = _rope_tables()
    dr, co = _bias_index()
    rpb = f(inputs["rpb"])[0]
    biasu = np.ascontiguousarray(rpb[:, dr, co].reshape(8, 128, 7 * 128))
    bgu = f(inputs["b_gate_up"])[0]
    bgu_l = np.ascontiguousarray(bgu.reshape(NE, 16, 128).transpose(0, 2, 1).reshape(NE * 128, 16))
    rowid = (np.arange(8)[None, :] * 128 + np.arange(128)[:, None]).astype(np.float32)
    blkth = np.broadcast_to((np.arange(NBLK, dtype=np.float32) * MB)[None, :, None], (128, NBLK, NE)).reshape(128, NBLK * NE)
    shared = {
        "w_ada": f(inputs["w_ada"])[0], "b_ada": f(inputs["b_ada"]).reshape(1, 6 * D),
        "w_a": wa, "w_b": wb, "sink": f(inputs["sink"]).reshape(1, 8), "biasu": biasu,
        "g_out_a": f(inputs["g_out_a"]).reshape(1, 512), "g_out_b": f(inputs["g_out_b"]).reshape(1, 512),
        "w_out": f(inputs["w_out"])[0], "w_router": f(inputs["w_router"])[0], "b_router": f(inputs["b_router"]).reshape(1, NE),
        "w_gate_up": f(inputs["w_gate_up"])[0].reshape(NE * D, 2 * DFF), "b_gate_up": bgu_l,
        "w_down": f(inputs["w_down"])[0].reshape(NE * DFF, D), "b_down": f(inputs["b_down"])[0],
        "g_final": f(inputs["g_final"]).reshape(1, D), "cosT": cosT, "sinT": sinT,
        "maska": np.ascontiguousarray(_mask_a().reshape(128, 384)),
        "maskb": np.ascontiguousarray(_MASKB.reshape(_NVAR, 128, 7 * 128)),
        "ident": np.eye(128, dtype=np.float32), "tri": np.triu(np.ones((128, 128), np.float32), 1),
        "rowid": rowid, "blkth": np.ascontiguousarray(blkth),
    }
    in_maps = []
    for b in range(8):
        m = dict(shared)
        m["x"] = x[b]
        m["cT"] = np.ascontiguousarray(c[b].reshape(8, 128).T)
        in_maps.append(m)
    return in_maps


def kernel(**inputs):
    in_maps = _prep_inputs(inputs)
    nc = build_program()
    res = run_bass_kernel_spmd(nc, in_maps, core_ids=list(range(8)))
    return np.stack([np.asarray(r["out"], dtype=np.float32) for r in res.results], axis=0)
```

```python
import bisect
from contextlib import ExitStack

import numpy as np
import concourse.bass as bass
import concourse.mybir as mybir
from concourse.bass_utils import run_bass_kernel_spmd

F32 = mybir.dt.float32
BF16 = mybir.dt.bfloat16
I32 = mybir.dt.int32
AF = mybir.ActivationFunctionType
ALU = mybir.AluOpType
AX = mybir.AxisListType

S = 4096
D = 1024
NQB = 32
NE = 32
DFF = 1024
EPS = 1e-5
MASKV = -240000.0
MB = 512
NBLK = 64
NSLOT = NBLK * MB
THETA = 500000.0


class _Op:
    __slots__ = ("eng", "fn", "deps", "dma", "need_inc", "target")


class Sched:
    def __init__(self, nc, es, nchan=10):
        self.nc = nc
        self.engs = dict(pe=nc.tensor, act=nc.scalar, dve=nc.vector, pool=nc.gpsimd, sp=nc.sync)
        self.sem = {e: es.enter_context(nc.semaphore("sem_" + e)) for e in ("pe", "act", "dve", "pool")}
        nch = {"sp": 12, "pool": 28}
        self.chan = {q: [es.enter_context(nc.semaphore(f"ch_{q}{i}")) for i in range(nch[q])] for q in ("sp", "pool")}
        self.chan_cnt = {q: [0] * nch[q] for q in ("sp", "pool")}
        self.chan_next = {q: 0 for q in ("sp", "pool")}
        self.ops = []
        self.flushed = 0
        self.last_writer = {}
        self.readers = {}
        self.cnt = {e: 0 for e in self.sem}
        self.incs = {e: ([], []) for e in self.sem}
        self.waited = {}

    def add(self, eng, fn, reads=(), writes=(), dma=False):
        op = _Op()
        op.eng, op.fn, op.dma, op.need_inc, op.target = eng, fn, dma, False, None
        deps = set()
        for r in reads:
            w = self.last_writer.get(r)
            if w is not None:
                deps.add(w)
        for w_ in writes:
            w = self.last_writer.get(w_)
            if w is not None:
                deps.add(w)
            for r in self.readers.get(w_, ()):
                deps.add(r)
        idx = len(self.ops)
        deps.discard(idx)
        op.deps = deps
        for r in reads:
            self.readers.setdefault(r, []).append(idx)
        for w_ in writes:
            self.last_writer[w_] = idx
            self.readers[w_] = []
        self.ops.append(op)
        return idx

    def dma(self, q, fn, reads=(), writes=()):
        return self.add(q, fn, reads, writes, dma=True)

    def _wait(self, ceng, sem, val):
        key = (ceng, id(sem))
        if self.waited.get(key, 0) >= val:
            return
        self.waited[key] = val
        self.engs[ceng].wait_ge(sem, val)

    def flush(self, final=False):
        ops = self.ops
        lo, hi = self.flushed, len(ops)
        last_of = {}
        for i in range(lo, hi):
            op = ops[i]
            if not op.dma:
                last_of[op.eng] = i
            for d in op.deps:
                dop = ops[d]
                if d >= lo and not dop.dma:
                    if dop.eng == "pe" and op.eng == "pe" and not op.dma:
                        continue
                    dop.need_inc = True
        for e, i in last_of.items():
            ops[i].need_inc = True
        for i in range(lo, hi):
            op = ops[i]
            ceng = op.eng
            if op.dma:
                q = ceng
                c = self.chan_next[q]
                self.chan_next[q] = (c + 1) % len(self.chan[q])
                csem = self.chan[q][c]
                if self.chan_cnt[q][c] > 0:
                    self._wait(q, csem, 16 * self.chan_cnt[q][c])
            for d in sorted(op.deps):
                dop = ops[d]
                if dop.dma:
                    self._wait(ceng, dop.target[0], dop.target[1])
                else:
                    if dop.eng == "pe" and ceng == "pe" and not op.dma:
                        continue
                    il, cl = self.incs[dop.eng]
                    if dop.target is not None:
                        tv = dop.target[1]
                    else:
                        j = bisect.bisect_left(il, d)
                        if j < len(il):
                            tv = cl[j]
                        else:
                            raise RuntimeError("no covering inc")
                    self._wait(ceng, self.sem[dop.eng], tv)
            ins = op.fn()
            if op.dma:
                self.chan_cnt[q][c] += 1
                ins.then_inc(csem, 16)
                op.target = (csem, 16 * self.chan_cnt[q][c])
            elif op.need_inc:
                self.cnt[ceng] += 1
                ins.then_inc(self.sem[ceng], 1)
                op.target = (self.sem[ceng], self.cnt[ceng])
                self.incs[ceng][0].append(i)
                self.incs[ceng][1].append(self.cnt[ceng])
            op.fn = None
        self.flushed = hi

    def barrier(self):
        self.flush()
        for ceng in ("pe", "act", "dve", "pool", "sp"):
            for e, sem in self.sem.items():
                if self.cnt[e] > 0:
                    self._wait(ceng, sem, self.cnt[e])
            for q in ("sp", "pool"):
                for c, csem in enumerate(self.chan[q]):
                    if self.chan_cnt[q][c] > 0:
                        self._wait(ceng, csem, 16 * self.chan_cnt[q][c])

    def finish(self, out_keys):
        self.flush()
        for k in out_keys:
            w = self.last_writer.get(k)
            if w is not None:
                t = self.ops[w].target
                self._wait("sp", t[0], t[1])
        for q in ("sp", "pool"):
            for c, csem in enumerate(self.chan[q]):
                if self.chan_cnt[q][c] > 0:
                    self._wait("sp", csem, 16 * self.chan_cnt[q][c])


def _rope_tables():
    inv_freq = (np.float32(THETA) ** (-np.arange(0, 16, 2, dtype=np.float32) / np.float32(16))).astype(np.float32)
    pos = np.arange(S, dtype=np.float32)
    ang = (pos[:, None] * inv_freq[None, :]).astype(np.float32)
    cos = np.cos(ang).astype(np.float32)
    sin = np.sin(ang).astype(np.float32)
    cosT = np.ones((128, S), np.float32)
    sinT = np.zeros((128, S), np.float32)
    for hh in range(2):
        b = hh * 64
        for d in range(8):
            cosT[b + d] = cos[:, d]
            cosT[b + 8 + d] = cos[:, d]
            sinT[b + d] = -sin[:, d]
            sinT[b + 8 + d] = sin[:, d]
    return cosT, sinT


def _mask_a():
    k = np.arange(128)[:, None, None]
    m = np.arange(3)[None, :, None]
    q = np.arange(128)[None, None, :]
    rel = (m - 1) * 128 + k - q
    return np.where(np.abs(rel) <= 128, 0.0, MASKV).astype(np.float32)


def _b_geometry():
    rows = 64
    rs = np.clip(np.arange(rows) - 4, 0, rows - 8)
    cs = np.clip(np.arange(64) - 8, 0, 64 - 16)

    def valid_row(kr, r):
        return (0 <= kr < rows) and (rs[r] <= kr < rs[r] + 8)

    colmask = np.zeros((64, 64), bool)
    for qc in range(64):
        colmask[qc, cs[qc]:cs[qc] + 16] = True
    masks = {}
    kbs = {}
    for i in range(NQB):
        mk = np.full((128, 7, 128), MASKV, np.float32)
        used = []
        for m in range(7):
            kb = i + m - 3
            if kb < 0 or kb > 31:
                continue
            anyv = False
            for a in range(2):
                for b in range(2):
                    if valid_row(2 * kb + a, 2 * i + b):
                        anyv = True
                        blk = np.where(colmask.T, 0.0, MASKV)
                        mk[a * 64:(a + 1) * 64, m, b * 64:(b + 1) * 64] = blk
            if anyv:
                used.append(m)
        masks[i] = mk
        kbs[i] = used
    variants = []
    var_of = {}
    for i in range(NQB):
        for vi, v in enumerate(variants):
            if np.array_equal(v, masks[i]):
                var_of[i] = vi
                break
        else:
            var_of[i] = len(variants)
            variants.append(masks[i])
    return np.stack(variants), var_of, kbs


def _bias_index():
    a = np.arange(2)[:, None, None, None, None]
    kc = np.arange(64)[None, :, None, None, None]
    m = np.arange(7)[None, None, :, None, None]
    b = np.arange(2)[None, None, None, :, None]
    qc = np.arange(64)[None, None, None, None, :]
    dr = np.clip(2 * (m - 3) + a - b, -7, 7) + 7
    co = np.clip(kc - qc, -15, 15) + 15
    dr = np.broadcast_to(dr, (2, 64, 7, 2, 64)).reshape(128, 7, 128)
    co = np.broadcast_to(co, (2, 64, 7, 2, 64)).reshape(128, 7, 128)
    return dr, co


_MASKB, _VAR_OF, _KBS = _b_geometry()
_NVAR = _MASKB.shape[0]
_PERM = np.concatenate([np.arange(8, 16), np.arange(0, 8), np.arange(16, 64)])

NWA = 1536 + 128
NWB = 1536


def _layout_w_in(w):
    qa, ka, va = w[:, 0:512], w[:, 512:640], w[:, 640:768]
    qb, kb, vb = w[:, 768:1280], w[:, 1280:1792], w[:, 1792:2304]
    qap = qa.reshape(D, 8, 64)[:, :, _PERM].reshape(D, 512)
    k0, k1 = ka[:, 0:64], ka[:, 64:128]
    k0p, k1p = k0[:, _PERM], k1[:, _PERM]
    wa = np.concatenate([qa, qap, k0, k0, k1, k1, k0p, k0p, k1p, k1p, va], axis=1)
    wb = np.concatenate([qb, kb, vb], axis=1)
    return np.ascontiguousarray(wa), np.ascontiguousarray(wb)


def build_program(stop_after=None, dbg=False):
    nc = bass.Bass("TRN2", target_bir_lowering=False)
    es = ExitStack()

    def din(name, shape, dt=F32):
        return nc.dram_tensor(name, list(shape), dt, kind="ExternalInput").ap()

    x_d = din("x", [S, D])
    cT_d = din("cT", [128, 8])
    wada_d = din("w_ada", [D, 6 * D])
    bada_d = din("b_ada", [1, 6 * D])
    wa_d = din("w_a", [D, NWA])
    wb_d = din("w_b", [D, NWB])
    sink_d = din("sink", [1, 8])
    biasu_d = din("biasu", [8, 128, 7 * 128])
    goa_d = din("g_out_a", [1, 512])
    gob_d = din("g_out_b", [1, 512])
    wout_d = din("w_out", [D, D])
    wr_d = din("w_router", [D, NE])
    br_d = din("b_router", [1, NE])
    if stop_after is None:
        wgu_d = din("w_gate_up", [NE * D, 2 * DFF])
        bgu_d = din("b_gate_up", [NE * 128, 16])
        wd_d = din("w_down", [NE * DFF, D])
        bd_d = din("b_down", [NE, D])
    gfin_d = din("g_final", [1, D])
    cos_d = din("cosT", [128, S])
    sin_d = din("sinT", [128, S])
    maska_d = din("maska", [128, 3 * 128])
    maskb_d = din("maskb", [_NVAR, 128, 7 * 128])
    ident_d = din("ident", [128, 128])
    tri_d = din("tri", [128, 128])
    rowid_d = din("rowid", [128, 8])
    blkth_d = din("blkth", [128, NBLK * NE])
    out_d = nc.dram_tensor("out", [S, D], F32, kind="ExternalOutput").ap()

    def dscr(name, shape, dt):
        kind = "ExternalOutput" if (dbg and name not in ("xs_s", "ys_s")) else "Internal"
        return nc.dram_tensor(name, list(shape), dt, kind=kind).ap()

    mixa_d = dscr("mixa_s", [S, 512], BF16)
    mixb_d = dscr("mixb_s", [S, 512], BF16)
    x1_d = dscr("x1_s", [S, D], F32)
    h2_d = dscr("h2_s", [S, D], BF16)
    if stop_after not in ("A", "B"):
        xs_d = dscr("xs_s", [NSLOT, D], BF16)
        ys_d = dscr("ys_s", [NSLOT, D], F32)
    dbg_d = {}
    if dbg:
        dbg_d["qta"] = nc.dram_tensor("dbg_qta", [128, 6 * S], BF16, kind="ExternalOutput").ap()
        dbg_d["gw"] = nc.dram_tensor("dbg_gw", [128, NQB * NE], F32, kind="ExternalOutput").ap()
        dbg_d["dsel"] = nc.dram_tensor("dbg_dsel", [128, NQB * 4], I32, kind="ExternalOutput").ap()
        dbg_d["blke"] = nc.dram_tensor("dbg_blke", [128, NBLK], F32, kind="ExternalOutput").ap()
        dbg_d["mod"] = nc.dram_tensor("dbg_mod", [128, 6 * D], F32, kind="ExternalOutput").ap()

    sc = Sched(nc, es)
    dbg_keys = []

    DUMPS = dict(ptA=([128, 384], BF16), poA=([128, 1024], F32), vpa=([128, NQB * 2 * 65], BF16), esink=([128, 8], F32),
                 goa=([128, 512], F32), oaA=([128, 512], F32), denA=([128, 8], F32), ssqA=([128, 1], F32))
    dump_d = {k: nc.dram_tensor("dbg_" + k, v[0], v[1], kind="ExternalOutput").ap() for k, v in DUMPS.items()} if dbg else {}

    def dump(name, ap, shape, dt, reads):
        if not dbg:
            return
        d = dump_d[name]
        sc.dma("sp", lambda: nc.sync.dma_start(out=d, in_=ap), reads=reads, writes=["dbg_" + name])
        dbg_keys.append("dbg_" + name)
    A = sc.add
    T, V, G, ACT, SP = nc.tensor, nc.vector, nc.gpsimd, nc.scalar, nc.sync

    def sb(stack, name, shape, dt=F32):
        return stack.enter_context(nc.sbuf_tensor("s_" + name, list(shape), dt))

    PS = [es.enter_context(nc.psum_tensor(f"ps{i}", [128, 1024], F32)) for i in range(4)]

    def bank(i):
        return PS[i // 2][:, (i % 2) * 512:(i % 2 + 1) * 512], ("ps", i)

    ident_f = sb(es, "ident_f", [128, 128], F32)
    ident_b = sb(es, "ident_b", [128, 128], BF16)
    ones_f = sb(es, "ones_f", [128, 128], F32)
    mod = sb(es, "mod", [128, 6 * D], F32)
    epsb = sb(es, "epsb", [128, 1], F32)
    sc.dma("sp", lambda: SP.dma_start(out=ident_f[:], in_=ident_d[:, :]), writes=["ident_f"])
    sc.dma("pool", lambda: G.dma_start(out=ident_b[:], in_=ident_d[:, :]), writes=["ident_b"])
    A("dve", lambda: V.memset(ones_f[:], 1.0), writes=["ones_f"])
    A("dve", lambda: V.memset(epsb[:], EPS), writes=["epsb"])

    with ExitStack() as p0:
        cT = sb(p0, "cT", [128, 8], F32)
        cact = sb(p0, "cact", [128, 8], F32)
        csig = sb(p0, "csig", [128, 8], F32)
        crep = sb(p0, "crep", [128, 8 * 128], F32)
        bada = sb(p0, "bada", [1, 6 * D], F32)
        wsl = [sb(p0, f"wsl{i}", [128, 8 * 512], F32) for i in range(2)]
        sc.dma("sp", lambda: SP.dma_start(out=cT[:], in_=cT_d[:, :]), writes=["cT"])
        sc.dma("sp", lambda: SP.dma_start(out=bada[:], in_=bada_d[:, :]), writes=["bada"])
        A("act", lambda: ACT.activation(out=csig[:], in_=cT[:], func=AF.Sigmoid), reads=["cT"], writes=["csig"])
        A("dve", lambda: V.tensor_tensor(out=cact[:], in0=cT[:], in1=csig[:], op=ALU.mult), reads=["cT", "csig"], writes=["cact"])
        A("dve", lambda: V.tensor_copy(out=crep[:].rearrange("p (c m) -> p c m", m=128),
                                       in_=cact[:].unsqueeze(2).to_broadcast([128, 8, 128])),
          reads=["cact"], writes=["crep"])
        for n in range(12):
            slot = n % 2
            w_t = wsl[slot]
            sc.dma("sp", lambda w_t=w_t, n=n: SP.dma_start(
                out=w_t[:].rearrange("p (c n) -> p c n", n=512),
                in_=wada_d[:, n * 512:(n + 1) * 512].rearrange("(c p) n -> p c n", p=128)),
                writes=[("wsl", slot)])
            pb, pk = bank(n % 2)
            for c in range(8):
                A("pe", lambda pb=pb, w_t=w_t, c=c: T.matmul(pb, lhsT=crep[:, c * 128:(c + 1) * 128],
                                                             rhs=w_t[:, c * 512:(c + 1) * 512], start=(c == 0), stop=False),
                  reads=["crep", ("wsl", slot)], writes=[pk])
            A("pe", lambda pb=pb, n=n: T.matmul(pb, lhsT=ones_f[0:1, :], rhs=bada[0:1, n * 512:(n + 1) * 512],
                                                start=False, stop=True),
              reads=["ones_f", "bada"], writes=[pk])
            if (n // 2) % 3 == 1:
                A("dve", lambda pb=pb, n=n: V.tensor_scalar(out=mod[:, n * 512:(n + 1) * 512], in0=pb, scalar1=1.0,
                                                            scalar2=None, op0=ALU.add), reads=[pk], writes=[("mod", n)])
            else:
                A("act", lambda pb=pb, n=n: ACT.copy(out=mod[:, n * 512:(n + 1) * 512], in_=pb), reads=[pk], writes=[("mod", n)])
        sc.barrier()
    MODK = [("mod", n) for n in range(12)]
    shift_m, scale1_m, gate_m = mod[:, 0:D], mod[:, D:2 * D], mod[:, 2 * D:3 * D]
    shift_f, scale1_f, gate_f = mod[:, 3 * D:4 * D], mod[:, 4 * D:5 * D], mod[:, 5 * D:6 * D]
    if dbg:
        sc.dma("sp", lambda: SP.dma_start(out=dbg_d["mod"][:, :], in_=mod[:]), reads=MODK, writes=["dbg_mod"])

    def rmsnorm_mod(stack_tiles, src, src_keys, scale1, shift, out_bf=None, out_f32=None, out_keys=(), tag=""):
        junk, ssq, rstd, tmp = stack_tiles
        A("act", lambda: ACT.activation(out=junk[:], in_=src, func=AF.Square, accum_out=ssq[:]),
          reads=list(src_keys), writes=["junk" + tag, "ssq" + tag])
        A("dve", lambda: V.tensor_scalar(out=rstd[:], in0=ssq[:], scalar1=1.0 / D, scalar2=EPS, op0=ALU.mult, op1=ALU.add),
          reads=["ssq" + tag], writes=["rstd" + tag])
        A("act", lambda: ACT.activation(out=rstd[:], in_=rstd[:], func=AF.Ln), reads=["rstd" + tag], writes=["rstd" + tag])
        A("act", lambda: ACT.activation(out=rstd[:], in_=rstd[:], func=AF.Exp, scale=-0.5), reads=["rstd" + tag], writes=["rstd" + tag])
        A("dve", lambda: V.scalar_tensor_tensor(out=tmp[:], in0=src, scalar=rstd[:, 0:1], in1=scale1, op0=ALU.mult, op1=ALU.mult),
          reads=list(src_keys) + ["rstd" + tag] + MODK, writes=["tmp" + tag])
        if out_f32 is not None:
            A("dve", lambda: V.tensor_tensor(out=out_f32, in0=tmp[:], in1=shift, op=ALU.add),
              reads=["tmp" + tag] + MODK, writes=list(out_keys))
            if out_bf is not None:
                A("act", lambda: ACT.copy(out=out_bf, in_=out_f32), reads=list(out_keys), writes=[k + ("bf",) for k in out_keys])
        else:
            A("dve", lambda: V.tensor_tensor(out=out_bf, in0=tmp[:], in1=shift, op=ALU.add),
              reads=["tmp" + tag] + MODK, writes=list(out_keys))

    def projection_pass(ps_, wmat_d, ncols, ngroups, emit_group, emit_v, vcol0, nvcols, tag):
        wsb = sb(ps_, "wsb" + tag, [128, 8 * ncols], BF16)
        w3 = wsb[:].rearrange("p (c n) -> p c n", n=ncols)
        for c in range(8):
            sc.dma("pool", lambda c=c: G.dma_start(out=w3[:, c, :], in_=wmat_d[c * 128:(c + 1) * 128, :]),
                   writes=[("wsb" + tag, c)])
        WK = [("wsb" + tag, c) for c in range(8)]
        xbl = [sb(ps_, f"xbl{tag}{i}", [128, D], F32) for i in range(2)]
        hbf = [sb(ps_, f"hbf{tag}{i}", [128, D], BF16) for i in range(2)]
        hT = [sb(ps_, f"hT{tag}{i}", [128, 8 * 512], BF16) for i in range(2)]
        tiles = [(sb(ps_, f"junk{tag}{j}", [128, D], BF16), sb(ps_, f"ssq{tag}{j}", [128, 1], F32),
                  sb(ps_, f"rstd{tag}{j}", [128, 1], F32), sb(ps_, f"tmp{tag}{j}", [128, D], F32)) for j in range(2)]
        for tc in range(8):
            hslot = tc % 2
            hT3 = hT[hslot][:].rearrange("p (c t) -> p c t", t=512)
            for sub in range(4):
                i = tc * 4 + sub
                xs_ = i % 2
                sc.dma("sp", lambda i=i, xs_=xs_: SP.dma_start(out=xbl[xs_][:], in_=x_d[i * 128:(i + 1) * 128, :]),
                       writes=[("xbl" + tag, xs_)])
                rmsnorm_mod(tiles[xs_], xbl[xs_][:], [("xbl" + tag, xs_)], scale1_m, shift_m, out_bf=hbf[xs_][:],
                            out_keys=[("hbf" + tag, xs_)], tag=tag + str(xs_))
                pbT = PS[0][:, (i % 2) * 512:(i % 2 + 1) * 512].bitcast(BF16)
                pkT = ("ps", i % 2)
                for c in range(8):
                    A("pe", lambda c=c, pbT=pbT, xs_=xs_: T.transpose(out=pbT[:, c * 128:(c + 1) * 128],
                                                                      in_=hbf[xs_][:, c * 128:(c + 1) * 128], identity=ident_b[:]),
                      reads=[("hbf" + tag, xs_), "ident_b"], writes=[pkT])
                A("act", lambda pbT=pbT, hT3=hT3, sub=sub: ACT.copy(out=hT3[:, :, sub * 128:(sub + 1) * 128],
                                                                    in_=pbT.rearrange("p (c t) -> p c t", t=128)),
                  reads=[pkT], writes=[("hT" + tag, hslot, sub)])
                pv, pvk = bank(2 + (i % 2))
                for c in range(8):
                    A("pe", lambda c=c, pv=pv, hT3=hT3, sub=sub: T.matmul(
                        pv[:, 0:nvcols], lhsT=hT3[:, c, sub * 128:(sub + 1) * 128], rhs=w3[:, c, vcol0:vcol0 + nvcols],
                        start=(c == 0), stop=(c == 7)),
                      reads=[("hT" + tag, hslot, sub)] + WK, writes=[pvk])
                emit_v(i, pv, pvk)
            HK = [("hT" + tag, hslot, s_) for s_ in range(4)]
            emit_group(tc, hT3, HK, w3, WK)
        return

    with ExitStack() as pa:
        qTa = sb(pa, "qTa", [128, 6 * S], BF16)
        qTa3 = qTa[:].rearrange("p (g t) -> p g t", t=S)
        vpa = sb(pa, "vpa", [128, NQB * 2 * 65], BF16)
        vpa4 = vpa[:].rearrange("p (i g d) -> p i g d", g=2, d=65)
        A("pool", lambda: G.memset(vpa[:], 1.0), writes=["vpa_init"])
        with ExitStack() as pa1:
            cosT = sb(pa1, "cosT", [128, S], F32)
            sinT = sb(pa1, "sinT", [128, S], F32)
            sc.dma("sp", lambda: SP.dma_start(out=cosT[:], in_=cos_d[:, :]), writes=["cosT"])
            sc.dma("sp", lambda: SP.dma_start(out=sinT[:], in_=sin_d[:, :]), writes=["sinT"])
            rt = [sb(pa1, f"rt{i}", [128, 512], F32) for i in range(4)]

            def emit_v_a(i, pv, pvk):
                A("act", lambda: ACT.copy(out=vpa4[:, i, :, 0:64], in_=pv[:, 0:128].rearrange("p (g d) -> p g d", d=64)),
                  reads=[pvk, "vpa_init"], writes=[("vpa", i)])

            def emit_group_a(tc, hT3, HK, w3, WK):
                for g in range(6):
                    c0 = g * 128 if g < 4 else 1024 + (g - 4) * 256
                    c1 = 512 + g * 128 if g < 4 else 1024 + 512 + (g - 4) * 256
                    if g >= 4:
                        c0 = 1024 + (g - 4) * 128
                        c1 = 1024 + 256 + (g - 4) * 128
                    pq, pqk = bank(4 + (g % 2) * 2)
                    pp, ppk = bank(5 + (g % 2) * 2)
                    for (pb, pk, col) in ((pq, pqk, c0), (pp, ppk, c1)):
                        for c in range(8):
                            A("pe", lambda pb=pb, c=c, col=col: T.matmul(pb, lhsT=w3[:, c, col:col + 128], rhs=hT3[:, c, :],
                                                                         start=(c == 0), stop=(c == 7)),
                              reads=HK + WK, writes=[pk])
                    r0, r1 = rt[(g % 2) * 2], rt[(g % 2) * 2 + 1]
                    k0, k1 = ("rt", (g % 2) * 2), ("rt", (g % 2) * 2 + 1)
                    tsl = slice(tc * 512, (tc + 1) * 512)
                    A("dve", lambda pq=pq, r0=r0, tsl=tsl: V.tensor_tensor(out=r0[:], in0=pq, in1=cosT[:, tsl], op=ALU.mult),
                      reads=[pqk, "cosT"], writes=[k0])
                    A("dve", lambda pp=pp, r1=r1, tsl=tsl: V.tensor_tensor(out=r1[:], in0=pp, in1=sinT[:, tsl], op=ALU.mult),
                      reads=[ppk, "sinT"], writes=[k1])
                    A("pool", lambda r0=r0, r1=r1, g=g, tsl=tsl: G.tensor_tensor(out=qTa3[:, g, tsl], in0=r0[:], in1=r1[:], op=ALU.add),
                      reads=[k0, k1], writes=[("qTa", g, tc)])

            projection_pass(pa1, wa_d, NWA, 12, emit_group_a, emit_v_a, 1536, 128, "A")
            sc.barrier()
        if dbg:
            sc.dma("sp", lambda: SP.dma_start(out=dbg_d["qta"][:, :], in_=qTa[:]),
                   reads=[("qTa", g, tc) for g in range(6) for tc in range(8)], writes=["dbg_qta"])

        with ExitStack() as pa2:
            if stop_after not in ("A", "B"):
                zt = sb(pa2, "zt", [128, 4 * D], BF16)
                A("pool", lambda: G.memset(zt[:], 0.0), writes=["zt"])
                for b in range(NBLK):
                    sc.dma("sp", lambda b=b: SP.dma_start(out=xs_d[b * MB:(b + 1) * MB, :].rearrange("(s p) d -> p s d", p=128),
                                                        in_=zt[:].rearrange("p (s d) -> p s d", d=D)), reads=["zt"], writes=["xs_d"])
            maska = sb(pa2, "maska", [128, 384], BF16)
            sc.dma("pool", lambda: G.dma_start(out=maska[:], in_=maska_d[:, :]), writes=["maska"])
            esink = sb(pa2, "esink", [128, 8], F32)
            sc.dma("sp", lambda: SP.dma_start(out=esink[:], in_=sink_d[:, :].partition_broadcast(128)), writes=["esink0"])
            A("act", lambda: ACT.activation(out=esink[:], in_=esink[:], func=AF.Exp), reads=["esink0"], writes=["esink"])
            goa = sb(pa2, "goa", [128, 512], F32)
            sc.dma("sp", lambda: SP.dma_start(out=goa[:], in_=goa_d[:, :].partition_broadcast(128)), writes=["goa"])
            pt = [sb(pa2, f"pta{i}", [128, 384], BF16) for i in range(3)]
            den = sb(pa2, "dena", [128, 8], F32)
            oa = sb(pa2, "oa", [128, 512], F32)
            junk2 = sb(pa2, "junk2a", [128, 512], BF16)
            ssq2 = sb(pa2, "ssq2a", [128, 1], F32)
            mixa = [sb(pa2, f"mixa{i}", [128, 512], BF16) for i in range(2)]
            for i in range(NQB):
                tcq = i // 4
                ms = [m for m in range(3) if 0 <= i + m - 1 < NQB]
                po = PS[3 - (i % 2)]
                pok = ("ps", 6 - 2 * (i % 2))
                po4 = po[:].rearrange("p (b x) -> p b x", b=2)[:, :, 0:260].rearrange("p b (h d) -> p b h d", d=65)
                def qk_a(h):
                    g, off = h // 2, (h % 2) * 64
                    kg = 4 + h // 4
                    pst, pstk = bank(h % 3)
                    for m in ms:
                        kb = i + m - 1
                        A("pe", lambda pst=pst, m=m, kb=kb, g=g, off=off, kg=kg, i=i: T.matmul(
                            pst[:, m * 128:(m + 1) * 128], lhsT=qTa3[off:off + 64, kg, kb * 128:(kb + 1) * 128],
                            rhs=qTa3[off:off + 64, g, i * 128:(i + 1) * 128], start=True, stop=False),
                          reads=[("qTa", kg, kb // 4), ("qTa", g, tcq)], writes=[pstk])
                        A("pe", lambda pst=pst, m=m: T.matmul(pst[:, m * 128:(m + 1) * 128], lhsT=ident_b[:],
                                                              rhs=maska[:, m * 128:(m + 1) * 128], start=False, stop=True),
                          reads=["ident_b", "maska"], writes=[pstk])

                qk_a(0)
                for h in range(8):
                    if h + 1 < 8:
                        qk_a(h + 1)
                    pst, pstk = bank(h % 3)
                    ptt = pt[h % 3]
                    ptk = ("pta", h % 3)
                    lo, hi = ms[0] * 128, (ms[-1] + 1) * 128
                    A("act", lambda pst=pst, ptt=ptt, lo=lo, hi=hi: ACT.activation(out=ptt[:, lo:hi], in_=pst[:, lo:hi],
                                                                                   func=AF.Exp, scale=0.125),
                      reads=[pstk], writes=[ptk])
                    for m in ms:
                        kb = i + m - 1
                        A("pe", lambda ptt=ptt, m=m, kb=kb, h=h, ms=ms, po=po: T.matmul(
                            po[:, (h // 4) * 512 + (h % 4) * 65:(h // 4) * 512 + (h % 4) * 65 + 65],
                            lhsT=ptt[:, m * 128:(m + 1) * 128], rhs=vpa4[:, kb, h // 4, :],
                            start=(m == ms[0]), stop=(m == ms[-1])),
                          reads=[ptk, ("vpa", kb)], writes=[pok])
                if i == 4:
                    dump("ptA", pt[7 % 3][:], [128, 384], BF16, [("pta", 7 % 3)])
                    if dbg:
                        podbg = sb(pa2, "podbg", [128, 1024], F32)
                        A("act", lambda: ACT.copy(out=podbg[:], in_=po[:]), reads=[pok], writes=["podbg"])
                        dump("poA", podbg[:], [128, 1024], F32, ["podbg"])
                    dump("vpa", vpa[:], [128, NQB * 2 * 65], BF16, [("vpa", kk) for kk in range(NQB)])
                    dump("esink", esink[:], [128, 8], F32, ["esink"])
                    dump("goa", goa[:], [128, 512], F32, ["goa"])
                A("dve", lambda po4=po4: V.tensor_tensor(out=den[:].rearrange("p (b h) -> p b h", b=2), in0=po4[:, :, :, 64],
                                                         in1=esink[:].rearrange("p (b h) -> p b h", b=2), op=ALU.add),
                  reads=[pok, "esink"], writes=["dena"])
                A("dve", lambda: V.reciprocal(out=den[:], in_=den[:]), reads=["dena"], writes=["dena"])
                A("dve", lambda po4=po4: V.tensor_tensor(
                    out=oa[:].rearrange("p (b h d) -> p b h d", b=2, d=64), in0=po4[:, :, :, 0:64],
                    in1=den[:].rearrange("p (b h) -> p b h", b=2).unsqueeze(3).to_broadcast([128, 2, 4, 64]), op=ALU.mult),
                  reads=[pok, "dena"], writes=["oa"])
                A("act", lambda: ACT.activation(out=junk2[:], in_=oa[:], func=AF.Square, accum_out=ssq2[:]),
                  reads=["oa"], writes=["junk2a", "ssq2a"])
                A("dve", lambda: V.tensor_scalar(out=ssq2[:], in0=ssq2[:], scalar1=1.0 / 512, scalar2=EPS, op0=ALU.mult, op1=ALU.add),
                  reads=["ssq2a"], writes=["ssq2a"])
                A("act", lambda: ACT.activation(out=ssq2[:], in_=ssq2[:], func=AF.Ln), reads=["ssq2a"], writes=["ssq2a"])
                A("act", lambda: ACT.activation(out=ssq2[:], in_=ssq2[:], func=AF.Exp, scale=-0.5), reads=["ssq2a"], writes=["ssq2a"])
                if i == 4:
                    dump("oaA", oa[:], [128, 512], F32, ["oa"])
                    dump("denA", den[:], [128, 8], F32, ["dena"])
                    dump("ssqA", ssq2[:], [128, 1], F32, ["ssq2a"])
                mx = mixa[i % 2]
                A("dve", lambda mx=mx: V.scalar_tensor_tensor(out=mx[:], in0=oa[:], scalar=ssq2[:, 0:1], in1=goa[:],
                                                              op0=ALU.mult, op1=ALU.mult),
                  reads=["oa", "ssq2a", "goa"], writes=[("mixa", i % 2)])
                sc.dma("sp", lambda mx=mx, i=i: SP.dma_start(out=mixa_d[i * 128:(i + 1) * 128, :], in_=mx[:]),
                       reads=[("mixa", i % 2)], writes=[("mixa_d", i)])
            sc.barrier()
    if stop_after == "A":
        sc.finish([("mixa_d", i) for i in range(NQB)] + ["dbg_qta", "dbg_mod"] + dbg_keys)
        es.close()
        return nc

    with ExitStack() as pb_:
        qTb = sb(pb_, "qTb", [128, 8 * S], BF16)
        qTb3 = qTb[:].rearrange("p (g t) -> p g t", t=S)
        vpb = sb(pb_, "vpb", [128, NQB * 8 * 65], BF16)
        vpb4 = vpb[:].rearrange("p (i g d) -> p i g d", g=8, d=65)
        A("pool", lambda: G.memset(vpb[:], 1.0), writes=["vpb_init"])
        with ExitStack() as pb1:
            def emit_v_b(i, pv, pvk):
                A("act", lambda: ACT.copy(out=vpb4[:, i, :, 0:64], in_=pv[:, 0:512].rearrange("p (g d) -> p g d", d=64)),
                  reads=[pvk, "vpb_init"], writes=[("vpb", i)])

            def emit_group_b(tc, hT3, HK, w3, WK):
                for g in range(8):
                    pq, pqk = bank(4 + g % 4)
                    for c in range(8):
                        A("pe", lambda pq=pq, c=c, g=g: T.matmul(pq, lhsT=w3[:, c, g * 128:(g + 1) * 128], rhs=hT3[:, c, :],
                                                                 start=(c == 0), stop=(c == 7)),
                          reads=HK + WK, writes=[pqk])
                    tsl = slice(tc * 512, (tc + 1) * 512)
                    if g % 2 == 0:
                        A("dve", lambda pq=pq, g=g, tsl=tsl: V.tensor_copy(out=qTb3[:, g, tsl], in_=pq), reads=[pqk], writes=[("qTb", g, tc)])
                    else:
                        A("act", lambda pq=pq, g=g, tsl=tsl: ACT.copy(out=qTb3[:, g, tsl], in_=pq), reads=[pqk], writes=[("qTb", g, tc)])

            projection_pass(pb1, wb_d, NWB, 8, emit_group_b, emit_v_b, 1024, 512, "B")
            sc.barrier()

        with ExitStack() as pb2:
            maskb = sb(pb2, "maskb", [128, _NVAR * 896], BF16)
            for v in range(_NVAR):
                sc.dma("pool", lambda v=v: G.dma_start(out=maskb[:, v * 896:(v + 1) * 896], in_=maskb_d[v, :, :]), writes=[("maskb", v)])
            biasu = sb(pb2, "biasu", [128, 8 * 896], F32)
            for h in range(8):
                sc.dma("sp", lambda h=h: SP.dma_start(out=biasu[:, h * 896:(h + 1) * 896], in_=biasu_d[h, :, :]), writes=[("biasu", h)])
            gob = sb(pb2, "gob", [128, 512], F32)
            sc.dma("sp", lambda: SP.dma_start(out=gob[:], in_=gob_d[:, :].partition_broadcast(128)), writes=["gob"])
            tt = [sb(pb2, f"ttb{i}", [128, 896], F32) for i in range(2)]
            pt = [sb(pb2, f"ptb{i}", [128, 896], BF16) for i in range(2)]
            den = sb(pb2, "denb", [128, 8], F32)
            ob = sb(pb2, "ob", [128, 512], F32)
            junk2 = sb(pb2, "junk2b", [128, 512], BF16)
            ssq2 = sb(pb2, "ssq2b", [128, 1], F32)
            mixb = [sb(pb2, f"mixb{i}", [128, 512], BF16) for i in range(2)]
            for i in range(NQB):
                tcq = i // 4
                ms = _KBS[i]
                var = _VAR_OF[i]
                po = PS[3 - (i % 2)]
                pok = ("ps", 6 - 2 * (i % 2))
                po4 = po[:].rearrange("p (b x) -> p b x", b=2)[:, :, 0:260].rearrange("p b (h d) -> p b h d", d=65)
                lo, hi = ms[0] * 128, (ms[-1] + 1) * 128
                def qk_b(h):
                    g, off, kg = h // 2, (h % 2) * 64, 4 + h // 2
                    sl = h % 2
                    pst = PS[sl]
                    pstk = [("ps", 2 * sl), ("ps", 2 * sl + 1)]
                    for m in ms:
                        kb = i + m - 3
                        A("pe", lambda pst=pst, m=m, kb=kb, g=g, off=off, kg=kg, i=i: T.matmul(
                            pst[:, m * 128:(m + 1) * 128], lhsT=qTb3[off:off + 64, kg, kb * 128:(kb + 1) * 128],
                            rhs=qTb3[off:off + 64, g, i * 128:(i + 1) * 128], start=True, stop=False),
                          reads=[("qTb", kg, kb // 4), ("qTb", g, tcq)], writes=pstk)
                        A("pe", lambda pst=pst, m=m, var=var: T.matmul(pst[:, m * 128:(m + 1) * 128], lhsT=ident_b[:],
                                                                       rhs=maskb[:, var * 896 + m * 128:var * 896 + (m + 1) * 128],
                                                                       start=False, stop=True),
                          reads=["ident_b", ("maskb", var)], writes=pstk)

                qk_b(0)
                for h in range(8):
                    if h + 1 < 8:
                        qk_b(h + 1)
                    sl = h % 2
                    pst = PS[sl]
                    pstk = [("ps", 2 * sl), ("ps", 2 * sl + 1)]
                    ttt, ptt = tt[sl], pt[sl]
                    A("dve", lambda pst=pst, ttt=ttt, h=h, lo=lo, hi=hi: V.scalar_tensor_tensor(
                        out=ttt[:, lo:hi], in0=pst[:, lo:hi], scalar=0.125, in1=biasu[:, h * 896 + lo:h * 896 + hi],
                        op0=ALU.mult, op1=ALU.add), reads=pstk + [("biasu", h)], writes=[("ttb", sl)])
                    A("act", lambda ttt=ttt, ptt=ptt, lo=lo, hi=hi: ACT.activation(out=ptt[:, lo:hi], in_=ttt[:, lo:hi], func=AF.Exp),
                      reads=[("ttb", sl)], writes=[("ptb", sl)])
                    for m in ms:
                        kb = i + m - 3
                        A("pe", lambda ptt=ptt, m=m, kb=kb, h=h, ms=ms, po=po: T.matmul(
                            po[:, (h // 4) * 512 + (h % 4) * 65:(h // 4) * 512 + (h % 4) * 65 + 65],
                            lhsT=ptt[:, m * 128:(m + 1) * 128], rhs=vpb4[:, kb, h, :],
                            start=(m == ms[0]), stop=(m == ms[-1])),
                          reads=[("ptb", sl), ("vpb", kb)], writes=[pok])
                A("dve", lambda po4=po4: V.reciprocal(out=den[:].rearrange("p (b h) -> p b h", b=2), in_=po4[:, :, :, 64]),
                  reads=[pok], writes=["denb"])
                A("dve", lambda po4=po4: V.tensor_tensor(
                    out=ob[:].rearrange("p (b h d) -> p b h d", b=2, d=64), in0=po4[:, :, :, 0:64],
                    in1=den[:].rearrange("p (b h) -> p b h", b=2).unsqueeze(3).to_broadcast([128, 2, 4, 64]), op=ALU.mult),
                  reads=[pok, "denb"], writes=["ob"])
                A("act", lambda: ACT.activation(out=junk2[:], in_=ob[:], func=AF.Square, accum_out=ssq2[:]),
                  reads=["ob"], writes=["junk2b", "ssq2b"])
                A("dve", lambda: V.tensor_scalar(out=ssq2[:], in0=ssq2[:], scalar1=1.0 / 512, scalar2=EPS, op0=ALU.mult, op1=ALU.add),
                  reads=["ssq2b"], writes=["ssq2b"])
                A("act", lambda: ACT.activation(out=ssq2[:], in_=ssq2[:], func=AF.Ln), reads=["ssq2b"], writes=["ssq2b"])
                A("act", lambda: ACT.activation(out=ssq2[:], in_=ssq2[:], func=AF.Exp, scale=-0.5), reads=["ssq2b"], writes=["ssq2b"])
                mx = mixb[i % 2]
                A("dve", lambda mx=mx: V.scalar_tensor_tensor(out=mx[:], in0=ob[:], scalar=ssq2[:, 0:1], in1=gob[:],
                                                              op0=ALU.mult, op1=ALU.mult),
                  reads=["ob", "ssq2b", "gob"], writes=[("mixb", i % 2)])
                sc.dma("sp", lambda mx=mx, i=i: SP.dma_start(out=mixb_d[i * 128:(i + 1) * 128, :], in_=mx[:]),
                       reads=[("mixb", i % 2)], writes=[("mixb_d", i)])
            sc.barrier()
    if stop_after == "B":
        sc.finish([("mixa_d", i) for i in range(NQB)] + [("mixb_d", i) for i in range(NQB)] + ["dbg_qta", "dbg_mod"])
        es.close()
        return nc

    rt_ = ExitStack()
    lg_all = sb(rt_, "lg_all", [128, NQB * NE], F32)
    m8_all = sb(rt_, "m8_all", [128, NQB * 8], F32)
    lg3 = lg_all[:].rearrange("p (i e) -> p i e", e=NE)
    m83 = m8_all[:].rearrange("p (i k) -> p i k", k=8)
    with ExitStack() as pc:
        wout = sb(pc, "wout", [128, 8 * D], BF16)
        wout3 = wout[:].rearrange("p (c n) -> p c n", n=D)
        for c in range(8):
            sc.dma("pool", lambda c=c: G.dma_start(out=wout3[:, c, :], in_=wout_d[c * 128:(c + 1) * 128, :]), writes=[("wout", c)])
        WOK = [("wout", c) for c in range(8)]
        wr = sb(pc, "wr", [128, 8 * NE], F32)
        sc.dma("sp", lambda: SP.dma_start(out=wr[:].rearrange("p (c e) -> p c e", e=NE),
                                          in_=wr_d[:, :].rearrange("(c p) e -> p c e", p=128)), writes=["wr"])
        brt = sb(pc, "brt", [1, NE], F32)
        sc.dma("sp", lambda: SP.dma_start(out=brt[:], in_=br_d[:, :]), writes=["brt"])
        mixab = [sb(pc, f"mixab{i}", [128, D], BF16) for i in range(2)]
        xb_ = [sb(pc, f"xc{i}", [128, D], F32) for i in range(2)]
        mixT_ = [sb(pc, f"mixT{j}", [128, D], BF16) for j in range(2)]
        t1_ = [sb(pc, f"t1{j}", [128, D], F32) for j in range(2)]
        x1t = [sb(pc, f"x1t{i}", [128, D], F32) for i in range(2)]
        h2f_ = [sb(pc, f"h2f{j}", [128, D], F32) for j in range(2)]
        h2b = [sb(pc, f"h2b{i}", [128, D], BF16) for i in range(2)]
        h2T_ = [sb(pc, f"h2T{j}", [128, D], F32) for j in range(2)]
        tilesC_ = [(sb(pc, f"junkC{j}", [128, D], BF16), sb(pc, f"ssqC{j}", [128, 1], F32),
                    sb(pc, f"rstdC{j}", [128, 1], F32), sb(pc, f"tmpC{j}", [128, D], F32)) for j in range(2)]
        def c_loads(i):
            s2 = i % 2
            sc.dma("sp", lambda i=i, s2=s2: SP.dma_start(out=mixab[s2][:, 0:512], in_=mixa_d[i * 128:(i + 1) * 128, :]),
                   reads=[("mixa_d", i)], writes=[("mixab", s2, 0)])
            sc.dma("sp", lambda i=i, s2=s2: SP.dma_start(out=mixab[s2][:, 512:1024], in_=mixb_d[i * 128:(i + 1) * 128, :]),
                   reads=[("mixb_d", i)], writes=[("mixab", s2, 1)])
            sc.dma("sp", lambda i=i, s2=s2: SP.dma_start(out=xb_[s2][:], in_=x_d[i * 128:(i + 1) * 128, :]), writes=[("xc", s2)])

        def c_a(i):
            s2 = i % 2
            mixT, t1, h2f, h2T, tilesC = mixT_[s2], t1_[s2], h2f_[s2], h2T_[s2], tilesC_[s2]
            pbT = PS[0][:, s2 * 512:(s2 + 1) * 512].bitcast(BF16)
            for c in range(8):
                A("pe", lambda c=c, pbT=pbT, s2=s2: T.transpose(out=pbT[:, c * 128:(c + 1) * 128],
                                                                in_=mixab[s2][:, c * 128:(c + 1) * 128], identity=ident_b[:]),
                  reads=[("mixab", s2, 0), ("mixab", s2, 1), "ident_b"], writes=[("ps", s2)])
            A("act", lambda pbT=pbT, mixT=mixT: ACT.copy(out=mixT[:], in_=pbT), reads=[("ps", s2)], writes=[("mixT", s2)])
            for n in range(2):
                py, pyk = bank(2 + n)
                for c in range(8):
                    A("pe", lambda py=py, c=c, n=n, mixT=mixT: T.matmul(py, lhsT=mixT[:, c * 128:(c + 1) * 128],
                                                             rhs=wout3[:, c, n * 512:(n + 1) * 512], start=(c == 0), stop=(c == 7)),
                      reads=[("mixT", s2)] + WOK, writes=[pyk])
                A("dve", lambda py=py, n=n, t1=t1: V.tensor_tensor(out=t1[:, n * 512:(n + 1) * 512], in0=py,
                                                            in1=gate_m[:, n * 512:(n + 1) * 512], op=ALU.mult),
                  reads=[pyk] + MODK, writes=[("t1", s2, n)])
            xt = x1t[s2]
            A("dve", lambda xt=xt, s2=s2, t1=t1: V.tensor_tensor(out=xt[:], in0=t1[:], in1=xb_[s2][:], op=ALU.add),
              reads=[("t1", s2, 0), ("t1", s2, 1), ("xc", s2)], writes=[("x1t", s2)])
            sc.dma("sp", lambda xt=xt, i=i: SP.dma_start(out=x1_d[i * 128:(i + 1) * 128, :], in_=xt[:]),
                   reads=[("x1t", s2)], writes=[("x1_d", i)])
            rmsnorm_mod(tilesC, xt[:], [("x1t", s2)], scale1_f, shift_f, out_bf=h2b[s2][:], out_f32=h2f[:],
                        out_keys=[("h2f", s2)], tag="C" + str(s2))
            sc.dma("sp", lambda i=i, s2=s2: SP.dma_start(out=h2_d[i * 128:(i + 1) * 128, :], in_=h2b[s2][:]),
                   reads=[("h2f", s2, "bf")], writes=[("h2_d", i)])

        def c_b(i):
            s2 = i % 2
            mixT, t1, h2f, h2T, tilesC = mixT_[s2], t1_[s2], h2f_[s2], h2T_[s2], tilesC_[s2]
            for r_ in range(2):
                pt_, ptk_ = bank(4 + r_)
                for c4 in range(4):
                    c = r_ * 4 + c4
                    A("pe", lambda pt_=pt_, c=c, c4=c4, h2f=h2f: T.transpose(out=pt_[:, c4 * 128:(c4 + 1) * 128],
                                                                    in_=h2f[:, c * 128:(c + 1) * 128], identity=ident_f[:]),
                      reads=[("h2f", s2), "ident_f"], writes=[ptk_])
                if r_ == 0:
                    A("dve", lambda pt_=pt_, r_=r_, h2T=h2T: V.tensor_copy(out=h2T[:, r_ * 512:(r_ + 1) * 512], in_=pt_), reads=[ptk_], writes=[("h2T", s2, r_)])
                else:
                    A("act", lambda pt_=pt_, r_=r_, h2T=h2T: ACT.copy(out=h2T[:, r_ * 512:(r_ + 1) * 512], in_=pt_), reads=[ptk_], writes=[("h2T", s2, r_)])
            pl, plk = bank(6 + s2)
            for c in range(8):
                A("pe", lambda pl=pl, c=c, h2T=h2T: T.matmul(pl[:, 0:NE], lhsT=h2T[:, c * 128:(c + 1) * 128], rhs=wr[:, c * NE:(c + 1) * NE],
                                                    start=(c == 0), stop=False),
                  reads=[("h2T", s2, 0), ("h2T", s2, 1), "wr"], writes=[plk])
            A("pe", lambda pl=pl: T.matmul(pl[:, 0:NE], lhsT=ones_f[0:1, :], rhs=brt[0:1, :], start=False, stop=True),
              reads=["ones_f", "brt"], writes=[plk])
            A("dve", lambda pl=pl, i=i: V.tensor_copy(out=lg3[:, i, :], in_=pl[:, 0:NE]), reads=[plk], writes=[("lg", i)])
            A("dve", lambda i=i: V.max(out=m83[:, i, :], in_=lg3[:, i, :]), reads=[("lg", i)], writes=[("m8", i)])

        c_loads(0)
        for i in range(NQB):
            if i + 1 < NQB:
                c_loads(i + 1)
            c_a(i)
            if i >= 1:
                c_b(i - 1)
        c_b(NQB - 1)
        sc.barrier()
    LGK = [("lg", i) for i in range(NQB)] + [("m8", i) for i in range(NQB)]

    gw_all = sb(rt_, "gw_all", [128, NQB * NE], F32)
    gw3 = gw_all[:].rearrange("p (i e) -> p i e", e=NE)
    dsel_i = sb(rt_, "dsel_i", [128, 4 * NQB], I32)
    gk = sb(rt_, "gk", [128, 4 * NQB], F32)
    idxw_i = sb(rt_, "idxw_i", [128, NBLK * 8], I32)
    idxg_i = sb(rt_, "idxg_i", [128, NBLK * 8], I32)
    idxb_i = sb(rt_, "idxb_i", [128, NBLK], I32)
    with ExitStack() as pr:
        tri = sb(pr, "tri", [128, 128], F32)
        sc.dma("sp", lambda: SP.dma_start(out=tri[:], in_=tri_d[:, :]), writes=["tri"])
        rowid = sb(pr, "rowid", [128, 8], F32)
        sc.dma("sp", lambda: SP.dma_start(out=rowid[:], in_=rowid_d[:, :]), writes=["rowid"])
        blkth = sb(pr, "blkth", [128, NBLK * NE], F32)
        sc.dma("sp", lambda: SP.dma_start(out=blkth[:], in_=blkth_d[:, :]), writes=["blkth"])
        msk = sb(pr, "msk", [128, NQB * NE], F32)
        msk3 = msk[:].rearrange("p (i e) -> p i e", e=NE)
        ex = sb(pr, "ex", [128, NQB * NE], F32)
        ex3 = ex[:].rearrange("p (i e) -> p i e", e=NE)
        ssum = sb(pr, "ssum", [128, NQB], F32)
        pos = sb(pr, "pos", [128, NQB * NE], F32)
        pos3 = pos[:].rearrange("p (i e) -> p i e", e=NE)
        oh = sb(pr, "oh", [128, NQB * NE], F32)
        oh3 = oh[:].rearrange("p (i e) -> p i e", e=NE)
        prod = sb(pr, "prod", [128, NQB * NE], F32)
        prod3 = prod[:].rearrange("p (i e) -> p i e", e=NE)
        dself = sb(pr, "dself", [128, 4 * NQB], F32)
        cnt = sb(pr, "cnt", [128, NE], F32)
        cs = [sb(pr, f"cs{i}", [128, NE], F32) for i in range(2)]
        padded = sb(pr, "padded", [128, NE], F32)
        pstart = sb(pr, "pstart", [128, NE], F32)
        cmpb = sb(pr, "cmpb", [128, NBLK * NE], F32)
        blke = sb(pr, "blke", [128, NBLK], F32)
        idxwf = sb(pr, "idxwf", [128, NBLK * 8], F32)
        idxbf = sb(pr, "idxbf", [128, NBLK], F32)

        A("dve", lambda: V.tensor_tensor(out=msk3, in0=lg3, in1=m83[:, :, 3:4].to_broadcast([128, NQB, NE]), op=ALU.is_ge),
          reads=LGK, writes=["msk"])
        mskb = sb(pr, "mskb", [128, NQB * NE], BF16)
        mskb3 = mskb[:].rearrange("p (i e) -> p i e", e=NE)
        ones_b = sb(pr, "ones_b", [128, 128], BF16)
        tri_b = sb(pr, "tri_b", [128, 128], BF16)
        A("act", lambda: ACT.copy(out=mskb[:], in_=msk[:]), reads=["msk"], writes=["mskb"])
        A("pool", lambda: G.memset(ones_b[:], 1.0), writes=["ones_b"])
        sc.dma("pool", lambda: G.dma_start(out=tri_b[:], in_=tri_d[:, :]), writes=["tri_b"])
        A("dve", lambda: V.tensor_tensor(out=ex3, in0=lg3, in1=m83[:, :, 0:1].to_broadcast([128, NQB, NE]), op=ALU.subtract),
          reads=LGK, writes=["ex"])
        A("act", lambda: ACT.activation(out=ex[:], in_=ex[:], func=AF.Exp), reads=["ex"], writes=["ex"])
        A("dve", lambda: V.tensor_tensor(out=ex[:], in0=ex[:], in1=msk[:], op=ALU.mult), reads=["ex", "msk"], writes=["ex"])
        A("dve", lambda: V.reduce_sum(out=ssum[:], in_=ex3, axis=AX.X), reads=["ex"], writes=["ssum"])
        A("dve", lambda: V.reciprocal(out=ssum[:], in_=ssum[:]), reads=["ssum"], writes=["ssum"])
        A("dve", lambda: V.tensor_tensor(out=gw3, in0=ex3, in1=ssum[:].unsqueeze(2).to_broadcast([128, NQB, NE]), op=ALU.mult),
          reads=["ex", "ssum"], writes=["gw"])
        for half in range(2):
            pp_, ppk_ = bank(half)
            for ii in range(16):
                i = half * 16 + ii
                for j in range(i):
                    A("pe", lambda pp_=pp_, ii=ii, j=j: T.matmul(pp_[:, ii * NE:(ii + 1) * NE], lhsT=ones_b[:], rhs=mskb3[:, j, :],
                                                                 start=(j == 0), stop=False),
                      reads=["ones_b", "mskb"], writes=[ppk_])
                A("pe", lambda pp_=pp_, ii=ii, i=i: T.matmul(pp_[:, ii * NE:(ii + 1) * NE], lhsT=tri_b[:], rhs=mskb3[:, i, :],
                                                             start=(i == 0), stop=True),
                  reads=["tri_b", "mskb"], writes=[ppk_])
            A("dve", lambda pp_=pp_, half=half: V.tensor_copy(out=pos[:, half * 512:(half + 1) * 512], in_=pp_),
              reads=[ppk_], writes=[("pos", half)])
        pc_, pck_ = bank(2)
        for j in range(NQB):
            A("pe", lambda j=j: T.matmul(pc_[:, 0:NE], lhsT=ones_b[:], rhs=mskb3[:, j, :], start=(j == 0), stop=(j == NQB - 1)),
              reads=["ones_b", "mskb"], writes=[pck_])
        A("dve", lambda: V.tensor_copy(out=cnt[:], in_=pc_[:, 0:NE]), reads=[pck_], writes=["cnt"])
        nbt = sb(pr, "nbt", [128, NE * 8], F32)
        A("dve", lambda: V.tensor_tensor(out=nbt[:].rearrange("p (e j) -> p e j", j=8),
                                         in0=cnt[:].unsqueeze(2).to_broadcast([128, NE, 8]),
                                         in1=blkth[:, 0:8 * NE].rearrange("p (b e) -> p e b", e=NE), op=ALU.is_gt),
          reads=["cnt", "blkth"], writes=["nbt"])
        A("dve", lambda: V.reduce_sum(out=padded[:], in_=nbt[:].rearrange("p (e j) -> p e j", j=8), axis=AX.X), reads=["nbt"], writes=["padded"])
        A("dve", lambda: V.tensor_scalar(out=padded[:], in0=padded[:], scalar1=float(MB), scalar2=None, op0=ALU.mult),
          reads=["padded"], writes=["padded"])
        A("dve", lambda: V.tensor_copy(out=cs[0][:], in_=padded[:]), reads=["padded"], writes=[("cs", 0)])
        cur = 0
        for sft in (1, 2, 4, 8, 16):
            nxt = 1 - cur
            A("dve", lambda cur=cur, nxt=nxt, sft=sft: V.tensor_copy(out=cs[nxt][:, 0:sft], in_=cs[cur][:, 0:sft]),
              reads=[("cs", cur)], writes=[("cs", nxt)])
            A("dve", lambda cur=cur, nxt=nxt, sft=sft: V.tensor_tensor(out=cs[nxt][:, sft:NE], in0=cs[cur][:, sft:NE],
                                                                       in1=cs[cur][:, 0:NE - sft], op=ALU.add),
              reads=[("cs", cur), ("cs", nxt)], writes=[("cs", nxt)])
            cur = nxt
        pend = cs[cur]
        pendk = ("cs", cur)
        A("dve", lambda: V.tensor_tensor(out=pstart[:], in0=pend[:], in1=padded[:], op=ALU.subtract), reads=[pendk, "padded"], writes=["pstart"])
        A("dve", lambda: V.tensor_tensor(out=pos3, in0=pos3, in1=pstart[:].unsqueeze(1).to_broadcast([128, NQB, NE]), op=ALU.add),
          reads=[("pos", 0), ("pos", 1), "pstart"], writes=["dest"])
        for k in range(4):
            A("dve", lambda k=k: V.tensor_tensor(out=oh3, in0=lg3, in1=m83[:, :, k:k + 1].to_broadcast([128, NQB, NE]), op=ALU.is_equal),
              reads=LGK, writes=["oh"])
            A("dve", lambda: V.tensor_tensor(out=prod[:], in0=oh[:], in1=pos[:], op=ALU.mult), reads=["oh", "dest"], writes=["prod"])
            A("dve", lambda k=k: V.reduce_sum(out=dself[:, k * NQB:(k + 1) * NQB], in_=prod3, axis=AX.X), reads=["prod"], writes=[("dself", k)])
            A("dve", lambda: V.tensor_tensor(out=prod[:], in0=oh[:], in1=gw_all[:], op=ALU.mult), reads=["oh", "gw"], writes=["prod"])
            A("dve", lambda k=k: V.reduce_sum(out=gk[:, k * NQB:(k + 1) * NQB], in_=prod3, axis=AX.X), reads=["prod"], writes=[("gk", k)])
        A("dve", lambda: V.tensor_copy(out=dsel_i[:], in_=dself[:]), reads=[("dself", k) for k in range(4)], writes=["dsel_i"])
        A("dve", lambda: V.tensor_tensor(out=cmpb[:].rearrange("p (b e) -> p b e", e=NE),
                                         in0=pend[:].unsqueeze(1).to_broadcast([128, NBLK, NE]),
                                         in1=blkth[:].rearrange("p (b e) -> p b e", e=NE), op=ALU.is_le),
          reads=[pendk, "blkth"], writes=["cmpb"])
        A("dve", lambda: V.reduce_sum(out=blke[:], in_=cmpb[:].rearrange("p (b e) -> p b e", e=NE), axis=AX.X), reads=["cmpb"], writes=["blke"])
        A("dve", lambda: V.tensor_scalar(out=blke[:], in0=blke[:], scalar1=float(NE - 1), scalar2=None, op0=ALU.min), reads=["blke"], writes=["blke"])
        A("dve", lambda: V.scalar_tensor_tensor(out=idxwf[:].rearrange("p (b c) -> p b c", c=8),
                                                in0=blke[:].unsqueeze(2).to_broadcast([128, NBLK, 8]), scalar=float(D),
                                                in1=rowid[:].unsqueeze(1).to_broadcast([128, NBLK, 8]), op0=ALU.mult, op1=ALU.add),
          reads=["blke", "rowid"], writes=["idxwf"])
        nused = sb(pr, "nused", [128, NBLK], F32)
        A("dve", lambda: V.tensor_scalar(out=nused[:], in0=blkth[:].rearrange("p (b e) -> p b e", e=NE)[:, :, 0],
                                         scalar1=pend[:, NE - 1:NE], scalar2=None, op0=ALU.is_ge),
          reads=[pendk, "blkth"], writes=["nused"])
        sameb = sb(pr, "sameb", [128, NBLK], F32)
        A("dve", lambda: V.memset(sameb[:], 0.0), writes=["sameb"])
        A("dve", lambda: V.tensor_tensor(out=sameb[:, 2:NBLK], in0=blke[:, 2:NBLK], in1=blke[:, 0:NBLK - 2], op=ALU.is_equal),
          reads=["blke", "sameb"], writes=["sameb"])
        A("dve", lambda: V.tensor_tensor(out=nused[:], in0=nused[:], in1=sameb[:], op=ALU.max), reads=["nused", "sameb"], writes=["nused"])
        idxgf = sb(pr, "idxgf", [128, NBLK * 8], F32)
        A("dve", lambda: V.scalar_tensor_tensor(out=idxgf[:].rearrange("p (b c) -> p b c", c=8),
                                                in0=nused[:].unsqueeze(2).to_broadcast([128, NBLK, 8]), scalar=40000.0,
                                                in1=idxwf[:].rearrange("p (b c) -> p b c", c=8), op0=ALU.mult, op1=ALU.add),
          reads=["nused", "idxwf"], writes=["idxgf"])
        A("dve", lambda: V.tensor_copy(out=idxg_i[:], in_=idxgf[:]), reads=["idxgf"], writes=["idxg_i"])
        A("dve", lambda: V.tensor_copy(out=idxw_i[:], in_=idxwf[:]), reads=["idxwf"], writes=["idxw_i"])
        A("dve", lambda: V.scalar_tensor_tensor(out=idxbf[:], in0=blke[:], scalar=128.0, in1=rowid[:, 0:1].to_broadcast([128, NBLK]),
                                                op0=ALU.mult, op1=ALU.add), reads=["blke", "rowid"], writes=["idxbf"])
        A("dve", lambda: V.scalar_tensor_tensor(out=idxbf[:], in0=nused[:], scalar=40000.0, in1=idxbf[:], op0=ALU.mult, op1=ALU.add),
          reads=["nused", "idxbf"], writes=["idxbf"])
        A("dve", lambda: V.tensor_copy(out=idxb_i[:], in_=idxbf[:]), reads=["idxbf"], writes=["idxb_i"])
        if dbg:
            sc.dma("sp", lambda: SP.dma_start(out=dbg_d["gw"][:, :], in_=gw_all[:]), reads=["gw"], writes=["dbg_gw"])
            sc.dma("sp", lambda: SP.dma_start(out=dbg_d["dsel"][:, :], in_=dsel_i[:]), reads=["dsel_i"], writes=["dbg_dsel"])
            sc.dma("sp", lambda: SP.dma_start(out=dbg_d["blke"][:, :], in_=blke[:]), reads=["blke"], writes=["dbg_blke"])
        h2r = [sb(pr, f"h2r{i}", [128, D], BF16) for i in range(4)]
        for i in range(NQB):
            s4 = i % 4
            sc.dma("sp", lambda i=i, s4=s4: SP.dma_start(out=h2r[s4][:], in_=h2_d[i * 128:(i + 1) * 128, :]),
                   reads=[("h2_d", i)], writes=[("h2r", s4)])
            for k in range(4):
                sc.dma("pool", lambda i=i, k=k, s4=s4: G.indirect_dma_start(
                    out=xs_d[:, :], out_offset=bass.IndirectOffsetOnAxis(ap=dsel_i[:, k * NQB + i:k * NQB + i + 1], axis=0),
                    in_=h2r[s4][:], in_offset=None), reads=[("h2r", s4), "dsel_i"], writes=["xs_d"])
        sc.barrier()
    if stop_after == "R":
        sc.finish(["dbg_gw", "dbg_dsel", "dbg_blke", "xs_d"] + [("x1_d", i) for i in range(NQB)])
        rt_.close()
        es.close()
        return nc

    with ExitStack() as pm:
        wgu = [sb(pm, f"wgu{i}", [128, 8 * 2 * DFF], BF16) for i in range(2)]
        wdn = [sb(pm, f"wdn{i}", [128, 8 * D], BF16) for i in range(2)]
        bgu = [sb(pm, f"bgu{i}", [128, 16], F32) for i in range(2)]
        xst = [sb(pm, "xst0", [128, 4 * D], BF16)]
        xsT = sb(pm, "xsT", [128, 8 * MB], BF16)
        xsT3 = xsT[:].rearrange("p (c t) -> p c t", t=MB)
        actT_ = [sb(pm, f"actT{j}", [128, 8 * MB], BF16) for j in range(2)]
        actT3_ = [a_[:].rearrange("p (f t) -> p f t", t=MB) for a_ in actT_]
        gt = [sb(pm, f"gt{i}", [128, MB], F32) for i in range(2)]
        sg = [sb(pm, f"sg{i}", [128, MB], F32) for i in range(2)]
        ut = [sb(pm, f"ut{i}", [128, MB], F32) for i in range(2)]
        yst = [sb(pm, f"yst{i}", [128, D], F32) for i in range(4)]

        bc_reg = [G.to_reg(NE * D - 1), G.to_reg(NE * 128 - 1)]

        def load_weights(b, which):
            sl = b % 2
            w3g = wgu[sl][:].rearrange("p (c n) -> p c n", n=2 * DFF)
            w3d = wdn[sl][:].rearrange("p (c n) -> p c n", n=D)
            for c in range(8 if which == "gu" else 0):
                sc.dma("pool", lambda c=c, w3g=w3g, b=b: G.indirect_dma_start(
                    out=w3g[:, c, :], out_offset=None, in_=wgu_d[:, :],
                    in_offset=bass.IndirectOffsetOnAxis(ap=idxg_i[:, b * 8 + c:b * 8 + c + 1], axis=0),
                    bounds_check=bc_reg[0], oob_is_err=False),
                    reads=["idxg_i"], writes=[("wgu", sl, c)])
            for c in range(8 if which == "wd" else 0):
                sc.dma("pool", lambda c=c, w3d=w3d, b=b: G.indirect_dma_start(
                    out=w3d[:, c, :], out_offset=None, in_=wd_d[:, :],
                    in_offset=bass.IndirectOffsetOnAxis(ap=idxg_i[:, b * 8 + c:b * 8 + c + 1], axis=0),
                    bounds_check=bc_reg[0], oob_is_err=False),
                    reads=["idxg_i"], writes=[("wdn", sl, c)])
            if which == "gu":
              sc.dma("pool", lambda b=b, sl=sl: G.indirect_dma_start(
                out=bgu[sl][:], out_offset=None, in_=bgu_d[:, :],
                in_offset=bass.IndirectOffsetOnAxis(ap=idxb_i[:, b:b + 1], axis=0),
                bounds_check=bc_reg[1], oob_is_err=False), reads=["idxb_i"], writes=[("bgu", sl)])

        def load_x(b):
            sl = 0
            sc.dma("sp", lambda b=b, sl=sl: SP.dma_start(out=xst[sl][:].rearrange("p (s d) -> p s d", d=D),
                                                        in_=xs_d[b * MB:(b + 1) * MB, :].rearrange("(s p) d -> p s d", p=128)),
                   reads=["xs_d"], writes=[("xst", sl)])

        for j in range(2):
            A("dve", lambda j=j: V.memset(wgu[j][:], 0.0), writes=[("wgu", j, c) for c in range(8)])
            A("dve", lambda j=j: V.memset(wdn[j][:], 0.0), writes=[("wdn", j, c) for c in range(8)])
            A("dve", lambda j=j: V.memset(bgu[j][:], 0.0), writes=[("bgu", j)])
        load_weights(0, "gu")
        load_x(0)

        def down_proj(b):
            sl = b % 2
            w3d = wdn[sl][:].rearrange("p (c n) -> p c n", n=D)
            WDK = [("wdn", sl, c) for c in range(8)]
            aT3 = actT3_[sl]
            AK = [("actT", sl, f) for f in range(8)]
            for s_ in range(4):
                ys_ = yst[s_]
                for n in range(2):
                    py, pyk = bank(6 + n)
                    for f in range(8):
                        A("pe", lambda py=py, f=f, s_=s_, n=n, w3d=w3d, aT3=aT3: T.matmul(
                            py, lhsT=aT3[:, f, s_ * 128:(s_ + 1) * 128], rhs=w3d[:, f, n * 512:(n + 1) * 512],
                            start=(f == 0), stop=(f == 7)), reads=AK + WDK, writes=[pyk])
                    A("act", lambda py=py, ys_=ys_, n=n: ACT.copy(out=ys_[:, n * 512:(n + 1) * 512], in_=py),
                      reads=[pyk], writes=[("yst", s_, n)])
                sc.dma("sp", lambda ys_=ys_, b=b, s_=s_: SP.dma_start(out=ys_d[b * MB + s_ * 128:b * MB + (s_ + 1) * 128, :], in_=ys_[:]),
                       reads=[("yst", s_, 0), ("yst", s_, 1)], writes=["ys_d"])

        for b in range(NBLK):
            sl = b % 2
            w3g = wgu[sl][:].rearrange("p (c n) -> p c n", n=2 * DFF)
            WGK = [("wgu", sl, c) for c in range(8)]
            aT3 = actT3_[sl]
            for s_ in range(4):
                pbT = PS[0][:, (s_ % 2) * 512:(s_ % 2 + 1) * 512].bitcast(BF16)
                pkT = ("ps", s_ % 2)
                for c in range(8):
                    A("pe", lambda c=c, pbT=pbT, s_=s_: T.transpose(
                        out=pbT[:, c * 128:(c + 1) * 128], in_=xst[0][:, s_ * D + c * 128:s_ * D + (c + 1) * 128], identity=ident_b[:]),
                      reads=[("xst", 0), "ident_b"], writes=[pkT])
                if s_ % 2 == 0:
                    A("dve", lambda pbT=pbT, s_=s_: V.tensor_copy(out=xsT3[:, :, s_ * 128:(s_ + 1) * 128],
                                                                  in_=pbT.rearrange("p (c t) -> p c t", t=128)),
                      reads=[pkT], writes=[("xsT", s_)])
                else:
                    A("act", lambda pbT=pbT, s_=s_: ACT.copy(out=xsT3[:, :, s_ * 128:(s_ + 1) * 128],
                                                             in_=pbT.rearrange("p (c t) -> p c t", t=128)),
                      reads=[pkT], writes=[("xsT", s_)])
            if b + 1 < NBLK:
                load_x(b + 1)
            if b >= 1:
                down_proj(b - 1)
            load_weights(b, "wd")
            if b + 1 < NBLK:
                load_weights(b + 1, "gu")
            XK = [("xsT", s_) for s_ in range(4)]
            for f in range(8):
                e2 = f % 2
                pg, pgk = bank(2 + e2 * 2)
                pu, puk = bank(3 + e2 * 2)
                for (pb, pk, col) in ((pg, pgk, f * 128), (pu, puk, DFF + f * 128)):
                    for c in range(8):
                        A("pe", lambda pb=pb, c=c, col=col, w3g=w3g: T.matmul(pb, lhsT=w3g[:, c, col:col + 128], rhs=xsT3[:, c, :],
                                                                              start=(c == 0), stop=(c == 7)),
                          reads=XK + WGK, writes=[pk])
                g_, s__, u_ = gt[e2], sg[e2], ut[e2]
                A("dve", lambda pg=pg, g_=g_, f=f, sl=sl: V.tensor_scalar(out=g_[:], in0=pg, scalar1=bgu[sl][:, f:f + 1], scalar2=7.0,
                                                                          op0=ALU.add, op1=ALU.min),
                  reads=[pgk, ("bgu", sl)], writes=[("gt", e2)])
                A("act", lambda g_=g_, s__=s__: ACT.activation(out=s__[:], in_=g_[:], func=AF.Sigmoid, scale=1.702),
                  reads=[("gt", e2)], writes=[("sg", e2)])
                A("dve", lambda pu=pu, u_=u_, f=f, sl=sl: V.tensor_scalar(out=u_[:], in0=pu, scalar1=bgu[sl][:, 8 + f:9 + f], scalar2=7.0,
                                                                          op0=ALU.add, op1=ALU.min),
                  reads=[puk, ("bgu", sl)], writes=[("ut", e2)])
                A("dve", lambda u_=u_: V.tensor_scalar(out=u_[:], in0=u_[:], scalar1=-7.0, scalar2=1.0, op0=ALU.max, op1=ALU.add),
                  reads=[("ut", e2)], writes=[("ut", e2)])
                A("dve", lambda g_=g_, s__=s__: V.tensor_tensor(out=g_[:], in0=g_[:], in1=s__[:], op=ALU.mult),
                  reads=[("gt", e2), ("sg", e2)], writes=[("gt", e2)])
                A("dve", lambda g_=g_, u_=u_, f=f, aT3=aT3: V.tensor_tensor(out=aT3[:, f, :], in0=g_[:], in1=u_[:], op=ALU.mult),
                  reads=[("gt", e2), ("ut", e2)], writes=[("actT", sl, f)])
            if b % 8 == 7:
                sc.flush()
        down_proj(NBLK - 1)
        sc.barrier()

    with ExitStack() as pf:
        bd = sb(pf, "bd", [NE, D], F32)
        sc.dma("sp", lambda: SP.dma_start(out=bd[:], in_=bd_d[:, :]), writes=["bd"])
        gfin = sb(pf, "gfin", [128, D], F32)
        sc.dma("sp", lambda: SP.dma_start(out=gfin[:], in_=gfin_d[:, :].partition_broadcast(128)), writes=["gfin"])
        x1f = [sb(pf, f"x1f{i}", [128, D], F32) for i in range(2)]
        yk_ = [[sb(pf, f"yk{j}_{i}", [128, D], F32) for i in range(4)] for j in range(2)]
        acc_ = [sb(pf, f"acc{j}", [128, D], F32) for j in range(2)]
        gwT_ = [sb(pf, f"gwT{j}", [NE, 128], F32) for j in range(2)]
        junkF = sb(pf, "junkF", [128, D], BF16)
        ssqF = sb(pf, "ssqF", [128, 1], F32)
        ot = [sb(pf, f"ot{i}", [128, D], F32) for i in range(2)]
        def f_loads(i):
            s2 = i % 2
            yk = yk_[s2]
            sc.dma("sp", lambda i=i, s2=s2: SP.dma_start(out=x1f[s2][:], in_=x1_d[i * 128:(i + 1) * 128, :]),
                   reads=[("x1_d", i)], writes=[("x1f", s2)])
            for k in range(4):
                sc.dma("pool", lambda i=i, k=k, yk=yk: G.indirect_dma_start(
                    out=yk[k][:], out_offset=None, in_=ys_d[:, :],
                    in_offset=bass.IndirectOffsetOnAxis(ap=dsel_i[:, k * NQB + i:k * NQB + i + 1], axis=0)),
                    reads=["ys_d", "dsel_i"], writes=[("yk", s2, k)])

        for i in range(NQB):
            s2 = i % 2
            yk, acc, gwT = yk_[s2], acc_[s2], gwT_[s2]
            ACCK, GWTK = ("acc", s2), ("gwT", s2)
            if i == 0:
                f_loads(0)
            if i + 1 < NQB:
                f_loads(i + 1)
            pt_, ptk_ = bank(s2)
            A("pe", lambda pt_=pt_, i=i: T.transpose(out=pt_[0:NE, 0:128], in_=gw3[:, i, :], identity=ident_f[:]),
              reads=["gw", "ident_f"], writes=[ptk_])
            A("act", lambda pt_=pt_, gwT=gwT: ACT.copy(out=gwT[:], in_=pt_[0:NE, 0:128]), reads=[ptk_], writes=[GWTK])
            pbs = []
            for n in range(2):
                pb, pbk = bank(2 + 2 * s2 + n)
                A("pe", lambda pb=pb, n=n, gwT=gwT: T.matmul(pb, lhsT=gwT[:], rhs=bd[:, n * 512:(n + 1) * 512], start=True, stop=True),
                  reads=[GWTK, "bd"], writes=[pbk])
                pbs.append((pb, pbk))
            A("dve", lambda i=i, acc=acc, yk=yk: V.tensor_scalar(out=acc[:], in0=yk[0][:], scalar1=gk[:, i:i + 1], scalar2=None, op0=ALU.mult),
              reads=[("yk", s2, 0)] + [("gk", k) for k in range(4)], writes=[ACCK])
            for k in range(1, 4):
                A("dve", lambda i=i, k=k, acc=acc, yk=yk: V.scalar_tensor_tensor(out=acc[:], in0=yk[k][:], scalar=gk[:, k * NQB + i:k * NQB + i + 1],
                                                                 in1=acc[:], op0=ALU.mult, op1=ALU.add),
                  reads=[("yk", s2, k), ACCK] + [("gk", kk) for kk in range(4)], writes=[ACCK])
            for n in range(2):
                pb, pbk = pbs[n]
                A("dve", lambda pb=pb, n=n, acc=acc: V.tensor_tensor(out=acc[:, n * 512:(n + 1) * 512], in0=pb, in1=acc[:, n * 512:(n + 1) * 512], op=ALU.add),
                  reads=[pbk, ACCK], writes=[ACCK])
            A("dve", lambda acc=acc: V.tensor_tensor(out=acc[:], in0=acc[:], in1=gate_f, op=ALU.mult), reads=[ACCK] + MODK, writes=[ACCK])
            A("dve", lambda s2=s2, acc=acc: V.tensor_tensor(out=acc[:], in0=acc[:], in1=x1f[s2][:], op=ALU.add), reads=[ACCK, ("x1f", s2)], writes=[ACCK])
            A("act", lambda acc=acc: ACT.activation(out=junkF[:], in_=acc[:], func=AF.Square, accum_out=ssqF[:]), reads=[ACCK], writes=["junkF", "ssqF"])
            A("dve", lambda: V.tensor_scalar(out=ssqF[:], in0=ssqF[:], scalar1=1.0 / D, scalar2=EPS, op0=ALU.mult, op1=ALU.add),
              reads=["ssqF"], writes=["ssqF"])
            A("act", lambda: ACT.activation(out=ssqF[:], in_=ssqF[:], func=AF.Ln), reads=["ssqF"], writes=["ssqF"])
            A("act", lambda: ACT.activation(out=ssqF[:], in_=ssqF[:], func=AF.Exp, scale=-0.5), reads=["ssqF"], writes=["ssqF"])
            o_ = ot[s2]
            A("dve", lambda o_=o_, acc=acc: V.scalar_tensor_tensor(out=o_[:], in0=acc[:], scalar=ssqF[:, 0:1], in1=gfin[:], op0=ALU.mult, op1=ALU.mult),
              reads=[ACCK, "ssqF", "gfin"], writes=[("ot", s2)])
            sc.dma("sp", lambda o_=o_, i=i: SP.dma_start(out=out_d[i * 128:(i + 1) * 128, :], in_=o_[:]),
                   reads=[("ot", s2)], writes=[("out_d", i)])
        sc.barrier()
    sc.finish([("out_d", i) for i in range(NQB)])
    rt_.close()
    es.close()
    return nc


def _prep_inputs(inputs):
    f = lambda a: np.ascontiguousarray(np.asarray(a, dtype=np.float32))
    x = f(inputs["x"])
    c = f(inputs["c"])
    w_in = f(inputs["w_in"])[0]
    wa, wb = _layout_w_in(w_in)
    cosT, sinT = _rope_tables()
    dr, co = _bias_index()
    rpb = f(inputs["rpb"])[0]
    biasu = np.ascontiguousarray(rpb[:, dr, co].reshape(8, 128, 7 * 128))
    bgu = f(inputs["b_gate_up"])[0]
    bgu_l = np.ascontiguousarray(bgu.reshape(NE, 16, 128).transpose(0, 2, 1).reshape(NE * 128, 16))
    rowid = (np.arange(8)[None, :] * 128 + np.arange(128)[:, None]).astype(np.float32)
    blkth = np.broadcast_to((np.arange(NBLK, dtype=np.float32) * MB)[None, :, None], (128, NBLK, NE)).reshape(128, NBLK * NE)
    shared = {
        "w_ada": f(inputs["w_ada"])[0], "b_ada": f(inputs["b_ada"]).reshape(1, 6 * D),
        "w_a": wa, "w_b": wb, "sink": f(inputs["sink"]).reshape(1, 8), "biasu": biasu,
        "g_out_a": f(inputs["g_out_a"]).reshape(1, 512), "g_out_b": f(inputs["g_out_b"]).reshape(1, 512),
        "w_out": f(inputs["w_out"])[0], "w_router": f(inputs["w_router"])[0], "b_router": f(inputs["b_router"]).reshape(1, NE),
        "w_gate_up": f(inputs["w_gate_up"])[0].reshape(NE * D, 2 * DFF), "b_gate_up": bgu_l,
        "w_down": f(inputs["w_down"])[0].reshape(NE * DFF, D), "b_down": f(inputs["b_down"])[0],
        "g_final": f(inputs["g_final"]).reshape(1, D), "cosT": cosT, "sinT": sinT,
        "maska": np.ascontiguousarray(_mask_a().reshape(128, 384)),
        "maskb": np.ascontiguousarray(_MASKB.reshape(_NVAR, 128, 7 * 128)),
        "ident": np.eye(128, dtype=np.float32), "tri": np.triu(np.ones((128, 128), np.float32), 1),
        "rowid": rowid, "blkth": np.ascontiguousarray(blkth),
    }
    in_maps = []
    for b in range(8):
        m = dict(shared)
        m["x"] = x[b]
        m["cT"] = np.ascontiguousarray(c[b].reshape(8, 128).T)
        in_maps.append(m)
    return in_maps


def kernel(**inputs):
    in_maps = _prep_inputs(inputs)
    nc = build_program()
    res = run_bass_kernel_spmd(nc, in_maps, core_ids=list(range(8)))
    return np.stack([np.asarray(r["out"], dtype=np.float32) for r in res.results], axis=0)
```

```python
import bisect
from contextlib import ExitStack

import numpy as np
import concourse.bass as bass
import concourse.mybir as mybir
from concourse.bass_utils import run_bass_kernel_spmd

F32 = mybir.dt.float32
BF16 = mybir.dt.bfloat16
I32 = mybir.dt.int32
AF = mybir.ActivationFunctionType
ALU = mybir.AluOpType
AX = mybir.AxisListType

S = 4096
D = 1024
NQB = 32
NE = 32
DFF = 1024
EPS = 1e-5
MASKV = -240000.0
MB = 512
NBLK = 63
NSLOT = NBLK * MB
THETA = 500000.0


class _Op:
    __slots__ = ("eng", "fn", "deps", "dma", "need_inc", "target")


class Sched:
    def __init__(self, nc, es, nchan=10):
        self.nc = nc
        self.engs = dict(pe=nc.tensor, act=nc.scalar, dve=nc.vector, pool=nc.gpsimd, sp=nc.sync)
        self.sem = {e: es.enter_context(nc.semaphore("sem_" + e)) for e in ("pe", "act", "dve", "pool")}
        nch = {"sp": 12, "pool": 28}
        self.chan = {q: [es.enter_context(nc.semaphore(f"ch_{q}{i}")) for i in range(nch[q])] for q in ("sp", "pool")}
        self.chan_cnt = {q: [0] * nch[q] for q in ("sp", "pool")}
        self.chan_next = {q: 0 for q in ("sp", "pool")}
        self.ops = []
        self.flushed = 0
        self.last_writer = {}
        self.readers = {}
        self.cnt = {e: 0 for e in self.sem}
        self.incs = {e: ([], []) for e in self.sem}
        self.waited = {}

    def add(self, eng, fn, reads=(), writes=(), dma=False):
        op = _Op()
        op.eng, op.fn, op.dma, op.need_inc, op.target = eng, fn, dma, False, None
        deps = set()
        for r in reads:
            w = self.last_writer.get(r)
            if w is not None:
                deps.add(w)
        for w_ in writes:
            w = self.last_writer.get(w_)
            if w is not None:
                deps.add(w)
            for r in self.readers.get(w_, ()):
                deps.add(r)
        idx = len(self.ops)
        deps.discard(idx)
        op.deps = deps
        for r in reads:
            self.readers.setdefault(r, []).append(idx)
        for w_ in writes:
            self.last_writer[w_] = idx
            self.readers[w_] = []
        self.ops.append(op)
        return idx

    def dma(self, q, fn, reads=(), writes=()):
        return self.add(q, fn, reads, writes, dma=True)

    def _wait(self, ceng, sem, val):
        key = (ceng, id(sem))
        if self.waited.get(key, 0) >= val:
            return
        self.waited[key] = val
        self.engs[ceng].wait_ge(sem, val)

    def flush(self, final=False):
        ops = self.ops
        lo, hi = self.flushed, len(ops)
        last_of = {}
        for i in range(lo, hi):
            op = ops[i]
            if not op.dma:
                last_of[op.eng] = i
            for d in op.deps:
                dop = ops[d]
                if d >= lo and not dop.dma:
                    if dop.eng == "pe" and op.eng == "pe" and not op.dma:
                        continue
                    dop.need_inc = True
        for e, i in last_of.items():
            ops[i].need_inc = True
        for i in range(lo, hi):
            op = ops[i]
            ceng = op.eng
            if op.dma:
                q = ceng
                c = self.chan_next[q]
                self.chan_next[q] = (c + 1) % len(self.chan[q])
                csem = self.chan[q][c]
                if self.chan_cnt[q][c] > 0:
                    self._wait(q, csem, 16 * self.chan_cnt[q][c])
            for d in sorted(op.deps):
                dop = ops[d]
                if dop.dma:
                    self._wait(ceng, dop.target[0], dop.target[1])
                else:
                    if dop.eng == "pe" and ceng == "pe" and not op.dma:
                        continue
                    il, cl = self.incs[dop.eng]
                    if dop.target is not None:
                        tv = dop.target[1]
                    else:
                        j = bisect.bisect_left(il, d)
                        if j < len(il):
                            tv = cl[j]
                        else:
                            raise RuntimeError("no covering inc")
                    self._wait(ceng, self.sem[dop.eng], tv)
            ins = op.fn()
            if op.dma:
                self.chan_cnt[q][c] += 1
                ins.then_inc(csem, 16)
                op.target = (csem, 16 * self.chan_cnt[q][c])
            elif op.need_inc:
                self.cnt[ceng] += 1
                ins.then_inc(self.sem[ceng], 1)
                op.target = (self.sem[ceng], self.cnt[ceng])
                self.incs[ceng][0].append(i)
                self.incs[ceng][1].append(self.cnt[ceng])
            op.fn = None
        self.flushed = hi

    def barrier(self):
        self.flush()
        for ceng in ("pe", "act", "dve", "pool", "sp"):
            for e, sem in self.sem.items():
                if self.cnt[e] > 0:
                    self._wait(ceng, sem, self.cnt[e])
            for q in ("sp", "pool"):
                for c, csem in enumerate(self.chan[q]):
                    if self.chan_cnt[q][c] > 0:
                        self._wait(ceng, csem, 16 * self.chan_cnt[q][c])

    def finish(self, out_keys):
        self.flush()
        for k in out_keys:
            w = self.last_writer.get(k)
            if w is not None:
                t = self.ops[w].target
                self._wait("sp", t[0], t[1])
        for q in ("sp", "pool"):
            for c, csem in enumerate(self.chan[q]):
                if self.chan_cnt[q][c] > 0:
                    self._wait("sp", csem, 16 * self.chan_cnt[q][c])


def _rope_tables():
    inv_freq = (np.float32(THETA) ** (-np.arange(0, 16, 2, dtype=np.float32) / np.float32(16))).astype(np.float32)
    pos = np.arange(S, dtype=np.float32)
    ang = (pos[:, None] * inv_freq[None, :]).astype(np.float32)
    cos = np.cos(ang).astype(np.float32)
    sin = np.sin(ang).astype(np.float32)
    cosT = np.ones((128, S), np.float32)
    sinT = np.zeros((128, S), np.float32)
    for hh in range(2):
        b = hh * 64
        for d in range(8):
            cosT[b + d] = cos[:, d]
            cosT[b + 8 + d] = cos[:, d]
            sinT[b + d] = -sin[:, d]
            sinT[b + 8 + d] = sin[:, d]
    return cosT, sinT


def _mask_a():
    k = np.arange(128)[:, None, None]
    m = np.arange(3)[None, :, None]
    q = np.arange(128)[None, None, :]
    rel = (m - 1) * 128 + k - q
    return np.where(np.abs(rel) <= 128, 0.0, MASKV).astype(np.float32)


def _b_geometry():
    rows = 64
    rs = np.clip(np.arange(rows) - 4, 0, rows - 8)
    cs = np.clip(np.arange(64) - 8, 0, 64 - 16)

    def valid_row(kr, r):
        return (0 <= kr < rows) and (rs[r] <= kr < rs[r] + 8)

    colmask = np.zeros((64, 64), bool)
    for qc in range(64):
        colmask[qc, cs[qc]:cs[qc] + 16] = True
    masks = {}
    kbs = {}
    for i in range(NQB):
        mk = np.full((128, 7, 128), MASKV, np.float32)
        used = []
        for m in range(7):
            kb = i + m - 3
            if kb < 0 or kb > 31:
                continue
            anyv = False
            for a in range(2):
                for b in range(2):
                    if valid_row(2 * kb + a, 2 * i + b):
                        anyv = True
                        blk = np.where(colmask.T, 0.0, MASKV)
                        mk[a * 64:(a + 1) * 64, m, b * 64:(b + 1) * 64] = blk
            if anyv:
                used.append(m)
        masks[i] = mk
        kbs[i] = used
    variants = []
    var_of = {}
    for i in range(NQB):
        for vi, v in enumerate(variants):
            if np.array_equal(v, masks[i]):
                var_of[i] = vi
                break
        else:
            var_of[i] = len(variants)
            variants.append(masks[i])
    return np.stack(variants), var_of, kbs


def _bias_index():
    a = np.arange(2)[:, None, None, None, None]
    kc = np.arange(64)[None, :, None, None, None]
    m = np.arange(7)[None, None, :, None, None]
    b = np.arange(2)[None, None, None, :, None]
    qc = np.arange(64)[None, None, None, None, :]
    dr = np.clip(2 * (m - 3) + a - b, -7, 7) + 7
    co = np.clip(kc - qc, -15, 15) + 15
    dr = np.broadcast_to(dr, (2, 64, 7, 2, 64)).reshape(128, 7, 128)
    co = np.broadcast_to(co, (2, 64, 7, 2, 64)).reshape(128, 7, 128)
    return dr, co


_MASKB, _VAR_OF, _KBS = _b_geometry()
_NVAR = _MASKB.shape[0]
_PERM = np.concatenate([np.arange(8, 16), np.arange(0, 8), np.arange(16, 64)])

NWA = 1536 + 128
NWB = 1536


def _layout_w_in(w):
    qa, ka, va = w[:, 0:512], w[:, 512:640], w[:, 640:768]
    qb, kb, vb = w[:, 768:1280], w[:, 1280:1792], w[:, 1792:2304]
    qap = qa.reshape(D, 8, 64)[:, :, _PERM].reshape(D, 512)
    k0, k1 = ka[:, 0:64], ka[:, 64:128]
    k0p, k1p = k0[:, _PERM], k1[:, _PERM]
    wa = np.concatenate([qa, qap, k0, k0, k1, k1, k0p, k0p, k1p, k1p, va], axis=1)
    wb = np.concatenate([qb, kb, vb], axis=1)
    return np.ascontiguousarray(wa), np.ascontiguousarray(wb)


def build_program(stop_after=None, dbg=False):
    nc = bass.Bass("TRN2", target_bir_lowering=False)
    es = ExitStack()

    def din(name, shape, dt=F32):
        return nc.dram_tensor(name, list(shape), dt, kind="ExternalInput").ap()

    x_d = din("x", [S, D])
    cT_d = din("cT", [128, 8])
    wada_d = din("w_ada", [D, 6 * D])
    bada_d = din("b_ada", [1, 6 * D])
    wa_d = din("w_a", [D, NWA])
    wb_d = din("w_b", [D, NWB])
    sink_d = din("sink", [1, 8])
    biasu_d = din("biasu", [8, 128, 7 * 128])
    goa_d = din("g_out_a", [1, 512])
    gob_d = din("g_out_b", [1, 512])
    wout_d = din("w_out", [D, D])
    wr_d = din("w_router", [D, NE])
    br_d = din("b_router", [1, NE])
    if stop_after is None:
        wgu_d = din("w_gate_up", [NE * D, 2 * DFF])
        bgu_d = din("b_gate_up", [NE * 128, 16])
        wd_d = din("w_down", [NE * DFF, D])
        bd_d = din("b_down", [NE, D])
    gfin_d = din("g_final", [1, D])
    cos_d = din("cosT", [128, S])
    sin_d = din("sinT", [128, S])
    maska_d = din("maska", [128, 3 * 128])
    maskb_d = din("maskb", [_NVAR, 128, 7 * 128])
    ident_d = din("ident", [128, 128])
    tri_d = din("tri", [128, 128])
    rowid_d = din("rowid", [128, 8])
    blkth_d = din("blkth", [128, NBLK * NE])
    out_d = nc.dram_tensor("out", [S, D], F32, kind="ExternalOutput").ap()

    def dscr(name, shape, dt):
        kind = "ExternalOutput" if (dbg and name not in ("xs_s", "ys_s")) else "Internal"
        return nc.dram_tensor(name, list(shape), dt, kind=kind).ap()

    mixa_d = dscr("mixa_s", [S, 512], BF16)
    mixb_d = dscr("mixb_s", [S, 512], BF16)
    x1_d = dscr("x1_s", [S, D], F32)
    h2_d = dscr("h2_s", [S, D], BF16)
    if stop_after not in ("A", "B"):
        xs_d = dscr("xs_s", [NSLOT, D], BF16)
        ys_d = dscr("ys_s", [NSLOT, D], F32)
    dbg_d = {}
    if dbg:
        dbg_d["qta"] = nc.dram_tensor("dbg_qta", [128, 6 * S], BF16, kind="ExternalOutput").ap()
        dbg_d["gw"] = nc.dram_tensor("dbg_gw", [128, NQB * NE], F32, kind="ExternalOutput").ap()
        dbg_d["dsel"] = nc.dram_tensor("dbg_dsel", [128, NQB * 4], I32, kind="ExternalOutput").ap()
        dbg_d["blke"] = nc.dram_tensor("dbg_blke", [128, NBLK], F32, kind="ExternalOutput").ap()
        dbg_d["mod"] = nc.dram_tensor("dbg_mod", [128, 6 * D], F32, kind="ExternalOutput").ap()

    sc = Sched(nc, es)
    dbg_keys = []

    DUMPS = dict(ptA=([128, 384], BF16), poA=([128, 1024], F32), vpa=([128, NQB * 2 * 65], BF16), esink=([128, 8], F32),
                 goa=([128, 512], F32), oaA=([128, 512], F32), denA=([128, 8], F32), ssqA=([128, 1], F32))
    dump_d = {k: nc.dram_tensor("dbg_" + k, v[0], v[1], kind="ExternalOutput").ap() for k, v in DUMPS.items()} if dbg else {}

    def dump(name, ap, shape, dt, reads):
        if not dbg:
            return
        d = dump_d[name]
        sc.dma("sp", lambda: nc.sync.dma_start(out=d, in_=ap), reads=reads, writes=["dbg_" + name])
        dbg_keys.append("dbg_" + name)
    A = sc.add
    T, V, G, ACT, SP = nc.tensor, nc.vector, nc.gpsimd, nc.scalar, nc.sync

    def sb(stack, name, shape, dt=F32):
        return stack.enter_context(nc.sbuf_tensor("s_" + name, list(shape), dt))

    PS = [es.enter_context(nc.psum_tensor(f"ps{i}", [128, 1024], F32)) for i in range(4)]

    def bank(i):
        return PS[i // 2][:, (i % 2) * 512:(i % 2 + 1) * 512], ("ps", i)

    ident_f = sb(es, "ident_f", [128, 128], F32)
    ident_b = sb(es, "ident_b", [128, 128], BF16)
    ones_f = sb(es, "ones_f", [128, 128], F32)
    mod = sb(es, "mod", [128, 6 * D], F32)
    epsb = sb(es, "epsb", [128, 1], F32)
    sc.dma("sp", lambda: SP.dma_start(out=ident_f[:], in_=ident_d[:, :]), writes=["ident_f"])
    sc.dma("pool", lambda: G.dma_start(out=ident_b[:], in_=ident_d[:, :]), writes=["ident_b"])
    A("dve", lambda: V.memset(ones_f[:], 1.0), writes=["ones_f"])
    A("dve", lambda: V.memset(epsb[:], EPS), writes=["epsb"])

    with ExitStack() as p0:
        cT = sb(p0, "cT", [128, 8], F32)
        cact = sb(p0, "cact", [128, 8], F32)
        csig = sb(p0, "csig", [128, 8], F32)
        crep = sb(p0, "crep", [128, 8 * 128], F32)
        bada = sb(p0, "bada", [1, 6 * D], F32)
        wsl = [sb(p0, f"wsl{i}", [128, 8 * 512], F32) for i in range(2)]
        sc.dma("sp", lambda: SP.dma_start(out=cT[:], in_=cT_d[:, :]), writes=["cT"])
        sc.dma("sp", lambda: SP.dma_start(out=bada[:], in_=bada_d[:, :]), writes=["bada"])
        A("act", lambda: ACT.activation(out=csig[:], in_=cT[:], func=AF.Sigmoid), reads=["cT"], writes=["csig"])
        A("dve", lambda: V.tensor_tensor(out=cact[:], in0=cT[:], in1=csig[:], op=ALU.mult), reads=["cT", "csig"], writes=["cact"])
        A("dve", lambda: V.tensor_copy(out=crep[:].rearrange("p (c m) -> p c m", m=128),
                                       in_=cact[:].unsqueeze(2).to_broadcast([128, 8, 128])),
          reads=["cact"], writes=["crep"])
        for n in range(12):
            slot = n % 2
            w_t = wsl[slot]
            sc.dma("sp", lambda w_t=w_t, n=n: SP.dma_start(
                out=w_t[:].rearrange("p (c n) -> p c n", n=512),
                in_=wada_d[:, n * 512:(n + 1) * 512].rearrange("(c p) n -> p c n", p=128)),
                writes=[("wsl", slot)])
            pb, pk = bank(n % 2)
            for c in range(8):
                A("pe", lambda pb=pb, w_t=w_t, c=c: T.matmul(pb, lhsT=crep[:, c * 128:(c + 1) * 128],
                                                             rhs=w_t[:, c * 512:(c + 1) * 512], start=(c == 0), stop=False),
                  reads=["crep", ("wsl", slot)], writes=[pk])
            A("pe", lambda pb=pb, n=n: T.matmul(pb, lhsT=ones_f[0:1, :], rhs=bada[0:1, n * 512:(n + 1) * 512],
                                                start=False, stop=True),
              reads=["ones_f", "bada"], writes=[pk])
            if (n // 2) % 3 == 1:
                A("dve", lambda pb=pb, n=n: V.tensor_scalar(out=mod[:, n * 512:(n + 1) * 512], in0=pb, scalar1=1.0,
                                                            scalar2=None, op0=ALU.add), reads=[pk], writes=[("mod", n)])
            else:
                A("act", lambda pb=pb, n=n: ACT.copy(out=mod[:, n * 512:(n + 1) * 512], in_=pb), reads=[pk], writes=[("mod", n)])
        sc.barrier()
    MODK = [("mod", n) for n in range(12)]
    shift_m, scale1_m, gate_m = mod[:, 0:D], mod[:, D:2 * D], mod[:, 2 * D:3 * D]
    shift_f, scale1_f, gate_f = mod[:, 3 * D:4 * D], mod[:, 4 * D:5 * D], mod[:, 5 * D:6 * D]
    if dbg:
        sc.dma("sp", lambda: SP.dma_start(out=dbg_d["mod"][:, :], in_=mod[:]), reads=MODK, writes=["dbg_mod"])

    def rmsnorm_mod(stack_tiles, src, src_keys, scale1, shift, out_bf=None, out_f32=None, out_keys=(), tag=""):
        junk, ssq, rstd, tmp = stack_tiles
        A("act", lambda: ACT.activation(out=junk[:], in_=src, func=AF.Square, accum_out=ssq[:]),
          reads=list(src_keys), writes=["junk" + tag, "ssq" + tag])
        A("dve", lambda: V.tensor_scalar(out=rstd[:], in0=ssq[:], scalar1=1.0 / D, scalar2=EPS, op0=ALU.mult, op1=ALU.add),
          reads=["ssq" + tag], writes=["rstd" + tag])
        A("act", lambda: ACT.activation(out=rstd[:], in_=rstd[:], func=AF.Ln), reads=["rstd" + tag], writes=["rstd" + tag])
        A("act", lambda: ACT.activation(out=rstd[:], in_=rstd[:], func=AF.Exp, scale=-0.5), reads=["rstd" + tag], writes=["rstd" + tag])
        A("dve", lambda: V.scalar_tensor_tensor(out=tmp[:], in0=src, scalar=rstd[:, 0:1], in1=scale1, op0=ALU.mult, op1=ALU.mult),
          reads=list(src_keys) + ["rstd" + tag] + MODK, writes=["tmp" + tag])
        if out_f32 is not None:
            A("dve", lambda: V.tensor_tensor(out=out_f32, in0=tmp[:], in1=shift, op=ALU.add),
              reads=["tmp" + tag] + MODK, writes=list(out_keys))
            if out_bf is not None:
                A("act", lambda: ACT.copy(out=out_bf, in_=out_f32), reads=list(out_keys), writes=[k + ("bf",) for k in out_keys])
        else:
            A("dve", lambda: V.tensor_tensor(out=out_bf, in0=tmp[:], in1=shift, op=ALU.add),
              reads=["tmp" + tag] + MODK, writes=list(out_keys))

    def projection_pass(ps_, wmat_d, ncols, ngroups, emit_group, emit_v, vcol0, nvcols, tag):
        wsb = sb(ps_, "wsb" + tag, [128, 8 * ncols], BF16)
        w3 = wsb[:].rearrange("p (c n) -> p c n", n=ncols)
        for c in range(8):
            sc.dma("pool", lambda c=c: G.dma_start(out=w3[:, c, :], in_=wmat_d[c * 128:(c + 1) * 128, :]),
                   writes=[("wsb" + tag, c)])
        WK = [("wsb" + tag, c) for c in range(8)]
        xbl = [sb(ps_, f"xbl{tag}{i}", [128, D], F32) for i in range(2)]
        hbf = [sb(ps_, f"hbf{tag}{i}", [128, D], BF16) for i in range(2)]
        hT = [sb(ps_, f"hT{tag}{i}", [128, 8 * 512], BF16) for i in range(2)]
        tiles = [(sb(ps_, f"junk{tag}{j}", [128, D], BF16), sb(ps_, f"ssq{tag}{j}", [128, 1], F32),
                  sb(ps_, f"rstd{tag}{j}", [128, 1], F32), sb(ps_, f"tmp{tag}{j}", [128, D], F32)) for j in range(2)]
        for tc in range(8):
            hslot = tc % 2
            hT3 = hT[hslot][:].rearrange("p (c t) -> p c t", t=512)
            for sub in range(4):
                i = tc * 4 + sub
                xs_ = i % 2
                sc.dma("sp", lambda i=i, xs_=xs_: SP.dma_start(out=xbl[xs_][:], in_=x_d[i * 128:(i + 1) * 128, :]),
                       writes=[("xbl" + tag, xs_)])
                rmsnorm_mod(tiles[xs_], xbl[xs_][:], [("xbl" + tag, xs_)], scale1_m, shift_m, out_bf=hbf[xs_][:],
                            out_keys=[("hbf" + tag, xs_)], tag=tag + str(xs_))
                pbT = PS[0][:, (i % 2) * 512:(i % 2 + 1) * 512].bitcast(BF16)
                pkT = ("ps", i % 2)
                for c in range(8):
                    A("pe", lambda c=c, pbT=pbT, xs_=xs_: T.transpose(out=pbT[:, c * 128:(c + 1) * 128],
                                                                      in_=hbf[xs_][:, c * 128:(c + 1) * 128], identity=ident_b[:]),
                      reads=[("hbf" + tag, xs_), "ident_b"], writes=[pkT])
                A("act", lambda pbT=pbT, hT3=hT3, sub=sub: ACT.copy(out=hT3[:, :, sub * 128:(sub + 1) * 128],
                                                                    in_=pbT.rearrange("p (c t) -> p c t", t=128)),
                  reads=[pkT], writes=[("hT" + tag, hslot, sub)])
                pv, pvk = bank(2 + (i % 2))
                for c in range(8):
                    A("pe", lambda c=c, pv=pv, hT3=hT3, sub=sub: T.matmul(
                        pv[:, 0:nvcols], lhsT=hT3[:, c, sub * 128:(sub + 1) * 128], rhs=w3[:, c, vcol0:vcol0 + nvcols],
                        start=(c == 0), stop=(c == 7)),
                      reads=[("hT" + tag, hslot, sub)] + WK, writes=[pvk])
                emit_v(i, pv, pvk)
            HK = [("hT" + tag, hslot, s_) for s_ in range(4)]
            emit_group(tc, hT3, HK, w3, WK)
        return

    with ExitStack() as pa:
        qTa = sb(pa, "qTa", [128, 6 * S], BF16)
        qTa3 = qTa[:].rearrange("p (g t) -> p g t", t=S)
        vpa = sb(pa, "vpa", [128, NQB * 2 * 65], BF16)
        vpa4 = vpa[:].rearrange("p (i g d) -> p i g d", g=2, d=65)
        A("pool", lambda: G.memset(vpa[:], 1.0), writes=["vpa_init"])
        with ExitStack() as pa1:
            cosT = sb(pa1, "cosT", [128, S], F32)
            sinT = sb(pa1, "sinT", [128, S], F32)
            sc.dma("sp", lambda: SP.dma_start(out=cosT[:], in_=cos_d[:, :]), writes=["cosT"])
            sc.dma("sp", lambda: SP.dma_start(out=sinT[:], in_=sin_d[:, :]), writes=["sinT"])
            rt = [sb(pa1, f"rt{i}", [128, 512], F32) for i in range(4)]

            def emit_v_a(i, pv, pvk):
                A("act", lambda: ACT.copy(out=vpa4[:, i, :, 0:64], in_=pv[:, 0:128].rearrange("p (g d) -> p g d", d=64)),
                  reads=[pvk, "vpa_init"], writes=[("vpa", i)])

            def emit_group_a(tc, hT3, HK, w3, WK):
                for g in range(6):
                    c0 = g * 128 if g < 4 else 1024 + (g - 4) * 256
                    c1 = 512 + g * 128 if g < 4 else 1024 + 512 + (g - 4) * 256
                    if g >= 4:
                        c0 = 1024 + (g - 4) * 128
                        c1 = 1024 + 256 + (g - 4) * 128
                    pq, pqk = bank(4 + (g % 2) * 2)
                    pp, ppk = bank(5 + (g % 2) * 2)
                    for (pb, pk, col) in ((pq, pqk, c0), (pp, ppk, c1)):
                        for c in range(8):
                            A("pe", lambda pb=pb, c=c, col=col: T.matmul(pb, lhsT=w3[:, c, col:col + 128], rhs=hT3[:, c, :],
                                                                         start=(c == 0), stop=(c == 7)),
                              reads=HK + WK, writes=[pk])
                    r0, r1 = rt[(g % 2) * 2], rt[(g % 2) * 2 + 1]
                    k0, k1 = ("rt", (g % 2) * 2), ("rt", (g % 2) * 2 + 1)
                    tsl = slice(tc * 512, (tc + 1) * 512)
                    A("dve", lambda pq=pq, r0=r0, tsl=tsl: V.tensor_tensor(out=r0[:], in0=pq, in1=cosT[:, tsl], op=ALU.mult),
                      reads=[pqk, "cosT"], writes=[k0])
                    A("dve", lambda pp=pp, r1=r1, tsl=tsl: V.tensor_tensor(out=r1[:], in0=pp, in1=sinT[:, tsl], op=ALU.mult),
                      reads=[ppk, "sinT"], writes=[k1])
                    A("pool", lambda r0=r0, r1=r1, g=g, tsl=tsl: G.tensor_tensor(out=qTa3[:, g, tsl], in0=r0[:], in1=r1[:], op=ALU.add),
                      reads=[k0, k1], writes=[("qTa", g, tc)])

            projection_pass(pa1, wa_d, NWA, 12, emit_group_a, emit_v_a, 1536, 128, "A")
            sc.barrier()
        if dbg:
            sc.dma("sp", lambda: SP.dma_start(out=dbg_d["qta"][:, :], in_=qTa[:]),
                   reads=[("qTa", g, tc) for g in range(6) for tc in range(8)], writes=["dbg_qta"])

        with ExitStack() as pa2:
            if stop_after not in ("A", "B"):
                zt = sb(pa2, "zt", [128, 4 * D], BF16)
                A("pool", lambda: G.memset(zt[:], 0.0), writes=["zt"])
                for b in range(NBLK):
                    sc.dma("sp", lambda b=b: SP.dma_start(out=xs_d[b * MB:(b + 1) * MB, :].rearrange("(s p) d -> p s d", p=128),
                                                        in_=zt[:].rearrange("p (s d) -> p s d", d=D)), reads=["zt"], writes=["xs_d"])
            maska = sb(pa2, "maska", [128, 384], BF16)
            sc.dma("pool", lambda: G.dma_start(out=maska[:], in_=maska_d[:, :]), writes=["maska"])
            esink = sb(pa2, "esink", [128, 8], F32)
            sc.dma("sp", lambda: SP.dma_start(out=esink[:], in_=sink_d[:, :].partition_broadcast(128)), writes=["esink0"])
            A("act", lambda: ACT.activation(out=esink[:], in_=esink[:], func=AF.Exp), reads=["esink0"], writes=["esink"])
            goa = sb(pa2, "goa", [128, 512], F32)
            sc.dma("sp", lambda: SP.dma_start(out=goa[:], in_=goa_d[:, :].partition_broadcast(128)), writes=["goa"])
            pt = [sb(pa2, f"pta{i}", [128, 384], BF16) for i in range(3)]
            den = sb(pa2, "dena", [128, 8], F32)
            oa = sb(pa2, "oa", [128, 512], F32)
            junk2 = sb(pa2, "junk2a", [128, 512], BF16)
            ssq2 = sb(pa2, "ssq2a", [128, 1], F32)
            mixa = [sb(pa2, f"mixa{i}", [128, 512], BF16) for i in range(2)]
            for i in range(NQB):
                tcq = i // 4
                ms = [m for m in range(3) if 0 <= i + m - 1 < NQB]
                po = PS[3 - (i % 2)]
                pok = ("ps", 6 - 2 * (i % 2))
                po4 = po[:].rearrange("p (b x) -> p b x", b=2)[:, :, 0:260].rearrange("p b (h d) -> p b h d", d=65)
                def qk_a(h):
                    g, off = h // 2, (h % 2) * 64
                    kg = 4 + h // 4
                    pst, pstk = bank(h % 3)
                    for m in ms:
                        kb = i + m - 1
                        A("pe", lambda pst=pst, m=m, kb=kb, g=g, off=off, kg=kg, i=i: T.matmul(
                            pst[:, m * 128:(m + 1) * 128], lhsT=qTa3[off:off + 64, kg, kb * 128:(kb + 1) * 128],
                            rhs=qTa3[off:off + 64, g, i * 128:(i + 1) * 128], start=True, stop=False),
                          reads=[("qTa", kg, kb // 4), ("qTa", g, tcq)], writes=[pstk])
                        A("pe", lambda pst=pst, m=m: T.matmul(pst[:, m * 128:(m + 1) * 128], lhsT=ident_b[:],
                                                              rhs=maska[:, m * 128:(m + 1) * 128], start=False, stop=True),
                          reads=["ident_b", "maska"], writes=[pstk])

                qk_a(0)
                for h in range(8):
                    if h + 1 < 8:
                        qk_a(h + 1)
                    pst, pstk = bank(h % 3)
                    ptt = pt[h % 3]
                    ptk = ("pta", h % 3)
                    lo, hi = ms[0] * 128, (ms[-1] + 1) * 128
                    A("act", lambda pst=pst, ptt=ptt, lo=lo, hi=hi: ACT.activation(out=ptt[:, lo:hi], in_=pst[:, lo:hi],
                                                                                   func=AF.Exp, scale=0.125),
                      reads=[pstk], writes=[ptk])
                    for m in ms:
                        kb = i + m - 1
                        A("pe", lambda ptt=ptt, m=m, kb=kb, h=h, ms=ms, po=po: T.matmul(
                            po[:, (h // 4) * 512 + (h % 4) * 65:(h // 4) * 512 + (h % 4) * 65 + 65],
                            lhsT=ptt[:, m * 128:(m + 1) * 128], rhs=vpa4[:, kb, h // 4, :],
                            start=(m == ms[0]), stop=(m == ms[-1])),
                          reads=[ptk, ("vpa", kb)], writes=[pok])
                if i == 4:
                    dump("ptA", pt[7 % 3][:], [128, 384], BF16, [("pta", 7 % 3)])
                    if dbg:
                        podbg = sb(pa2, "podbg", [128, 1024], F32)
                        A("act", lambda: ACT.copy(out=podbg[:], in_=po[:]), reads=[pok], writes=["podbg"])
                        dump("poA", podbg[:], [128, 1024], F32, ["podbg"])
                    dump("vpa", vpa[:], [128, NQB * 2 * 65], BF16, [("vpa", kk) for kk in range(NQB)])
                    dump("esink", esink[:], [128, 8], F32, ["esink"])
                    dump("goa", goa[:], [128, 512], F32, ["goa"])
                A("dve", lambda po4=po4: V.tensor_tensor(out=den[:].rearrange("p (b h) -> p b h", b=2), in0=po4[:, :, :, 64],
                                                         in1=esink[:].rearrange("p (b h) -> p b h", b=2), op=ALU.add),
                  reads=[pok, "esink"], writes=["dena"])
                A("dve", lambda: V.reciprocal(out=den[:], in_=den[:]), reads=["dena"], writes=["dena"])
                A("dve", lambda po4=po4: V.tensor_tensor(
                    out=oa[:].rearrange("p (b h d) -> p b h d", b=2, d=64), in0=po4[:, :, :, 0:64],
                    in1=den[:].rearrange("p (b h) -> p b h", b=2).unsqueeze(3).to_broadcast([128, 2, 4, 64]), op=ALU.mult),
                  reads=[pok, "dena"], writes=["oa"])
                A("act", lambda: ACT.activation(out=junk2[:], in_=oa[:], func=AF.Square, accum_out=ssq2[:]),
                  reads=["oa"], writes=["junk2a", "ssq2a"])
                A("dve", lambda: V.tensor_scalar(out=ssq2[:], in0=ssq2[:], scalar1=1.0 / 512, scalar2=EPS, op0=ALU.mult, op1=ALU.add),
                  reads=["ssq2a"], writes=["ssq2a"])
                A("act", lambda: ACT.activation(out=ssq2[:], in_=ssq2[:], func=AF.Ln), reads=["ssq2a"], writes=["ssq2a"])
                A("act", lambda: ACT.activation(out=ssq2[:], in_=ssq2[:], func=AF.Exp, scale=-0.5), reads=["ssq2a"], writes=["ssq2a"])
                if i == 4:
                    dump("oaA", oa[:], [128, 512], F32, ["oa"])
                    dump("denA", den[:], [128, 8], F32, ["dena"])
                    dump("ssqA", ssq2[:], [128, 1], F32, ["ssq2a"])
                mx = mixa[i % 2]
                A("dve", lambda mx=mx: V.scalar_tensor_tensor(out=mx[:], in0=oa[:], scalar=ssq2[:, 0:1], in1=goa[:],
                                                              op0=ALU.mult, op1=ALU.mult),
                  reads=["oa", "ssq2a", "goa"], writes=[("mixa", i % 2)])
                sc.dma("sp", lambda mx=mx, i=i: SP.dma_start(out=mixa_d[i * 128:(i + 1) * 128, :], in_=mx[:]),
                       reads=[("mixa", i % 2)], writes=[("mixa_d", i)])
            sc.barrier()
    if stop_after == "A":
        sc.finish([("mixa_d", i) for i in range(NQB)] + ["dbg_qta", "dbg_mod"] + dbg_keys)
        es.close()
        return nc

    with ExitStack() as pb_:
        qTb = sb(pb_, "qTb", [128, 8 * S], BF16)
        qTb3 = qTb[:].rearrange("p (g t) -> p g t", t=S)
        vpb = sb(pb_, "vpb", [128, NQB * 8 * 65], BF16)
        vpb4 = vpb[:].rearrange("p (i g d) -> p i g d", g=8, d=65)
        A("pool", lambda: G.memset(vpb[:], 1.0), writes=["vpb_init"])
        with ExitStack() as pb1:
            def emit_v_b(i, pv, pvk):
                A("act", lambda: ACT.copy(out=vpb4[:, i, :, 0:64], in_=pv[:, 0:512].rearrange("p (g d) -> p g d", d=64)),
                  reads=[pvk, "vpb_init"], writes=[("vpb", i)])

            def emit_group_b(tc, hT3, HK, w3, WK):
                for g in range(8):
                    pq, pqk = bank(4 + g % 4)
                    for c in range(8):
                        A("pe", lambda pq=pq, c=c, g=g: T.matmul(pq, lhsT=w3[:, c, g * 128:(g + 1) * 128], rhs=hT3[:, c, :],
                                                                 start=(c == 0), stop=(c == 7)),
                          reads=HK + WK, writes=[pqk])
                    tsl = slice(tc * 512, (tc + 1) * 512)
                    if g % 2 == 0:
                        A("dve", lambda pq=pq, g=g, tsl=tsl: V.tensor_copy(out=qTb3[:, g, tsl], in_=pq), reads=[pqk], writes=[("qTb", g, tc)])
                    else:
                        A("act", lambda pq=pq, g=g, tsl=tsl: ACT.copy(out=qTb3[:, g, tsl], in_=pq), reads=[pqk], writes=[("qTb", g, tc)])

            projection_pass(pb1, wb_d, NWB, 8, emit_group_b, emit_v_b, 1024, 512, "B")
            sc.barrier()

        with ExitStack() as pb2:
            maskb = sb(pb2, "maskb", [128, _NVAR * 896], BF16)
            for v in range(_NVAR):
                sc.dma("pool", lambda v=v: G.dma_start(out=maskb[:, v * 896:(v + 1) * 896], in_=maskb_d[v, :, :]), writes=[("maskb", v)])
            biasu = sb(pb2, "biasu", [128, 8 * 896], F32)
            for h in range(8):
                sc.dma("sp", lambda h=h: SP.dma_start(out=biasu[:, h * 896:(h + 1) * 896], in_=biasu_d[h, :, :]), writes=[("biasu", h)])
            gob = sb(pb2, "gob", [128, 512], F32)
            sc.dma("sp", lambda: SP.dma_start(out=gob[:], in_=gob_d[:, :].partition_broadcast(128)), writes=["gob"])
            tt = [sb(pb2, f"ttb{i}", [128, 896], F32) for i in range(2)]
            pt = [sb(pb2, f"ptb{i}", [128, 896], BF16) for i in range(2)]
            den = sb(pb2, "denb", [128, 8], F32)
            ob = sb(pb2, "ob", [128, 512], F32)
            junk2 = sb(pb2, "junk2b", [128, 512], BF16)
            ssq2 = sb(pb2, "ssq2b", [128, 1], F32)
            mixb = [sb(pb2, f"mixb{i}", [128, 512], BF16) for i in range(2)]
            for i in range(NQB):
                tcq = i // 4
                ms = _KBS[i]
                var = _VAR_OF[i]
                po = PS[3 - (i % 2)]
                pok = ("ps", 6 - 2 * (i % 2))
                po4 = po[:].rearrange("p (b x) -> p b x", b=2)[:, :, 0:260].rearrange("p b (h d) -> p b h d", d=65)
                lo, hi = ms[0] * 128, (ms[-1] + 1) * 128
                def qk_b(h):
                    g, off, kg = h // 2, (h % 2) * 64, 4 + h // 2
                    sl = h % 2
                    pst = PS[sl]
                    pstk = [("ps", 2 * sl), ("ps", 2 * sl + 1)]
                    for m in ms:
                        kb = i + m - 3
                        A("pe", lambda pst=pst, m=m, kb=kb, g=g, off=off, kg=kg, i=i: T.matmul(
                            pst[:, m * 128:(m + 1) * 128], lhsT=qTb3[off:off + 64, kg, kb * 128:(kb + 1) * 128],
                            rhs=qTb3[off:off + 64, g, i * 128:(i + 1) * 128], start=True, stop=False),
                          reads=[("qTb", kg, kb // 4), ("qTb", g, tcq)], writes=pstk)
                        A("pe", lambda pst=pst, m=m, var=var: T.matmul(pst[:, m * 128:(m + 1) * 128], lhsT=ident_b[:],
                                                                       rhs=maskb[:, var * 896 + m * 128:var * 896 + (m + 1) * 128],
                                                                       start=False, stop=True),
                          reads=["ident_b", ("maskb", var)], writes=pstk)

                qk_b(0)
                for h in range(8):
                    if h + 1 < 8:
                        qk_b(h + 1)
                    sl = h % 2
                    pst = PS[sl]
                    pstk = [("ps", 2 * sl), ("ps", 2 * sl + 1)]
                    ttt, ptt = tt[sl], pt[sl]
                    A("dve", lambda pst=pst, ttt=ttt, h=h, lo=lo, hi=hi: V.scalar_tensor_tensor(
                        out=ttt[:, lo:hi], in0=pst[:, lo:hi], scalar=0.125, in1=biasu[:, h * 896 + lo:h * 896 + hi],
                        op0=ALU.mult, op1=ALU.add), reads=pstk + [("biasu", h)], writes=[("ttb", sl)])
                    A("act", lambda ttt=ttt, ptt=ptt, lo=lo, hi=hi: ACT.activation(out=ptt[:, lo:hi], in_=ttt[:, lo:hi], func=AF.Exp),
                      reads=[("ttb", sl)], writes=[("ptb", sl)])
                    for m in ms:
                        kb = i + m - 3
                        A("pe", lambda ptt=ptt, m=m, kb=kb, h=h, ms=ms, po=po: T.matmul(
                            po[:, (h // 4) * 512 + (h % 4) * 65:(h // 4) * 512 + (h % 4) * 65 + 65],
                            lhsT=ptt[:, m * 128:(m + 1) * 128], rhs=vpb4[:, kb, h, :],
                            start=(m == ms[0]), stop=(m == ms[-1])),
                          reads=[("ptb", sl), ("vpb", kb)], writes=[pok])
                A("dve", lambda po4=po4: V.reciprocal(out=den[:].rearrange("p (b h) -> p b h", b=2), in_=po4[:, :, :, 64]),
                  reads=[pok], writes=["denb"])
                A("dve", lambda po4=po4: V.tensor_tensor(
                    out=ob[:].rearrange("p (b h d) -> p b h d", b=2, d=64), in0=po4[:, :, :, 0:64],
                    in1=den[:].rearrange("p (b h) -> p b h", b=2).unsqueeze(3).to_broadcast([128, 2, 4, 64]), op=ALU.mult),
                  reads=[pok, "denb"], writes=["ob"])
                A("act", lambda: ACT.activation(out=junk2[:], in_=ob[:], func=AF.Square, accum_out=ssq2[:]),
                  reads=["ob"], writes=["junk2b", "ssq2b"])
                A("dve", lambda: V.tensor_scalar(out=ssq2[:], in0=ssq2[:], scalar1=1.0 / 512, scalar2=EPS, op0=ALU.mult, op1=ALU.add),
                  reads=["ssq2b"], writes=["ssq2b"])
                A("act", lambda: ACT.activation(out=ssq2[:], in_=ssq2[:], func=AF.Ln), reads=["ssq2b"], writes=["ssq2b"])
                A("act", lambda: ACT.activation(out=ssq2[:], in_=ssq2[:], func=AF.Exp, scale=-0.5), reads=["ssq2b"], writes=["ssq2b"])
                mx = mixb[i % 2]
                A("dve", lambda mx=mx: V.scalar_tensor_tensor(out=mx[:], in0=ob[:], scalar=ssq2[:, 0:1], in1=gob[:],
                                                              op0=ALU.mult, op1=ALU.mult),
                  reads=["ob", "ssq2b", "gob"], writes=[("mixb", i % 2)])
                sc.dma("sp", lambda mx=mx, i=i: SP.dma_start(out=mixb_d[i * 128:(i + 1) * 128, :], in_=mx[:]),
                       reads=[("mixb", i % 2)], writes=[("mixb_d", i)])
            sc.barrier()
    if stop_after == "B":
        sc.finish([("mixa_d", i) for i in range(NQB)] + [("mixb_d", i) for i in range(NQB)] + ["dbg_qta", "dbg_mod"])
        es.close()
        return nc

    rt_ = ExitStack()
    lg_all = sb(rt_, "lg_all", [128, NQB * NE], F32)
    m8_all = sb(rt_, "m8_all", [128, NQB * 8], F32)
    lg3 = lg_all[:].rearrange("p (i e) -> p i e", e=NE)
    m83 = m8_all[:].rearrange("p (i k) -> p i k", k=8)
    with ExitStack() as pc:
        wout = sb(pc, "wout", [128, 8 * D], BF16)
        wout3 = wout[:].rearrange("p (c n) -> p c n", n=D)
        for c in range(8):
            sc.dma("pool", lambda c=c: G.dma_start(out=wout3[:, c, :], in_=wout_d[c * 128:(c + 1) * 128, :]), writes=[("wout", c)])
        WOK = [("wout", c) for c in range(8)]
        wr = sb(pc, "wr", [128, 8 * NE], F32)
        sc.dma("sp", lambda: SP.dma_start(out=wr[:].rearrange("p (c e) -> p c e", e=NE),
                                          in_=wr_d[:, :].rearrange("(c p) e -> p c e", p=128)), writes=["wr"])
        brt = sb(pc, "brt", [1, NE], F32)
        sc.dma("sp", lambda: SP.dma_start(out=brt[:], in_=br_d[:, :]), writes=["brt"])
        mixab = [sb(pc, f"mixab{i}", [128, D], BF16) for i in range(2)]
        xb_ = [sb(pc, f"xc{i}", [128, D], F32) for i in range(2)]
        mixT_ = [sb(pc, f"mixT{j}", [128, D], BF16) for j in range(2)]
        t1_ = [sb(pc, f"t1{j}", [128, D], F32) for j in range(2)]
        x1t = [sb(pc, f"x1t{i}", [128, D], F32) for i in range(2)]
        h2f_ = [sb(pc, f"h2f{j}", [128, D], F32) for j in range(2)]
        h2b = [sb(pc, f"h2b{i}", [128, D], BF16) for i in range(2)]
        h2T_ = [sb(pc, f"h2T{j}", [128, D], F32) for j in range(2)]
        tilesC_ = [(sb(pc, f"junkC{j}", [128, D], BF16), sb(pc, f"ssqC{j}", [128, 1], F32),
                    sb(pc, f"rstdC{j}", [128, 1], F32), sb(pc, f"tmpC{j}", [128, D], F32)) for j in range(2)]
        def c_loads(i):
            s2 = i % 2
            sc.dma("sp", lambda i=i, s2=s2: SP.dma_start(out=mixab[s2][:, 0:512], in_=mixa_d[i * 128:(i + 1) * 128, :]),
                   reads=[("mixa_d", i)], writes=[("mixab", s2, 0)])
            sc.dma("sp", lambda i=i, s2=s2: SP.dma_start(out=mixab[s2][:, 512:1024], in_=mixb_d[i * 128:(i + 1) * 128, :]),
                   reads=[("mixb_d", i)], writes=[("mixab", s2, 1)])
            sc.dma("sp", lambda i=i, s2=s2: SP.dma_start(out=xb_[s2][:], in_=x_d[i * 128:(i + 1) * 128, :]), writes=[("xc", s2)])

        def c_a(i):
            s2 = i % 2
            mixT, t1, h2f, h2T, tilesC = mixT_[s2], t1_[s2], h2f_[s2], h2T_[s2], tilesC_[s2]
            pbT = PS[0][:, s2 * 512:(s2 + 1) * 512].bitcast(BF16)
            for c in range(8):
                A("pe", lambda c=c, pbT=pbT, s2=s2: T.transpose(out=pbT[:, c * 128:(c + 1) * 128],
                                                                in_=mixab[s2][:, c * 128:(c + 1) * 128], identity=ident_b[:]),
                  reads=[("mixab", s2, 0), ("mixab", s2, 1), "ident_b"], writes=[("ps", s2)])
            A("act", lambda pbT=pbT, mixT=mixT: ACT.copy(out=mixT[:], in_=pbT), reads=[("ps", s2)], writes=[("mixT", s2)])
            for n in range(2):
                py, pyk = bank(2 + n)
                for c in range(8):
                    A("pe", lambda py=py, c=c, n=n, mixT=mixT: T.matmul(py, lhsT=mixT[:, c * 128:(c + 1) * 128],
                                                             rhs=wout3[:, c, n * 512:(n + 1) * 512], start=(c == 0), stop=(c == 7)),
                      reads=[("mixT", s2)] + WOK, writes=[pyk])
                A("dve", lambda py=py, n=n, t1=t1: V.tensor_tensor(out=t1[:, n * 512:(n + 1) * 512], in0=py,
                                                            in1=gate_m[:, n * 512:(n + 1) * 512], op=ALU.mult),
                  reads=[pyk] + MODK, writes=[("t1", s2, n)])
            xt = x1t[s2]
            A("dve", lambda xt=xt, s2=s2, t1=t1: V.tensor_tensor(out=xt[:], in0=t1[:], in1=xb_[s2][:], op=ALU.add),
              reads=[("t1", s2, 0), ("t1", s2, 1), ("xc", s2)], writes=[("x1t", s2)])
            sc.dma("sp", lambda xt=xt, i=i: SP.dma_start(out=x1_d[i * 128:(i + 1) * 128, :], in_=xt[:]),
                   reads=[("x1t", s2)], writes=[("x1_d", i)])
            rmsnorm_mod(tilesC, xt[:], [("x1t", s2)], scale1_f, shift_f, out_bf=h2b[s2][:], out_f32=h2f[:],
                        out_keys=[("h2f", s2)], tag="C" + str(s2))
            sc.dma("sp", lambda i=i, s2=s2: SP.dma_start(out=h2_d[i * 128:(i + 1) * 128, :], in_=h2b[s2][:]),
                   reads=[("h2f", s2, "bf")], writes=[("h2_d", i)])

        def c_b(i):
            s2 = i % 2
            mixT, t1, h2f, h2T, tilesC = mixT_[s2], t1_[s2], h2f_[s2], h2T_[s2], tilesC_[s2]
            for r_ in range(2):
                pt_, ptk_ = bank(4 + r_)
                for c4 in range(4):
                    c = r_ * 4 + c4
                    A("pe", lambda pt_=pt_, c=c, c4=c4, h2f=h2f: T.transpose(out=pt_[:, c4 * 128:(c4 + 1) * 128],
                                                                    in_=h2f[:, c * 128:(c + 1) * 128], identity=ident_f[:]),
                      reads=[("h2f", s2), "ident_f"], writes=[ptk_])
                if r_ == 0:
                    A("dve", lambda pt_=pt_, r_=r_, h2T=h2T: V.tensor_copy(out=h2T[:, r_ * 512:(r_ + 1) * 512], in_=pt_), reads=[ptk_], writes=[("h2T", s2, r_)])
                else:
                    A("act", lambda pt_=pt_, r_=r_, h2T=h2T: ACT.copy(out=h2T[:, r_ * 512:(r_ + 1) * 512], in_=pt_), reads=[ptk_], writes=[("h2T", s2, r_)])
            pl, plk = bank(6 + s2)
            for c in range(8):
                A("pe", lambda pl=pl, c=c, h2T=h2T: T.matmul(pl[:, 0:NE], lhsT=h2T[:, c * 128:(c + 1) * 128], rhs=wr[:, c * NE:(c + 1) * NE],
                                                    start=(c == 0), stop=False),
                  reads=[("h2T", s2, 0), ("h2T", s2, 1), "wr"], writes=[plk])
            A("pe", lambda pl=pl: T.matmul(pl[:, 0:NE], lhsT=ones_f[0:1, :], rhs=brt[0:1, :], start=False, stop=True),
              reads=["ones_f", "brt"], writes=[plk])
            A("dve", lambda pl=pl, i=i: V.tensor_copy(out=lg3[:, i, :], in_=pl[:, 0:NE]), reads=[plk], writes=[("lg", i)])
            A("dve", lambda i=i: V.max(out=m83[:, i, :], in_=lg3[:, i, :]), reads=[("lg", i)], writes=[("m8", i)])

        c_loads(0)
        for i in range(NQB):
            if i + 1 < NQB:
                c_loads(i + 1)
            c_a(i)
            if i >= 1:
                c_b(i - 1)
        c_b(NQB - 1)
        sc.barrier()
    LGK = [("lg", i) for i in range(NQB)] + [("m8", i) for i in range(NQB)]

    gw_all = sb(rt_, "gw_all", [128, NQB * NE], F32)
    gw3 = gw_all[:].rearrange("p (i e) -> p i e", e=NE)
    dsel_i = sb(rt_, "dsel_i", [128, 4 * NQB], I32)
    gk = sb(rt_, "gk", [128, 4 * NQB], F32)
    idxw_i = sb(rt_, "idxw_i", [128, NBLK * 8], I32)
    idxg_i = sb(rt_, "idxg_i", [128, NBLK * 8], I32)
    idxb_i = sb(rt_, "idxb_i", [128, NBLK], I32)
    with ExitStack() as pr:
        tri = sb(pr, "tri", [128, 128], F32)
        sc.dma("sp", lambda: SP.dma_start(out=tri[:], in_=tri_d[:, :]), writes=["tri"])
        rowid = sb(pr, "rowid", [128, 8], F32)
        sc.dma("sp", lambda: SP.dma_start(out=rowid[:], in_=rowid_d[:, :]), writes=["rowid"])
        blkth = sb(pr, "blkth", [128, NBLK * NE], F32)
        sc.dma("sp", lambda: SP.dma_start(out=blkth[:], in_=blkth_d[:, :]), writes=["blkth"])
        msk = sb(pr, "msk", [128, NQB * NE], F32)
        msk3 = msk[:].rearrange("p (i e) -> p i e", e=NE)
        ex = sb(pr, "ex", [128, NQB * NE], F32)
        ex3 = ex[:].rearrange("p (i e) -> p i e", e=NE)
        ssum = sb(pr, "ssum", [128, NQB], F32)
        pos = sb(pr, "pos", [128, NQB * NE], F32)
        pos3 = pos[:].rearrange("p (i e) -> p i e", e=NE)
        oh = sb(pr, "oh", [128, NQB * NE], F32)
        oh3 = oh[:].rearrange("p (i e) -> p i e", e=NE)
        prod = sb(pr, "prod", [128, NQB * NE], F32)
        prod3 = prod[:].rearrange("p (i e) -> p i e", e=NE)
        dself = sb(pr, "dself", [128, 4 * NQB], F32)
        cnt = sb(pr, "cnt", [128, NE], F32)
        cs = [sb(pr, f"cs{i}", [128, NE], F32) for i in range(2)]
        padded = sb(pr, "padded", [128, NE], F32)
        pstart = sb(pr, "pstart", [128, NE], F32)
        cmpb = sb(pr, "cmpb", [128, NBLK * NE], F32)
        blke = sb(pr, "blke", [128, NBLK], F32)
        idxwf = sb(pr, "idxwf", [128, NBLK * 8], F32)
        idxbf = sb(pr, "idxbf", [128, NBLK], F32)

        A("dve", lambda: V.tensor_tensor(out=msk3, in0=lg3, in1=m83[:, :, 3:4].to_broadcast([128, NQB, NE]), op=ALU.is_ge),
          reads=LGK, writes=["msk"])
        mskb = sb(pr, "mskb", [128, NQB * NE], BF16)
        mskb3 = mskb[:].rearrange("p (i e) -> p i e", e=NE)
        ones_b = sb(pr, "ones_b", [128, 128], BF16)
        tri_b = sb(pr, "tri_b", [128, 128], BF16)
        A("act", lambda: ACT.copy(out=mskb[:], in_=msk[:]), reads=["msk"], writes=["mskb"])
        A("pool", lambda: G.memset(ones_b[:], 1.0), writes=["ones_b"])
        sc.dma("pool", lambda: G.dma_start(out=tri_b[:], in_=tri_d[:, :]), writes=["tri_b"])
        A("dve", lambda: V.tensor_tensor(out=ex3, in0=lg3, in1=m83[:, :, 0:1].to_broadcast([128, NQB, NE]), op=ALU.subtract),
          reads=LGK, writes=["ex"])
        A("act", lambda: ACT.activation(out=ex[:], in_=ex[:], func=AF.Exp), reads=["ex"], writes=["ex"])
        A("dve", lambda: V.tensor_tensor(out=ex[:], in0=ex[:], in1=msk[:], op=ALU.mult), reads=["ex", "msk"], writes=["ex"])
        A("dve", lambda: V.reduce_sum(out=ssum[:], in_=ex3, axis=AX.X), reads=["ex"], writes=["ssum"])
        A("dve", lambda: V.reciprocal(out=ssum[:], in_=ssum[:]), reads=["ssum"], writes=["ssum"])
        A("dve", lambda: V.tensor_tensor(out=gw3, in0=ex3, in1=ssum[:].unsqueeze(2).to_broadcast([128, NQB, NE]), op=ALU.mult),
          reads=["ex", "ssum"], writes=["gw"])
        for half in range(2):
            pp_, ppk_ = bank(half)
            for ii in range(16):
                i = half * 16 + ii
                for j in range(i):
                    A("pe", lambda pp_=pp_, ii=ii, j=j: T.matmul(pp_[:, ii * NE:(ii + 1) * NE], lhsT=ones_b[:], rhs=mskb3[:, j, :],
                                                                 start=(j == 0), stop=False),
                      reads=["ones_b", "mskb"], writes=[ppk_])
                A("pe", lambda pp_=pp_, ii=ii, i=i: T.matmul(pp_[:, ii * NE:(ii + 1) * NE], lhsT=tri_b[:], rhs=mskb3[:, i, :],
                                                             start=(i == 0), stop=True),
                  reads=["tri_b", "mskb"], writes=[ppk_])
            A("dve", lambda pp_=pp_, half=half: V.tensor_copy(out=pos[:, half * 512:(half + 1) * 512], in_=pp_),
              reads=[ppk_], writes=[("pos", half)])
        pc_, pck_ = bank(2)
        for j in range(NQB):
            A("pe", lambda j=j: T.matmul(pc_[:, 0:NE], lhsT=ones_b[:], rhs=mskb3[:, j, :], start=(j == 0), stop=(j == NQB - 1)),
              reads=["ones_b", "mskb"], writes=[pck_])
        A("dve", lambda: V.tensor_copy(out=cnt[:], in_=pc_[:, 0:NE]), reads=[pck_], writes=["cnt"])
        nbt = sb(pr, "nbt", [128, NE * 8], F32)
        A("dve", lambda: V.tensor_tensor(out=nbt[:].rearrange("p (e j) -> p e j", j=8),
                                         in0=cnt[:].unsqueeze(2).to_broadcast([128, NE, 8]),
                                         in1=blkth[:, 0:8 * NE].rearrange("p (b e) -> p e b", e=NE), op=ALU.is_gt),
          reads=["cnt", "blkth"], writes=["nbt"])
        A("dve", lambda: V.reduce_sum(out=padded[:], in_=nbt[:].rearrange("p (e j) -> p e j", j=8), axis=AX.X), reads=["nbt"], writes=["padded"])
        A("dve", lambda: V.tensor_scalar(out=padded[:], in0=padded[:], scalar1=float(MB), scalar2=None, op0=ALU.mult),
          reads=["padded"], writes=["padded"])
        A("dve", lambda: V.tensor_copy(out=cs[0][:], in_=padded[:]), reads=["padded"], writes=[("cs", 0)])
        cur = 0
        for sft in (1, 2, 4, 8, 16):
            nxt = 1 - cur
            A("dve", lambda cur=cur, nxt=nxt, sft=sft: V.tensor_copy(out=cs[nxt][:, 0:sft], in_=cs[cur][:, 0:sft]),
              reads=[("cs", cur)], writes=[("cs", nxt)])
            A("dve", lambda cur=cur, nxt=nxt, sft=sft: V.tensor_tensor(out=cs[nxt][:, sft:NE], in0=cs[cur][:, sft:NE],
                                                                       in1=cs[cur][:, 0:NE - sft], op=ALU.add),
              reads=[("cs", cur), ("cs", nxt)], writes=[("cs", nxt)])
            cur = nxt
        pend = cs[cur]
        pendk = ("cs", cur)
        A("dve", lambda: V.tensor_tensor(out=pstart[:], in0=pend[:], in1=padded[:], op=ALU.subtract), reads=[pendk, "padded"], writes=["pstart"])
        A("dve", lambda: V.tensor_tensor(out=pos3, in0=pos3, in1=pstart[:].unsqueeze(1).to_broadcast([128, NQB, NE]), op=ALU.add),
          reads=[("pos", 0), ("pos", 1), "pstart"], writes=["dest"])
        for k in range(4):
            A("dve", lambda k=k: V.tensor_tensor(out=oh3, in0=lg3, in1=m83[:, :, k:k + 1].to_broadcast([128, NQB, NE]), op=ALU.is_equal),
              reads=LGK, writes=["oh"])
            A("dve", lambda: V.tensor_tensor(out=prod[:], in0=oh[:], in1=pos[:], op=ALU.mult), reads=["oh", "dest"], writes=["prod"])
            A("dve", lambda k=k: V.reduce_sum(out=dself[:, k * NQB:(k + 1) * NQB], in_=prod3, axis=AX.X), reads=["prod"], writes=[("dself", k)])
            A("dve", lambda: V.tensor_tensor(out=prod[:], in0=oh[:], in1=gw_all[:], op=ALU.mult), reads=["oh", "gw"], writes=["prod"])
            A("dve", lambda k=k: V.reduce_sum(out=gk[:, k * NQB:(k + 1) * NQB], in_=prod3, axis=AX.X), reads=["prod"], writes=[("gk", k)])
        A("dve", lambda: V.tensor_copy(out=dsel_i[:], in_=dself[:]), reads=[("dself", k) for k in range(4)], writes=["dsel_i"])
        A("dve", lambda: V.tensor_tensor(out=cmpb[:].rearrange("p (b e) -> p b e", e=NE),
                                         in0=pend[:].unsqueeze(1).to_broadcast([128, NBLK, NE]),
                                         in1=blkth[:].rearrange("p (b e) -> p b e", e=NE), op=ALU.is_le),
          reads=[pendk, "blkth"], writes=["cmpb"])
        A("dve", lambda: V.reduce_sum(out=blke[:], in_=cmpb[:].rearrange("p (b e) -> p b e", e=NE), axis=AX.X), reads=["cmpb"], writes=["blke"])
        A("dve", lambda: V.tensor_scalar(out=blke[:], in0=blke[:], scalar1=float(NE - 1), scalar2=None, op0=ALU.min), reads=["blke"], writes=["blke"])
        A("dve", lambda: V.scalar_tensor_tensor(out=idxwf[:].rearrange("p (b c) -> p b c", c=8),
                                                in0=blke[:].unsqueeze(2).to_broadcast([128, NBLK, 8]), scalar=float(D),
                                                in1=rowid[:].unsqueeze(1).to_broadcast([128, NBLK, 8]), op0=ALU.mult, op1=ALU.add),
          reads=["blke", "rowid"], writes=["idxwf"])
        nused = sb(pr, "nused", [128, NBLK], F32)
        A("dve", lambda: V.tensor_scalar(out=nused[:], in0=blkth[:].rearrange("p (b e) -> p b e", e=NE)[:, :, 0],
                                         scalar1=pend[:, NE - 1:NE], scalar2=None, op0=ALU.is_ge),
          reads=[pendk, "blkth"], writes=["nused"])
        sameb = sb(pr, "sameb", [128, NBLK], F32)
        A("dve", lambda: V.memset(sameb[:], 0.0), writes=["sameb"])
        A("dve", lambda: V.tensor_tensor(out=sameb[:, 2:NBLK], in0=blke[:, 2:NBLK], in1=blke[:, 0:NBLK - 2], op=ALU.is_equal),
          reads=["blke", "sameb"], writes=["sameb"])
        A("dve", lambda: V.tensor_tensor(out=nused[:], in0=nused[:], in1=sameb[:], op=ALU.max), reads=["nused", "sameb"], writes=["nused"])
        idxgf = sb(pr, "idxgf", [128, NBLK * 8], F32)
        A("dve", lambda: V.scalar_tensor_tensor(out=idxgf[:].rearrange("p (b c) -> p b c", c=8),
                                                in0=nused[:].unsqueeze(2).to_broadcast([128, NBLK, 8]), scalar=40000.0,
                                                in1=idxwf[:].rearrange("p (b c) -> p b c", c=8), op0=ALU.mult, op1=ALU.add),
          reads=["nused", "idxwf"], writes=["idxgf"])
        A("dve", lambda: V.tensor_copy(out=idxg_i[:], in_=idxgf[:]), reads=["idxgf"], writes=["idxg_i"])
        A("dve", lambda: V.tensor_copy(out=idxw_i[:], in_=idxwf[:]), reads=["idxwf"], writes=["idxw_i"])
        A("dve", lambda: V.scalar_tensor_tensor(out=idxbf[:], in0=blke[:], scalar=128.0, in1=rowid[:, 0:1].to_broadcast([128, NBLK]),
                                                op0=ALU.mult, op1=ALU.add), reads=["blke", "rowid"], writes=["idxbf"])
        A("dve", lambda: V.scalar_tensor_tensor(out=idxbf[:], in0=nused[:], scalar=40000.0, in1=idxbf[:], op0=ALU.mult, op1=ALU.add),
          reads=["nused", "idxbf"], writes=["idxbf"])
        A("dve", lambda: V.tensor_copy(out=idxb_i[:], in_=idxbf[:]), reads=["idxbf"], writes=["idxb_i"])
        if dbg:
            sc.dma("sp", lambda: SP.dma_start(out=dbg_d["gw"][:, :], in_=gw_all[:]), reads=["gw"], writes=["dbg_gw"])
            sc.dma("sp", lambda: SP.dma_start(out=dbg_d["dsel"][:, :], in_=dsel_i[:]), reads=["dsel_i"], writes=["dbg_dsel"])
            sc.dma("sp", lambda: SP.dma_start(out=dbg_d["blke"][:, :], in_=blke[:]), reads=["blke"], writes=["dbg_blke"])
        h2r = [sb(pr, f"h2r{i}", [128, D], BF16) for i in range(4)]
        for i in range(NQB):
            s4 = i % 4
            sc.dma("sp", lambda i=i, s4=s4: SP.dma_start(out=h2r[s4][:], in_=h2_d[i * 128:(i + 1) * 128, :]),
                   reads=[("h2_d", i)], writes=[("h2r", s4)])
            for k in range(4):
                sc.dma("pool", lambda i=i, k=k, s4=s4: G.indirect_dma_start(
                    out=xs_d[:, :], out_offset=bass.IndirectOffsetOnAxis(ap=dsel_i[:, k * NQB + i:k * NQB + i + 1], axis=0),
                    in_=h2r[s4][:], in_offset=None), reads=[("h2r", s4), "dsel_i"], writes=["xs_d"])
        sc.barrier()
    if stop_after == "R":
        sc.finish(["dbg_gw", "dbg_dsel", "dbg_blke", "xs_d"] + [("x1_d", i) for i in range(NQB)])
        rt_.close()
        es.close()
        return nc

    with ExitStack() as pm:
        wgu = [sb(pm, f"wgu{i}", [128, 8 * 2 * DFF], BF16) for i in range(2)]
        wdn = [sb(pm, f"wdn{i}", [128, 8 * D], BF16) for i in range(2)]
        bgu = [sb(pm, f"bgu{i}", [128, 16], F32) for i in range(2)]
        xst = [sb(pm, "xst0", [128, 4 * D], BF16)]
        xsT = sb(pm, "xsT", [128, 8 * MB], BF16)
        xsT3 = xsT[:].rearrange("p (c t) -> p c t", t=MB)
        actT_ = [sb(pm, f"actT{j}", [128, 8 * MB], BF16) for j in range(2)]
        actT3_ = [a_[:].rearrange("p (f t) -> p f t", t=MB) for a_ in actT_]
        gt = [sb(pm, f"gt{i}", [128, MB], F32) for i in range(2)]
        sg = [sb(pm, f"sg{i}", [128, MB], F32) for i in range(2)]
        ut = [sb(pm, f"ut{i}", [128, MB], F32) for i in range(2)]
        yst = [sb(pm, f"yst{i}", [128, D], F32) for i in range(4)]

        bc_reg = [G.to_reg(NE * D - 1), G.to_reg(NE * 128 - 1)]

        def load_weights(b, which):
            sl = b % 2
            w3g = wgu[sl][:].rearrange("p (c n) -> p c n", n=2 * DFF)
            w3d = wdn[sl][:].rearrange("p (c n) -> p c n", n=D)
            for c in range(8 if which == "gu" else 0):
                sc.dma("pool", lambda c=c, w3g=w3g, b=b: G.indirect_dma_start(
                    out=w3g[:, c, :], out_offset=None, in_=wgu_d[:, :],
                    in_offset=bass.IndirectOffsetOnAxis(ap=idxg_i[:, b * 8 + c:b * 8 + c + 1], axis=0),
                    bounds_check=bc_reg[0], oob_is_err=False),
                    reads=["idxg_i"], writes=[("wgu", sl, c)])
            for c in range(8 if which == "wd" else 0):
                sc.dma("pool", lambda c=c, w3d=w3d, b=b: G.indirect_dma_start(
                    out=w3d[:, c, :], out_offset=None, in_=wd_d[:, :],
                    in_offset=bass.IndirectOffsetOnAxis(ap=idxg_i[:, b * 8 + c:b * 8 + c + 1], axis=0),
                    bounds_check=bc_reg[0], oob_is_err=False),
                    reads=["idxg_i"], writes=[("wdn", sl, c)])
            if which == "gu":
              sc.dma("pool", lambda b=b, sl=sl: G.indirect_dma_start(
                out=bgu[sl][:], out_offset=None, in_=bgu_d[:, :],
                in_offset=bass.IndirectOffsetOnAxis(ap=idxb_i[:, b:b + 1], axis=0),
                bounds_check=bc_reg[1], oob_is_err=False), reads=["idxb_i"], writes=[("bgu", sl)])

        def load_x(b):
            sl = 0
            sc.dma("sp", lambda b=b, sl=sl: SP.dma_start(out=xst[sl][:].rearrange("p (s d) -> p s d", d=D),
                                                        in_=xs_d[b * MB:(b + 1) * MB, :].rearrange("(s p) d -> p s d", p=128)),
                   reads=["xs_d"], writes=[("xst", sl)])

        for j in range(2):
            A("dve", lambda j=j: V.memset(wgu[j][:], 0.0), writes=[("wgu", j, c) for c in range(8)])
            A("dve", lambda j=j: V.memset(wdn[j][:], 0.0), writes=[("wdn", j, c) for c in range(8)])
            A("dve", lambda j=j: V.memset(bgu[j][:], 0.0), writes=[("bgu", j)])
        load_weights(0, "gu")
        load_x(0)

        def down_proj(b):
            sl = b % 2
            w3d = wdn[sl][:].rearrange("p (c n) -> p c n", n=D)
            WDK = [("wdn", sl, c) for c in range(8)]
            aT3 = actT3_[sl]
            AK = [("actT", sl, f) for f in range(8)]
            for s_ in range(4):
                ys_ = yst[s_]
                for n in range(2):
                    py, pyk = bank(6 + n)
                    for f in range(8):
                        A("pe", lambda py=py, f=f, s_=s_, n=n, w3d=w3d, aT3=aT3: T.matmul(
                            py, lhsT=aT3[:, f, s_ * 128:(s_ + 1) * 128], rhs=w3d[:, f, n * 512:(n + 1) * 512],
                            start=(f == 0), stop=(f == 7)), reads=AK + WDK, writes=[pyk])
                    A("act", lambda py=py, ys_=ys_, n=n: ACT.copy(out=ys_[:, n * 512:(n + 1) * 512], in_=py),
                      reads=[pyk], writes=[("yst", s_, n)])
                sc.dma("sp", lambda ys_=ys_, b=b, s_=s_: SP.dma_start(out=ys_d[b * MB + s_ * 128:b * MB + (s_ + 1) * 128, :], in_=ys_[:]),
                       reads=[("yst", s_, 0), ("yst", s_, 1)], writes=["ys_d"])

        for b in range(NBLK):
            sl = b % 2
            w3g = wgu[sl][:].rearrange("p (c n) -> p c n", n=2 * DFF)
            WGK = [("wgu", sl, c) for c in range(8)]
            aT3 = actT3_[sl]
            for s_ in range(4):
                pbT = PS[0][:, (s_ % 2) * 512:(s_ % 2 + 1) * 512].bitcast(BF16)
                pkT = ("ps", s_ % 2)
                for c in range(8):
                    A("pe", lambda c=c, pbT=pbT, s_=s_: T.transpose(
                        out=pbT[:, c * 128:(c + 1) * 128], in_=xst[0][:, s_ * D + c * 128:s_ * D + (c + 1) * 128], identity=ident_b[:]),
                      reads=[("xst", 0), "ident_b"], writes=[pkT])
                if s_ % 2 == 0:
                    A("dve", lambda pbT=pbT, s_=s_: V.tensor_copy(out=xsT3[:, :, s_ * 128:(s_ + 1) * 128],
                                                                  in_=pbT.rearrange("p (c t) -> p c t", t=128)),
                      reads=[pkT], writes=[("xsT", s_)])
                else:
                    A("act", lambda pbT=pbT, s_=s_: ACT.copy(out=xsT3[:, :, s_ * 128:(s_ + 1) * 128],
                                                             in_=pbT.rearrange("p (c t) -> p c t", t=128)),
                      reads=[pkT], writes=[("xsT", s_)])
            if b + 1 < NBLK:
                load_x(b + 1)
            if b >= 1:
                down_proj(b - 1)
            load_weights(b, "wd")
            if b + 1 < NBLK:
                load_weights(b + 1, "gu")
            XK = [("xsT", s_) for s_ in range(4)]
            for f in range(8):
                e2 = f % 2
                pg, pgk = bank(2 + e2 * 2)
                pu, puk = bank(3 + e2 * 2)
                for (pb, pk, col) in ((pg, pgk, f * 128), (pu, puk, DFF + f * 128)):
                    for c in range(8):
                        A("pe", lambda pb=pb, c=c, col=col, w3g=w3g: T.matmul(pb, lhsT=w3g[:, c, col:col + 128], rhs=xsT3[:, c, :],
                                                                              start=(c == 0), stop=(c == 7)),
                          reads=XK + WGK, writes=[pk])
                g_, s__, u_ = gt[e2], sg[e2], ut[e2]
                A("dve", lambda pg=pg, g_=g_, f=f, sl=sl: V.tensor_scalar(out=g_[:], in0=pg, scalar1=bgu[sl][:, f:f + 1], scalar2=7.0,
                                                                          op0=ALU.add, op1=ALU.min),
                  reads=[pgk, ("bgu", sl)], writes=[("gt", e2)])
                A("act", lambda g_=g_, s__=s__: ACT.activation(out=s__[:], in_=g_[:], func=AF.Sigmoid, scale=1.702),
                  reads=[("gt", e2)], writes=[("sg", e2)])
                A("dve", lambda pu=pu, u_=u_, f=f, sl=sl: V.tensor_scalar(out=u_[:], in0=pu, scalar1=bgu[sl][:, 8 + f:9 + f], scalar2=7.0,
                                                                          op0=ALU.add, op1=ALU.min),
                  reads=[puk, ("bgu", sl)], writes=[("ut", e2)])
                A("dve", lambda u_=u_: V.tensor_scalar(out=u_[:], in0=u_[:], scalar1=-7.0, scalar2=1.0, op0=ALU.max, op1=ALU.add),
                  reads=[("ut", e2)], writes=[("ut", e2)])
                A("dve", lambda g_=g_, s__=s__: V.tensor_tensor(out=g_[:], in0=g_[:], in1=s__[:], op=ALU.mult),
                  reads=[("gt", e2), ("sg", e2)], writes=[("gt", e2)])
                A("dve", lambda g_=g_, u_=u_, f=f, aT3=aT3: V.tensor_tensor(out=aT3[:, f, :], in0=g_[:], in1=u_[:], op=ALU.mult),
                  reads=[("gt", e2), ("ut", e2)], writes=[("actT", sl, f)])
            if b % 8 == 7:
                sc.flush()
        down_proj(NBLK - 1)
        sc.barrier()

    with ExitStack() as pf:
        bd = sb(pf, "bd", [NE, D], F32)
        sc.dma("sp", lambda: SP.dma_start(out=bd[:], in_=bd_d[:, :]), writes=["bd"])
        gfin = sb(pf, "gfin", [128, D], F32)
        sc.dma("sp", lambda: SP.dma_start(out=gfin[:], in_=gfin_d[:, :].partition_broadcast(128)), writes=["gfin"])
        x1f = [sb(pf, f"x1f{i}", [128, D], F32) for i in range(2)]
        yk_ = [[sb(pf, f"yk{j}_{i}", [128, D], F32) for i in range(4)] for j in range(2)]
        acc_ = [sb(pf, f"acc{j}", [128, D], F32) for j in range(2)]
        gwT_ = [sb(pf, f"gwT{j}", [NE, 128], F32) for j in range(2)]
        junkF = sb(pf, "junkF", [128, D], BF16)
        ssqF = sb(pf, "ssqF", [128, 1], F32)
        ot = [sb(pf, f"ot{i}", [128, D], F32) for i in range(2)]
        def f_loads(i):
            s2 = i % 2
            yk = yk_[s2]
            sc.dma("sp", lambda i=i, s2=s2: SP.dma_start(out=x1f[s2][:], in_=x1_d[i * 128:(i + 1) * 128, :]),
                   reads=[("x1_d", i)], writes=[("x1f", s2)])
            for k in range(4):
                sc.dma("pool", lambda i=i, k=k, yk=yk: G.indirect_dma_start(
                    out=yk[k][:], out_offset=None, in_=ys_d[:, :],
                    in_offset=bass.IndirectOffsetOnAxis(ap=dsel_i[:, k * NQB + i:k * NQB + i + 1], axis=0)),
                    reads=["ys_d", "dsel_i"], writes=[("yk", s2, k)])

        for i in range(NQB):
            s2 = i % 2
            yk, acc, gwT = yk_[s2], acc_[s2], gwT_[s2]
            ACCK, GWTK = ("acc", s2), ("gwT", s2)
            if i == 0:
                f_loads(0)
            if i + 1 < NQB:
                f_loads(i + 1)
            pt_, ptk_ = bank(s2)
            A("pe", lambda pt_=pt_, i=i: T.transpose(out=pt_[0:NE, 0:128], in_=gw3[:, i, :], identity=ident_f[:]),
              reads=["gw", "ident_f"], writes=[ptk_])
            A("act", lambda pt_=pt_, gwT=gwT: ACT.copy(out=gwT[:], in_=pt_[0:NE, 0:128]), reads=[ptk_], writes=[GWTK])
            pbs = []
            for n in range(2):
                pb, pbk = bank(2 + 2 * s2 + n)
                A("pe", lambda pb=pb, n=n, gwT=gwT: T.matmul(pb, lhsT=gwT[:], rhs=bd[:, n * 512:(n + 1) * 512], start=True, stop=True),
                  reads=[GWTK, "bd"], writes=[pbk])
                pbs.append((pb, pbk))
            A("dve", lambda i=i, acc=acc, yk=yk: V.tensor_scalar(out=acc[:], in0=yk[0][:], scalar1=gk[:, i:i + 1], scalar2=None, op0=ALU.mult),
              reads=[("yk", s2, 0)] + [("gk", k) for k in range(4)], writes=[ACCK])
            for k in range(1, 4):
                A("dve", lambda i=i, k=k, acc=acc, yk=yk: V.scalar_tensor_tensor(out=acc[:], in0=yk[k][:], scalar=gk[:, k * NQB + i:k * NQB + i + 1],
                                                                 in1=acc[:], op0=ALU.mult, op1=ALU.add),
                  reads=[("yk", s2, k), ACCK] + [("gk", kk) for kk in range(4)], writes=[ACCK])
            for n in range(2):
                pb, pbk = pbs[n]
                A("dve", lambda pb=pb, n=n, acc=acc: V.tensor_tensor(out=acc[:, n * 512:(n + 1) * 512], in0=pb, in1=acc[:, n * 512:(n + 1) * 512], op=ALU.add),
                  reads=[pbk, ACCK], writes=[ACCK])
            A("dve", lambda acc=acc: V.tensor_tensor(out=acc[:], in0=acc[:], in1=gate_f, op=ALU.mult), reads=[ACCK] + MODK, writes=[ACCK])
            A("dve", lambda s2=s2, acc=acc: V.tensor_tensor(out=acc[:], in0=acc[:], in1=x1f[s2][:], op=ALU.add), reads=[ACCK, ("x1f", s2)], writes=[ACCK])
            A("act", lambda acc=acc: ACT.activation(out=junkF[:], in_=acc[:], func=AF.Square, accum_out=ssqF[:]), reads=[ACCK], writes=["junkF", "ssqF"])
            A("dve", lambda: V.tensor_scalar(out=ssqF[:], in0=ssqF[:], scalar1=1.0 / D, scalar2=EPS, op0=ALU.mult, op1=ALU.add),
              reads=["ssqF"], writes=["ssqF"])
            A("act", lambda: ACT.activation(out=ssqF[:], in_=ssqF[:], func=AF.Ln), reads=["ssqF"], writes=["ssqF"])
            A("act", lambda: ACT.activation(out=ssqF[:], in_=ssqF[:], func=AF.Exp, scale=-0.5), reads=["ssqF"], writes=["ssqF"])
            o_ = ot[s2]
            A("dve", lambda o_=o_, acc=acc: V.scalar_tensor_tensor(out=o_[:], in0=acc[:], scalar=ssqF[:, 0:1], in1=gfin[:], op0=ALU.mult, op1=ALU.mult),
              reads=[ACCK, "ssqF", "gfin"], writes=[("ot", s2)])
            sc.dma("sp", lambda o_=o_, i=i: SP.dma_start(out=out_d[i * 128:(i + 1) * 128, :], in_=o_[:]),
                   reads=[("ot", s2)], writes=[("out_d", i)])
        sc.barrier()
    sc.finish([("out_d", i) for i in range(NQB)])
    rt_.close()
    es.close()
    return nc


def _prep_inputs(inputs):
    f = lambda a: np.ascontiguousarray(np.asarray(a, dtype=np.float32))
    x = f(inputs["x"])
    c = f(inputs["c"])
    w_in = f(inputs["w_in"])[0]
    wa, wb = _layout_w_in(w_in)
    cosT, sinT = _rope_tables()
    dr, co = _bias_index()
    rpb = f(inputs["rpb"])[0]
    biasu = np.ascontiguousarray(rpb[:, dr, co].reshape(8, 128, 7 * 128))
    bgu = f(inputs["b_gate_up"])[0]
    bgu_l = np.ascontiguousarray(bgu.reshape(NE, 16, 128).transpose(0, 2, 1).reshape(NE * 128, 16))
    rowid = (np.arange(8)[None, :] * 128 + np.arange(128)[:, None]).astype(np.float32)
    blkth = np.broadcast_to((np.arange(NBLK, dtype=np.float32) * MB)[None, :, None], (128, NBLK, NE)).reshape(128, NBLK * NE)
    shared = {
        "w_ada": f(inputs["w_ada"])[0], "b_ada": f(inputs["b_ada"]).reshape(1, 6 * D),
        "w_a": wa, "w_b": wb, "sink": f(inputs["sink"]).reshape(1, 8), "biasu": biasu,
        "g_out_a": f(inputs["g_out_a"]).reshape(1, 512), "g_out_b": f(inputs["g_out_b"]).reshape(1, 512),
        "w_out": f(inputs["w_out"])[0], "w_router": f(inputs["w_router"])[0], "b_router": f(inputs["b_router"]).reshape(1, NE),
        "w_gate_up": f(inputs["w_gate_up"])[0].reshape(NE * D, 2 * DFF), "b_gate_up": bgu_l,
        "w_down": f(inputs["w_down"])[0].reshape(NE * DFF, D), "b_down": f(inputs["b_down"])[0],
        "g_final": f(inputs["g_final"]).reshape(1, D), "cosT": cosT, "sinT": sinT,
        "maska": np.ascontiguousarray(_mask_a().reshape(128, 384)),
        "maskb": np.ascontiguousarray(_MASKB.reshape(_NVAR, 128, 7 * 128)),
        "ident": np.eye(128, dtype=np.float32), "tri": np.triu(np.ones((128, 128), np.float32), 1),
        "rowid": rowid, "blkth": np.ascontiguousarray(blkth),
    }
    in_maps = []
    for b in range(8):
        m = dict(shared)
        m["x"] = x[b]
        m["cT"] = np.ascontiguousarray(c[b].reshape(8, 128).T)
        in_maps.append(m)
    return in_maps


def kernel(**inputs):
    in_maps = _prep_inputs(inputs)
    nc = build_program()
    res = run_bass_kernel_spmd(nc, in_maps, core_ids=list(range(8)))
    return np.stack([np.asarray(r["out"], dtype=np.float32) for r in res.results], axis=0)
```
